# Optimizing a Trainium2 kernel written in Bass

```python
import math
import jax
import jax.numpy as jnp
from jax import lax
import numpy as np

D_MODEL = 1024
BATCH = 2
SEQ = 16384
DEPTH = 1

CHUNK = 64
N_META = 16
D_MIX = D_MODEL
ATT_W = D_MIX // 2
ATT_HEADS = 4
HEAD_DIM = ATT_W // (2 * ATT_HEADS)
ATT_Q = ATT_HEADS * 2 * HEAD_DIM
ATT_V = ATT_HEADS * 2 * HEAD_DIM
SSM_W = D_MIX - ATT_W
SSM_CG = 16
SSM_G = SSM_W // SSM_CG
SSM_N = 64
IN_COLS = 2 * ATT_Q + ATT_V + SSM_W
N_BUCKETS = 32
MAX_DISTANCE = 128
Q_BLOCK = 128
N_EXPERTS = 32
TOP_K = 4
D_FF = D_MODEL
SWIGLU_LIMIT = 7.0
SWIGLU_ALPHA = 1.702
MOE_BLOCK = 256
LN_EPS = 1e-5
NEG_INF = -1e30
DEEPNORM_ALPHA = (2.0 * DEPTH) ** 0.25
DEEPNORM_BETA = (8.0 * DEPTH) ** -0.25

kernel_name = "hymba_diffattn_s5_moe_deepnorm"


def layer_norm(x, g, b):
    xf = x.astype(jnp.float32)
    mu = jnp.mean(xf, axis=-1, keepdims=True)
    var = jnp.mean(jnp.square(xf - mu), axis=-1, keepdims=True)
    y = (xf - mu) * lax.rsqrt(var + LN_EPS) * g.astype(jnp.float32) + b.astype(jnp.float32)
    return y.astype(x.dtype)


def rms_norm(x, g):
    xf = x.astype(jnp.float32)
    y = xf * lax.rsqrt(jnp.mean(jnp.square(xf), axis=-1, keepdims=True) + LN_EPS) * g.astype(jnp.float32)
    return y.astype(x.dtype)


def t5_bucket(rel):
    nb = N_BUCKETS // 2
    max_exact = nb // 2
    ret = jnp.where(rel > 0, nb, 0)
    n = jnp.abs(rel)
    n_f = jnp.maximum(n, 1).astype(jnp.float32)
    large = max_exact + (jnp.log(n_f / max_exact) / math.log(MAX_DISTANCE / max_exact)
                         * (nb - max_exact)).astype(jnp.int32)
    large = jnp.minimum(large, nb - 1)
    return ret + jnp.where(n < max_exact, n, large)


def diff_attention(q, k, v, lam, lam_init, rel_bias, subln_g, pos, q_chunk, key_chunk):
    bsz, lp = q.shape[0], q.shape[1]
    qh = jnp.transpose(q, (0, 2, 3, 1, 4)) * (HEAD_DIM ** -0.5)
    kh = jnp.transpose(k, (0, 2, 3, 1, 4))
    vh = jnp.transpose(v, (0, 2, 1, 3))
    table = rel_bias.astype(jnp.float32)
    n_blocks = lp // Q_BLOCK

    def one_block(i):
        start = i * Q_BLOCK
        qb = lax.dynamic_slice_in_dim(qh, start, Q_BLOCK, axis=3)
        qpos = lax.dynamic_slice_in_dim(pos, start, Q_BLOCK)
        qc = lax.dynamic_slice_in_dim(q_chunk, start, Q_BLOCK)
        s = jnp.einsum('bhmqd,bhmkd->bhmqk', qb, kh).astype(jnp.float32)
        bias = table[t5_bucket(pos[None, :] - qpos[:, None])]
        s = s + jnp.transpose(bias, (2, 0, 1))[None, :, None]
        visible = key_chunk[None, :] <= qc[:, None]
        s = jnp.where(visible[None, None, None], s, NEG_INF)
        p = jax.nn.softmax(s, axis=-1)
        w = p[:, :, 0] - lam * p[:, :, 1]
        return jnp.einsum('bhqk,bhke->bqhe', w.astype(vh.dtype), vh)

    o = lax.map(one_block, jnp.arange(n_blocks))
    o = jnp.transpose(o, (1, 0, 2, 3, 4)).reshape(bsz, lp, ATT_HEADS, 2 * HEAD_DIM)
    o = rms_norm(o, subln_g) * (1.0 - lam_init)
    return o.reshape(bsz, lp, ATT_W)


def s5_ssm(u, a_re, a_im, log_step, b_re, b_im, c_re, c_im, d_skip, w_glu, b_glu):
    f32 = jnp.float32
    bsz, lp, _ = u.shape
    uf = u.astype(f32).reshape(bsz, lp, SSM_G, SSM_CG)
    step = jnp.exp(log_step.astype(f32))[:, None]
    ar = jnp.minimum(a_re.astype(f32), -1e-4)
    ai = a_im.astype(f32)
    mag = jnp.exp(step * ar)
    ph = step * ai
    abar_re = mag * jnp.cos(ph)
    abar_im = mag * jnp.sin(ph)
    den = ar * ar + ai * ai
    e_re = abar_re - 1.0
    e_im = abar_im
    f_re = (e_re * ar + e_im * ai) / den
    f_im = (e_im * ar - e_re * ai) / den
    br = b_re.astype(f32)
    bi = b_im.astype(f32)
    bb_re = f_re[..., None] * br - f_im[..., None] * bi
    bb_im = f_re[..., None] * bi + f_im[..., None] * br
    bu_re = jnp.einsum('blgc,gnc->blgn', uf, bb_re)
    bu_im = jnp.einsum('blgc,gnc->blgn', uf, bb_im)
    a_seq_re = jnp.broadcast_to(abar_re[None, None], (1, lp, SSM_G, SSM_N))
    a_seq_im = jnp.broadcast_to(abar_im[None, None], (1, lp, SSM_G, SSM_N))

    def combine(e1, e2):
        a1r, a1i, b1r, b1i = e1
        a2r, a2i, b2r, b2i = e2
        return (a1r * a2r - a1i * a2i,
                a1r * a2i + a1i * a2r,
                a2r * b1r - a2i * b1i + b2r,
                a2r * b1i + a2i * b1r + b2i)

    _, _, xr, xi = lax.associative_scan(combine, (a_seq_re, a_seq_im, bu_re, bu_im), axis=1)
    y = (jnp.einsum('blgn,gcn->blgc', xr, c_re.astype(f32))
         - jnp.einsum('blgn,gcn->blgc', xi, c_im.astype(f32))
         + d_skip.astype(f32).reshape(SSM_G, SSM_CG) * uf)
    y = jax.nn.gelu(y.reshape(bsz, lp, SSM_W))
    y = y * jax.nn.sigmoid(y @ w_glu.astype(f32) + b_glu.astype(f32))
    return y.astype(u.dtype)


def moe_ffn(t, w_router, b_router, w_gate, b_gate, w_up, b_up, w_down, b_down):
    n_tok, dm = t.shape
    logits = t.astype(jnp.float32) @ w_router.astype(jnp.float32) + b_router.astype(jnp.float32)
    top_val, top_idx = lax.top_k(logits, TOP_K)
    gates = jax.nn.softmax(top_val, axis=-1)
    nk = n_tok * TOP_K
    flat_e = top_idx.reshape(nk).astype(jnp.int32)
    order = jnp.argsort(flat_e)
    sorted_e = flat_e[order]
    counts = jnp.bincount(flat_e, length=N_EXPERTS)
    padded = (counts + MOE_BLOCK - 1) // MOE_BLOCK * MOE_BLOCK
    padded_end = jnp.cumsum(padded)
    padded_start = padded_end - padded
    group_start = jnp.cumsum(counts) - counts
    dest = padded_start[sorted_e] + jnp.arange(nk, dtype=jnp.int32) - group_start[sorted_e]
    n_blocks = -(-nk // MOE_BLOCK) + N_EXPERTS
    cap = n_blocks * MOE_BLOCK
    buf_tok = jnp.full((cap,), n_tok, jnp.int32).at[dest].set((order // TOP_K).astype(jnp.int32))
    buf_gate = jnp.zeros((cap,), jnp.float32).at[dest].set(gates.reshape(nk)[order])
    block_expert = jnp.minimum(
        jnp.searchsorted(padded_end, jnp.arange(n_blocks, dtype=jnp.int32) * MOE_BLOCK, side='right'),
        N_EXPERTS - 1)
    t_pad = jnp.concatenate([t, jnp.zeros((1, dm), t.dtype)], axis=0)

    def expert_block(args):
        tok, g, e = args
        xb = t_pad[tok]
        gate = xb @ w_gate[e] + b_gate[e]
        up = xb @ w_up[e] + b_up[e]
        gate = jnp.minimum(gate, SWIGLU_LIMIT)
        up = jnp.clip(up, -SWIGLU_LIMIT, SWIGLU_LIMIT)
        y = ((up + 1.0) * gate * jax.nn.sigmoid(gate * SWIGLU_ALPHA)) @ w_down[e] + b_down[e]
        return (y * g[:, None]).astype(t.dtype)

    out = lax.map(expert_block, (buf_tok.reshape(n_blocks, MOE_BLOCK),
                                 buf_gate.reshape(n_blocks, MOE_BLOCK), block_expert))
    y = jnp.zeros((n_tok + 1, dm), t.dtype).at[buf_tok].add(out.reshape(cap, dm))
    return y[:n_tok]


def setup_inputs(seed: int = 0) -> dict:
    key = jax.random.key(seed)
    ks = jax.random.split(key, 40)
    f32 = jnp.float32

    def nrm(k, shape, s):
        return jax.random.normal(k, shape, f32) * s

    col_scale = jnp.concatenate([jnp.ones((2 * ATT_Q,), f32),
                                 jnp.full((ATT_V,), DEEPNORM_BETA, f32),
                                 jnp.ones((SSM_W,), f32)])
    return {
        'x': nrm(ks[0], (BATCH, SEQ, D_MODEL), 1.0),
        'meta_tokens': nrm(ks[1], (N_META, D_MODEL), 1.0),
        'ln_in_g': 1.0 + nrm(ks[2], (D_MODEL,), 0.02),
        'ln_in_b': nrm(ks[3], (D_MODEL,), 0.02),
        'rel_bias': nrm(ks[4], (N_BUCKETS, ATT_HEADS), 0.3),
        'w_in': nrm(ks[5], (DEPTH, D_MODEL, IN_COLS), D_MODEL ** -0.5) * col_scale,
        'lambda_q1': nrm(ks[6], (DEPTH, HEAD_DIM), 0.1),
        'lambda_k1': nrm(ks[7], (DEPTH, HEAD_DIM), 0.1),
        'lambda_q2': nrm(ks[8], (DEPTH, HEAD_DIM), 0.1),
        'lambda_k2': nrm(ks[9], (DEPTH, HEAD_DIM), 0.1),
        'subln_g': 1.0 + nrm(ks[10], (DEPTH, 2 * HEAD_DIM), 0.02),
        'a_re': -0.5 + nrm(ks[11], (DEPTH, SSM_G, SSM_N), 0.01),
        'a_im': math.pi * jnp.arange(SSM_N, dtype=f32) + nrm(ks[12], (DEPTH, SSM_G, SSM_N), 0.01),
        'log_step': jax.random.uniform(ks[13], (DEPTH, SSM_G), f32, math.log(1e-3), math.log(1e-1)),
        'b_re': nrm(ks[14], (DEPTH, SSM_G, SSM_N, SSM_CG), (2.0 * SSM_CG) ** -0.5),
        'b_im': nrm(ks[15], (DEPTH, SSM_G, SSM_N, SSM_CG), (2.0 * SSM_CG) ** -0.5),
        'c_re': nrm(ks[16], (DEPTH, SSM_G, SSM_CG, SSM_N), SSM_N ** -0.5),
        'c_im': nrm(ks[17], (DEPTH, SSM_G, SSM_CG, SSM_N), SSM_N ** -0.5),
        'd_skip': nrm(ks[18], (DEPTH, SSM_W), 1.0),
        'w_glu': nrm(ks[19], (DEPTH, SSM_W, SSM_W), SSM_W ** -0.5),
        'b_glu': nrm(ks[20], (DEPTH, SSM_W), 0.02),
        'w_out': nrm(ks[21], (DEPTH, D_MIX, D_MODEL), D_MIX ** -0.5 * DEEPNORM_BETA),
        'ln1_g': 1.0 + nrm(ks[22], (DEPTH, D_MODEL), 0.02),
        'ln1_b': nrm(ks[23], (DEPTH, D_MODEL), 0.02),
        'w_router': nrm(ks[24], (DEPTH, D_MODEL, N_EXPERTS), D_MODEL ** -0.5),
        'b_router': nrm(ks[25], (DEPTH, N_EXPERTS), 0.01),
        'w_gate': nrm(ks[26], (DEPTH, N_EXPERTS, D_MODEL, D_FF), D_MODEL ** -0.5),
        'b_gate': nrm(ks[27], (DEPTH, N_EXPERTS, D_FF), 0.02),
        'w_up': nrm(ks[28], (DEPTH, N_EXPERTS, D_MODEL, D_FF), D_MODEL ** -0.5),
        'b_up': nrm(ks[29], (DEPTH, N_EXPERTS, D_FF), 0.02),
        'w_down': nrm(ks[30], (DEPTH, N_EXPERTS, D_FF, D_MODEL), D_FF ** -0.5 * DEEPNORM_BETA),
        'b_down': nrm(ks[31], (DEPTH, N_EXPERTS, D_MODEL), 0.02),
        'ln2_g': 1.0 + nrm(ks[32], (DEPTH, D_MODEL), 0.02),
        'ln2_b': nrm(ks[33], (DEPTH, D_MODEL), 0.02),
    }


def reference(x, meta_tokens, ln_in_g, ln_in_b, rel_bias, w_in, lambda_q1, lambda_k1,
              lambda_q2, lambda_k2, subln_g, a_re, a_im, log_step, b_re, b_im, c_re, c_im,
              d_skip, w_glu, b_glu, w_out, ln1_g, ln1_b, w_router, b_router, w_gate, b_gate,
              w_up, b_up, w_down, b_down, ln2_g, ln2_b):
    f32 = jnp.float32
    bsz, seq, dm = x.shape
    total = N_META + seq
    lp = -(-total // Q_BLOCK) * Q_BLOCK
    h = jnp.concatenate([jnp.broadcast_to(meta_tokens.astype(x.dtype)[None], (bsz, N_META, dm)), x], axis=1)
    h = layer_norm(h, ln_in_g, ln_in_b)
    h = jnp.pad(h, ((0, 0), (0, lp - total), (0, 0)))
    pos = jnp.arange(lp, dtype=jnp.int32)
    chunk = jnp.where(pos < N_META, 0, (pos - N_META) // CHUNK + 1)
    key_chunk = jnp.where(pos < total, chunk, jnp.int32(2 ** 30))

    for l in range(DEPTH):
        proj = h @ w_in[l]
        q = proj[..., :ATT_Q].reshape(bsz, lp, ATT_HEADS, 2, HEAD_DIM)
        k = proj[..., ATT_Q:2 * ATT_Q].reshape(bsz, lp, ATT_HEADS, 2, HEAD_DIM)
        v = proj[..., 2 * ATT_Q:2 * ATT_Q + ATT_V].reshape(bsz, lp, ATT_HEADS, 2 * HEAD_DIM)
        u = proj[..., 2 * ATT_Q + ATT_V:]
        lam_init = 0.8 - 0.6 * math.exp(-0.3 * l)
        lam = (jnp.exp(jnp.sum(lambda_q1[l].astype(f32) * lambda_k1[l].astype(f32)))
               - jnp.exp(jnp.sum(lambda_q2[l].astype(f32) * lambda_k2[l].astype(f32))) + lam_init)
        att = diff_attention(q, k, v, lam, lam_init, rel_bias, subln_g[l], pos, chunk, key_chunk)
        ssm = s5_ssm(u, a_re[l], a_im[l], log_step[l], b_re[l], b_im[l], c_re[l], c_im[l],
                     d_skip[l], w_glu[l], b_glu[l])
        mix = jnp.concatenate([att, ssm], axis=-1) @ w_out[l]
        h = layer_norm(DEEPNORM_ALPHA * h + mix, ln1_g[l], ln1_b[l])
        ffn = moe_ffn(h.reshape(bsz * lp, dm), w_router[l], b_router[l], w_gate[l], b_gate[l],
                      w_up[l], b_up[l], w_down[l], b_down[l]).reshape(bsz, lp, dm)
        h = layer_norm(DEEPNORM_ALPHA * h + ffn, ln2_g[l], ln2_b[l])

    return h[:, N_META:N_META + seq]
```

```python
import math
from contextlib import ExitStack

import numpy as np
import concourse.bass as bass
import concourse.mybir as mybir
from concourse.bass_utils import run_bass_kernel_spmd

F32 = mybir.dt.float32
BF16 = mybir.dt.bfloat16
I32 = mybir.dt.int32
AF = mybir.ActivationFunctionType
ALU = mybir.AluOpType
AX = mybir.AxisListType

D = 1024
NEXP = 32
LN_EPS = 1e-5
ALPHA = 2.0 ** 0.25
NEG = -30000.0
EPOCH = 30000


class Sync:
    def __init__(self, nc, stack, n_dma_sems=10):
        self.nc = nc
        self.stack = stack
        self.eng = {"pe": nc.tensor, "act": nc.scalar, "dve": nc.vector,
                    "pool": nc.gpsimd, "sp": nc.sync}
        self.sems = {}
        self.nsem = 0
        self.cur = {e: [self._new_sem(), 0] for e in self.eng}
        self.seen = {e: {} for e in self.eng}
        self.lastw = {}
        self.readers = {}
        self.dma_pool = {}
        self.dma_rr = {}
        for q in ("sp", "pool", "act"):
            self.dma_pool[q] = [[self._new_sem(), 0] for _ in range(n_dma_sems)]
            self.dma_rr[q] = 0
        self.n_inst = 0
        self.n_wait = 0

    def _new_sem(self):
        sid = self.nsem
        self.nsem += 1
        self.sems[sid] = self.stack.enter_context(self.nc.semaphore(f"s{sid}"))
        return sid

    def _wait(self, e, ticket):
        sid, val, owner = ticket
        if owner == e and e == "pe":
            return
        if self.seen[e].get(sid, 0) >= val:
            return
        self.eng[e].wait_ge(self.sems[sid], val)
        self.seen[e][sid] = val
        self.n_wait += 1

    def _deps(self, e, reads, writes):
        for b in list(reads) + list(writes):
            t = self.lastw.get(b)
            if t is not None:
                self._wait(e, t)
        for b in writes:
            for t in self.readers.get(b, ()):
                self._wait(e, t)

    def _commit(self, ticket, reads, writes):
        for b in writes:
            self.lastw[b] = ticket
            self.readers[b] = []
        for b in reads:
            lst = self.readers.setdefault(b, [])
            lst.append(ticket)
            if len(lst) > 24:
                best = {}
                for t in lst:
                    if t[0] not in best or best[t[0]][1] < t[1]:
                        best[t[0]] = t
                self.readers[b] = list(best.values())

    def op(self, e, fn, reads=(), writes=()):
        self._deps(e, reads, writes)
        c = self.cur[e]
        if c[1] >= EPOCH:
            c[0] = self._new_sem()
            c[1] = 0
        ins = fn()
        c[1] += 1
        ins.then_inc(self.sems[c[0]], 1)
        ticket = (c[0], c[1], e)
        self._commit(ticket, reads, writes)
        self.n_inst += 1
        return ticket

    def dma(self, q, fn, reads=(), writes=(), grp=None, ngrp=6):
        self._deps(q, reads, writes)
        if grp is None:
            pk = q
        else:
            pk = (q, grp)
            if pk not in self.dma_pool:
                self.dma_pool[pk] = [[self._new_sem(), 0] for _ in range(ngrp)]
                self.dma_rr[pk] = 0
        pool = self.dma_pool[pk]
        i = self.dma_rr[pk]
        self.dma_rr[pk] = (i + 1) % len(pool)
        slot = pool[i]
        if slot[1] > 0:
            self._wait(q, (slot[0], slot[1], "dma"))
        if slot[1] >= EPOCH:
            slot[0] = self._new_sem()
            slot[1] = 0
        ins = fn()
        slot[1] += 16
        ins.then_inc(self.sems[slot[0]], 16)
        ticket = (slot[0], slot[1], "dma")
        self._commit(ticket, reads, writes)
        self.n_inst += 1
        return ticket

    def waitfor(self, e, keys):
        for b in keys:
            t = self.lastw.get(b)
            if t is not None:
                self._wait(e, t)

    def wait_all(self, e):
        for b, t in list(self.lastw.items()):
            self._wait(e, t)
        for q, pool in self.dma_pool.items():
            for slot in pool:
                if slot[1] > 0:
                    self._wait(e, (slot[0], slot[1], "dma"))


def _bucket_thresholds():
    n = np.arange(1, 400, dtype=np.int32)
    n_f = np.maximum(n, 1).astype(np.float32)
    large = 8 + (np.log(n_f / np.float32(8)) / np.float32(math.log(128 / 8)) * np.float32(8)).astype(np.int32)
    large = np.minimum(large, 15)
    bucket = np.where(n < 8, n, large)
    thr = {}
    for j in range(9, 16):
        thr[j] = int(n[np.argmax(bucket >= j)])
    return thr


def build(NX, CAP, debug=False, full=True, stop_after=None):
    nc = bass.Bass("TRN2", target_bir_lowering=False)
    NT = NX // 128 + 1
    NCOL = NT * 128
    NG = NX // 512
    NTOK2 = NX // 4
    NT2 = NTOK2 // 128
    NG2 = NTOK2 // 512
    NRT = CAP // 128
    NSLOT = NEXP * CAP

    def din(name, shape, dt=F32):
        return nc.dram_tensor(name, list(shape), dt, kind="ExternalInput").ap()

    x_b = din("x_b", [NX, D])
    meta = din("meta", [16, D])
    ln_in_g = din("ln_in_g", [1, D]); ln_in_b = din("ln_in_b", [1, D])
    w4 = din("w4", [D, 512])
    relb = din("relb", [1, 32])
    lam4 = din("lam4", [4, 64])
    subln_g = din("subln_g", [1, 128])
    a_re = din("a_re", [1, 512]); a_im = din("a_im", [1, 512]); log_step = din("log_step", [1, 8])
    b_re = din("b_re", [8, 64, 16]); b_im = din("b_im", [8, 64, 16])
    c_re = din("c_re", [8, 16, 64]); c_im = din("c_im", [8, 16, 64])
    d_skip = din("d_skip", [1, 128])
    if full:
        x2 = din("x2", [NTOK2, D])
        gidx = din("gidx", [128, 8 * NG2], I32)
        w_glu = din("w_glu", [512, 512]); b_glu = din("b_glu", [1, 512])
        w_out = din("w_out", [D, D])
        ln1_g = din("ln1_g", [1, D]); ln1_b = din("ln1_b", [1, D])
        w_router = din("w_router", [D, NEXP]); b_router = din("b_router", [1, NEXP])
        w_gate = din("w_gate", [NEXP, D, D]); b_gate = din("b_gate", [NEXP, D])
        w_up = din("w_up", [NEXP, D, D]); b_up = din("b_up", [NEXP, D])
        w_down = din("w_down", [NEXP, D, D]); b_down = din("b_down", [NEXP, D])
        ln2_g = din("ln2_g", [1, D]); ln2_b = din("ln2_b", [1, D])
        out_d = nc.dram_tensor("out", [NTOK2, D], F32, kind="ExternalOutput").ap()

    slab = nc.dram_tensor("slab", [256 * NG, 512], BF16).ap()
    gath = nc.dram_tensor("gath", [4 * 256 * NG, 512], BF16).ap()
    slab3 = slab.rearrange("(g f) c -> f g c", f=256)
    vaug_d = nc.dram_tensor("vaug_d", [128, NT, 129], BF16).ap()
    h1_d = nc.dram_tensor("h1_d", [NTOK2, D], F32).ap()
    h1b_d = nc.dram_tensor("h1b_d", [NTOK2, D], BF16).ap()
    tokslot_d = nc.dram_tensor("tokslot_d", [NSLOT + 128, 1], I32).ap()
    ybuf_d = nc.dram_tensor("ybuf_d", [NSLOT, D], F32).ap()
    dbg = {}
    if debug:
        dbg["qT"] = nc.dram_tensor("dbg_qT", [128, NCOL], BF16, kind="ExternalOutput").ap()
        dbg["kT"] = nc.dram_tensor("dbg_kT", [128, NCOL], BF16, kind="ExternalOutput").ap()
        dbg["uT"] = nc.dram_tensor("dbg_uT", [128, NCOL], BF16, kind="ExternalOutput").ap()
        dbg["v"] = nc.dram_tensor("dbg_v", [128, NT, 129], BF16, kind="ExternalOutput").ap()
        dbg["slab"] = nc.dram_tensor("dbg_slab", [256 * NG, 512], BF16, kind="ExternalOutput").ap()
        dbg["Wb"] = nc.dram_tensor("dbg_Wb", [128, 1024], F32, kind="ExternalOutput").ap()
        dbg["ident"] = nc.dram_tensor("dbg_ident", [128, 128], BF16, kind="ExternalOutput").ap()
        dbg["ob"] = nc.dram_tensor("dbg_ob", [128, 4, 128], BF16, kind="ExternalOutput").ap()
        dbg["g08"] = nc.dram_tensor("dbg_g08", [128, 128], F32, kind="ExternalOutput").ap()
        dbg["po"] = nc.dram_tensor("dbg_po", [128, 3, 512], F32, kind="ExternalOutput").ap()
        dbg["rr"] = nc.dram_tensor("dbg_rr", [128, 8], F32, kind="ExternalOutput").ap()
        dbg["pT"] = nc.dram_tensor("dbg_pT", [128, 2, 512], BF16, kind="ExternalOutput").ap()
        if full:
            dbg["h1"] = nc.dram_tensor("dbg_h1", [NTOK2, D], F32, kind="ExternalOutput").ap()
            dbg["lg"] = nc.dram_tensor("dbg_lg", [NTOK2, 32], F32, kind="ExternalOutput").ap()

    thr = _bucket_thresholds()

    with ExitStack() as st0:
        S = Sync(nc, st0)
        V, A_, P_, T_ = nc.vector, nc.scalar, nc.gpsimd, nc.tensor

        def mk(stack):
            def sb(name, shape, dt=F32):
                return stack.enter_context(nc.sbuf_tensor(name, list(shape), dt))

            def ps(name, shape, dt=F32):
                return stack.enter_context(nc.psum_tensor(name, list(shape), dt))
            return sb, ps

        sb0, ps0 = mk(st0)

        ident = sb0("ident", [128, 128], BF16)
        identf = sb0("identf", [128, 128], F32)
        tri = sb0("tri", [128, 128], BF16)
        stri = sb0("stri", [128, 128], BF16)
        ones = sb0("ones", [128, 128], BF16)
        iop = sb0("iop", [128, 1], F32)
        iop_i = sb0("iop_i", [128, 1], I32)
        S.op("pool", lambda: P_.memset(identf[:], 0.0), writes=["identf"])
        S.op("pool", lambda: P_.affine_select(out=identf[:], in_=identf[:], pattern=[[-1, 128]],
                                              compare_op=ALU.not_equal, fill=1.0, base=0, channel_multiplier=1),
             reads=["identf"], writes=["identf"])
        S.op("dve", lambda: V.tensor_copy(out=ident[:], in_=identf[:]), reads=["identf"], writes=["ident"])
        S.op("pool", lambda: P_.memset(ones[:], 1.0), writes=["ones"])
        S.op("pool", lambda: P_.affine_select(out=tri[:], in_=ones[:], pattern=[[1, 128]],
                                              compare_op=ALU.is_ge, fill=0.0, base=0, channel_multiplier=-1),
             reads=["ones"], writes=["tri"])
        S.op("pool", lambda: P_.affine_select(out=stri[:], in_=ones[:], pattern=[[1, 128]],
                                              compare_op=ALU.is_gt, fill=0.0, base=0, channel_multiplier=-1),
             reads=["ones"], writes=["stri"])
        S.op("pool", lambda: P_.iota(iop_i[:], pattern=[[0, 1]], base=0, channel_multiplier=1), writes=["iop_i"])
        S.op("dve", lambda: V.tensor_copy(out=iop[:], in_=iop_i[:]), reads=["iop_i"], writes=["iop"])

        def bc_load(name, src, n, stack_sb, q="sp"):
            t = stack_sb(name, [128, n], F32)
            S.dma(q, lambda: nc.sync.dma_start(out=t[:], in_=src.partition_broadcast(128)), writes=[name])
            return t

        lng = bc_load("lng", ln_in_g[0], D, sb0)
        lnb = bc_load("lnb", ln_in_b[0], D, sb0)

        cnt = {"ln": 0}

        def layernorm(xt, kx, g_bc, kg, b_bc, kb, tmp, kt, out_ap, kout, sbs):
            i = cnt["ln"]; cnt["ln"] += 1
            st6, mv, rstd, nmr = sbs
            ks = ("lnstat", id(st6))
            S.op("dve", lambda: V.bn_stats(out=st6[:, 0:6], in_=xt[:, 0:512]), reads=[kx], writes=[ks])
            S.op("dve", lambda: V.bn_stats(out=st6[:, 6:12], in_=xt[:, 512:1024]), reads=[kx], writes=[ks])
            S.op("dve", lambda: V.bn_aggr(out=mv[:, 0:2], in_=st6[:, 0:12]), reads=[ks], writes=[ks])
            S.op("dve", lambda: V.tensor_scalar(out=rstd[:], in0=mv[:, 1:2], scalar1=LN_EPS, scalar2=None, op0=ALU.add), reads=[ks], writes=[ks])
            S.op("dve", lambda: V.tensor_scalar(out=nmr[:], in0=mv[:, 0:1], scalar1=-1.0, scalar2=None, op0=ALU.mult), reads=[ks], writes=[ks])
            S.op("act", lambda: A_.activation(out=rstd[:], in_=rstd[:], func=AF.Ln), reads=[ks], writes=[ks])
            S.op("act", lambda: A_.activation(out=rstd[:], in_=rstd[:], func=AF.Exp, scale=-0.5), reads=[ks], writes=[ks])
            S.op("act", lambda: A_.activation(out=nmr[:], in_=nmr[:], func=AF.Copy, scale=rstd[:, 0:1]), reads=[ks], writes=[ks])
            S.op("act", lambda: A_.activation(out=tmp[:], in_=xt[:], func=AF.Identity, bias=nmr[:, 0:1], scale=rstd[:, 0:1]),
                 reads=[kx, ks], writes=[kt])
            S.op("dve", lambda: V.tensor_tensor(out=tmp[:], in0=tmp[:], in1=g_bc[:], op=ALU.mult), reads=[kt, kg], writes=[kt])
            S.op("dve", lambda: V.tensor_tensor(out=out_ap, in0=tmp[:], in1=b_bc[:], op=ALU.add), reads=[kt, kb], writes=[kout])

        st_att = ExitStack()
        sbA, psA = mk(st_att)
        qT = sbA("qT", [128, NCOL], BF16)
        kT = sbA("kT", [128, NCOL], BF16)
        st_u = ExitStack()
        sbU, _ = mk(st_u)
        uT = sbU("uT", [128, NCOL], BF16)

        st3 = ExitStack()
        sb, ps = mk(st3)
        PI = math.pi
        Tnr = sb("Tnr", [128, 512], F32); Tni = sb("Tni", [128, 512], F32)
        Ppr = sb("Ppr", [128, 4, 128], F32); Ppi = sb("Ppi", [128, 4, 128], F32)
        Bblk = sb("Bblk", [128, 1024], BF16)
        Cmat = sb("Cmat", [128, 8, 128], BF16)
        dsk = sb("dsk", [128, 1], F32)
        S.dma("sp", lambda: nc.sync.dma_start(out=dsk[:], in_=d_skip.rearrange("o (p q) -> (o p) q", q=1)), writes=["dsk"])
        negpi = sb("negpi", [128, 1], F32)
        S.op("dve", lambda: V.memset(negpi[:], -PI), writes=["negpi"])
        with ExitStack() as stt:
            sbt, _ = mk(stt)
            are = bc_load("are", a_re[0], 512, sbt)
            aim = bc_load("aim", a_im[0], 512, sbt)
            ls8 = bc_load("ls8", log_step[0], 8, sbt)
            S.op("act", lambda: A_.activation(out=ls8[:], in_=ls8[:], func=AF.Exp), reads=["ls8"], writes=["ls8"])
            S.op("dve", lambda: V.tensor_scalar(out=are[:], in0=are[:], scalar1=-1e-4, scalar2=None, op0=ALU.min), reads=["are"], writes=["are"])
            sa = sbt("sa", [128, 512], F32); sp_ = sbt("sp_", [128, 512], F32)
            st8 = ls8[:, 0:8].unsqueeze(2).to_broadcast([128, 8, 64])
            S.op("dve", lambda: V.tensor_tensor(out=sa[:].rearrange("p (g n) -> p g n", n=64), in0=are[:].rearrange("p (g n) -> p g n", n=64),
                                                in1=st8, op=ALU.mult), reads=["are", "ls8"], writes=["sa"])
            S.op("dve", lambda: V.tensor_tensor(out=sp_[:].rearrange("p (g n) -> p g n", n=64), in0=aim[:].rearrange("p (g n) -> p g n", n=64),
                                                in1=st8, op=ALU.mult), reads=["aim", "ls8"], writes=["sp_"])
            t_a = sbt("t_a", [128, 512], F32); t_b = sbt("t_b", [128, 512], F32); t_c = sbt("t_c", [128, 512], F32)
            t_d = sbt("t_d", [128, 512], F32); t_e = sbt("t_e", [128, 512], F32)
            sp1 = sbt("sp1", [128, 1], F32); nsp1 = sbt("nsp1", [128, 1], F32)
            S.op("dve", lambda: V.tensor_scalar(out=sp1[:], in0=iop[:], scalar1=1.0, scalar2=None, op0=ALU.add), reads=["iop"], writes=["sp1"])
            S.op("dve", lambda: V.tensor_scalar(out=nsp1[:], in0=sp1[:], scalar1=-1.0, scalar2=None, op0=ALU.mult), reads=["sp1"], writes=["nsp1"])

            rki = sbt("rki", [128, 512], I32)
            rkf = sbt("rkf", [128, 512], F32)
            C1 = 6.28125
            C2 = 2 * PI - C1

            def reduce_sin(ph, kph, shift, out_ap, kout, scratch, ksc):
                n = ph.shape[-1]
                S.op("dve", lambda: V.tensor_scalar(out=scratch, in0=ph, scalar1=shift, scalar2=1.0 / (2 * PI), op0=ALU.add, op1=ALU.mult),
                     reads=[kph], writes=[ksc])
                S.op("dve", lambda: V.tensor_copy(out=rki[:, 0:n], in_=scratch), reads=[ksc], writes=["rki"])
                S.op("dve", lambda: V.tensor_copy(out=rkf[:, 0:n], in_=rki[:, 0:n]), reads=["rki"], writes=["rkf"])
                S.op("dve", lambda: V.tensor_scalar(out=scratch, in0=ph, scalar1=shift, scalar2=None, op0=ALU.add), reads=[kph], writes=[ksc])
                S.op("dve", lambda: V.scalar_tensor_tensor(out=scratch, in0=rkf[:, 0:n], scalar=-C1, in1=scratch, op0=ALU.mult, op1=ALU.add),
                     reads=["rkf", ksc], writes=[ksc])
                S.op("dve", lambda: V.scalar_tensor_tensor(out=scratch, in0=rkf[:, 0:n], scalar=-C2, in1=scratch, op0=ALU.mult, op1=ALU.add),
                     reads=["rkf", ksc], writes=[ksc])
                S.op("dve", lambda: V.tensor_scalar(out=rkf[:, 0:n], in0=scratch, scalar1=PI, scalar2=-2 * PI, op0=ALU.is_gt, op1=ALU.mult),
                     reads=[ksc], writes=["rkf"])
                S.op("dve", lambda: V.tensor_tensor(out=scratch, in0=scratch, in1=rkf[:, 0:n], op=ALU.add), reads=["rkf", ksc], writes=[ksc])
                S.op("dve", lambda: V.tensor_scalar(out=rkf[:, 0:n], in0=scratch, scalar1=-PI, scalar2=2 * PI, op0=ALU.is_lt, op1=ALU.mult),
                     reads=[ksc], writes=["rkf"])
                S.op("dve", lambda: V.tensor_tensor(out=scratch, in0=scratch, in1=rkf[:, 0:n], op=ALU.add), reads=["rkf", ksc], writes=[ksc])
                S.op("act", lambda: A_.activation(out=out_ap, in_=scratch, func=AF.Sin), reads=[ksc], writes=[kout])

            def sincos(ph, kph, out_s, ks, out_c, kc, scratch, ksc):
                reduce_sin(ph, kph, 0.0, out_s, ks, scratch, ksc)
                reduce_sin(ph, kph, 0.5 * PI, out_c, kc, scratch, ksc)

            S.op("dve", lambda: V.tensor_scalar(out=t_a[:], in0=sa[:], scalar1=nsp1[:, 0:1], scalar2=None, op0=ALU.mult), reads=["sa", "nsp1"], writes=["t_a"])
            S.op("act", lambda: A_.activation(out=t_a[:], in_=t_a[:], func=AF.Exp), reads=["t_a"], writes=["t_a"])
            S.op("dve", lambda: V.tensor_scalar(out=t_b[:], in0=sp_[:], scalar1=sp1[:, 0:1], scalar2=None, op0=ALU.mult), reads=["sp_", "sp1"], writes=["t_b"])
            sincos(t_b[:], "t_b", t_c[:], "t_c", t_d[:], "t_d", t_e[:], "t_e")
            S.op("dve", lambda: V.tensor_tensor(out=Tnr[:], in0=t_a[:], in1=t_d[:], op=ALU.mult), reads=["t_a", "t_d"], writes=["Tnr"])
            S.op("dve", lambda: V.scalar_tensor_tensor(out=Tni[:], in0=t_a[:], scalar=-1.0, in1=t_c[:], op0=ALU.mult, op1=ALU.mult),
                 reads=["t_a", "t_c"], writes=["Tni"])
            S.op("act", lambda: A_.activation(out=t_a[:], in_=sa[:], func=AF.Exp), reads=["sa", "Tnr", "Tni"], writes=["t_a"])
            sincos(sp_[:], "sp_", t_c[:], "t_c", t_d[:], "t_d", t_e[:], "t_e")
            S.op("dve", lambda: V.tensor_tensor(out=t_d[:], in0=t_a[:], in1=t_d[:], op=ALU.mult), reads=["t_a", "t_d"], writes=["t_d"])
            S.op("dve", lambda: V.tensor_scalar(out=t_d[:], in0=t_d[:], scalar1=-1.0, scalar2=None, op0=ALU.add), reads=["t_d"], writes=["t_d"])
            S.op("dve", lambda: V.tensor_tensor(out=t_c[:], in0=t_a[:], in1=t_c[:], op=ALU.mult), reads=["t_a", "t_c"], writes=["t_c"])
            S.op("dve", lambda: V.tensor_tensor(out=t_a[:], in0=are[:], in1=are[:], op=ALU.mult), reads=["are", "t_c", "t_d"], writes=["t_a"])
            S.op("dve", lambda: V.tensor_tensor(out=t_b[:], in0=aim[:], in1=aim[:], op=ALU.mult), reads=["aim"], writes=["t_b"])
            S.op("dve", lambda: V.tensor_tensor(out=t_a[:], in0=t_a[:], in1=t_b[:], op=ALU.add), reads=["t_a", "t_b"], writes=["t_a"])
            S.op("dve", lambda: V.reciprocal(out=t_a[:], in_=t_a[:]), reads=["t_a"], writes=["t_a"])
            S.op("dve", lambda: V.tensor_tensor(out=t_b[:], in0=t_d[:], in1=are[:], op=ALU.mult), reads=["t_d", "are"], writes=["t_b"])
            S.op("dve", lambda: V.tensor_tensor(out=t_e[:], in0=t_c[:], in1=aim[:], op=ALU.mult), reads=["t_c", "aim"], writes=["t_e"])
            S.op("dve", lambda: V.tensor_tensor(out=t_b[:], in0=t_b[:], in1=t_e[:], op=ALU.add), reads=["t_b", "t_e"], writes=["t_b"])
            S.op("dve", lambda: V.tensor_tensor(out=t_b[:], in0=t_b[:], in1=t_a[:], op=ALU.mult), reads=["t_b", "t_a"], writes=["t_b"])
            S.op("dve", lambda: V.tensor_tensor(out=t_e[:], in0=t_c[:], in1=are[:], op=ALU.mult), reads=["t_c", "are", "t_b"], writes=["t_e"])
            S.op("dve", lambda: V.tensor_tensor(out=t_c[:], in0=t_d[:], in1=aim[:], op=ALU.mult), reads=["t_d", "aim", "t_e"], writes=["t_c"])
            S.op("dve", lambda: V.tensor_tensor(out=t_e[:], in0=t_e[:], in1=t_c[:], op=ALU.subtract), reads=["t_e", "t_c"], writes=["t_e"])
            S.op("dve", lambda: V.tensor_tensor(out=t_e[:], in0=t_e[:], in1=t_a[:], op=ALU.mult), reads=["t_e", "t_a"], writes=["t_e"])
            Brr = sbt("Brr", [128, 512], F32); Bri = sbt("Bri", [128, 512], F32)
            S.op("pool", lambda: P_.memset(Brr[:], 0.0), writes=["Brr"])
            S.op("pool", lambda: P_.memset(Bri[:], 0.0), writes=["Bri"])
            for gi in range(8):
                S.dma("sp", lambda: nc.sync.dma_start(out=Brr[16 * gi:16 * gi + 16, 64 * gi:64 * gi + 64], in_=b_re[gi].rearrange("n c -> c n"),
                                                      allow_slow_non_contiguous=True), reads=["Brr"], writes=[("Brr", gi)])
                S.dma("sp", lambda: nc.sync.dma_start(out=Bri[16 * gi:16 * gi + 16, 64 * gi:64 * gi + 64], in_=b_im[gi].rearrange("n c -> c n"),
                                                      allow_slow_non_contiguous=True), reads=["Bri"], writes=[("Bri", gi)])
            bk = [("Brr", gi) for gi in range(8)] + [("Bri", gi) for gi in range(8)]
            S.op("dve", lambda: V.tensor_tensor(out=t_a[:], in0=t_b[:], in1=Brr[:], op=ALU.mult), reads=["t_b", "t_a"] + bk, writes=["t_a"])
            S.op("dve", lambda: V.tensor_tensor(out=t_c[:], in0=t_e[:], in1=Bri[:], op=ALU.mult), reads=["t_e", "t_c"] + bk, writes=["t_c"])
            S.op("dve", lambda: V.tensor_tensor(out=Bblk[:, 0:512], in0=t_a[:], in1=t_c[:], op=ALU.subtract), reads=["t_a", "t_c"], writes=["Bblk"])
            S.op("dve", lambda: V.tensor_tensor(out=t_a[:], in0=t_b[:], in1=Bri[:], op=ALU.mult), reads=["t_b", "t_a", "Bblk"] + bk, writes=["t_a"])
            S.op("dve", lambda: V.tensor_tensor(out=t_c[:], in0=t_e[:], in1=Brr[:], op=ALU.mult), reads=["t_e", "t_c", "Bblk"] + bk, writes=["t_c"])
            S.op("dve", lambda: V.tensor_tensor(out=Bblk[:, 512:1024], in0=t_a[:], in1=t_c[:], op=ALU.add), reads=["t_a", "t_c"], writes=["Bblk"])
            Cst = sbt("Cst", [128, 8, 128], F32)
            S.op("pool", lambda: P_.memset(Cst[:], 0.0), writes=["Cst"])
            ck = []
            for gi in range(8):
                k, gl = gi // 2, gi % 2
                S.dma("sp", lambda: nc.sync.dma_start(out=Cst[64 * gl:64 * gl + 64, k, 16 * gi:16 * gi + 16], in_=c_re[gi].rearrange("c n -> n c"),
                                                      allow_slow_non_contiguous=True), reads=["Cst"], writes=[("Cst", gi, 0)])
                S.dma("sp", lambda: nc.sync.dma_start(out=Cst[64 * gl:64 * gl + 64, 4 + k, 16 * gi:16 * gi + 16], in_=c_im[gi].rearrange("c n -> n c"),
                                                      allow_slow_non_contiguous=True), reads=["Cst"], writes=[("Cst", gi, 1)])
                ck += [("Cst", gi, 0), ("Cst", gi, 1)]
            S.op("dve", lambda: V.tensor_copy(out=Cmat[:, 0:4, :], in_=Cst[:, 0:4, :]), reads=ck, writes=["Cmat"])
            S.op("dve", lambda: V.tensor_scalar(out=Cmat[:, 4:8, :], in0=Cst[:, 4:8, :], scalar1=-1.0, scalar2=None, op0=ALU.mult), reads=ck, writes=["Cmat"])
            arc = sbt("arc", [128, 4], F32); aic = sbt("aic", [128, 4], F32); stc = sbt("stc", [128, 4], F32)
            S.dma("sp", lambda: nc.sync.dma_start(out=arc[:], in_=a_re.rearrange("o (k p) -> (o p) k", p=128), allow_slow_non_contiguous=True), writes=["arc"])
            S.dma("sp", lambda: nc.sync.dma_start(out=aic[:], in_=a_im.rearrange("o (k p) -> (o p) k", p=128), allow_slow_non_contiguous=True), writes=["aic"])
            for gi in range(8):
                k, gl = gi // 2, gi % 2
                S.dma("sp", lambda: nc.sync.dma_start(out=stc[64 * gl:64 * gl + 64, k:k + 1], in_=log_step[0, gi:gi + 1].partition_broadcast(64)),
                      writes=[("stc", gi)])
            S.op("act", lambda: A_.activation(out=stc[:], in_=stc[:], func=AF.Exp), reads=[("stc", gi) for gi in range(8)], writes=["stc"])
            S.op("dve", lambda: V.tensor_scalar(out=arc[:], in0=arc[:], scalar1=-1e-4, scalar2=None, op0=ALU.min), reads=["arc"], writes=["arc"])
            S.op("dve", lambda: V.tensor_tensor(out=arc[:], in0=arc[:], in1=stc[:], op=ALU.mult), reads=["arc", "stc"], writes=["arc"])
            S.op("dve", lambda: V.tensor_tensor(out=aic[:], in0=aic[:], in1=stc[:], op=ALU.mult), reads=["aic", "stc"], writes=["aic"])
            tp1i = sbt("tp1i", [128, 128], I32); tp1 = sbt("tp1", [128, 128], F32)
            S.op("pool", lambda: P_.iota(tp1i[:], pattern=[[1, 128]], base=1, channel_multiplier=0), writes=["tp1i"])
            S.op("dve", lambda: V.tensor_copy(out=tp1[:], in_=tp1i[:]), reads=["tp1i"], writes=["tp1"])
            for k in range(4):
                S.op("dve", lambda: V.tensor_scalar(out=t_a[:, 0:128], in0=tp1[:], scalar1=arc[:, k:k + 1], scalar2=None, op0=ALU.mult),
                     reads=["tp1", "arc", "Bblk"], writes=["t_a"])
                S.op("act", lambda: A_.activation(out=t_a[:, 0:128], in_=t_a[:, 0:128], func=AF.Exp), reads=["t_a"], writes=["t_a"])
                S.op("dve", lambda: V.tensor_scalar(out=t_b[:, 0:128], in0=tp1[:], scalar1=aic[:, k:k + 1], scalar2=None, op0=ALU.mult),
                     reads=["tp1", "aic", "Bblk"], writes=["t_b"])
                sincos(t_b[:, 0:128], "t_b", t_c[:, 0:128], "t_c", t_d[:, 0:128], "t_d", t_e[:, 0:128], "t_e")
                S.op("dve", lambda: V.tensor_tensor(out=Ppr[:, k, :], in0=t_a[:, 0:128], in1=t_d[:, 0:128], op=ALU.mult), reads=["t_a", "t_d"], writes=["Ppr"])
                S.op("dve", lambda: V.tensor_tensor(out=Ppi[:, k, :], in0=t_a[:, 0:128], in1=t_c[:, 0:128], op=ALU.mult), reads=["t_a", "t_c"], writes=["Ppi"])
            for e in ("pe", "act", "dve", "pool", "sp"):
                S.wait_all(e)

        pbu = ps("pbu", [128, 2, 512], F32)
        pz = [ps("pz0", [128, 8, 128], F32)] * 2
        py = ps("py", [128, 512], F32)
        wq = [[sb(f"w{k}_{i}", [128, 512], F32) for k in range(4)] for i in range(2)]
        Wbs = [sb(f"Wbs{i}", [128, 1024], BF16) for i in range(2)]
        zp = sb("zp", [128, 8, 128], F32)
        xq = [[sb(f"x{k}_0", [128, 4, 128], F32) for k in range(4)]] * 2
        XTs = [sb(f"XT{i}", [128, 8, 128], BF16) for i in range(2)]
        car = [sb(f"car{i}", [128, 8], F32) for i in range(2)]
        yf = sb("yf", [128, 512], F32); y2 = sb("y2", [128, 512], F32); ysg = sb("ysg", [128, 512], F32)
        ybs = [sb(f"yb{i}", [128, 512], BF16) for i in range(2)]
        S.op("dve", lambda: V.memset(car[0][:], 0.0), writes=[("car", 0)])

        def bu(ct):
            for half in range(2):
                S.op("pe", lambda: T_.matmul(pbu[:, half, :], lhsT=uT[:, ct * 128:(ct + 1) * 128], rhs=Bblk[:, half * 512:(half + 1) * 512],
                                             start=True, stop=True), reads=[("uT", (ct + 3) // 4), "Bblk"], writes=["pbu"])

        def wmod(ct):
            b2 = ct % 2
            w1, w2, w3, w4_ = wq[b2]
            kw = ("wq", b2)
            S.op("dve", lambda: V.tensor_tensor(out=w1[:], in0=pbu[:, 0, :], in1=Tnr[:], op=ALU.mult), reads=["pbu", "Tnr"], writes=[kw])
            S.op("dve", lambda: V.tensor_tensor(out=w2[:], in0=pbu[:, 1, :], in1=Tni[:], op=ALU.mult), reads=["pbu", "Tni"], writes=[kw])
            S.op("dve", lambda: V.tensor_tensor(out=w3[:], in0=pbu[:, 1, :], in1=Tnr[:], op=ALU.mult), reads=["pbu", "Tnr"], writes=[kw])
            S.op("dve", lambda: V.tensor_tensor(out=w4_[:], in0=pbu[:, 0, :], in1=Tni[:], op=ALU.mult), reads=["pbu", "Tni"], writes=[kw])
            S.op("pool", lambda: P_.tensor_tensor(out=Wbs[b2][:, 0:512], in0=w1[:], in1=w2[:], op=ALU.subtract), reads=[kw], writes=[("Wbs", b2)])
            S.op("pool", lambda: P_.tensor_tensor(out=Wbs[b2][:, 512:1024], in0=w3[:], in1=w4_[:], op=ALU.add), reads=[kw], writes=[("Wbs", b2)])

        def zmm(ct):
            b2 = ct % 2
            for k in range(8):
                S.op("pe", lambda: T_.matmul(pz[b2][:, k, :], lhsT=Wbs[b2][:, k * 128:(k + 1) * 128], rhs=tri[:], start=True, stop=True),
                     reads=[("Wbs", b2), "tri"], writes=[("pz", 0)])

        def xmod(ct):
            b2 = ct % 2
            xa, xb_, xc, xd = xq[b2]
            kx = ("xq", 0)
            cin, cout = car[b2], car[1 - b2]
            S.op("dve", lambda: V.tensor_tensor(out=zp[:], in0=pz[b2][:], in1=cin[:, 0:8].unsqueeze(2).to_broadcast([128, 8, 128]), op=ALU.add),
                 reads=[("pz", 0), ("car", b2)], writes=["zp"])
            S.op("dve", lambda: V.tensor_tensor(out=xa[:], in0=zp[:, 0:4, :], in1=Ppr[:], op=ALU.mult), reads=["zp", "Ppr"], writes=[kx])
            S.op("dve", lambda: V.tensor_tensor(out=xb_[:], in0=zp[:, 4:8, :], in1=Ppi[:], op=ALU.mult), reads=["zp", "Ppi"], writes=[kx])
            S.op("dve", lambda: V.tensor_tensor(out=xc[:], in0=zp[:, 0:4, :], in1=Ppi[:], op=ALU.mult), reads=["zp", "Ppi"], writes=[kx])
            S.op("dve", lambda: V.tensor_tensor(out=xd[:], in0=zp[:, 4:8, :], in1=Ppr[:], op=ALU.mult), reads=["zp", "Ppr"], writes=[kx])
            S.op("dve", lambda: V.tensor_tensor(out=cout[:, 0:4], in0=xa[:, :, 127], in1=xb_[:, :, 127], op=ALU.subtract),
                 reads=[kx], writes=[("car", 1 - b2)])
            S.op("dve", lambda: V.tensor_tensor(out=cout[:, 4:8], in0=xc[:, :, 127], in1=xd[:, :, 127], op=ALU.add),
                 reads=[kx], writes=[("car", 1 - b2)])
            if ct == 0:
                return
            S.op("pool", lambda: P_.tensor_tensor(out=XTs[b2][:, 0:4, :], in0=xa[:], in1=xb_[:], op=ALU.subtract), reads=[kx], writes=[("XT", b2)])
            S.op("pool", lambda: P_.tensor_tensor(out=XTs[b2][:, 4:8, :], in0=xc[:], in1=xd[:], op=ALU.add), reads=[kx], writes=[("XT", b2)])

        def ymm(ct):
            if ct == 0:
                return
            b2 = ct % 2
            ci = (ct - 1) % 4
            for k in range(8):
                S.op("pe", lambda: T_.matmul(py[:, ci * 128:(ci + 1) * 128], lhsT=Cmat[:, k, :], rhs=XTs[b2][:, k, :], start=(k == 0), stop=(k == 7)),
                     reads=["Cmat", ("XT", b2)], writes=["py"])
            if ci == 3:
                gq = (ct - 1) // 4
                c0 = 128 + 512 * gq
                yb = ybs[gq % 2]; kyb = ("yb", gq % 2)
                S.op("dve", lambda: V.scalar_tensor_tensor(out=yf[:], in0=uT[:, c0:c0 + 512], scalar=dsk[:, 0:1], in1=py[:], op0=ALU.mult, op1=ALU.add),
                     reads=[("uT", gq + 1), "dsk", "py"], writes=["yf"])
                S.op("pool", lambda: P_.tensor_tensor(out=y2[:], in0=yf[:], in1=yf[:], op=ALU.mult), reads=["yf"], writes=["y2"])
                S.op("pool", lambda: P_.tensor_scalar(out=y2[:], in0=y2[:], scalar1=0.044715, scalar2=1.0, op0=ALU.mult, op1=ALU.add), reads=["y2"], writes=["y2"])
                S.op("pool", lambda: P_.tensor_tensor(out=y2[:], in0=y2[:], in1=yf[:], op=ALU.mult), reads=["y2", "yf"], writes=["y2"])
                S.op("act", lambda: A_.activation(out=ysg[:], in_=y2[:], func=AF.Sigmoid, scale=1.5957691216057308), reads=["y2"], writes=["ysg"])
                S.op("pool", lambda: P_.tensor_tensor(out=yb[:], in0=yf[:], in1=ysg[:], op=ALU.mult), reads=["yf", "ysg"], writes=[kyb])
                S.dma("sp", lambda: nc.sync.dma_start(out=slab3[128:256, gq, :], in_=yb[:]), reads=[kyb], writes=[("slabY", gq)])


        with ExitStack() as st1:
            sb, ps = mk(st1)
            w4b = sb("w4b", [128, 8, 512], BF16)
            biasq = sb("biasq", [128, 1], F32); biask = sb("biask", [128, 1], F32); biasu = sb("biasu", [128, 1], F32)
            bv_bc = sb("bv_bc", [128, 128], F32)
            with ExitStack() as stw:
                sbw, psw = mk(stw)
                wf = sbw("wf", [128, 8, 512], F32)
                gcol = sbw("gcol", [128, 8], F32); bcol = sbw("bcol", [128, 8], F32)
                bcr = sbw("bcr", [128, 8, 128], F32)
                pb = psw("pb", [128, 4], F32)
                pbv = psw("pbv", [128, 128], F32)
                S.dma("sp", lambda: nc.sync.dma_start(out=wf[:], in_=w4.rearrange("(k p) n -> p k n", p=128)), writes=["wf"])
                S.dma("sp", lambda: nc.sync.dma_start(out=gcol[:], in_=ln_in_g.rearrange("o (k p) -> (o p) k", p=128), allow_slow_non_contiguous=True), writes=["gcol"])
                S.dma("sp", lambda: nc.sync.dma_start(out=bcol[:], in_=ln_in_b.rearrange("o (k p) -> (o p) k", p=128), allow_slow_non_contiguous=True), writes=["bcol"])
                for k in range(8):
                    S.op("dve", lambda: V.tensor_scalar(out=w4b[:, k, :], in0=wf[:, k, :], scalar1=gcol[:, k:k + 1], scalar2=None, op0=ALU.mult),
                         reads=["wf", "gcol"], writes=["w4b"])
                    S.op("pool", lambda: P_.tensor_copy(out=bcr[:, k, :], in_=bcol[:, k:k + 1].to_broadcast([128, 128])), reads=["bcol"], writes=["bcr"])
                for bi, (dstb, kb, sc) in enumerate(((biasq, "biasq", 0.125), (biask, "biask", 1.0), (None, None, None), (biasu, "biasu", 1.0))):
                    if dstb is None:
                        continue
                    for k in range(8):
                        S.op("pe", lambda: T_.matmul(pb[:, bi:bi + 1], lhsT=wf[:, k, bi * 128:(bi + 1) * 128], rhs=bcol[:, k:k + 1],
                                                     start=(k == 0), stop=(k == 7)), reads=["wf", "bcol"], writes=["pb"])
                    S.op("dve", lambda: V.tensor_scalar(out=dstb[:], in0=pb[:, bi:bi + 1], scalar1=sc, scalar2=None, op0=ALU.mult), reads=["pb"], writes=[kb])
                for k in range(8):
                    S.op("pe", lambda: T_.matmul(pbv[:], lhsT=bcr[:, k, :], rhs=wf[:, k, 256:384], start=(k == 0), stop=(k == 7)),
                         reads=["wf", "bcr"], writes=["pbv"])
                S.op("dve", lambda: V.tensor_copy(out=bv_bc[:], in_=pbv[:]), reads=["pbv"], writes=["bv_bc"])
                for e in ("pe", "act", "dve", "pool", "sp"):
                    S.wait_all(e)
            NB = 3
            xts = [sb(f"xt{i}", [128, D], F32) for i in range(NB)]
            tmps = [None] * NB
            hbs = [sb(f"hb{i}", [128, D], BF16) for i in range(NB)]
            hTs = [sb(f"hT{i}", [128, 8, 512], BF16) for i in range(2)]
            Vst = [sb(f"Vst{i}", [128, 4, 129], BF16) for i in range(2)]
            for i_ in range(2):
                S.op("pool", lambda: P_.memset(Vst[i_][:, :, 128:129], 1.0), writes=[("Vst1", i_)])
            lnsb = [(sb(f"st6_{i}", [128, 12], F32), sb(f"mv_{i}", [128, 2], F32), sb(f"rstd_{i}", [128, 1], F32),
                     sb(f"nmr_{i}", [128, 1], F32)) for i in range(NB)]
            ptr = [ps("ptr0", [128, 8, 128], BF16)] * 2
            pproj = [ps("pproj0", [128, 512], F32)] * 2
            pv = ps("pv", [128, 4, 128], F32)
            groups = [[0]] + [list(range(1 + 4 * g, 5 + 4 * g)) for g in range(NG)]
            flat = [(gi, ti, ct) for gi, tiles in enumerate(groups) for ti, ct in enumerate(tiles)]
            pcnt = [0]

            def stA(t):
                gi, ti, ct = flat[t]
                s = t % NB
                xt, tmp, hb = xts[s], tmps[s], hbs[s]
                if ct == 0:
                    S.op("dve", lambda: V.memset(xt[0:112, :], 0.0), writes=[("xt", s)])
                    S.dma("sp", lambda: nc.sync.dma_start(out=xt[112:128, :], in_=meta), writes=[("xt", s)])
                else:
                    S.dma("sp", lambda: nc.sync.dma_start(out=xt[:], in_=x_b[(ct - 1) * 128:ct * 128, :]), writes=[("xt", s)])
                st6, mv, rstd, nmr = lnsb[s]
                ks = ("lnstat", id(st6))
                kx = ("xt", s)
                S.op("dve", lambda: V.bn_stats(out=st6[:, 0:6], in_=xt[:, 0:512]), reads=[kx], writes=[ks])
                S.op("dve", lambda: V.bn_stats(out=st6[:, 6:12], in_=xt[:, 512:1024]), reads=[kx], writes=[ks])
                S.op("dve", lambda: V.bn_aggr(out=mv[:, 0:2], in_=st6[:, 0:12]), reads=[ks], writes=[ks])
                S.op("dve", lambda: V.tensor_scalar(out=rstd[:], in0=mv[:, 1:2], scalar1=LN_EPS, scalar2=None, op0=ALU.add), reads=[ks], writes=[ks])
                S.op("dve", lambda: V.tensor_scalar(out=nmr[:], in0=mv[:, 0:1], scalar1=-1.0, scalar2=None, op0=ALU.mult), reads=[ks], writes=[ks])
                S.op("act", lambda: A_.activation(out=rstd[:], in_=rstd[:], func=AF.Ln), reads=[ks], writes=[ks])
                S.op("act", lambda: A_.activation(out=rstd[:], in_=rstd[:], func=AF.Exp, scale=-0.5), reads=[ks], writes=[ks])
                S.op("act", lambda: A_.activation(out=nmr[:], in_=nmr[:], func=AF.Copy, scale=rstd[:, 0:1]), reads=[ks], writes=[ks])
                S.op("act", lambda: A_.activation(out=hb[:], in_=xt[:], func=AF.Identity, bias=nmr[:, 0:1], scale=rstd[:, 0:1]),
                     reads=[kx, ks], writes=[("hb", s)])

            def stB(t):
                gi, ti, ct = flat[t]
                s = t % NB
                hb = hbs[s]
                hT = hTs[gi % 2]; khT = ("hT", gi % 2)
                pt = ptr[0]
                for k in range(8):
                    S.op("pe", lambda: T_.transpose(out=pt[:, k, :], in_=hb[:, k * 128:(k + 1) * 128], identity=ident[:]),
                         reads=[("hb", s), "ident"], writes=[("ptr", 0)])
                S.op("act", lambda: A_.copy(out=hT[:, :, ti * 128:(ti + 1) * 128], in_=pt[:]),
                     reads=[("ptr", 0)], writes=[khT])

            def stC(gi):
                tiles = groups[gi]
                hT = hTs[gi % 2]; khT = ("hT", gi % 2)
                ncols = 128 * len(tiles)
                c0 = tiles[0] * 128
                for bi, (dst, kd) in enumerate(((qT, "qT"), (kT, "kT"), (None, None), (uT, ("uT", gi)))):
                    if dst is None:
                        continue
                    pp = pproj[0]; kp = ("pproj", 0)
                    for k in range(8):
                        S.op("pe", lambda: T_.matmul(pp[:, 0:ncols], lhsT=w4b[:, k, bi * 128:(bi + 1) * 128], rhs=hT[:, k, 0:ncols],
                                                     start=(k == 0), stop=(k == 7)), reads=["w4b", khT], writes=[kp])
                    if bi == 0:
                        S.op("act", lambda: A_.activation(out=dst[:, c0:c0 + ncols], in_=pp[:, 0:ncols], func=AF.Identity, bias=biasq[:, 0:1], scale=0.125),
                             reads=[kp, "biasq"], writes=[kd])
                    else:
                        bb_ = biask if bi == 1 else biasu
                        S.op("dve", lambda: V.tensor_scalar(out=dst[:, c0:c0 + ncols], in0=pp[:, 0:ncols], scalar1=bb_[:, 0:1], scalar2=None, op0=ALU.add),
                             reads=[kp, "biask", "biasu"], writes=[kd])
                        if bi == 3 and gi == 0:
                            S.op("dve", lambda: V.memset(uT[:, 0:112], 0.0), reads=[kd], writes=[kd])
                for ti, ct in enumerate(tiles):
                    for k in range(8):
                        S.op("pe", lambda: T_.matmul(pv[:, ti, :], lhsT=hT[:, k, ti * 128:(ti + 1) * 128], rhs=w4b[:, k, 256:384],
                                                     start=(k == 0), stop=(k == 7)), reads=["w4b", khT], writes=["pv"])
                nt_ = len(tiles)
                vs_ = gi % 2
                S.op("dve", lambda: V.tensor_tensor(out=Vst[vs_][:, 0:nt_, 0:128], in0=pv[:, 0:nt_, :],
                                                    in1=bv_bc[:, 0:128].unsqueeze(1).to_broadcast([128, nt_, 128]), op=ALU.add),
                     reads=["pv", "bv_bc"], writes=[("Vst", vs_)])
                S.dma("sp", lambda: nc.sync.dma_start(out=vaug_d[:, tiles[0]:tiles[0] + nt_, :], in_=Vst[vs_][:, 0:nt_, :]),
                      reads=[("Vst", vs_), ("Vst1", vs_)], writes=[("vaug_d", gi)])

            nfl = len(flat)
            stA(0)
            if nfl > 1:
                stA(1)
            ssm_next = [0]

            def ssm_run(upto):
                while ssm_next[0] < upto:
                    ct_ = ssm_next[0]
                    if ct_ == 0:
                        bu(0)
                        wmod(0)
                    if ct_ + 1 < NT:
                        bu(ct_ + 1)
                    zmm(ct_)
                    if ct_ >= 1:
                        ymm(ct_ - 1)
                    if ct_ + 1 < NT:
                        wmod(ct_ + 1)
                    xmod(ct_)
                    ssm_next[0] += 1

            print("SBUF bytes remaining in fused phase:", nc.sbuf_bytes_remaining, flush=True)
            allowed = 0
            for t in range(nfl):
                if t + 2 < nfl:
                    stA(t + 2)
                stB(t)
                gi, ti, ct = flat[t]
                if ti == len(groups[gi]) - 1:
                    stC(gi)
                    if gi >= 1:
                        allowed = groups[gi - 1][-1]
                if ssm_next[0] < allowed:
                    ssm_run(ssm_next[0] + 1)
                    if allowed - ssm_next[0] > 4:
                        ssm_run(ssm_next[0] + 1)
            ssm_run(NT)
            ymm(NT - 1)
            if debug:
                S.dma("sp", lambda: nc.sync.dma_start(out=dbg["qT"], in_=qT[:]), reads=["qT"], writes=["dq"])
                S.dma("sp", lambda: nc.sync.dma_start(out=dbg["kT"], in_=kT[:]), reads=["kT"], writes=["dk"])
            for e in ("pe", "act", "dve", "pool", "sp"):
                S.wait_all(e)
        st3.close()
        st_u.close()
        print("P1a+S5 done: inst", S.n_inst, "waits", S.n_wait, "sems", S.nsem, flush=True)

        XCH = min(1024, 128 * NG)
        NXC = 256 * NG // XCH

        GPC = XCH // 256

        def ag_chunk(c_):
            keys = []
            for g_ in range(c_ * GPC, (c_ + 1) * GPC):
                keys += [("slabA", g_), ("slabY", g_)]
            S.waitfor("pool", keys)
            S.op("pool", lambda: P_.collective_compute("AllGather", ALU.bypass, replica_groups=[[0, 1, 2, 3], [4, 5, 6, 7]],
                                                       ins=[slab[c_ * XCH:(c_ + 1) * XCH, :].opt()],
                                                       outs=[gath[c_ * 4 * XCH:(c_ + 1) * 4 * XCH, :].opt()]), writes=[("gath", c_)])

        with ExitStack() as st2:
            sb, ps = mk(st2)
            Vaug = sb("Vaug", [128, NT, 129], BF16)
            S.dma("sp", lambda: nc.sync.dma_start(out=Vaug[:], in_=vaug_d), reads=[("vaug_d", gi_) for gi_ in range(NG + 1)], writes=["Vaug"])
            tb = bc_load("tb", relb[0], 32, sb)
            Wb = sb("Wb", [128, 1024], F32)
            Bm0 = sb("Bm0", [128, 512], F32)
            with ExitStack() as stt:
                sbt, _ = mk(stt)
                reli = sbt("reli", [128, 1024], I32)
                relv = sbt("relv", [128, 1024], F32)
                stp = sbt("stp", [128, 1024], F32)
                iom = sbt("iom", [128, 1024], F32)
                dl = sbt("dl", [128, 32], F32)
                thrp = sbt("thrp", [128, 1], F32)
                S.op("pool", lambda: P_.iota(reli[:], pattern=[[-1, 1024]], base=384, channel_multiplier=1), writes=["reli"])
                S.op("dve", lambda: V.tensor_copy(out=relv[:], in_=reli[:]), reads=["reli"], writes=["relv"])
                S.op("pool", lambda: P_.iota(reli[:], pattern=[[1, 1024]], base=0, channel_multiplier=0), reads=["relv"], writes=["reli"])
                S.op("dve", lambda: V.tensor_copy(out=iom[:], in_=reli[:]), reads=["reli"], writes=["iom"])
                steps = []
                for j in range(15, 8, -1):
                    steps.append((-thr[j] + 1, j - 1))
                for n in range(7, -1, -1):
                    steps.append((-n, n))
                for n in range(1, 8):
                    steps.append((n, 16 + n))
                steps.append((8, 24))
                for j in range(9, 16):
                    steps.append((thr[j], 16 + j))
                prev = 15
                S.op("dve", lambda: V.tensor_scalar(out=Wb[:], in0=relv[:], scalar1=0.0, scalar2=tb[:, 15:16],
                                                    op0=ALU.mult, op1=ALU.add), reads=["relv", "tb"], writes=["Wb"])
                for si, (tv, bk) in enumerate(steps):
                    S.op("dve", lambda: V.tensor_tensor(out=dl[:, si:si + 1], in0=tb[:, bk:bk + 1], in1=tb[:, prev:prev + 1],
                                                        op=ALU.subtract), reads=["tb"], writes=["dl"])
                    S.op("dve", lambda: V.tensor_scalar(out=stp[:], in0=relv[:], scalar1=float(tv), scalar2=dl[:, si:si + 1],
                                                        op0=ALU.is_ge, op1=ALU.mult), reads=["relv", "dl"], writes=["stp"])
                    S.op("dve", lambda: V.tensor_tensor(out=Wb[:], in0=Wb[:], in1=stp[:], op=ALU.add), reads=["stp", "Wb"], writes=["Wb"])
                    prev = bk
                S.op("pool", lambda: P_.affine_select(out=Wb[0:64, :], in_=Wb[0:64, :], pattern=[[1, 1024]], compare_op=ALU.is_ge,
                                                      fill=NEG, base=-384, channel_multiplier=0), reads=["Wb"], writes=["Wb"])
                S.op("pool", lambda: P_.affine_select(out=Wb[64:128, :], in_=Wb[64:128, :], pattern=[[1, 1024]], compare_op=ALU.is_ge,
                                                      fill=NEG, base=-448, channel_multiplier=0), reads=["Wb"], writes=["Wb"])
                S.op("dve", lambda: V.tensor_copy(out=Bm0[:], in_=Wb[:, 512:1024]), reads=["Wb"], writes=["Bm0"])
                S.op("dve", lambda: V.memset(Bm0[0:112, :], NEG), reads=["Bm0"], writes=["Bm0"])
                for e in ("pe", "act", "dve", "pool", "sp"):
                    S.wait_all(e)
            Wbb = sb("Wbb", [128, 1024], BF16)
            Bm0b = sb("Bm0b", [128, 512], BF16)
            S.op("dve", lambda: V.tensor_copy(out=Wbb[:], in_=Wb[:]), reads=["Wb"], writes=["Wbb"])
            S.op("dve", lambda: V.tensor_copy(out=Bm0b[:], in_=Bm0[:]), reads=["Bm0"], writes=["Bm0b"])
            bfar = sb("bfar", [128, 1], F32)
            bfarm = sb("bfarm", [128, 1], F32)
            S.op("dve", lambda: V.tensor_copy(out=bfar[:], in_=tb[:, 15:16]), reads=["tb"], writes=["bfar"])
            S.op("dve", lambda: V.tensor_copy(out=bfarm[:], in_=tb[:, 15:16]), reads=["tb"], writes=["bfarm"])
            S.op("dve", lambda: V.memset(bfarm[0:112, :], NEG), reads=["bfarm"], writes=["bfarm"])
            lamt = sb("lamt", [128, 4, 64], F32)
            S.dma("sp", lambda: nc.sync.dma_start(out=lamt[:], in_=lam4.rearrange("a n -> (a n)").partition_broadcast(128)), writes=["lamt"])
            lsum = sb("lsum", [128, 2], F32)
            lprod = sb("lprod", [128, 2, 64], F32)
            neglam = sb("neglam", [128, 1], F32)
            S.op("dve", lambda: V.tensor_tensor(out=lprod[:, 0, :], in0=lamt[:, 0, :], in1=lamt[:, 1, :], op=ALU.mult), reads=["lamt"], writes=["lprod"])
            S.op("dve", lambda: V.tensor_tensor(out=lprod[:, 1, :], in0=lamt[:, 2, :], in1=lamt[:, 3, :], op=ALU.mult), reads=["lamt"], writes=["lprod"])
            S.op("dve", lambda: V.reduce_sum(out=lsum[:], in_=lprod[:], axis=AX.X), reads=["lprod"], writes=["lsum"])
            S.op("act", lambda: A_.activation(out=lsum[:], in_=lsum[:], func=AF.Exp), reads=["lsum"], writes=["lsum"])
            S.op("dve", lambda: V.tensor_tensor(out=neglam[:], in0=lsum[:, 1:2], in1=lsum[:, 0:1], op=ALU.subtract), reads=["lsum"], writes=["neglam"])
            S.op("dve", lambda: V.tensor_scalar(out=neglam[:], in0=neglam[:], scalar1=-0.2, scalar2=None, op0=ALU.add), reads=["neglam"], writes=["neglam"])
            g08 = bc_load("g08", subln_g[0], 128, sb)
            S.op("dve", lambda: V.tensor_scalar(out=g08[:], in0=g08[:], scalar1=0.8, scalar2=None, op0=ALU.mult), reads=["g08"], writes=["g08"])

            pss = [ps(f"pss{i}", [128, 2, 512], F32) for i in range(2)]
            po = ps("po", [128, 3, 512], F32)
            ptt = ps("ptt", [128, 4, 128], BF16)
            pTs = [sb(f"pT{i}", [128, 2, 512], BF16) for i in range(3)]
            sn = [sb(f"sn{i}", [128, 2, 512], F32) for i in range(2)]
            osb = sb("osb", [128, 128], F32)
            o2 = sb("o2", [128, 128], F32)
            ob = sb("ob", [128, 4, 128], BF16)
            rr = sb("rr", [128, 8], F32)
            attT = [sb(f"attT{i}", [128, 512], BF16) for i in range(2)]

            def acc(a):
                return po[:, a // 3, (a % 3) * 129:(a % 3) * 129 + 129]

            o4 = sb("o4", [128, 4, 128], F32)
            ms = sb("ms", [128, 4], F32)
            units = [(g, j) for g in range(NG) for j in range(4 * g + 5)]
            ncnt = [0]

            def near_of(g, j):
                if j == 0:
                    return Bm0b[:, :] if g == 0 else None
                if j < 4 * g:
                    return None
                tp = j - (4 * g + 1)
                return Wbb[:, 384 - 128 * tp:384 - 128 * tp + 512]

            def qk(i):
                g, j = units[i]
                u = i % 2
                q0 = 128 + 512 * g
                nb = near_of(g, j)
                for m in range(2):
                    S.op("pe", lambda: T_.matmul(pss[u][:, m, :], lhsT=kT[64 * m:64 * m + 64, j * 128:(j + 1) * 128],
                                                 rhs=qT[64 * m:64 * m + 64, q0:q0 + 512], start=True, stop=(nb is None)),
                         reads=["kT", "qT"], writes=[("pss", u)])
                    if nb is not None:
                        S.op("pe", lambda: T_.matmul(pss[u][:, m, :], lhsT=ident[:], rhs=nb, start=False, stop=True),
                             reads=["ident", "Wbb", "Bm0b"], writes=[("pss", u)])

            def ex(i):
                g, j = units[i]
                u = i % 2
                v3 = i % 3
                pS, pT = pss[u], pTs[v3]
                if j == 0 and g > 0:
                    S.op("act", lambda: A_.activation(out=pT[:], in_=pS[:], func=AF.Exp, bias=bfarm[:, 0:1], scale=1.0),
                         reads=[("pss", u), "bfarm"], writes=[("pT", v3)])
                elif 0 < j < 4 * g:
                    S.op("act", lambda: A_.activation(out=pT[:], in_=pS[:], func=AF.Exp, bias=bfar[:, 0:1], scale=1.0),
                         reads=[("pss", u), "bfar"], writes=[("pT", v3)])
                else:
                    S.op("act", lambda: A_.activation(out=pT[:], in_=pS[:], func=AF.Exp), reads=[("pss", u)], writes=[("pT", v3)])

            def pv(i):
                g, j = units[i]
                u = i % 3
                last = 4 * g + 4
                for m in range(2):
                    for qs in range(4):
                        S.op("pe", lambda: T_.matmul(acc(m * 4 + qs), lhsT=pTs[u][:, m, qs * 128:(qs + 1) * 128], rhs=Vaug[:, j, :],
                                                     start=(j == 0 and (m * 4 + qs) % 3 == 0), stop=(j == last), skip_group_check=True),
                             reads=[("pT", u), "Vaug"], writes=["po"])

            posn = sb("posn", [128, 3, 512], F32)

            def accs_(a):
                return posn[:, a // 3, (a % 3) * 129:(a % 3) * 129 + 129]

            def fin_dve1(g):
                S.op("dve", lambda: V.tensor_copy(out=posn[:, 0:2, 0:387], in_=po[:, 0:2, 0:387]), reads=["po"], writes=["posn"])
                S.op("dve", lambda: V.tensor_copy(out=posn[:, 2, 0:258], in_=po[:, 2, 0:258]), reads=["po"], writes=["posn"])
                for a in range(8):
                    S.op("dve", lambda: V.reciprocal(out=rr[:, a:a + 1], in_=accs_(a)[:, 128:129]), reads=["posn"], writes=["rr"])
                S.op("dve", lambda: V.tensor_scalar(out=rr[:, 4:8], in0=rr[:, 4:8], scalar1=neglam[:, 0:1], scalar2=None, op0=ALU.mult),
                     reads=["rr", "neglam"], writes=["rr"])
                for qs in range(4):
                    S.op("dve", lambda: V.tensor_scalar(out=o4[:, qs, :], in0=accs_(qs)[:, 0:128], scalar1=rr[:, qs:qs + 1], scalar2=None, op0=ALU.mult),
                         reads=["posn", "rr"], writes=["o4"])
                    S.op("dve", lambda: V.scalar_tensor_tensor(out=o4[:, qs, :], in0=accs_(4 + qs)[:, 0:128], scalar=rr[:, 4 + qs:5 + qs], in1=o4[:, qs, :],
                                                               op0=ALU.mult, op1=ALU.add), reads=["posn", "rr", "o4"], writes=["o4"])
                    S.op("dve", lambda: V.tensor_tensor(out=o2[:], in0=o4[:, qs, :], in1=o4[:, qs, :], op=ALU.mult), reads=["o4"], writes=["o2"])
                    S.op("dve", lambda: V.reduce_sum(out=ms[:, qs:qs + 1], in_=o2[:], axis=AX.X), reads=["o2", "ms"], writes=["ms"])
                S.op("dve", lambda: V.tensor_scalar(out=ms[:], in0=ms[:], scalar1=1.0 / 128.0, scalar2=LN_EPS, op0=ALU.mult, op1=ALU.add),
                     reads=["ms"], writes=["ms"])

            def fin_rest(g):
                aT = attT[g % 2]; kaT = ("attT", g % 2)
                S.op("act", lambda: A_.activation(out=ms[:], in_=ms[:], func=AF.Ln), reads=["ms"], writes=["ms"])
                S.op("act", lambda: A_.activation(out=ms[:], in_=ms[:], func=AF.Exp, scale=-0.5), reads=["ms"], writes=["ms"])
                for qs in range(4):
                    S.op("dve", lambda: V.scalar_tensor_tensor(out=ob[:, qs, :], in0=o4[:, qs, :], scalar=ms[:, qs:qs + 1], in1=g08[:],
                                                               op0=ALU.mult, op1=ALU.mult), reads=["o4", "ms", "g08"], writes=["ob"])
                for qs in range(4):
                    S.op("pe", lambda: T_.transpose(out=ptt[:, qs, :], in_=ob[:, qs, :], identity=ident[:]), reads=["ob", "ident"], writes=["ptt"])
                S.op("dve", lambda: V.tensor_copy(out=aT[:], in_=ptt[:].rearrange("p a b -> p (a b)")), reads=["ptt"], writes=[kaT])
                S.dma("sp", lambda: nc.sync.dma_start(out=slab3[0:128, g, :], in_=aT[:]), reads=[kaT], writes=[("slabA", g)])
                if (g + 1) % GPC == 0:
                    ag_chunk(g // GPC)

            qk(0)
            pending = None
            nun = len(units)
            for i in range(nun + 1):
                if i + 1 < nun:
                    qk(i + 1)
                if i < nun:
                    ex(i)
                if i >= 1:
                    pv(i - 1)
                    gp, jp = units[i - 1]
                    if pending is not None and jp == 3:
                        fin_rest(pending)
                        pending = None
                    if jp == 4 * gp + 4:
                        fin_dve1(gp)
                        pending = gp
            fin_rest(pending)
            for e in ("pe", "act", "dve", "pool", "sp"):
                S.wait_all(e)
        st_att.close()
        print("P1b done: inst", S.n_inst, "waits", S.n_wait, "sems", S.nsem, flush=True)
        if debug:
            S.waitfor("sp", [("slabA", g) for g in range(NG)] + [("slabY", g) for g in range(NG)])
            S.dma("sp", lambda: nc.sync.dma_start(out=dbg["slab"], in_=slab), writes=["dslab"])
        if not full:
            for e in ("pe", "act", "dve", "pool", "sp"):
                S.wait_all(e)
            return nc
        S.waitfor("pool", [("gath", c_) for c_ in range(NXC)])

        def barrier():
            for e_ in ("pe", "act", "dve", "pool", "sp"):
                S.wait_all(e_)

        IOA = bass.IndirectOffsetOnAxis
        with ExitStack() as st4:
            sb, ps = mk(st4)
            g1 = bc_load("g1", ln1_g[0], D, sb); b1 = bc_load("b1", ln1_b[0], D, sb)
            g2 = bc_load("g2", ln2_g[0], D, sb); b2 = bc_load("b2", ln2_b[0], D, sb)
            slot_ga = sb("slot_ga", [128, NT2, 4], I32)
            gate_all = sb("gate_all", [128, NT2, 4], F32)
            lnsb2 = [(sb(f"st6b_{i}", [128, 12], F32), sb(f"mvb_{i}", [128, 2], F32), sb(f"rstdb_{i}", [128, 1], F32),
                      sb(f"nmrb_{i}", [128, 1], F32)) for i in range(2)]
            ztile = sb("ztile", [128, NSLOT // 128 + 1], I32)
            S.op("pool", lambda: P_.memset(ztile[:], 0), writes=["ztile"])
            S.dma("sp", lambda: nc.sync.dma_start(out=tokslot_d.rearrange("(p f) o -> p (f o)", p=128), in_=ztile[:]), reads=["ztile"], writes=["tokslot0"])
            sc_keys = []
            h1_keys = []
            with ExitStack() as st5:
                sb5, ps5 = mk(st5)
                wglu = sb5("wglu", [128, 4, 512], BF16)
                wout = sb5("wout", [128, 8, 1024], BF16)
                wr = sb5("wr", [128, 8, 32], F32)
                S.dma("pool", lambda: P_.dma_start(out=wglu[:], in_=w_glu.rearrange("(k p) n -> p k n", p=128)), writes=["wglu"])
                S.dma("pool", lambda: P_.dma_start(out=wout[:], in_=w_out.rearrange("(k p) n -> p k n", p=128)), writes=["wout"])
                S.dma("sp", lambda: nc.sync.dma_start(out=wr[:], in_=w_router.rearrange("(k p) n -> p k n", p=128)), writes=["wr"])
                bglu = sb5("bglu", [128, 4], F32)
                S.dma("sp", lambda: nc.sync.dma_start(out=bglu[:], in_=b_glu.rearrange("o (j p) -> (o p) j", p=128), allow_slow_non_contiguous=True), writes=["bglu"])
                brt = bc_load("brt", b_router[0], NEXP, sb5)
                gidx_t = sb5("gidx_t", [128, 8 * NG2], I32)
                S.dma("sp", lambda: nc.sync.dma_start(out=gidx_t[:], in_=gidx), writes=["gidx"])
                ebi = sb5("ebi", [128, NEXP], I32); ebase = sb5("ebase", [128, NEXP], F32)
                S.op("pool", lambda: P_.iota(ebi[:], pattern=[[CAP, NEXP]], base=0, channel_multiplier=0), writes=["ebi"])
                S.op("dve", lambda: V.tensor_copy(out=ebase[:], in_=ebi[:]), reads=["ebi"], writes=["ebase"])
                cntbc = sb5("cntbc", [128, NEXP], F32)
                S.op("dve", lambda: V.memset(cntbc[:], 0.0), writes=["cntbc"])
                catT = [sb5(f"catT{i}", [128, 4, 512], BF16) for i in range(2)]
                yT = [sb5(f"yT{i}", [128, 4, 512], BF16) for i in range(2)]
                ssmT = sb5("ssmT", [128, 4, 512], BF16)
                sg = sb5("sg", [128, 512], F32)
                xt2 = [sb5(f"xt2_{i}", [128, D], F32) for i in range(2)]
                tmpa = sb5("tmpa", [128, D], F32)
                h0 = sb5("h0", [128, D], F32)
                pre = sb5("pre", [128, D], F32)
                h1 = [sb5(f"h1_{i}", [128, D], F32) for i in range(2)]
                h1b = [sb5(f"h1b_{i}", [128, D], BF16) for i in range(2)]
                h1T = sb5("h1T", [128, 8, 128], F32)
                lg = sb5("lg", [128, NEXP], F32); mx8 = sb5("mx8", [128, 8], F32); negm = sb5("negm", [128, 1], F32)
                ex = sb5("ex", [128, NEXP], F32); msk = sb5("msk", [128, NEXP], F32); mskb = sb5("mskb", [128, NEXP], BF16)
                den = sb5("den", [128, 1], F32); gfull = sb5("gfull", [128, NEXP], F32)
                rank = sb5("rank", [128, NEXP], F32); ovf = sb5("ovf", [128, NEXP], F32); keep = sb5("keep", [128, NEXP], F32)
                sga = sb5("sga", [128, NEXP], F32); ssc = sb5("ssc", [128, NEXP], F32)
                oh = sb5("oh", [128, NEXP], F32); tmq = sb5("tmq", [128, NEXP], F32)
                sgak = sb5("sgak", [128, 4], F32); ssck = sb5("ssck", [128, 4], F32)
                ssci = [sb5(f"ssci{i}", [128, 4], I32) for i in range(2)]
                tokid = [sb5(f"tokid{i}", [128, 1], I32) for i in range(2)]
                pzg = [ps5(f"pzg{i}", [128, 512], F32) for i in range(2)]
                pms = [ps5(f"pm{i}", [128, 1024], F32) for i in range(2)]
                ptf = [ps5("ptf0", [128, 4, 128], F32)] * 2
                pk = ps5("pk", [128, 512], F32)
                Q3 = sb5("Q3", [128, 3, NEXP], F32)
                tm3 = sb5("tm3", [128, 3, NEXP], F32)
                R4 = sb5("R4", [128, 4, 3], F32)
                lnsb3 = [(sb5(f"st6c_{i}", [128, 12], F32), sb5(f"mvc_{i}", [128, 2], F32), sb5(f"rstdc_{i}", [128, 1], F32),
                          sb5(f"nmrc_{i}", [128, 1], F32)) for i in range(2)]

                h0s = [h0, sb5("h0b", [128, D], F32)]
                tmpb = sb5("tmpb", [128, D], F32)

                def stage0(t2):
                    s2 = t2 % 2
                    xt = xt2[s2]
                    S.dma("sp", lambda: nc.sync.dma_start(out=xt[:], in_=x2[t2 * 128:(t2 + 1) * 128, :]), writes=[("xt2", s2)])
                    layernorm(xt, ("xt2", s2), lng, "lng", lnb, "lnb", tmpb, "tmpb", h0s[s2][:], ("h0", s2), lnsb3[0])

                def stage1_mm(t2, tt, cT, kc):
                    pm = pms[t2 % 2]
                    for half in range(2):
                        for k in range(8):
                            lhs = cT[:, k, tt * 128:(tt + 1) * 128] if k < 4 else ssmT[:, k - 4, tt * 128:(tt + 1) * 128]
                            S.op("pe", lambda: T_.matmul(pm[:, half * 512:(half + 1) * 512], lhsT=lhs, rhs=wout[:, k, half * 512:(half + 1) * 512],
                                                         start=(k == 0), stop=(k == 7)), reads=["wout", kc, "ssmT"], writes=[("pm", t2 % 2)])

                def stage1(t2, tg, tt, cT, kc):
                    s2 = t2 % 2
                    pm = pms[s2]
                    S.op("dve", lambda: V.scalar_tensor_tensor(out=pre[:], in0=h0s[s2][:], scalar=ALPHA, in1=pm[:], op0=ALU.mult, op1=ALU.add),
                         reads=[("h0", s2), ("pm", s2)], writes=["pre"])
                    h1t = h1[s2]; kh1 = ("h1", s2)
                    layernorm(pre, "pre", g1, "g1", b1, "b1", tmpa, "tmpa", h1t[:], kh1, lnsb3[1])
                    S.op("act", lambda: A_.copy(out=h1b[s2][:], in_=h1t[:]), reads=[kh1], writes=[("h1b", s2)])
                    S.dma("sp", lambda: nc.sync.dma_start(out=h1_d[t2 * 128:(t2 + 1) * 128, :], in_=h1t[:]), reads=[kh1], writes=[("h1_d", t2)])
                    S.dma("sp", lambda: nc.sync.dma_start(out=h1b_d[t2 * 128:(t2 + 1) * 128, :], in_=h1b[s2][:]), reads=[("h1b", s2)], writes=[("h1b_d", t2)])
                    h1_keys.append(("h1b_d", t2))
                    if debug:
                        S.dma("sp", lambda: nc.sync.dma_start(out=dbg["h1"][t2 * 128:(t2 + 1) * 128, :], in_=h1t[:]), reads=[kh1], writes=[("dh1", t2)])

                def stage2(t2):
                    s2 = t2 % 2
                    h1t = h1[s2]; kh1 = ("h1", s2)
                    for hh in range(2):
                        pf = ptf[hh]
                        for k4 in range(4):
                            k = hh * 4 + k4
                            S.op("pe", lambda: T_.transpose(out=pf[:, k4, :], in_=h1t[:, k * 128:(k + 1) * 128], identity=identf[:]),
                                 reads=[kh1, "identf"], writes=[("ptf", 0)])
                        S.op("act", lambda: A_.copy(out=h1T[:, hh * 4:hh * 4 + 4, :], in_=pf[:]), reads=[("ptf", 0)], writes=["h1T"])
                    for k in range(8):
                        S.op("pe", lambda: T_.matmul(pk[:, 0:32], lhsT=h1T[:, k, :], rhs=wr[:, k, :], start=(k == 0), stop=(k == 7)),
                             reads=["h1T", "wr"], writes=["pk"])
                    S.op("dve", lambda: V.tensor_tensor(out=lg[:], in0=pk[:, 0:32], in1=brt[:], op=ALU.add), reads=["pk", "brt"], writes=["lg"])
                    if debug:
                        S.dma("sp", lambda: nc.sync.dma_start(out=dbg["lg"][t2 * 128:(t2 + 1) * 128, :], in_=lg[:]), reads=["lg"], writes=[("dlg", t2)])
                    S.op("dve", lambda: V.max(out=mx8[:], in_=lg[:]), reads=["lg"], writes=["mx8"])
                    S.op("dve", lambda: V.tensor_scalar(out=negm[:], in0=mx8[:, 0:1], scalar1=-1.0, scalar2=None, op0=ALU.mult), reads=["mx8"], writes=["negm"])
                    S.op("act", lambda: A_.activation(out=ex[:], in_=lg[:], func=AF.Exp, bias=negm[:, 0:1], scale=1.0), reads=["lg", "negm"], writes=["ex"])
                    S.op("dve", lambda: V.tensor_scalar(out=msk[:], in0=lg[:], scalar1=mx8[:, 3:4], scalar2=None, op0=ALU.is_ge), reads=["lg", "mx8"], writes=["msk"])
                    S.op("dve", lambda: V.tensor_copy(out=mskb[:], in_=msk[:]), reads=["msk"], writes=["mskb"])
                    S.op("pe", lambda: T_.matmul(pk[:, 32:64], lhsT=stri[:], rhs=mskb[:], start=True, stop=True), reads=["stri", "mskb"], writes=["pk"])
                    S.op("pe", lambda: T_.matmul(pk[:, 64:96], lhsT=ones[:], rhs=mskb[:], start=True, stop=True), reads=["ones", "mskb"], writes=["pk"])
                    S.op("dve", lambda: V.tensor_tensor(out=ex[:], in0=ex[:], in1=msk[:], op=ALU.mult), reads=["ex", "msk"], writes=["ex"])
                    S.op("dve", lambda: V.reduce_sum(out=den[:], in_=ex[:], axis=AX.X), reads=["ex"], writes=["den"])
                    S.op("dve", lambda: V.reciprocal(out=den[:], in_=den[:]), reads=["den"], writes=["den"])
                    S.op("dve", lambda: V.tensor_tensor(out=rank[:], in0=pk[:, 32:64], in1=cntbc[:], op=ALU.add), reads=["pk", "cntbc"], writes=["rank"])
                    S.op("dve", lambda: V.tensor_tensor(out=cntbc[:], in0=pk[:, 64:96], in1=cntbc[:], op=ALU.add), reads=["pk", "cntbc", "rank"], writes=["cntbc"])
                    S.op("dve", lambda: V.tensor_scalar(out=ovf[:], in0=rank[:], scalar1=float(CAP), scalar2=None, op0=ALU.is_ge), reads=["rank"], writes=["ovf"])
                    S.op("dve", lambda: V.tensor_scalar(out=keep[:], in0=ovf[:], scalar1=-1.0, scalar2=1.0, op0=ALU.mult, op1=ALU.add), reads=["ovf"], writes=["keep"])
                    S.op("dve", lambda: V.scalar_tensor_tensor(out=Q3[:, 2, :], in0=ex[:], scalar=den[:, 0:1], in1=keep[:], op0=ALU.mult, op1=ALU.mult),
                         reads=["ex", "den", "keep"], writes=["Q3"])
                    S.op("dve", lambda: V.scalar_tensor_tensor(out=Q3[:, 0, :], in0=rank[:], scalar=float(CAP - 1), in1=ebase[:], op0=ALU.min, op1=ALU.add),
                         reads=["rank", "ebase"], writes=["Q3"])
                    S.op("dve", lambda: V.tensor_tensor(out=ssc[:], in0=rank[:], in1=ebase[:], op=ALU.add), reads=["rank", "ebase"], writes=["ssc"])
                    S.op("dve", lambda: V.tensor_tensor(out=ssc[:], in0=ssc[:], in1=keep[:], op=ALU.mult), reads=["ssc", "keep"], writes=["ssc"])
                    S.op("dve", lambda: V.scalar_tensor_tensor(out=Q3[:, 1, :], in0=ovf[:], scalar=float(NSLOT), in1=ssc[:], op0=ALU.mult, op1=ALU.add),
                         reads=["ovf", "ssc"], writes=["Q3"])
                    for k in range(4):
                        S.op("dve", lambda: V.tensor_scalar(out=oh[:], in0=lg[:], scalar1=mx8[:, k:k + 1], scalar2=None, op0=ALU.is_equal), reads=["lg", "mx8"], writes=["oh"])
                        S.op("dve", lambda: V.tensor_tensor(out=tm3[:], in0=Q3[:], in1=oh[:, 0:NEXP].unsqueeze(1).to_broadcast([128, 3, NEXP]), op=ALU.mult),
                             reads=["Q3", "oh"], writes=["tm3"])
                        S.op("dve", lambda: V.reduce_sum(out=R4[:, k, :], in_=tm3[:], axis=AX.X), reads=["tm3"], writes=["R4"])
                    S.op("dve", lambda: V.tensor_copy(out=slot_ga[:, t2, :], in_=R4[:, :, 0]), reads=["R4"], writes=["slot_ga"])
                    S.op("dve", lambda: V.tensor_copy(out=ssci[s2][:], in_=R4[:, :, 1]), reads=["R4"], writes=[("ssci", s2)])
                    S.op("dve", lambda: V.tensor_copy(out=gate_all[:, t2, :], in_=R4[:, :, 2]), reads=["R4"], writes=["gate_all"])
                    S.op("pool", lambda: P_.iota(tokid[s2][:], pattern=[[0, 1]], base=t2 * 128, channel_multiplier=1), writes=[("tokid", s2)])
                    for k in range(4):
                        S.dma("pool", lambda: P_.indirect_dma_start(out=tokslot_d[:, :], out_offset=IOA(ap=ssci[s2][:, k:k + 1], axis=0),
                                                                    in_=tokid[s2][:, 0:1], in_offset=None),
                              reads=[("ssci", s2), ("tokid", s2), "tokslot0"], writes=[("sc", t2, k)])
                        sc_keys.append(("sc", t2, k))

                for tg in range(NG2):
                    cT, yT_ = catT[tg % 2], yT[tg % 2]
                    kc, ky = ("catT", tg % 2), ("yT", tg % 2)
                    for j in range(4):
                        for half in range(2):
                            col = (tg * 4 + j) * 2 + half
                            dst = cT[:, j, :] if half == 0 else yT_[:, j, :]
                            S.dma("pool", lambda: P_.indirect_dma_start(out=dst, out_offset=None, in_=gath[:, :],
                                                                        in_offset=IOA(ap=gidx_t[:, col:col + 1], axis=0)),
                                  reads=["gidx"], writes=[kc if half == 0 else ky])
                    for j2 in range(4):
                        pz_ = pzg[j2 % 2]; kpz = ("pzg", j2 % 2)
                        for j1 in range(4):
                            S.op("pe", lambda: T_.matmul(pz_[:], lhsT=wglu[:, j1, j2 * 128:(j2 + 1) * 128], rhs=yT_[:, j1, :],
                                                         start=(j1 == 0), stop=(j1 == 3)), reads=["wglu", ky], writes=[kpz])
                        S.op("act", lambda: A_.activation(out=sg[:], in_=pz_[:], func=AF.Sigmoid, bias=bglu[:, j2:j2 + 1], scale=1.0),
                             reads=[kpz, "bglu"], writes=["sg"])
                        S.op("dve", lambda: V.tensor_tensor(out=ssmT[:, j2, :], in0=yT_[:, j2, :], in1=sg[:], op=ALU.mult),
                             reads=[ky, "sg"], writes=["ssmT"])
                    stage1_mm(tg * 4, 0, cT, kc)
                    for tt in range(4):
                        t2 = tg * 4 + tt
                        if t2 == 0:
                            stage0(0)
                        if t2 + 1 < NT2:
                            stage0(t2 + 1)
                        if tt < 3:
                            stage1_mm(t2 + 1, tt + 1, cT, kc)
                        stage1(t2, tg, tt, cT, kc)
                        if t2 >= 1:
                            stage2(t2 - 1)
                stage2(NT2 - 1)
                barrier()
            print("P2a done: inst", S.n_inst, "waits", S.n_wait, "sems", S.nsem, flush=True)

            chunks = [(c0, min(c0 + 512, CAP)) for c0 in range(0, CAP, 512)]
            y_keys = []
            with ExitStack() as st6:
                sb6, ps6 = mk(st6)
                wgs = [sb6(f"wg{i}", [128, 8, 1024], BF16) for i in range(2)]
                wus = [sb6(f"wu{i}", [128, 8, 1024], BF16) for i in range(2)]
                wds = [sb6(f"wd{i}", [128, 8, 1024], BF16) for i in range(2)]
                bgall = sb6("bgall", [128, NEXP, 8], F32); buall = sb6("buall", [128, NEXP, 8], F32)
                for e in range(NEXP):
                    S.dma("sp", lambda: nc.sync.dma_start(out=bgall[:, e, :], in_=b_gate[e].rearrange("(j p) -> p j", p=128), allow_slow_non_contiguous=True),
                          writes=[("bgall", e)])
                    S.dma("sp", lambda: nc.sync.dma_start(out=buall[:, e, :], in_=b_up[e].rearrange("(j p) -> p j", p=128), allow_slow_non_contiguous=True),
                          writes=[("buall", e)])
                bdbc = [sb6(f"bdbc{i}", [128, D], F32) for i in range(2)]
                idxs = [sb6(f"idx{i}", [128, NRT], I32) for i in range(2)]
                xg = [sb6(f"xg{i}", [128, D], BF16) for i in range(2)]
                xT = sb6("xT", [128, 8, CAP], BF16)
                actT = sb6("actT", [128, 8, CAP], BF16)
                gsb = [sb6(f"gsb{i}", [128, 512], F32) for i in range(2)]
                sgs = [sb6(f"sgs{i}", [128, 512], F32) for i in range(2)]
                usb = [sb6(f"usb{i}", [128, 512], F32) for i in range(2)]
                yo = [sb6(f"yo{i}", [128, D], F32) for i in range(2)]
                ptx = ps6("ptx", [128, 8, 128], BF16)
                pgc = [ps6(f"pgc{i}", [128, 512], F32) for i in range(2)]
                puc = [ps6(f"puc{i}", [128, 512], F32) for i in range(2)]
                pdh = [ps6(f"pd{i}", [128, 512], F32) for i in range(2)]

                WSRC = {"wg": (wgs, w_gate), "wu": (wus, w_up), "wd": (wds, w_down)}

                def load_piece(e, nm, k2):
                    s_ = e % 2
                    wt, src = WSRC[nm]
                    S.dma("pool", lambda: P_.dma_start(out=wt[s_][:, 2 * k2:2 * k2 + 2, :],
                                                       in_=src[e, k2 * 256:(k2 + 1) * 256, :].rearrange("(k p) n -> p k n", p=128)),
                          writes=[(nm, s_, 2 * k2), (nm, s_, 2 * k2 + 1)], grp="w", ngrp=9)

                WPLAN = [[("wg", 0), ("wu", 0)], [("wd", 0)], [("wg", 1), ("wu", 1)], [("wd", 1)],
                         [("wg", 2), ("wu", 2)], [("wd", 2)], [("wg", 3), ("wu", 3)], [("wd", 3)]]

                def load_wk(e, fj):
                    for nm, k2 in WPLAN[fj]:
                        load_piece(e, nm, k2)

                def load_w(e):
                    for fj in range(8):
                        load_wk(e, fj)

                xT2 = [xT, sb6("xTb", [128, 8, CAP], BF16)]

                def prep_idx(e):
                    S.dma("sp", lambda: nc.sync.dma_start(out=idxs[e % 2][:], in_=tokslot_d[e * CAP:(e + 1) * CAP, :].rearrange("(r p) o -> p (r o)", p=128),
                                                          allow_slow_non_contiguous=True), writes=[("idx", e % 2)])
                    S.dma("sp", lambda: nc.sync.dma_start(out=bdbc[e % 2][:], in_=b_down[e].partition_broadcast(128)), writes=[("bdbc", e % 2)])

                gcnt = [0]

                def prep_g(e, rt):
                    xs = rt % 2
                    S.dma("pool", lambda: P_.indirect_dma_start(out=xg[xs][:, :], out_offset=None, in_=h1b_d[:, :],
                                                                in_offset=IOA(ap=idxs[e % 2][:, rt:rt + 1], axis=0)),
                          reads=[("idx", e % 2)], writes=[("xg", xs)])

                def prep_t(e, rt):
                    xs = rt % 2
                    for k in range(8):
                        S.op("pe", lambda: T_.transpose(out=ptx[:, k, :], in_=xg[xs][:, k * 128:(k + 1) * 128], identity=ident[:]),
                             reads=[("xg", xs), "ident"], writes=["ptx"])
                    S.op("act", lambda: A_.copy(out=xT2[e % 2][:, :, rt * 128:(rt + 1) * 128], in_=ptx[:]), reads=["ptx"], writes=[("xT", e % 2)])

                def prep_rt(e, rt):
                    if rt + 1 < NRT:
                        prep_g(e, rt + 1)
                    prep_t(e, rt)

                load_w(0)
                S.waitfor("sp", sc_keys)
                S.waitfor("pool", h1_keys)
                prep_idx(0)
                prep_g(0, 0)
                for rt in range(NRT):
                    prep_rt(0, rt)
                cc = 0
                for e in range(NEXP):
                    s_ = e % 2
                    xTe = xT2[s_]
                    if e + 1 < NEXP:
                        prep_idx(e + 1)
                        prep_g(e + 1, 0)
                    nxt = list(range(NRT)) if e + 1 < NEXP else []
                    for fj in range(8):
                        for (c0, c1) in chunks:
                            b_ = cc % 2; cc += 1
                            n_ = c1 - c0
                            for (W, kw, P, kp) in ((wgs[s_], ("wg", s_), pgc[b_], ("pgc", b_)), (wus[s_], ("wu", s_), puc[b_], ("puc", b_))):
                                for k in range(8):
                                    S.op("pe", lambda: T_.matmul(P[:, 0:n_], lhsT=W[:, k, fj * 128:(fj + 1) * 128], rhs=xTe[:, k, c0:c1],
                                                                 start=(k == 0), stop=(k == 7)), reads=[kw + (k,), ("xT", s_)], writes=[kp])
                            gs, sg_, us = gsb[b_], sgs[b_], usb[b_]
                            S.op("dve", lambda: V.tensor_scalar(out=gs[:, 0:n_], in0=pgc[b_][:, 0:n_], scalar1=bgall[:, e, fj:fj + 1], scalar2=7.0,
                                                                op0=ALU.add, op1=ALU.min), reads=[("pgc", b_), ("bgall", e)], writes=[("gsb", b_)])
                            S.op("act", lambda: A_.activation(out=sg_[:, 0:n_], in_=gs[:, 0:n_], func=AF.Sigmoid, scale=1.702), reads=[("gsb", b_)], writes=[("sgs", b_)])
                            S.op("dve", lambda: V.tensor_scalar(out=us[:, 0:n_], in0=puc[b_][:, 0:n_], scalar1=buall[:, e, fj:fj + 1], scalar2=7.0,
                                                                op0=ALU.add, op1=ALU.min), reads=[("puc", b_), ("buall", e)], writes=[("usb", b_)])
                            S.op("dve", lambda: V.tensor_scalar(out=us[:, 0:n_], in0=us[:, 0:n_], scalar1=-7.0, scalar2=1.0, op0=ALU.max, op1=ALU.add),
                                 reads=[("usb", b_)], writes=[("usb", b_)])
                            S.op("dve", lambda: V.tensor_tensor(out=gs[:, 0:n_], in0=gs[:, 0:n_], in1=sg_[:, 0:n_], op=ALU.mult),
                                 reads=[("gsb", b_), ("sgs", b_)], writes=[("gsb", b_)])
                            S.op("dve", lambda: V.tensor_tensor(out=actT[:, fj, c0:c1], in0=gs[:, 0:n_], in1=us[:, 0:n_], op=ALU.mult),
                                 reads=[("gsb", b_), ("usb", b_)], writes=["actT"])
                        if e + 1 < NEXP:
                            load_wk(e + 1, fj)
                        if nxt:
                            prep_rt(e + 1, nxt.pop(0))
                    while nxt:
                        prep_rt(e + 1, nxt.pop(0))
                    for rt in range(NRT):
                        ys = rt % 2
                        for half in range(2):
                            for k in range(8):
                                S.op("pe", lambda: T_.matmul(pdh[half][:, :], lhsT=actT[:, k, rt * 128:(rt + 1) * 128],
                                                             rhs=wds[s_][:, k, half * 512:(half + 1) * 512], start=(k == 0), stop=(k == 7)),
                                     reads=["actT", ("wd", s_, k)], writes=[("pd", half)])
                            S.op("dve", lambda: V.tensor_tensor(out=yo[ys][:, half * 512:(half + 1) * 512], in0=pdh[half][:, :],
                                                                in1=bdbc[s_][:, half * 512:(half + 1) * 512], op=ALU.add),
                                 reads=[("pd", half), ("bdbc", s_)], writes=[("yo", ys, half)])
                        r0 = e * CAP + rt * 128
                        S.dma("sp", lambda: nc.sync.dma_start(out=ybuf_d[r0:r0 + 128, :], in_=yo[ys][:]), reads=[("yo", ys, 0), ("yo", ys, 1)],
                              writes=[("ybuf", e, rt)])
                        y_keys.append(("ybuf", e, rt))
                barrier()
            print("P2b done: inst", S.n_inst, "waits", S.n_wait, "sems", S.nsem, flush=True)

            with ExitStack() as st7:
                sb7, ps7 = mk(st7)
                h1c = [sb7(f"h1c{i}", [128, D], F32) for i in range(2)]
                yk = [[sb7(f"yk{i}_{k}", [128, D], F32) for k in range(4)] for i in range(2)]
                accs = [sb7(f"acc{i}", [128, D], F32) for i in range(2)]
                tmpc = sb7("tmpc", [128, D], F32)
                ot = [sb7(f"ot{i}", [128, D], F32) for i in range(2)]
                S.waitfor("pool", y_keys)

                def c_fetch(t2):
                    s2 = t2 % 2
                    S.dma("sp", lambda: nc.sync.dma_start(out=h1c[s2][:], in_=h1_d[t2 * 128:(t2 + 1) * 128, :]), reads=[("h1_d", t2)], writes=[("h1c", s2)])
                    for k in range(4):
                        S.dma("pool", lambda: P_.indirect_dma_start(out=yk[s2][k][:, :], out_offset=None, in_=ybuf_d[:, :],
                                                                    in_offset=IOA(ap=slot_ga[:, t2, k:k + 1], axis=0)),
                              reads=["slot_ga"], writes=[("yk", s2, k)])

                def c_comp(t2):
                    s2 = t2 % 2
                    ac = accs[s2]; ka = ("acc", s2)
                    S.op("act", lambda: A_.activation(out=ac[:], in_=h1c[s2][:], func=AF.Copy, scale=ALPHA), reads=[("h1c", s2)], writes=[ka])
                    for k in range(4):
                        S.op("dve", lambda: V.scalar_tensor_tensor(out=ac[:], in0=yk[s2][k][:], scalar=gate_all[:, t2, k:k + 1], in1=ac[:],
                                                                   op0=ALU.mult, op1=ALU.add), reads=[("yk", s2, k), "gate_all", ka], writes=[ka])
                    layernorm(ac, ka, g2, "g2", b2, "b2", tmpc, "tmpc", ot[s2][:], ("ot", s2), lnsb2[s2])
                    S.dma("sp", lambda: nc.sync.dma_start(out=out_d[t2 * 128:(t2 + 1) * 128, :], in_=ot[s2][:]), reads=[("ot", s2)], writes=[("out", t2)])

                c_fetch(0)
                for t2 in range(NT2):
                    if t2 + 1 < NT2:
                        c_fetch(t2 + 1)
                    c_comp(t2)
                barrier()
        for e in ("pe", "act", "dve", "pool", "sp"):
            S.wait_all(e)
    return nc


def make_maps(inp, NX, full=True):
    f = lambda a: np.ascontiguousarray(np.asarray(a, dtype=np.float32))
    NG = NX // 512
    NTOK2 = NX // 4
    NG2 = NTOK2 // 512
    XCH = min(1024, 128 * NG)
    w_in = np.asarray(inp["w_in"])[0]
    maps = []
    for c in range(8):
        b, r = c // 4, c % 4
        m = {}
        m["x_b"] = f(inp["x"][b, :NX])
        m["meta"] = f(inp["meta_tokens"])
        m["ln_in_g"] = f(inp["ln_in_g"]).reshape(1, D)
        m["ln_in_b"] = f(inp["ln_in_b"]).reshape(1, D)
        m["w4"] = f(np.concatenate([w_in[:, r * 128:(r + 1) * 128], w_in[:, 512 + r * 128:512 + (r + 1) * 128],
                                    w_in[:, 1024 + r * 128:1024 + (r + 1) * 128], w_in[:, 1536 + r * 128:1536 + (r + 1) * 128]], axis=1))
        m["relb"] = f(np.asarray(inp["rel_bias"])[:, r]).reshape(1, 32)
        m["lam4"] = f(np.stack([inp["lambda_q1"][0], inp["lambda_k1"][0], inp["lambda_q2"][0], inp["lambda_k2"][0]]))
        m["subln_g"] = f(inp["subln_g"][0]).reshape(1, 128)
        gs = slice(8 * r, 8 * r + 8)
        m["a_re"] = f(inp["a_re"][0][gs]).reshape(1, 512)
        m["a_im"] = f(inp["a_im"][0][gs]).reshape(1, 512)
        m["log_step"] = f(inp["log_step"][0][gs]).reshape(1, 8)
        m["b_re"] = f(inp["b_re"][0][gs]); m["b_im"] = f(inp["b_im"][0][gs])
        m["c_re"] = f(inp["c_re"][0][gs]); m["c_im"] = f(inp["c_im"][0][gs])
        m["d_skip"] = f(inp["d_skip"][0][128 * r:128 * r + 128]).reshape(1, 128)
        if full:
            m["x2"] = f(inp["x"][b, r * NTOK2:(r + 1) * NTOK2])
            gi = np.zeros((128, 8 * NG2), np.int32)
            p = np.arange(128)
            for tg in range(NG2):
                for j in range(4):
                    for half in range(2):
                        rho = (r * NG2 + tg) * 256 + half * 128 + p
                        gi[:, (tg * 4 + j) * 2 + half] = (rho // XCH) * 4 * XCH + j * XCH + rho % XCH
            m["gidx"] = gi
            m["w_glu"] = f(inp["w_glu"][0]); m["b_glu"] = f(inp["b_glu"][0]).reshape(1, 512)
            m["w_out"] = f(inp["w_out"][0])
            m["ln1_g"] = f(inp["ln1_g"][0]).reshape(1, D); m["ln1_b"] = f(inp["ln1_b"][0]).reshape(1, D)
            m["w_router"] = f(inp["w_router"][0]); m["b_router"] = f(inp["b_router"][0]).reshape(1, NEXP)
            m["w_gate"] = f(inp["w_gate"][0]); m["b_gate"] = f(inp["b_gate"][0])
            m["w_up"] = f(inp["w_up"][0]); m["b_up"] = f(inp["b_up"][0])
            m["w_down"] = f(inp["w_down"][0]); m["b_down"] = f(inp["b_down"][0])
            m["ln2_g"] = f(inp["ln2_g"][0]).reshape(1, D); m["ln2_b"] = f(inp["ln2_b"][0]).reshape(1, D)
        maps.append(m)
    return maps


def kernel(**inputs):
    NX = int(np.asarray(inputs["x"]).shape[1])
    CAP = 768 if NX >= 16384 else 128 * max(1, int(math.ceil(1.5 * NX / 8 / 128)))
    nc = build(NX, CAP, debug=False, full=True)
    maps = make_maps(inputs, NX, full=True)
    res = run_bass_kernel_spmd(nc, maps, core_ids=list(range(8)))
    NTOK2 = NX // 4
    out = np.zeros((2, NX, D), np.float32)
    for c in range(8):
        b, r = c // 4, c % 4
        out[b, r * NTOK2:(r + 1) * NTOK2] = np.asarray(res.results[c]["out"], dtype=np.float32)
    return out
```

```python
import math
from contextlib import ExitStack

import numpy as np
import concourse.bass as bass
import concourse.mybir as mybir
from concourse.bass_utils import run_bass_kernel_spmd

F32 = mybir.dt.float32
BF16 = mybir.dt.bfloat16
I32 = mybir.dt.int32
AF = mybir.ActivationFunctionType
ALU = mybir.AluOpType
AX = mybir.AxisListType

D = 1024
NEXP = 32
LN_EPS = 1e-5
ALPHA = 2.0 ** 0.25
NEG = -30000.0
EPOCH = 30000


class Sync:
    def __init__(self, nc, stack, n_dma_sems=10):
        self.nc = nc
        self.stack = stack
        self.eng = {"pe": nc.tensor, "act": nc.scalar, "dve": nc.vector,
                    "pool": nc.gpsimd, "sp": nc.sync}
        self.sems = {}
        self.nsem = 0
        self.cur = {e: [self._new_sem(), 0] for e in self.eng}
        self.seen = {e: {} for e in self.eng}
        self.lastw = {}
        self.readers = {}
        self.dma_pool = {}
        self.dma_rr = {}
        for q in ("sp", "pool", "act"):
            self.dma_pool[q] = [[self._new_sem(), 0] for _ in range(n_dma_sems)]
            self.dma_rr[q] = 0
        self.n_inst = 0
        self.n_wait = 0

    def _new_sem(self):
        sid = self.nsem
        self.nsem += 1
        self.sems[sid] = self.stack.enter_context(self.nc.semaphore(f"s{sid}"))
        return sid

    def _wait(self, e, ticket):
        sid, val, owner = ticket
        if owner == e and e == "pe":
            return
        if self.seen[e].get(sid, 0) >= val:
            return
        self.eng[e].wait_ge(self.sems[sid], val)
        self.seen[e][sid] = val
        self.n_wait += 1

    def _deps(self, e, reads, writes):
        for b in list(reads) + list(writes):
            t = self.lastw.get(b)
            if t is not None:
                self._wait(e, t)
        for b in writes:
            for t in self.readers.get(b, ()):
                self._wait(e, t)

    def _commit(self, ticket, reads, writes):
        for b in writes:
            self.lastw[b] = ticket
            self.readers[b] = []
        for b in reads:
            lst = self.readers.setdefault(b, [])
            lst.append(ticket)
            if len(lst) > 24:
                best = {}
                for t in lst:
                    if t[0] not in best or best[t[0]][1] < t[1]:
                        best[t[0]] = t
                self.readers[b] = list(best.values())

    def op(self, e, fn, reads=(), writes=()):
        self._deps(e, reads, writes)
        c = self.cur[e]
        if c[1] >= EPOCH:
            c[0] = self._new_sem()
            c[1] = 0
        ins = fn()
        c[1] += 1
        ins.then_inc(self.sems[c[0]], 1)
        ticket = (c[0], c[1], e)
        self._commit(ticket, reads, writes)
        self.n_inst += 1
        return ticket

    def dma(self, q, fn, reads=(), writes=(), grp=None, ngrp=6):
        self._deps(q, reads, writes)
        if grp is None:
            pk = q
        else:
            pk = (q, grp)
            if pk not in self.dma_pool:
                self.dma_pool[pk] = [[self._new_sem(), 0] for _ in range(ngrp)]
                self.dma_rr[pk] = 0
        pool = self.dma_pool[pk]
        i = self.dma_rr[pk]
        self.dma_rr[pk] = (i + 1) % len(pool)
        slot = pool[i]
        if slot[1] > 0:
            self._wait(q, (slot[0], slot[1], "dma"))
        if slot[1] >= EPOCH:
            slot[0] = self._new_sem()
            slot[1] = 0
        ins = fn()
        slot[1] += 16
        ins.then_inc(self.sems[slot[0]], 16)
        ticket = (slot[0], slot[1], "dma")
        self._commit(ticket, reads, writes)
        self.n_inst += 1
        return ticket

    def waitfor(self, e, keys):
        for b in keys:
            t = self.lastw.get(b)
            if t is not None:
                self._wait(e, t)

    def wait_all(self, e):
        for b, t in list(self.lastw.items()):
            self._wait(e, t)
        for q, pool in self.dma_pool.items():
            for slot in pool:
                if slot[1] > 0:
                    self._wait(e, (slot[0], slot[1], "dma"))


def _bucket_thresholds():
    n = np.arange(1, 400, dtype=np.int32)
    n_f = np.maximum(n, 1).astype(np.float32)
    large = 8 + (np.log(n_f / np.float32(8)) / np.float32(math.log(128 / 8)) * np.float32(8)).astype(np.int32)
    large = np.minimum(large, 15)
    bucket = np.where(n < 8, n, large)
    thr = {}
    for j in range(9, 16):
        thr[j] = int(n[np.argmax(bucket >= j)])
    return thr


def build(NX, CAP, debug=False, full=True, stop_after=None):
    nc = bass.Bass("TRN2", target_bir_lowering=False)
    NT = NX // 128 + 1
    NCOL = NT * 128
    NG = NX // 512
    NTOK2 = NX // 4
    NT2 = NTOK2 // 128
    NG2 = NTOK2 // 512
    NRT = CAP // 128
    NSLOT = NEXP * CAP

    def din(name, shape, dt=F32):
        return nc.dram_tensor(name, list(shape), dt, kind="ExternalInput").ap()

    x_b = din("x_b", [NX, D])
    meta = din("meta", [16, D])
    ln_in_g = din("ln_in_g", [1, D]); ln_in_b = din("ln_in_b", [1, D])
    w4 = din("w4", [D, 512])
    relb = din("relb", [1, 32])
    lam4 = din("lam4", [4, 64])
    subln_g = din("subln_g", [1, 128])
    a_re = din("a_re", [1, 512]); a_im = din("a_im", [1, 512]); log_step = din("log_step", [1, 8])
    b_re = din("b_re", [8, 64, 16]); b_im = din("b_im", [8, 64, 16])
    c_re = din("c_re", [8, 16, 64]); c_im = din("c_im", [8, 16, 64])
    d_skip = din("d_skip", [1, 128])
    if full:
        x2 = din("x2", [NTOK2, D])
        gidx = din("gidx", [128, 8 * NG2], I32)
        w_glu = din("w_glu", [512, 512]); b_glu = din("b_glu", [1, 512])
        w_out = din("w_out", [D, D])
        ln1_g = din("ln1_g", [1, D]); ln1_b = din("ln1_b", [1, D])
        w_router = din("w_router", [D, NEXP]); b_router = din("b_router", [1, NEXP])
        w_gate = din("w_gate", [NEXP, D, D]); b_gate = din("b_gate", [NEXP, D])
        w_up = din("w_up", [NEXP, D, D]); b_up = din("b_up", [NEXP, D])
        w_down = din("w_down", [NEXP, D, D]); b_down = din("b_down", [NEXP, D])
        ln2_g = din("ln2_g", [1, D]); ln2_b = din("ln2_b", [1, D])
        out_d = nc.dram_tensor("out", [NTOK2, D], F32, kind="ExternalOutput").ap()

    slab = nc.dram_tensor("slab", [256 * NG, 512], BF16).ap()
    gath = nc.dram_tensor("gath", [4 * 256 * NG, 512], BF16).ap()
    slab3 = slab.rearrange("(g f) c -> f g c", f=256)
    vaug_d = nc.dram_tensor("vaug_d", [128, NT, 129], BF16).ap()
    h1_d = nc.dram_tensor("h1_d", [NTOK2, D], F32).ap()
    h1b_d = nc.dram_tensor("h1b_d", [NTOK2, D], BF16).ap()
    tokslot_d = nc.dram_tensor("tokslot_d", [NSLOT + 128, 1], I32).ap()
    ybuf_d = nc.dram_tensor("ybuf_d", [NSLOT, D], F32).ap()
    dbg = {}
    if debug:
        dbg["qT"] = nc.dram_tensor("dbg_qT", [128, NCOL], BF16, kind="ExternalOutput").ap()
        dbg["kT"] = nc.dram_tensor("dbg_kT", [128, NCOL], BF16, kind="ExternalOutput").ap()
        dbg["uT"] = nc.dram_tensor("dbg_uT", [128, NCOL], BF16, kind="ExternalOutput").ap()
        dbg["v"] = nc.dram_tensor("dbg_v", [128, NT, 129], BF16, kind="ExternalOutput").ap()
        dbg["slab"] = nc.dram_tensor("dbg_slab", [256 * NG, 512], BF16, kind="ExternalOutput").ap()
        dbg["Wb"] = nc.dram_tensor("dbg_Wb", [128, 1024], F32, kind="ExternalOutput").ap()
        dbg["ident"] = nc.dram_tensor("dbg_ident", [128, 128], BF16, kind="ExternalOutput").ap()
        dbg["ob"] = nc.dram_tensor("dbg_ob", [128, 4, 128], BF16, kind="ExternalOutput").ap()
        dbg["g08"] = nc.dram_tensor("dbg_g08", [128, 128], F32, kind="ExternalOutput").ap()
        dbg["po"] = nc.dram_tensor("dbg_po", [128, 3, 512], F32, kind="ExternalOutput").ap()
        dbg["rr"] = nc.dram_tensor("dbg_rr", [128, 8], F32, kind="ExternalOutput").ap()
        dbg["pT"] = nc.dram_tensor("dbg_pT", [128, 2, 512], BF16, kind="ExternalOutput").ap()
        if full:
            dbg["h1"] = nc.dram_tensor("dbg_h1", [NTOK2, D], F32, kind="ExternalOutput").ap()
            dbg["lg"] = nc.dram_tensor("dbg_lg", [NTOK2, 32], F32, kind="ExternalOutput").ap()

    thr = _bucket_thresholds()

    with ExitStack() as st0:
        S = Sync(nc, st0)
        V, A_, P_, T_ = nc.vector, nc.scalar, nc.gpsimd, nc.tensor

        def mk(stack):
            def sb(name, shape, dt=F32):
                return stack.enter_context(nc.sbuf_tensor(name, list(shape), dt))

            def ps(name, shape, dt=F32):
                return stack.enter_context(nc.psum_tensor(name, list(shape), dt))
            return sb, ps

        sb0, ps0 = mk(st0)

        ident = sb0("ident", [128, 128], BF16)
        identf = sb0("identf", [128, 128], F32)
        tri = sb0("tri", [128, 128], BF16)
        stri = sb0("stri", [128, 128], BF16)
        ones = sb0("ones", [128, 128], BF16)
        iop = sb0("iop", [128, 1], F32)
        iop_i = sb0("iop_i", [128, 1], I32)
        S.op("pool", lambda: P_.memset(identf[:], 0.0), writes=["identf"])
        S.op("pool", lambda: P_.affine_select(out=identf[:], in_=identf[:], pattern=[[-1, 128]],
                                              compare_op=ALU.not_equal, fill=1.0, base=0, channel_multiplier=1),
             reads=["identf"], writes=["identf"])
        S.op("dve", lambda: V.tensor_copy(out=ident[:], in_=identf[:]), reads=["identf"], writes=["ident"])
        S.op("pool", lambda: P_.memset(ones[:], 1.0), writes=["ones"])
        S.op("pool", lambda: P_.affine_select(out=tri[:], in_=ones[:], pattern=[[1, 128]],
                                              compare_op=ALU.is_ge, fill=0.0, base=0, channel_multiplier=-1),
             reads=["ones"], writes=["tri"])
        S.op("pool", lambda: P_.affine_select(out=stri[:], in_=ones[:], pattern=[[1, 128]],
                                              compare_op=ALU.is_gt, fill=0.0, base=0, channel_multiplier=-1),
             reads=["ones"], writes=["stri"])
        S.op("pool", lambda: P_.iota(iop_i[:], pattern=[[0, 1]], base=0, channel_multiplier=1), writes=["iop_i"])
        S.op("dve", lambda: V.tensor_copy(out=iop[:], in_=iop_i[:]), reads=["iop_i"], writes=["iop"])

        def bc_load(name, src, n, stack_sb, q="sp"):
            t = stack_sb(name, [128, n], F32)
            S.dma(q, lambda: nc.sync.dma_start(out=t[:], in_=src.partition_broadcast(128)), writes=[name])
            return t

        lng = bc_load("lng", ln_in_g[0], D, sb0)
        lnb = bc_load("lnb", ln_in_b[0], D, sb0)

        cnt = {"ln": 0}

        def layernorm(xt, kx, g_bc, kg, b_bc, kb, tmp, kt, out_ap, kout, sbs):
            i = cnt["ln"]; cnt["ln"] += 1
            st6, mv, rstd, nmr = sbs
            ks = ("lnstat", id(st6))
            S.op("dve", lambda: V.bn_stats(out=st6[:, 0:6], in_=xt[:, 0:512]), reads=[kx], writes=[ks])
            S.op("dve", lambda: V.bn_stats(out=st6[:, 6:12], in_=xt[:, 512:1024]), reads=[kx], writes=[ks])
            S.op("dve", lambda: V.bn_aggr(out=mv[:, 0:2], in_=st6[:, 0:12]), reads=[ks], writes=[ks])
            S.op("dve", lambda: V.tensor_scalar(out=rstd[:], in0=mv[:, 1:2], scalar1=LN_EPS, scalar2=None, op0=ALU.add), reads=[ks], writes=[ks])
            S.op("dve", lambda: V.tensor_scalar(out=nmr[:], in0=mv[:, 0:1], scalar1=-1.0, scalar2=None, op0=ALU.mult), reads=[ks], writes=[ks])
            S.op("act", lambda: A_.activation(out=rstd[:], in_=rstd[:], func=AF.Ln), reads=[ks], writes=[ks])
            S.op("act", lambda: A_.activation(out=rstd[:], in_=rstd[:], func=AF.Exp, scale=-0.5), reads=[ks], writes=[ks])
            S.op("act", lambda: A_.activation(out=nmr[:], in_=nmr[:], func=AF.Copy, scale=rstd[:, 0:1]), reads=[ks], writes=[ks])
            S.op("act", lambda: A_.activation(out=tmp[:], in_=xt[:], func=AF.Identity, bias=nmr[:, 0:1], scale=rstd[:, 0:1]),
                 reads=[kx, ks], writes=[kt])
            S.op("dve", lambda: V.tensor_tensor(out=tmp[:], in0=tmp[:], in1=g_bc[:], op=ALU.mult), reads=[kt, kg], writes=[kt])
            S.op("dve", lambda: V.tensor_tensor(out=out_ap, in0=tmp[:], in1=b_bc[:], op=ALU.add), reads=[kt, kb], writes=[kout])

        st_att = ExitStack()
        sbA, psA = mk(st_att)
        qT = sbA("qT", [128, NCOL], BF16)
        kT = sbA("kT", [128, NCOL], BF16)
        st_u = ExitStack()
        sbU, _ = mk(st_u)
        uT = sbU("uT", [128, NCOL], BF16)

        st3 = ExitStack()
        sb, ps = mk(st3)
        PI = math.pi
        Tnr = sb("Tnr", [128, 512], F32); Tni = sb("Tni", [128, 512], F32)
        Ppr = sb("Ppr", [128, 4, 128], F32); Ppi = sb("Ppi", [128, 4, 128], F32)
        Bblk = sb("Bblk", [128, 1024], BF16)
        Cmat = sb("Cmat", [128, 8, 128], BF16)
        dsk = sb("dsk", [128, 1], F32)
        S.dma("sp", lambda: nc.sync.dma_start(out=dsk[:], in_=d_skip.rearrange("o (p q) -> (o p) q", q=1)), writes=["dsk"])
        negpi = sb("negpi", [128, 1], F32)
        S.op("dve", lambda: V.memset(negpi[:], -PI), writes=["negpi"])
        with ExitStack() as stt:
            sbt, _ = mk(stt)
            are = bc_load("are", a_re[0], 512, sbt)
            aim = bc_load("aim", a_im[0], 512, sbt)
            ls8 = bc_load("ls8", log_step[0], 8, sbt)
            S.op("act", lambda: A_.activation(out=ls8[:], in_=ls8[:], func=AF.Exp), reads=["ls8"], writes=["ls8"])
            S.op("dve", lambda: V.tensor_scalar(out=are[:], in0=are[:], scalar1=-1e-4, scalar2=None, op0=ALU.min), reads=["are"], writes=["are"])
            sa = sbt("sa", [128, 512], F32); sp_ = sbt("sp_", [128, 512], F32)
            st8 = ls8[:, 0:8].unsqueeze(2).to_broadcast([128, 8, 64])
            S.op("dve", lambda: V.tensor_tensor(out=sa[:].rearrange("p (g n) -> p g n", n=64), in0=are[:].rearrange("p (g n) -> p g n", n=64),
                                                in1=st8, op=ALU.mult), reads=["are", "ls8"], writes=["sa"])
            S.op("dve", lambda: V.tensor_tensor(out=sp_[:].rearrange("p (g n) -> p g n", n=64), in0=aim[:].rearrange("p (g n) -> p g n", n=64),
                                                in1=st8, op=ALU.mult), reads=["aim", "ls8"], writes=["sp_"])
            t_a = sbt("t_a", [128, 512], F32); t_b = sbt("t_b", [128, 512], F32); t_c = sbt("t_c", [128, 512], F32)
            t_d = sbt("t_d", [128, 512], F32); t_e = sbt("t_e", [128, 512], F32)
            sp1 = sbt("sp1", [128, 1], F32); nsp1 = sbt("nsp1", [128, 1], F32)
            S.op("dve", lambda: V.tensor_scalar(out=sp1[:], in0=iop[:], scalar1=1.0, scalar2=None, op0=ALU.add), reads=["iop"], writes=["sp1"])
            S.op("dve", lambda: V.tensor_scalar(out=nsp1[:], in0=sp1[:], scalar1=-1.0, scalar2=None, op0=ALU.mult), reads=["sp1"], writes=["nsp1"])

            rki = sbt("rki", [128, 512], I32)
            rkf = sbt("rkf", [128, 512], F32)
            C1 = 6.28125
            C2 = 2 * PI - C1

            def reduce_sin(ph, kph, shift, out_ap, kout, scratch, ksc):
                n = ph.shape[-1]
                S.op("dve", lambda: V.tensor_scalar(out=scratch, in0=ph, scalar1=shift, scalar2=1.0 / (2 * PI), op0=ALU.add, op1=ALU.mult),
                     reads=[kph], writes=[ksc])
                S.op("dve", lambda: V.tensor_copy(out=rki[:, 0:n], in_=scratch), reads=[ksc], writes=["rki"])
                S.op("dve", lambda: V.tensor_copy(out=rkf[:, 0:n], in_=rki[:, 0:n]), reads=["rki"], writes=["rkf"])
                S.op("dve", lambda: V.tensor_scalar(out=scratch, in0=ph, scalar1=shift, scalar2=None, op0=ALU.add), reads=[kph], writes=[ksc])
                S.op("dve", lambda: V.scalar_tensor_tensor(out=scratch, in0=rkf[:, 0:n], scalar=-C1, in1=scratch, op0=ALU.mult, op1=ALU.add),
                     reads=["rkf", ksc], writes=[ksc])
                S.op("dve", lambda: V.scalar_tensor_tensor(out=scratch, in0=rkf[:, 0:n], scalar=-C2, in1=scratch, op0=ALU.mult, op1=ALU.add),
                     reads=["rkf", ksc], writes=[ksc])
                S.op("dve", lambda: V.tensor_scalar(out=rkf[:, 0:n], in0=scratch, scalar1=PI, scalar2=-2 * PI, op0=ALU.is_gt, op1=ALU.mult),
                     reads=[ksc], writes=["rkf"])
                S.op("dve", lambda: V.tensor_tensor(out=scratch, in0=scratch, in1=rkf[:, 0:n], op=ALU.add), reads=["rkf", ksc], writes=[ksc])
                S.op("dve", lambda: V.tensor_scalar(out=rkf[:, 0:n], in0=scratch, scalar1=-PI, scalar2=2 * PI, op0=ALU.is_lt, op1=ALU.mult),
                     reads=[ksc], writes=["rkf"])
                S.op("dve", lambda: V.tensor_tensor(out=scratch, in0=scratch, in1=rkf[:, 0:n], op=ALU.add), reads=["rkf", ksc], writes=[ksc])
                S.op("act", lambda: A_.activation(out=out_ap, in_=scratch, func=AF.Sin), reads=[ksc], writes=[kout])

            def sincos(ph, kph, out_s, ks, out_c, kc, scratch, ksc):
                reduce_sin(ph, kph, 0.0, out_s, ks, scratch, ksc)
                reduce_sin(ph, kph, 0.5 * PI, out_c, kc, scratch, ksc)

            S.op("dve", lambda: V.tensor_scalar(out=t_a[:], in0=sa[:], scalar1=nsp1[:, 0:1], scalar2=None, op0=ALU.mult), reads=["sa", "nsp1"], writes=["t_a"])
            S.op("act", lambda: A_.activation(out=t_a[:], in_=t_a[:], func=AF.Exp), reads=["t_a"], writes=["t_a"])
            S.op("dve", lambda: V.tensor_scalar(out=t_b[:], in0=sp_[:], scalar1=sp1[:, 0:1], scalar2=None, op0=ALU.mult), reads=["sp_", "sp1"], writes=["t_b"])
            sincos(t_b[:], "t_b", t_c[:], "t_c", t_d[:], "t_d", t_e[:], "t_e")
            S.op("dve", lambda: V.tensor_tensor(out=Tnr[:], in0=t_a[:], in1=t_d[:], op=ALU.mult), reads=["t_a", "t_d"], writes=["Tnr"])
            S.op("dve", lambda: V.scalar_tensor_tensor(out=Tni[:], in0=t_a[:], scalar=-1.0, in1=t_c[:], op0=ALU.mult, op1=ALU.mult),
                 reads=["t_a", "t_c"], writes=["Tni"])
            S.op("act", lambda: A_.activation(out=t_a[:], in_=sa[:], func=AF.Exp), reads=["sa", "Tnr", "Tni"], writes=["t_a"])
            sincos(sp_[:], "sp_", t_c[:], "t_c", t_d[:], "t_d", t_e[:], "t_e")
            S.op("dve", lambda: V.tensor_tensor(out=t_d[:], in0=t_a[:], in1=t_d[:], op=ALU.mult), reads=["t_a", "t_d"], writes=["t_d"])
            S.op("dve", lambda: V.tensor_scalar(out=t_d[:], in0=t_d[:], scalar1=-1.0, scalar2=None, op0=ALU.add), reads=["t_d"], writes=["t_d"])
            S.op("dve", lambda: V.tensor_tensor(out=t_c[:], in0=t_a[:], in1=t_c[:], op=ALU.mult), reads=["t_a", "t_c"], writes=["t_c"])
            S.op("dve", lambda: V.tensor_tensor(out=t_a[:], in0=are[:], in1=are[:], op=ALU.mult), reads=["are", "t_c", "t_d"], writes=["t_a"])
            S.op("dve", lambda: V.tensor_tensor(out=t_b[:], in0=aim[:], in1=aim[:], op=ALU.mult), reads=["aim"], writes=["t_b"])
            S.op("dve", lambda: V.tensor_tensor(out=t_a[:], in0=t_a[:], in1=t_b[:], op=ALU.add), reads=["t_a", "t_b"], writes=["t_a"])
            S.op("dve", lambda: V.reciprocal(out=t_a[:], in_=t_a[:]), reads=["t_a"], writes=["t_a"])
            S.op("dve", lambda: V.tensor_tensor(out=t_b[:], in0=t_d[:], in1=are[:], op=ALU.mult), reads=["t_d", "are"], writes=["t_b"])
            S.op("dve", lambda: V.tensor_tensor(out=t_e[:], in0=t_c[:], in1=aim[:], op=ALU.mult), reads=["t_c", "aim"], writes=["t_e"])
            S.op("dve", lambda: V.tensor_tensor(out=t_b[:], in0=t_b[:], in1=t_e[:], op=ALU.add), reads=["t_b", "t_e"], writes=["t_b"])
            S.op("dve", lambda: V.tensor_tensor(out=t_b[:], in0=t_b[:], in1=t_a[:], op=ALU.mult), reads=["t_b", "t_a"], writes=["t_b"])
            S.op("dve", lambda: V.tensor_tensor(out=t_e[:], in0=t_c[:], in1=are[:], op=ALU.mult), reads=["t_c", "are", "t_b"], writes=["t_e"])
            S.op("dve", lambda: V.tensor_tensor(out=t_c[:], in0=t_d[:], in1=aim[:], op=ALU.mult), reads=["t_d", "aim", "t_e"], writes=["t_c"])
            S.op("dve", lambda: V.tensor_tensor(out=t_e[:], in0=t_e[:], in1=t_c[:], op=ALU.subtract), reads=["t_e", "t_c"], writes=["t_e"])
            S.op("dve", lambda: V.tensor_tensor(out=t_e[:], in0=t_e[:], in1=t_a[:], op=ALU.mult), reads=["t_e", "t_a"], writes=["t_e"])
            Brr = sbt("Brr", [128, 512], F32); Bri = sbt("Bri", [128, 512], F32)
            S.op("pool", lambda: P_.memset(Brr[:], 0.0), writes=["Brr"])
            S.op("pool", lambda: P_.memset(Bri[:], 0.0), writes=["Bri"])
            for gi in range(8):
                S.dma("sp", lambda: nc.sync.dma_start(out=Brr[16 * gi:16 * gi + 16, 64 * gi:64 * gi + 64], in_=b_re[gi].rearrange("n c -> c n"),
                                                      allow_slow_non_contiguous=True), reads=["Brr"], writes=[("Brr", gi)])
                S.dma("sp", lambda: nc.sync.dma_start(out=Bri[16 * gi:16 * gi + 16, 64 * gi:64 * gi + 64], in_=b_im[gi].rearrange("n c -> c n"),
                                                      allow_slow_non_contiguous=True), reads=["Bri"], writes=[("Bri", gi)])
            bk = [("Brr", gi) for gi in range(8)] + [("Bri", gi) for gi in range(8)]
            S.op("dve", lambda: V.tensor_tensor(out=t_a[:], in0=t_b[:], in1=Brr[:], op=ALU.mult), reads=["t_b", "t_a"] + bk, writes=["t_a"])
            S.op("dve", lambda: V.tensor_tensor(out=t_c[:], in0=t_e[:], in1=Bri[:], op=ALU.mult), reads=["t_e", "t_c"] + bk, writes=["t_c"])
            S.op("dve", lambda: V.tensor_tensor(out=Bblk[:, 0:512], in0=t_a[:], in1=t_c[:], op=ALU.subtract), reads=["t_a", "t_c"], writes=["Bblk"])
            S.op("dve", lambda: V.tensor_tensor(out=t_a[:], in0=t_b[:], in1=Bri[:], op=ALU.mult), reads=["t_b", "t_a", "Bblk"] + bk, writes=["t_a"])
            S.op("dve", lambda: V.tensor_tensor(out=t_c[:], in0=t_e[:], in1=Brr[:], op=ALU.mult), reads=["t_e", "t_c", "Bblk"] + bk, writes=["t_c"])
            S.op("dve", lambda: V.tensor_tensor(out=Bblk[:, 512:1024], in0=t_a[:], in1=t_c[:], op=ALU.add), reads=["t_a", "t_c"], writes=["Bblk"])
            Cst = sbt("Cst", [128, 8, 128], F32)
            S.op("pool", lambda: P_.memset(Cst[:], 0.0), writes=["Cst"])
            ck = []
            for gi in range(8):
                k, gl = gi // 2, gi % 2
                S.dma("sp", lambda: nc.sync.dma_start(out=Cst[64 * gl:64 * gl + 64, k, 16 * gi:16 * gi + 16], in_=c_re[gi].rearrange("c n -> n c"),
                                                      allow_slow_non_contiguous=True), reads=["Cst"], writes=[("Cst", gi, 0)])
                S.dma("sp", lambda: nc.sync.dma_start(out=Cst[64 * gl:64 * gl + 64, 4 + k, 16 * gi:16 * gi + 16], in_=c_im[gi].rearrange("c n -> n c"),
                                                      allow_slow_non_contiguous=True), reads=["Cst"], writes=[("Cst", gi, 1)])
                ck += [("Cst", gi, 0), ("Cst", gi, 1)]
            S.op("dve", lambda: V.tensor_copy(out=Cmat[:, 0:4, :], in_=Cst[:, 0:4, :]), reads=ck, writes=["Cmat"])
            S.op("dve", lambda: V.tensor_scalar(out=Cmat[:, 4:8, :], in0=Cst[:, 4:8, :], scalar1=-1.0, scalar2=None, op0=ALU.mult), reads=ck, writes=["Cmat"])
            arc = sbt("arc", [128, 4], F32); aic = sbt("aic", [128, 4], F32); stc = sbt("stc", [128, 4], F32)
            S.dma("sp", lambda: nc.sync.dma_start(out=arc[:], in_=a_re.rearrange("o (k p) -> (o p) k", p=128), allow_slow_non_contiguous=True), writes=["arc"])
            S.dma("sp", lambda: nc.sync.dma_start(out=aic[:], in_=a_im.rearrange("o (k p) -> (o p) k", p=128), allow_slow_non_contiguous=True), writes=["aic"])
            for gi in range(8):
                k, gl = gi // 2, gi % 2
                S.dma("sp", lambda: nc.sync.dma_start(out=stc[64 * gl:64 * gl + 64, k:k + 1], in_=log_step[0, gi:gi + 1].partition_broadcast(64)),
                      writes=[("stc", gi)])
            S.op("act", lambda: A_.activation(out=stc[:], in_=stc[:], func=AF.Exp), reads=[("stc", gi) for gi in range(8)], writes=["stc"])
            S.op("dve", lambda: V.tensor_scalar(out=arc[:], in0=arc[:], scalar1=-1e-4, scalar2=None, op0=ALU.min), reads=["arc"], writes=["arc"])
            S.op("dve", lambda: V.tensor_tensor(out=arc[:], in0=arc[:], in1=stc[:], op=ALU.mult), reads=["arc", "stc"], writes=["arc"])
            S.op("dve", lambda: V.tensor_tensor(out=aic[:], in0=aic[:], in1=stc[:], op=ALU.mult), reads=["aic", "stc"], writes=["aic"])
            tp1i = sbt("tp1i", [128, 128], I32); tp1 = sbt("tp1", [128, 128], F32)
            S.op("pool", lambda: P_.iota(tp1i[:], pattern=[[1, 128]], base=1, channel_multiplier=0), writes=["tp1i"])
            S.op("dve", lambda: V.tensor_copy(out=tp1[:], in_=tp1i[:]), reads=["tp1i"], writes=["tp1"])
            for k in range(4):
                S.op("dve", lambda: V.tensor_scalar(out=t_a[:, 0:128], in0=tp1[:], scalar1=arc[:, k:k + 1], scalar2=None, op0=ALU.mult),
                     reads=["tp1", "arc", "Bblk"], writes=["t_a"])
                S.op("act", lambda: A_.activation(out=t_a[:, 0:128], in_=t_a[:, 0:128], func=AF.Exp), reads=["t_a"], writes=["t_a"])
                S.op("dve", lambda: V.tensor_scalar(out=t_b[:, 0:128], in0=tp1[:], scalar1=aic[:, k:k + 1], scalar2=None, op0=ALU.mult),
                     reads=["tp1", "aic", "Bblk"], writes=["t_b"])
                sincos(t_b[:, 0:128], "t_b", t_c[:, 0:128], "t_c", t_d[:, 0:128], "t_d", t_e[:, 0:128], "t_e")
                S.op("dve", lambda: V.tensor_tensor(out=Ppr[:, k, :], in0=t_a[:, 0:128], in1=t_d[:, 0:128], op=ALU.mult), reads=["t_a", "t_d"], writes=["Ppr"])
                S.op("dve", lambda: V.tensor_tensor(out=Ppi[:, k, :], in0=t_a[:, 0:128], in1=t_c[:, 0:128], op=ALU.mult), reads=["t_a", "t_c"], writes=["Ppi"])
            for e in ("pe", "act", "dve", "pool", "sp"):
                S.wait_all(e)

        pbu = ps("pbu", [128, 2, 512], F32)
        pz = [ps("pz0", [128, 8, 128], F32)] * 2
        py = ps("py", [128, 512], F32)
        wq = [[sb(f"w{k}_{i}", [128, 512], F32) for k in range(4)] for i in range(2)]
        Wbs = [sb(f"Wbs{i}", [128, 1024], BF16) for i in range(2)]
        zp = sb("zp", [128, 8, 128], F32)
        xq = [[sb(f"x{k}_0", [128, 4, 128], F32) for k in range(4)]] * 2
        XTs = [sb(f"XT{i}", [128, 8, 128], BF16) for i in range(2)]
        car = [sb(f"car{i}", [128, 8], F32) for i in range(2)]
        yf = sb("yf", [128, 512], F32); y2 = sb("y2", [128, 512], F32); ysg = sb("ysg", [128, 512], F32)
        ybs = [sb(f"yb{i}", [128, 512], BF16) for i in range(2)]
        S.op("dve", lambda: V.memset(car[0][:], 0.0), writes=[("car", 0)])

        def bu(ct):
            for half in range(2):
                S.op("pe", lambda: T_.matmul(pbu[:, half, :], lhsT=uT[:, ct * 128:(ct + 1) * 128], rhs=Bblk[:, half * 512:(half + 1) * 512],
                                             start=True, stop=True), reads=[("uT", (ct + 3) // 4), "Bblk"], writes=["pbu"])

        def wmod(ct):
            b2 = ct % 2
            w1, w2, w3, w4_ = wq[b2]
            kw = ("wq", b2)
            S.op("dve", lambda: V.tensor_tensor(out=w1[:], in0=pbu[:, 0, :], in1=Tnr[:], op=ALU.mult), reads=["pbu", "Tnr"], writes=[kw])
            S.op("dve", lambda: V.tensor_tensor(out=w2[:], in0=pbu[:, 1, :], in1=Tni[:], op=ALU.mult), reads=["pbu", "Tni"], writes=[kw])
            S.op("dve", lambda: V.tensor_tensor(out=w3[:], in0=pbu[:, 1, :], in1=Tnr[:], op=ALU.mult), reads=["pbu", "Tnr"], writes=[kw])
            S.op("dve", lambda: V.tensor_tensor(out=w4_[:], in0=pbu[:, 0, :], in1=Tni[:], op=ALU.mult), reads=["pbu", "Tni"], writes=[kw])
            S.op("pool", lambda: P_.tensor_tensor(out=Wbs[b2][:, 0:512], in0=w1[:], in1=w2[:], op=ALU.subtract), reads=[kw], writes=[("Wbs", b2)])
            S.op("pool", lambda: P_.tensor_tensor(out=Wbs[b2][:, 512:1024], in0=w3[:], in1=w4_[:], op=ALU.add), reads=[kw], writes=[("Wbs", b2)])

        def zmm(ct):
            b2 = ct % 2
            for k in range(8):
                S.op("pe", lambda: T_.matmul(pz[b2][:, k, :], lhsT=Wbs[b2][:, k * 128:(k + 1) * 128], rhs=tri[:], start=True, stop=True),
                     reads=[("Wbs", b2), "tri"], writes=[("pz", 0)])

        def xmod(ct):
            b2 = ct % 2
            xa, xb_, xc, xd = xq[b2]
            kx = ("xq", 0)
            cin, cout = car[b2], car[1 - b2]
            S.op("dve", lambda: V.tensor_tensor(out=zp[:], in0=pz[b2][:], in1=cin[:, 0:8].unsqueeze(2).to_broadcast([128, 8, 128]), op=ALU.add),
                 reads=[("pz", 0), ("car", b2)], writes=["zp"])
            S.op("dve", lambda: V.tensor_tensor(out=xa[:], in0=zp[:, 0:4, :], in1=Ppr[:], op=ALU.mult), reads=["zp", "Ppr"], writes=[kx])
            S.op("dve", lambda: V.tensor_tensor(out=xb_[:], in0=zp[:, 4:8, :], in1=Ppi[:], op=ALU.mult), reads=["zp", "Ppi"], writes=[kx])
            S.op("dve", lambda: V.tensor_tensor(out=xc[:], in0=zp[:, 0:4, :], in1=Ppi[:], op=ALU.mult), reads=["zp", "Ppi"], writes=[kx])
            S.op("dve", lambda: V.tensor_tensor(out=xd[:], in0=zp[:, 4:8, :], in1=Ppr[:], op=ALU.mult), reads=["zp", "Ppr"], writes=[kx])
            S.op("dve", lambda: V.tensor_tensor(out=cout[:, 0:4], in0=xa[:, :, 127], in1=xb_[:, :, 127], op=ALU.subtract),
                 reads=[kx], writes=[("car", 1 - b2)])
            S.op("dve", lambda: V.tensor_tensor(out=cout[:, 4:8], in0=xc[:, :, 127], in1=xd[:, :, 127], op=ALU.add),
                 reads=[kx], writes=[("car", 1 - b2)])
            if ct == 0:
                return
            S.op("pool", lambda: P_.tensor_tensor(out=XTs[b2][:, 0:4, :], in0=xa[:], in1=xb_[:], op=ALU.subtract), reads=[kx], writes=[("XT", b2)])
            S.op("pool", lambda: P_.tensor_tensor(out=XTs[b2][:, 4:8, :], in0=xc[:], in1=xd[:], op=ALU.add), reads=[kx], writes=[("XT", b2)])

        def ymm(ct):
            if ct == 0:
                return
            b2 = ct % 2
            ci = (ct - 1) % 4
            for k in range(8):
                S.op("pe", lambda: T_.matmul(py[:, ci * 128:(ci + 1) * 128], lhsT=Cmat[:, k, :], rhs=XTs[b2][:, k, :], start=(k == 0), stop=(k == 7)),
                     reads=["Cmat", ("XT", b2)], writes=["py"])
            if ci == 3:
                gq = (ct - 1) // 4
                c0 = 128 + 512 * gq
                yb = ybs[gq % 2]; kyb = ("yb", gq % 2)
                S.op("dve", lambda: V.scalar_tensor_tensor(out=yf[:], in0=uT[:, c0:c0 + 512], scalar=dsk[:, 0:1], in1=py[:], op0=ALU.mult, op1=ALU.add),
                     reads=[("uT", gq + 1), "dsk", "py"], writes=["yf"])
                S.op("pool", lambda: P_.tensor_tensor(out=y2[:], in0=yf[:], in1=yf[:], op=ALU.mult), reads=["yf"], writes=["y2"])
                S.op("pool", lambda: P_.tensor_scalar(out=y2[:], in0=y2[:], scalar1=0.044715, scalar2=1.0, op0=ALU.mult, op1=ALU.add), reads=["y2"], writes=["y2"])
                S.op("pool", lambda: P_.tensor_tensor(out=y2[:], in0=y2[:], in1=yf[:], op=ALU.mult), reads=["y2", "yf"], writes=["y2"])
                S.op("act", lambda: A_.activation(out=ysg[:], in_=y2[:], func=AF.Sigmoid, scale=1.5957691216057308), reads=["y2"], writes=["ysg"])
                S.op("pool", lambda: P_.tensor_tensor(out=yb[:], in0=yf[:], in1=ysg[:], op=ALU.mult), reads=["yf", "ysg"], writes=[kyb])
                S.dma("sp", lambda: nc.sync.dma_start(out=slab3[128:256, gq, :], in_=yb[:]), reads=[kyb], writes=[("slabY", gq)])


        with ExitStack() as st1:
            sb, ps = mk(st1)
            w4b = sb("w4b", [128, 8, 512], BF16)
            biasq = sb("biasq", [128, 1], F32); biask = sb("biask", [128, 1], F32); biasu = sb("biasu", [128, 1], F32)
            bv_bc = sb("bv_bc", [128, 128], F32)
            with ExitStack() as stw:
                sbw, psw = mk(stw)
                wf = sbw("wf", [128, 8, 512], F32)
                gcol = sbw("gcol", [128, 8], F32); bcol = sbw("bcol", [128, 8], F32)
                bcr = sbw("bcr", [128, 8, 128], F32)
                pb = psw("pb", [128, 4], F32)
                pbv = psw("pbv", [128, 128], F32)
                S.dma("sp", lambda: nc.sync.dma_start(out=wf[:], in_=w4.rearrange("(k p) n -> p k n", p=128)), writes=["wf"])
                S.dma("sp", lambda: nc.sync.dma_start(out=gcol[:], in_=ln_in_g.rearrange("o (k p) -> (o p) k", p=128), allow_slow_non_contiguous=True), writes=["gcol"])
                S.dma("sp", lambda: nc.sync.dma_start(out=bcol[:], in_=ln_in_b.rearrange("o (k p) -> (o p) k", p=128), allow_slow_non_contiguous=True), writes=["bcol"])
                for k in range(8):
                    S.op("dve", lambda: V.tensor_scalar(out=w4b[:, k, :], in0=wf[:, k, :], scalar1=gcol[:, k:k + 1], scalar2=None, op0=ALU.mult),
                         reads=["wf", "gcol"], writes=["w4b"])
                    S.op("pool", lambda: P_.tensor_copy(out=bcr[:, k, :], in_=bcol[:, k:k + 1].to_broadcast([128, 128])), reads=["bcol"], writes=["bcr"])
                for bi, (dstb, kb, sc) in enumerate(((biasq, "biasq", 0.125), (biask, "biask", 1.0), (None, None, None), (biasu, "biasu", 1.0))):
                    if dstb is None:
                        continue
                    for k in range(8):
                        S.op("pe", lambda: T_.matmul(pb[:, bi:bi + 1], lhsT=wf[:, k, bi * 128:(bi + 1) * 128], rhs=bcol[:, k:k + 1],
                                                     start=(k == 0), stop=(k == 7)), reads=["wf", "bcol"], writes=["pb"])
                    S.op("dve", lambda: V.tensor_scalar(out=dstb[:], in0=pb[:, bi:bi + 1], scalar1=sc, scalar2=None, op0=ALU.mult), reads=["pb"], writes=[kb])
                for k in range(8):
                    S.op("pe", lambda: T_.matmul(pbv[:], lhsT=bcr[:, k, :], rhs=wf[:, k, 256:384], start=(k == 0), stop=(k == 7)),
                         reads=["wf", "bcr"], writes=["pbv"])
                S.op("dve", lambda: V.tensor_copy(out=bv_bc[:], in_=pbv[:]), reads=["pbv"], writes=["bv_bc"])
                for e in ("pe", "act", "dve", "pool", "sp"):
                    S.wait_all(e)
            NB = 3
            xts = [sb(f"xt{i}", [128, D], F32) for i in range(NB)]
            tmps = [None] * NB
            hbs = [sb(f"hb{i}", [128, D], BF16) for i in range(NB)]
            hTs = [sb(f"hT{i}", [128, 8, 512], BF16) for i in range(2)]
            Vst = [sb(f"Vst{i}", [128, 4, 129], BF16) for i in range(2)]
            for i_ in range(2):
                S.op("pool", lambda: P_.memset(Vst[i_][:, :, 128:129], 1.0), writes=[("Vst1", i_)])
            lnsb = [(sb(f"st6_{i}", [128, 12], F32), sb(f"mv_{i}", [128, 2], F32), sb(f"rstd_{i}", [128, 1], F32),
                     sb(f"nmr_{i}", [128, 1], F32)) for i in range(NB)]
            ptr = [ps("ptr0", [128, 8, 128], BF16)] * 2
            pproj = [ps("pproj0", [128, 512], F32)] * 2
            pv = ps("pv", [128, 4, 128], F32)
            groups = [[0]] + [list(range(1 + 4 * g, 5 + 4 * g)) for g in range(NG)]
            flat = [(gi, ti, ct) for gi, tiles in enumerate(groups) for ti, ct in enumerate(tiles)]
            pcnt = [0]

            def stA(t):
                gi, ti, ct = flat[t]
                s = t % NB
                xt, tmp, hb = xts[s], tmps[s], hbs[s]
                if ct == 0:
                    S.op("dve", lambda: V.memset(xt[0:112, :], 0.0), writes=[("xt", s)])
                    S.dma("sp", lambda: nc.sync.dma_start(out=xt[112:128, :], in_=meta), writes=[("xt", s)])
                else:
                    S.dma("sp", lambda: nc.sync.dma_start(out=xt[:], in_=x_b[(ct - 1) * 128:ct * 128, :]), writes=[("xt", s)])
                st6, mv, rstd, nmr = lnsb[s]
                ks = ("lnstat", id(st6))
                kx = ("xt", s)
                S.op("dve", lambda: V.bn_stats(out=st6[:, 0:6], in_=xt[:, 0:512]), reads=[kx], writes=[ks])
                S.op("dve", lambda: V.bn_stats(out=st6[:, 6:12], in_=xt[:, 512:1024]), reads=[kx], writes=[ks])
                S.op("dve", lambda: V.bn_aggr(out=mv[:, 0:2], in_=st6[:, 0:12]), reads=[ks], writes=[ks])
                S.op("dve", lambda: V.tensor_scalar(out=rstd[:], in0=mv[:, 1:2], scalar1=LN_EPS, scalar2=None, op0=ALU.add), reads=[ks], writes=[ks])
                S.op("dve", lambda: V.tensor_scalar(out=nmr[:], in0=mv[:, 0:1], scalar1=-1.0, scalar2=None, op0=ALU.mult), reads=[ks], writes=[ks])
                S.op("act", lambda: A_.activation(out=rstd[:], in_=rstd[:], func=AF.Ln), reads=[ks], writes=[ks])
                S.op("act", lambda: A_.activation(out=rstd[:], in_=rstd[:], func=AF.Exp, scale=-0.5), reads=[ks], writes=[ks])
                S.op("act", lambda: A_.activation(out=nmr[:], in_=nmr[:], func=AF.Copy, scale=rstd[:, 0:1]), reads=[ks], writes=[ks])
                S.op("act", lambda: A_.activation(out=hb[:], in_=xt[:], func=AF.Identity, bias=nmr[:, 0:1], scale=rstd[:, 0:1]),
                     reads=[kx, ks], writes=[("hb", s)])

            def stB(t):
                gi, ti, ct = flat[t]
                s = t % NB
                hb = hbs[s]
                hT = hTs[gi % 2]; khT = ("hT", gi % 2)
                pt = ptr[0]
                for k in range(8):
                    S.op("pe", lambda: T_.transpose(out=pt[:, k, :], in_=hb[:, k * 128:(k + 1) * 128], identity=ident[:]),
                         reads=[("hb", s), "ident"], writes=[("ptr", 0)])
                S.op("act", lambda: A_.copy(out=hT[:, :, ti * 128:(ti + 1) * 128], in_=pt[:]),
                     reads=[("ptr", 0)], writes=[khT])

            def stC(gi):
                tiles = groups[gi]
                hT = hTs[gi % 2]; khT = ("hT", gi % 2)
                ncols = 128 * len(tiles)
                c0 = tiles[0] * 128
                for bi, (dst, kd) in enumerate(((qT, "qT"), (kT, "kT"), (None, None), (uT, ("uT", gi)))):
                    if dst is None:
                        continue
                    pp = pproj[0]; kp = ("pproj", 0)
                    for k in range(8):
                        S.op("pe", lambda: T_.matmul(pp[:, 0:ncols], lhsT=w4b[:, k, bi * 128:(bi + 1) * 128], rhs=hT[:, k, 0:ncols],
                                                     start=(k == 0), stop=(k == 7)), reads=["w4b", khT], writes=[kp])
                    if bi == 0:
                        S.op("act", lambda: A_.activation(out=dst[:, c0:c0 + ncols], in_=pp[:, 0:ncols], func=AF.Identity, bias=biasq[:, 0:1], scale=0.125),
                             reads=[kp, "biasq"], writes=[kd])
                    else:
                        bb_ = biask if bi == 1 else biasu
                        S.op("dve", lambda: V.tensor_scalar(out=dst[:, c0:c0 + ncols], in0=pp[:, 0:ncols], scalar1=bb_[:, 0:1], scalar2=None, op0=ALU.add),
                             reads=[kp, "biask", "biasu"], writes=[kd])
                        if bi == 3 and gi == 0:
                            S.op("dve", lambda: V.memset(uT[:, 0:112], 0.0), reads=[kd], writes=[kd])
                for ti, ct in enumerate(tiles):
                    for k in range(8):
                        S.op("pe", lambda: T_.matmul(pv[:, ti, :], lhsT=hT[:, k, ti * 128:(ti + 1) * 128], rhs=w4b[:, k, 256:384],
                                                     start=(k == 0), stop=(k == 7)), reads=["w4b", khT], writes=["pv"])
                nt_ = len(tiles)
                vs_ = gi % 2
                S.op("dve", lambda: V.tensor_tensor(out=Vst[vs_][:, 0:nt_, 0:128], in0=pv[:, 0:nt_, :],
                                                    in1=bv_bc[:, 0:128].unsqueeze(1).to_broadcast([128, nt_, 128]), op=ALU.add),
                     reads=["pv", "bv_bc"], writes=[("Vst", vs_)])
                S.dma("sp", lambda: nc.sync.dma_start(out=vaug_d[:, tiles[0]:tiles[0] + nt_, :], in_=Vst[vs_][:, 0:nt_, :]),
                      reads=[("Vst", vs_), ("Vst1", vs_)], writes=[("vaug_d", gi)])

            nfl = len(flat)
            stA(0)
            if nfl > 1:
                stA(1)
            ssm_next = [0]

            def ssm_run(upto):
                while ssm_next[0] < upto:
                    ct_ = ssm_next[0]
                    if ct_ == 0:
                        bu(0)
                        wmod(0)
                    if ct_ + 1 < NT:
                        bu(ct_ + 1)
                    zmm(ct_)
                    if ct_ >= 1:
                        ymm(ct_ - 1)
                    if ct_ + 1 < NT:
                        wmod(ct_ + 1)
                    xmod(ct_)
                    ssm_next[0] += 1

            print("SBUF bytes remaining in fused phase:", nc.sbuf_bytes_remaining, flush=True)
            allowed = 0
            for t in range(nfl):
                if t + 2 < nfl:
                    stA(t + 2)
                stB(t)
                gi, ti, ct = flat[t]
                if ti == len(groups[gi]) - 1:
                    stC(gi)
                    if gi >= 1:
                        allowed = groups[gi - 1][-1]
                if ssm_next[0] < allowed:
                    ssm_run(ssm_next[0] + 1)
                    if allowed - ssm_next[0] > 4:
                        ssm_run(ssm_next[0] + 1)
            ssm_run(NT)
            ymm(NT - 1)
            if debug:
                S.dma("sp", lambda: nc.sync.dma_start(out=dbg["qT"], in_=qT[:]), reads=["qT"], writes=["dq"])
                S.dma("sp", lambda: nc.sync.dma_start(out=dbg["kT"], in_=kT[:]), reads=["kT"], writes=["dk"])
            for e in ("pe", "act", "dve", "pool", "sp"):
                S.wait_all(e)
        st3.close()
        st_u.close()
        print("P1a+S5 done: inst", S.n_inst, "waits", S.n_wait, "sems", S.nsem, flush=True)

        XCH = min(1024, 128 * NG)
        NXC = 256 * NG // XCH

        GPC = XCH // 256

        def ag_chunk(c_):
            keys = []
            for g_ in range(c_ * GPC, (c_ + 1) * GPC):
                keys += [("slabA", g_), ("slabY", g_)]
            S.waitfor("pool", keys)
            S.op("pool", lambda: P_.collective_compute("AllGather", ALU.bypass, replica_groups=[[0, 1, 2, 3], [4, 5, 6, 7]],
                                                       ins=[slab[c_ * XCH:(c_ + 1) * XCH, :].opt()],
                                                       outs=[gath[c_ * 4 * XCH:(c_ + 1) * 4 * XCH, :].opt()]), writes=[("gath", c_)])

        with ExitStack() as st2:
            sb, ps = mk(st2)
            Vaug = sb("Vaug", [128, NT, 129], BF16)
            S.dma("sp", lambda: nc.sync.dma_start(out=Vaug[:], in_=vaug_d), reads=[("vaug_d", gi_) for gi_ in range(NG + 1)], writes=["Vaug"])
            tb = bc_load("tb", relb[0], 32, sb)
            Wb = sb("Wb", [128, 1024], F32)
            Bm0 = sb("Bm0", [128, 512], F32)
            with ExitStack() as stt:
                sbt, _ = mk(stt)
                reli = sbt("reli", [128, 1024], I32)
                relv = sbt("relv", [128, 1024], F32)
                stp = sbt("stp", [128, 1024], F32)
                iom = sbt("iom", [128, 1024], F32)
                dl = sbt("dl", [128, 32], F32)
                thrp = sbt("thrp", [128, 1], F32)
                S.op("pool", lambda: P_.iota(reli[:], pattern=[[-1, 1024]], base=384, channel_multiplier=1), writes=["reli"])
                S.op("dve", lambda: V.tensor_copy(out=relv[:], in_=reli[:]), reads=["reli"], writes=["relv"])
                S.op("pool", lambda: P_.iota(reli[:], pattern=[[1, 1024]], base=0, channel_multiplier=0), reads=["relv"], writes=["reli"])
                S.op("dve", lambda: V.tensor_copy(out=iom[:], in_=reli[:]), reads=["reli"], writes=["iom"])
                steps = []
                for j in range(15, 8, -1):
                    steps.append((-thr[j] + 1, j - 1))
                for n in range(7, -1, -1):
                    steps.append((-n, n))
                for n in range(1, 8):
                    steps.append((n, 16 + n))
                steps.append((8, 24))
                for j in range(9, 16):
                    steps.append((thr[j], 16 + j))
                prev = 15
                S.op("dve", lambda: V.tensor_scalar(out=Wb[:], in0=relv[:], scalar1=0.0, scalar2=tb[:, 15:16],
                                                    op0=ALU.mult, op1=ALU.add), reads=["relv", "tb"], writes=["Wb"])
                for si, (tv, bk) in enumerate(steps):
                    S.op("dve", lambda: V.tensor_tensor(out=dl[:, si:si + 1], in0=tb[:, bk:bk + 1], in1=tb[:, prev:prev + 1],
                                                        op=ALU.subtract), reads=["tb"], writes=["dl"])
                    S.op("dve", lambda: V.tensor_scalar(out=stp[:], in0=relv[:], scalar1=float(tv), scalar2=dl[:, si:si + 1],
                                                        op0=ALU.is_ge, op1=ALU.mult), reads=["relv", "dl"], writes=["stp"])
                    S.op("dve", lambda: V.tensor_tensor(out=Wb[:], in0=Wb[:], in1=stp[:], op=ALU.add), reads=["stp", "Wb"], writes=["Wb"])
                    prev = bk
                S.op("pool", lambda: P_.affine_select(out=Wb[0:64, :], in_=Wb[0:64, :], pattern=[[1, 1024]], compare_op=ALU.is_ge,
                                                      fill=NEG, base=-384, channel_multiplier=0), reads=["Wb"], writes=["Wb"])
                S.op("pool", lambda: P_.affine_select(out=Wb[64:128, :], in_=Wb[64:128, :], pattern=[[1, 1024]], compare_op=ALU.is_ge,
                                                      fill=NEG, base=-448, channel_multiplier=0), reads=["Wb"], writes=["Wb"])
                S.op("dve", lambda: V.tensor_copy(out=Bm0[:], in_=Wb[:, 512:1024]), reads=["Wb"], writes=["Bm0"])
                S.op("dve", lambda: V.memset(Bm0[0:112, :], NEG), reads=["Bm0"], writes=["Bm0"])
                for e in ("pe", "act", "dve", "pool", "sp"):
                    S.wait_all(e)
            Wbb = sb("Wbb", [128, 1024], BF16)
            Bm0b = sb("Bm0b", [128, 512], BF16)
            S.op("dve", lambda: V.tensor_copy(out=Wbb[:], in_=Wb[:]), reads=["Wb"], writes=["Wbb"])
            S.op("dve", lambda: V.tensor_copy(out=Bm0b[:], in_=Bm0[:]), reads=["Bm0"], writes=["Bm0b"])
            bfar = sb("bfar", [128, 1], F32)
            bfarm = sb("bfarm", [128, 1], F32)
            S.op("dve", lambda: V.tensor_copy(out=bfar[:], in_=tb[:, 15:16]), reads=["tb"], writes=["bfar"])
            S.op("dve", lambda: V.tensor_copy(out=bfarm[:], in_=tb[:, 15:16]), reads=["tb"], writes=["bfarm"])
            S.op("dve", lambda: V.memset(bfarm[0:112, :], NEG), reads=["bfarm"], writes=["bfarm"])
            lamt = sb("lamt", [128, 4, 64], F32)
            S.dma("sp", lambda: nc.sync.dma_start(out=lamt[:], in_=lam4.rearrange("a n -> (a n)").partition_broadcast(128)), writes=["lamt"])
            lsum = sb("lsum", [128, 2], F32)
            lprod = sb("lprod", [128, 2, 64], F32)
            neglam = sb("neglam", [128, 1], F32)
            S.op("dve", lambda: V.tensor_tensor(out=lprod[:, 0, :], in0=lamt[:, 0, :], in1=lamt[:, 1, :], op=ALU.mult), reads=["lamt"], writes=["lprod"])
            S.op("dve", lambda: V.tensor_tensor(out=lprod[:, 1, :], in0=lamt[:, 2, :], in1=lamt[:, 3, :], op=ALU.mult), reads=["lamt"], writes=["lprod"])
            S.op("dve", lambda: V.reduce_sum(out=lsum[:], in_=lprod[:], axis=AX.X), reads=["lprod"], writes=["lsum"])
            S.op("act", lambda: A_.activation(out=lsum[:], in_=lsum[:], func=AF.Exp), reads=["lsum"], writes=["lsum"])
            S.op("dve", lambda: V.tensor_tensor(out=neglam[:], in0=lsum[:, 1:2], in1=lsum[:, 0:1], op=ALU.subtract), reads=["lsum"], writes=["neglam"])
            S.op("dve", lambda: V.tensor_scalar(out=neglam[:], in0=neglam[:], scalar1=-0.2, scalar2=None, op0=ALU.add), reads=["neglam"], writes=["neglam"])
            g08 = bc_load("g08", subln_g[0], 128, sb)
            S.op("dve", lambda: V.tensor_scalar(out=g08[:], in0=g08[:], scalar1=0.8, scalar2=None, op0=ALU.mult), reads=["g08"], writes=["g08"])

            pss = [ps(f"pss{i}", [128, 2, 512], F32) for i in range(2)]
            po = ps("po", [128, 3, 512], F32)
            ptt = ps("ptt", [128, 4, 128], BF16)
            pTs = [sb(f"pT{i}", [128, 2, 512], BF16) for i in range(3)]
            sn = [sb(f"sn{i}", [128, 2, 512], F32) for i in range(2)]
            osb = sb("osb", [128, 128], F32)
            o2 = sb("o2", [128, 128], F32)
            ob = sb("ob", [128, 4, 128], BF16)
            rr = sb("rr", [128, 8], F32)
            attT = [sb(f"attT{i}", [128, 512], BF16) for i in range(2)]

            def acc(a):
                return po[:, a // 3, (a % 3) * 129:(a % 3) * 129 + 129]

            o4 = sb("o4", [128, 4, 128], F32)
            ms = sb("ms", [128, 4], F32)
            units = [(g, j) for g in range(NG) for j in range(4 * g + 5)]
            ncnt = [0]

            def near_of(g, j):
                if j == 0:
                    return Bm0b[:, :] if g == 0 else None
                if j < 4 * g:
                    return None
                tp = j - (4 * g + 1)
                return Wbb[:, 384 - 128 * tp:384 - 128 * tp + 512]

            def qk(i):
                g, j = units[i]
                u = i % 2
                q0 = 128 + 512 * g
                nb = near_of(g, j)
                for m in range(2):
                    S.op("pe", lambda: T_.matmul(pss[u][:, m, :], lhsT=kT[64 * m:64 * m + 64, j * 128:(j + 1) * 128],
                                                 rhs=qT[64 * m:64 * m + 64, q0:q0 + 512], start=True, stop=(nb is None)),
                         reads=["kT", "qT"], writes=[("pss", u)])
                    if nb is not None:
                        S.op("pe", lambda: T_.matmul(pss[u][:, m, :], lhsT=ident[:], rhs=nb, start=False, stop=True),
                             reads=["ident", "Wbb", "Bm0b"], writes=[("pss", u)])

            def ex(i):
                g, j = units[i]
                u = i % 2
                v3 = i % 3
                pS, pT = pss[u], pTs[v3]
                if j == 0 and g > 0:
                    S.op("act", lambda: A_.activation(out=pT[:], in_=pS[:], func=AF.Exp, bias=bfarm[:, 0:1], scale=1.0),
                         reads=[("pss", u), "bfarm"], writes=[("pT", v3)])
                elif 0 < j < 4 * g:
                    S.op("act", lambda: A_.activation(out=pT[:], in_=pS[:], func=AF.Exp, bias=bfar[:, 0:1], scale=1.0),
                         reads=[("pss", u), "bfar"], writes=[("pT", v3)])
                else:
                    S.op("act", lambda: A_.activation(out=pT[:], in_=pS[:], func=AF.Exp), reads=[("pss", u)], writes=[("pT", v3)])

            def pv(i):
                g, j = units[i]
                u = i % 3
                last = 4 * g + 4
                for m in range(2):
                    for qs in range(4):
                        S.op("pe", lambda: T_.matmul(acc(m * 4 + qs), lhsT=pTs[u][:, m, qs * 128:(qs + 1) * 128], rhs=Vaug[:, j, :],
                                                     start=(j == 0 and (m * 4 + qs) % 3 == 0), stop=(j == last), skip_group_check=True),
                             reads=[("pT", u), "Vaug"], writes=["po"])

            posn = sb("posn", [128, 3, 512], F32)

            def accs_(a):
                return posn[:, a // 3, (a % 3) * 129:(a % 3) * 129 + 129]

            def fin_dve1(g):
                S.op("dve", lambda: V.tensor_copy(out=posn[:, 0:2, 0:387], in_=po[:, 0:2, 0:387]), reads=["po"], writes=["posn"])
                S.op("dve", lambda: V.tensor_copy(out=posn[:, 2, 0:258], in_=po[:, 2, 0:258]), reads=["po"], writes=["posn"])
                for a in range(8):
                    S.op("dve", lambda: V.reciprocal(out=rr[:, a:a + 1], in_=accs_(a)[:, 128:129]), reads=["posn"], writes=["rr"])
                S.op("dve", lambda: V.tensor_scalar(out=rr[:, 4:8], in0=rr[:, 4:8], scalar1=neglam[:, 0:1], scalar2=None, op0=ALU.mult),
                     reads=["rr", "neglam"], writes=["rr"])
                for qs in range(4):
                    S.op("dve", lambda: V.tensor_scalar(out=o4[:, qs, :], in0=accs_(qs)[:, 0:128], scalar1=rr[:, qs:qs + 1], scalar2=None, op0=ALU.mult),
                         reads=["posn", "rr"], writes=["o4"])
                    S.op("dve", lambda: V.scalar_tensor_tensor(out=o4[:, qs, :], in0=accs_(4 + qs)[:, 0:128], scalar=rr[:, 4 + qs:5 + qs], in1=o4[:, qs, :],
                                                               op0=ALU.mult, op1=ALU.add), reads=["posn", "rr", "o4"], writes=["o4"])
                    S.op("dve", lambda: V.tensor_tensor(out=o2[:], in0=o4[:, qs, :], in1=o4[:, qs, :], op=ALU.mult), reads=["o4"], writes=["o2"])
                    S.op("dve", lambda: V.reduce_sum(out=ms[:, qs:qs + 1], in_=o2[:], axis=AX.X), reads=["o2", "ms"], writes=["ms"])
                S.op("dve", lambda: V.tensor_scalar(out=ms[:], in0=ms[:], scalar1=1.0 / 128.0, scalar2=LN_EPS, op0=ALU.mult, op1=ALU.add),
                     reads=["ms"], writes=["ms"])

            def fin_rest(g):
                aT = attT[g % 2]; kaT = ("attT", g % 2)
                S.op("act", lambda: A_.activation(out=ms[:], in_=ms[:], func=AF.Ln), reads=["ms"], writes=["ms"])
                S.op("act", lambda: A_.activation(out=ms[:], in_=ms[:], func=AF.Exp, scale=-0.5), reads=["ms"], writes=["ms"])
                for qs in range(4):
                    S.op("dve", lambda: V.scalar_tensor_tensor(out=ob[:, qs, :], in0=o4[:, qs, :], scalar=ms[:, qs:qs + 1], in1=g08[:],
                                                               op0=ALU.mult, op1=ALU.mult), reads=["o4", "ms", "g08"], writes=["ob"])
                for qs in range(4):
                    S.op("pe", lambda: T_.transpose(out=ptt[:, qs, :], in_=ob[:, qs, :], identity=ident[:]), reads=["ob", "ident"], writes=["ptt"])
                S.op("dve", lambda: V.tensor_copy(out=aT[:], in_=ptt[:].rearrange("p a b -> p (a b)")), reads=["ptt"], writes=[kaT])
                S.dma("sp", lambda: nc.sync.dma_start(out=slab3[0:128, g, :], in_=aT[:]), reads=[kaT], writes=[("slabA", g)])
                if (g + 1) % GPC == 0:
                    ag_chunk(g // GPC)

            qk(0)
            pending = None
            nun = len(units)
            for i in range(nun + 1):
                if i + 1 < nun:
                    qk(i + 1)
                if i < nun:
                    ex(i)
                if i >= 1:
                    pv(i - 1)
                    gp, jp = units[i - 1]
                    if pending is not None and jp == 3:
                        fin_rest(pending)
                        pending = None
                    if jp == 4 * gp + 4:
                        fin_dve1(gp)
                        pending = gp
            fin_rest(pending)
            for e in ("pe", "act", "dve", "pool", "sp"):
                S.wait_all(e)
        st_att.close()
        print("P1b done: inst", S.n_inst, "waits", S.n_wait, "sems", S.nsem, flush=True)
        if debug:
            S.waitfor("sp", [("slabA", g) for g in range(NG)] + [("slabY", g) for g in range(NG)])
            S.dma("sp", lambda: nc.sync.dma_start(out=dbg["slab"], in_=slab), writes=["dslab"])
        if not full:
            for e in ("pe", "act", "dve", "pool", "sp"):
                S.wait_all(e)
            return nc
        S.waitfor("pool", [("gath", c_) for c_ in range(NXC)])

        def barrier():
            for e_ in ("pe", "act", "dve", "pool", "sp"):
                S.wait_all(e_)

        IOA = bass.IndirectOffsetOnAxis
        with ExitStack() as st4:
            sb, ps = mk(st4)
            g1 = bc_load("g1", ln1_g[0], D, sb); b1 = bc_load("b1", ln1_b[0], D, sb)
            g2 = bc_load("g2", ln2_g[0], D, sb); b2 = bc_load("b2", ln2_b[0], D, sb)
            slot_ga = sb("slot_ga", [128, NT2, 4], I32)
            gate_all = sb("gate_all", [128, NT2, 4], F32)
            lnsb2 = [(sb(f"st6b_{i}", [128, 12], F32), sb(f"mvb_{i}", [128, 2], F32), sb(f"rstdb_{i}", [128, 1], F32),
                      sb(f"nmrb_{i}", [128, 1], F32)) for i in range(2)]
            ztile = sb("ztile", [128, NSLOT // 128 + 1], I32)
            S.op("pool", lambda: P_.memset(ztile[:], 0), writes=["ztile"])
            S.dma("sp", lambda: nc.sync.dma_start(out=tokslot_d.rearrange("(p f) o -> p (f o)", p=128), in_=ztile[:]), reads=["ztile"], writes=["tokslot0"])
            sc_keys = []
            h1_keys = []
            with ExitStack() as st5:
                sb5, ps5 = mk(st5)
                wglu = sb5("wglu", [128, 4, 512], BF16)
                wout = sb5("wout", [128, 8, 1024], BF16)
                wr = sb5("wr", [128, 8, 32], F32)
                S.dma("pool", lambda: P_.dma_start(out=wglu[:], in_=w_glu.rearrange("(k p) n -> p k n", p=128)), writes=["wglu"])
                S.dma("pool", lambda: P_.dma_start(out=wout[:], in_=w_out.rearrange("(k p) n -> p k n", p=128)), writes=["wout"])
                S.dma("sp", lambda: nc.sync.dma_start(out=wr[:], in_=w_router.rearrange("(k p) n -> p k n", p=128)), writes=["wr"])
                bglu = sb5("bglu", [128, 4], F32)
                S.dma("sp", lambda: nc.sync.dma_start(out=bglu[:], in_=b_glu.rearrange("o (j p) -> (o p) j", p=128), allow_slow_non_contiguous=True), writes=["bglu"])
                brt = bc_load("brt", b_router[0], NEXP, sb5)
                gidx_t = sb5("gidx_t", [128, 8 * NG2], I32)
                S.dma("sp", lambda: nc.sync.dma_start(out=gidx_t[:], in_=gidx), writes=["gidx"])
                ebi = sb5("ebi", [128, NEXP], I32); ebase = sb5("ebase", [128, NEXP], F32)
                S.op("pool", lambda: P_.iota(ebi[:], pattern=[[CAP, NEXP]], base=0, channel_multiplier=0), writes=["ebi"])
                S.op("dve", lambda: V.tensor_copy(out=ebase[:], in_=ebi[:]), reads=["ebi"], writes=["ebase"])
                cntbc = sb5("cntbc", [128, NEXP], F32)
                S.op("dve", lambda: V.memset(cntbc[:], 0.0), writes=["cntbc"])
                catT = [sb5(f"catT{i}", [128, 4, 512], BF16) for i in range(2)]
                yT = [sb5(f"yT{i}", [128, 4, 512], BF16) for i in range(2)]
                ssmT = sb5("ssmT", [128, 4, 512], BF16)
                sg = sb5("sg", [128, 512], F32)
                xt2 = [sb5(f"xt2_{i}", [128, D], F32) for i in range(2)]
                tmpa = sb5("tmpa", [128, D], F32)
                h0 = sb5("h0", [128, D], F32)
                pre = sb5("pre", [128, D], F32)
                h1 = [sb5(f"h1_{i}", [128, D], F32) for i in range(2)]
                h1b = [sb5(f"h1b_{i}", [128, D], BF16) for i in range(2)]
                h1T = sb5("h1T", [128, 8, 128], F32)
                lg = sb5("lg", [128, NEXP], F32); mx8 = sb5("mx8", [128, 8], F32); negm = sb5("negm", [128, 1], F32)
                ex = sb5("ex", [128, NEXP], F32); msk = sb5("msk", [128, NEXP], F32); mskb = sb5("mskb", [128, NEXP], BF16)
                den = sb5("den", [128, 1], F32); gfull = sb5("gfull", [128, NEXP], F32)
                rank = sb5("rank", [128, NEXP], F32); ovf = sb5("ovf", [128, NEXP], F32); keep = sb5("keep", [128, NEXP], F32)
                sga = sb5("sga", [128, NEXP], F32); ssc = sb5("ssc", [128, NEXP], F32)
                oh = sb5("oh", [128, NEXP], F32); tmq = sb5("tmq", [128, NEXP], F32)
                sgak = sb5("sgak", [128, 4], F32); ssck = sb5("ssck", [128, 4], F32)
                ssci = [sb5(f"ssci{i}", [128, 4], I32) for i in range(2)]
                tokid = [sb5(f"tokid{i}", [128, 1], I32) for i in range(2)]
                pzg = [ps5(f"pzg{i}", [128, 512], F32) for i in range(2)]
                pms = [ps5(f"pm{i}", [128, 1024], F32) for i in range(2)]
                ptf = [ps5("ptf0", [128, 4, 128], F32)] * 2
                pk = ps5("pk", [128, 512], F32)
                Q3 = sb5("Q3", [128, 3, NEXP], F32)
                tm3 = sb5("tm3", [128, 3, NEXP], F32)
                R4 = sb5("R4", [128, 4, 3], F32)
                lnsb3 = [(sb5(f"st6c_{i}", [128, 12], F32), sb5(f"mvc_{i}", [128, 2], F32), sb5(f"rstdc_{i}", [128, 1], F32),
                          sb5(f"nmrc_{i}", [128, 1], F32)) for i in range(2)]

                h0s = [h0, sb5("h0b", [128, D], F32)]
                tmpb = sb5("tmpb", [128, D], F32)

                def stage0(t2):
                    s2 = t2 % 2
                    xt = xt2[s2]
                    S.dma("sp", lambda: nc.sync.dma_start(out=xt[:], in_=x2[t2 * 128:(t2 + 1) * 128, :]), writes=[("xt2", s2)])
                    layernorm(xt, ("xt2", s2), lng, "lng", lnb, "lnb", tmpb, "tmpb", h0s[s2][:], ("h0", s2), lnsb3[0])

                def stage1_mm(t2, tt, cT, kc):
                    pm = pms[t2 % 2]
                    for half in range(2):
                        for k in range(8):
                            lhs = cT[:, k, tt * 128:(tt + 1) * 128] if k < 4 else ssmT[:, k - 4, tt * 128:(tt + 1) * 128]
                            S.op("pe", lambda: T_.matmul(pm[:, half * 512:(half + 1) * 512], lhsT=lhs, rhs=wout[:, k, half * 512:(half + 1) * 512],
                                                         start=(k == 0), stop=(k == 7)), reads=["wout", kc, "ssmT"], writes=[("pm", t2 % 2)])

                def stage1(t2, tg, tt, cT, kc):
                    s2 = t2 % 2
                    pm = pms[s2]
                    S.op("dve", lambda: V.scalar_tensor_tensor(out=pre[:], in0=h0s[s2][:], scalar=ALPHA, in1=pm[:], op0=ALU.mult, op1=ALU.add),
                         reads=[("h0", s2), ("pm", s2)], writes=["pre"])
                    h1t = h1[s2]; kh1 = ("h1", s2)
                    layernorm(pre, "pre", g1, "g1", b1, "b1", tmpa, "tmpa", h1t[:], kh1, lnsb3[1])
                    S.op("act", lambda: A_.copy(out=h1b[s2][:], in_=h1t[:]), reads=[kh1], writes=[("h1b", s2)])
                    S.dma("sp", lambda: nc.sync.dma_start(out=h1_d[t2 * 128:(t2 + 1) * 128, :], in_=h1t[:]), reads=[kh1], writes=[("h1_d", t2)])
                    S.dma("sp", lambda: nc.sync.dma_start(out=h1b_d[t2 * 128:(t2 + 1) * 128, :], in_=h1b[s2][:]), reads=[("h1b", s2)], writes=[("h1b_d", t2)])
                    h1_keys.append(("h1b_d", t2))
                    if debug:
                        S.dma("sp", lambda: nc.sync.dma_start(out=dbg["h1"][t2 * 128:(t2 + 1) * 128, :], in_=h1t[:]), reads=[kh1], writes=[("dh1", t2)])

                def stage2(t2):
                    s2 = t2 % 2
                    h1t = h1[s2]; kh1 = ("h1", s2)
                    for hh in range(2):
                        pf = ptf[hh]
                        for k4 in range(4):
                            k = hh * 4 + k4
                            S.op("pe", lambda: T_.transpose(out=pf[:, k4, :], in_=h1t[:, k * 128:(k + 1) * 128], identity=identf[:]),
                                 reads=[kh1, "identf"], writes=[("ptf", 0)])
                        S.op("act", lambda: A_.copy(out=h1T[:, hh * 4:hh * 4 + 4, :], in_=pf[:]), reads=[("ptf", 0)], writes=["h1T"])
                    for k in range(8):
                        S.op("pe", lambda: T_.matmul(pk[:, 0:32], lhsT=h1T[:, k, :], rhs=wr[:, k, :], start=(k == 0), stop=(k == 7)),
                             reads=["h1T", "wr"], writes=["pk"])
                    S.op("dve", lambda: V.tensor_tensor(out=lg[:], in0=pk[:, 0:32], in1=brt[:], op=ALU.add), reads=["pk", "brt"], writes=["lg"])
                    if debug:
                        S.dma("sp", lambda: nc.sync.dma_start(out=dbg["lg"][t2 * 128:(t2 + 1) * 128, :], in_=lg[:]), reads=["lg"], writes=[("dlg", t2)])
                    S.op("dve", lambda: V.max(out=mx8[:], in_=lg[:]), reads=["lg"], writes=["mx8"])
                    S.op("dve", lambda: V.tensor_scalar(out=negm[:], in0=mx8[:, 0:1], scalar1=-1.0, scalar2=None, op0=ALU.mult), reads=["mx8"], writes=["negm"])
                    S.op("act", lambda: A_.activation(out=ex[:], in_=lg[:], func=AF.Exp, bias=negm[:, 0:1], scale=1.0), reads=["lg", "negm"], writes=["ex"])
                    S.op("dve", lambda: V.tensor_scalar(out=msk[:], in0=lg[:], scalar1=mx8[:, 3:4], scalar2=None, op0=ALU.is_ge), reads=["lg", "mx8"], writes=["msk"])
                    S.op("dve", lambda: V.tensor_copy(out=mskb[:], in_=msk[:]), reads=["msk"], writes=["mskb"])
                    S.op("pe", lambda: T_.matmul(pk[:, 32:64], lhsT=stri[:], rhs=mskb[:], start=True, stop=True), reads=["stri", "mskb"], writes=["pk"])
                    S.op("pe", lambda: T_.matmul(pk[:, 64:96], lhsT=ones[:], rhs=mskb[:], start=True, stop=True), reads=["ones", "mskb"], writes=["pk"])
                    S.op("dve", lambda: V.tensor_tensor(out=ex[:], in0=ex[:], in1=msk[:], op=ALU.mult), reads=["ex", "msk"], writes=["ex"])
                    S.op("dve", lambda: V.reduce_sum(out=den[:], in_=ex[:], axis=AX.X), reads=["ex"], writes=["den"])
                    S.op("dve", lambda: V.reciprocal(out=den[:], in_=den[:]), reads=["den"], writes=["den"])
                    S.op("dve", lambda: V.tensor_tensor(out=rank[:], in0=pk[:, 32:64], in1=cntbc[:], op=ALU.add), reads=["pk", "cntbc"], writes=["rank"])
                    S.op("dve", lambda: V.tensor_tensor(out=cntbc[:], in0=pk[:, 64:96], in1=cntbc[:], op=ALU.add), reads=["pk", "cntbc", "rank"], writes=["cntbc"])
                    S.op("dve", lambda: V.tensor_scalar(out=ovf[:], in0=rank[:], scalar1=float(CAP), scalar2=None, op0=ALU.is_ge), reads=["rank"], writes=["ovf"])
                    S.op("dve", lambda: V.tensor_scalar(out=keep[:], in0=ovf[:], scalar1=-1.0, scalar2=1.0, op0=ALU.mult, op1=ALU.add), reads=["ovf"], writes=["keep"])
                    S.op("dve", lambda: V.scalar_tensor_tensor(out=Q3[:, 2, :], in0=ex[:], scalar=den[:, 0:1], in1=keep[:], op0=ALU.mult, op1=ALU.mult),
                         reads=["ex", "den", "keep"], writes=["Q3"])
                    S.op("dve", lambda: V.scalar_tensor_tensor(out=Q3[:, 0, :], in0=rank[:], scalar=float(CAP - 1), in1=ebase[:], op0=ALU.min, op1=ALU.add),
                         reads=["rank", "ebase"], writes=["Q3"])
                    S.op("dve", lambda: V.tensor_tensor(out=ssc[:], in0=rank[:], in1=ebase[:], op=ALU.add), reads=["rank", "ebase"], writes=["ssc"])
                    S.op("dve", lambda: V.tensor_tensor(out=ssc[:], in0=ssc[:], in1=keep[:], op=ALU.mult), reads=["ssc", "keep"], writes=["ssc"])
                    S.op("dve", lambda: V.scalar_tensor_tensor(out=Q3[:, 1, :], in0=ovf[:], scalar=float(NSLOT), in1=ssc[:], op0=ALU.mult, op1=ALU.add),
                         reads=["ovf", "ssc"], writes=["Q3"])
                    for k in range(4):
                        S.op("dve", lambda: V.tensor_scalar(out=oh[:], in0=lg[:], scalar1=mx8[:, k:k + 1], scalar2=None, op0=ALU.is_equal), reads=["lg", "mx8"], writes=["oh"])
                        S.op("dve", lambda: V.tensor_tensor(out=tm3[:], in0=Q3[:], in1=oh[:, 0:NEXP].unsqueeze(1).to_broadcast([128, 3, NEXP]), op=ALU.mult),
                             reads=["Q3", "oh"], writes=["tm3"])
                        S.op("dve", lambda: V.reduce_sum(out=R4[:, k, :], in_=tm3[:], axis=AX.X), reads=["tm3"], writes=["R4"])
                    S.op("dve", lambda: V.tensor_copy(out=slot_ga[:, t2, :], in_=R4[:, :, 0]), reads=["R4"], writes=["slot_ga"])
                    S.op("dve", lambda: V.tensor_copy(out=ssci[s2][:], in_=R4[:, :, 1]), reads=["R4"], writes=[("ssci", s2)])
                    S.op("dve", lambda: V.tensor_copy(out=gate_all[:, t2, :], in_=R4[:, :, 2]), reads=["R4"], writes=["gate_all"])
                    S.op("pool", lambda: P_.iota(tokid[s2][:], pattern=[[0, 1]], base=t2 * 128, channel_multiplier=1), writes=[("tokid", s2)])
                    for k in range(4):
                        S.dma("pool", lambda: P_.indirect_dma_start(out=tokslot_d[:, :], out_offset=IOA(ap=ssci[s2][:, k:k + 1], axis=0),
                                                                    in_=tokid[s2][:, 0:1], in_offset=None),
                              reads=[("ssci", s2), ("tokid", s2), "tokslot0"], writes=[("sc", t2, k)])
                        sc_keys.append(("sc", t2, k))

                def fetch_group(tg):
                    for j in range(4):
                        for half in range(2):
                            col = (tg * 4 + j) * 2 + half
                            dst = catT[tg % 2][:, j, :] if half == 0 else yT[tg % 2][:, j, :]
                            S.dma("pool", lambda: P_.indirect_dma_start(out=dst, out_offset=None, in_=gath[:, :],
                                                                        in_offset=IOA(ap=gidx_t[:, col:col + 1], axis=0)),
                                  reads=["gidx"], writes=[("catT", tg % 2) if half == 0 else ("yT", tg % 2)])

                fetch_group(0)
                for tg in range(NG2):
                    cT, yT_ = catT[tg % 2], yT[tg % 2]
                    kc, ky = ("catT", tg % 2), ("yT", tg % 2)
                    if tg + 1 < NG2:
                        fetch_group(tg + 1)
                    for j2 in range(4):
                        pz_ = pzg[j2 % 2]; kpz = ("pzg", j2 % 2)
                        for j1 in range(4):
                            S.op("pe", lambda: T_.matmul(pz_[:], lhsT=wglu[:, j1, j2 * 128:(j2 + 1) * 128], rhs=yT_[:, j1, :],
                                                         start=(j1 == 0), stop=(j1 == 3)), reads=["wglu", ky], writes=[kpz])
                        S.op("act", lambda: A_.activation(out=sg[:], in_=pz_[:], func=AF.Sigmoid, bias=bglu[:, j2:j2 + 1], scale=1.0),
                             reads=[kpz, "bglu"], writes=["sg"])
                        S.op("dve", lambda: V.tensor_tensor(out=ssmT[:, j2, :], in0=yT_[:, j2, :], in1=sg[:], op=ALU.mult),
                             reads=[ky, "sg"], writes=["ssmT"])
                    stage1_mm(tg * 4, 0, cT, kc)
                    for tt in range(4):
                        t2 = tg * 4 + tt
                        if t2 == 0:
                            stage0(0)
                        if t2 + 1 < NT2:
                            stage0(t2 + 1)
                        if tt < 3:
                            stage1_mm(t2 + 1, tt + 1, cT, kc)
                        stage1(t2, tg, tt, cT, kc)
                        if t2 >= 1:
                            stage2(t2 - 1)
                stage2(NT2 - 1)
                barrier()
            print("P2a done: inst", S.n_inst, "waits", S.n_wait, "sems", S.nsem, flush=True)

            chunks = [(c0, min(c0 + 512, CAP)) for c0 in range(0, CAP, 512)]
            y_keys = []
            with ExitStack() as st6:
                sb6, ps6 = mk(st6)
                wgs = [sb6(f"wg{i}", [128, 8, 1024], BF16) for i in range(2)]
                wus = [sb6(f"wu{i}", [128, 8, 1024], BF16) for i in range(2)]
                wds = [sb6(f"wd{i}", [128, 8, 1024], BF16) for i in range(2)]
                bgall = sb6("bgall", [128, NEXP, 8], F32); buall = sb6("buall", [128, NEXP, 8], F32)
                for e in range(NEXP):
                    S.dma("sp", lambda: nc.sync.dma_start(out=bgall[:, e, :], in_=b_gate[e].rearrange("(j p) -> p j", p=128), allow_slow_non_contiguous=True),
                          writes=[("bgall", e)])
                    S.dma("sp", lambda: nc.sync.dma_start(out=buall[:, e, :], in_=b_up[e].rearrange("(j p) -> p j", p=128), allow_slow_non_contiguous=True),
                          writes=[("buall", e)])
                bdbc = [sb6(f"bdbc{i}", [128, D], F32) for i in range(2)]
                idxs = [sb6(f"idx{i}", [128, NRT], I32) for i in range(2)]
                xg = [sb6(f"xg{i}", [128, D], BF16) for i in range(2)]
                xT = sb6("xT", [128, 8, CAP], BF16)
                actT = sb6("actT", [128, 8, CAP], BF16)
                gsb = [sb6(f"gsb{i}", [128, 512], F32) for i in range(2)]
                sgs = [sb6(f"sgs{i}", [128, 512], F32) for i in range(2)]
                usb = [sb6(f"usb{i}", [128, 512], F32) for i in range(2)]
                yo = [sb6(f"yo{i}", [128, D], F32) for i in range(2)]
                ptx = ps6("ptx", [128, 8, 128], BF16)
                pgc = [ps6(f"pgc{i}", [128, 512], F32) for i in range(2)]
                puc = [ps6(f"puc{i}", [128, 512], F32) for i in range(2)]
                pdh = [ps6(f"pd{i}", [128, 512], F32) for i in range(2)]

                WSRC = {"wg": (wgs, w_gate), "wu": (wus, w_up), "wd": (wds, w_down)}

                def load_piece(e, nm, k2):
                    s_ = e % 2
                    wt, src = WSRC[nm]
                    S.dma("pool", lambda: P_.dma_start(out=wt[s_][:, 2 * k2:2 * k2 + 2, :],
                                                       in_=src[e, k2 * 256:(k2 + 1) * 256, :].rearrange("(k p) n -> p k n", p=128)),
                          writes=[(nm, s_, 2 * k2), (nm, s_, 2 * k2 + 1)], grp="w", ngrp=9)

                WPLAN = [[("wg", 0), ("wu", 0)], [("wd", 0)], [("wg", 1), ("wu", 1)], [("wd", 1)],
                         [("wg", 2), ("wu", 2)], [("wd", 2)], [("wg", 3), ("wu", 3)], [("wd", 3)]]

                def load_wk(e, fj):
                    for nm, k2 in WPLAN[fj]:
                        load_piece(e, nm, k2)

                def load_w(e):
                    for fj in range(8):
                        load_wk(e, fj)

                xT2 = [xT, sb6("xTb", [128, 8, CAP], BF16)]

                def prep_idx(e):
                    S.dma("sp", lambda: nc.sync.dma_start(out=idxs[e % 2][:], in_=tokslot_d[e * CAP:(e + 1) * CAP, :].rearrange("(r p) o -> p (r o)", p=128),
                                                          allow_slow_non_contiguous=True), writes=[("idx", e % 2)])
                    S.dma("sp", lambda: nc.sync.dma_start(out=bdbc[e % 2][:], in_=b_down[e].partition_broadcast(128)), writes=[("bdbc", e % 2)])

                gcnt = [0]

                def prep_g(e, rt):
                    xs = rt % 2
                    S.dma("pool", lambda: P_.indirect_dma_start(out=xg[xs][:, :], out_offset=None, in_=h1b_d[:, :],
                                                                in_offset=IOA(ap=idxs[e % 2][:, rt:rt + 1], axis=0)),
                          reads=[("idx", e % 2)], writes=[("xg", xs)])

                def prep_t(e, rt):
                    xs = rt % 2
                    for k in range(8):
                        S.op("pe", lambda: T_.transpose(out=ptx[:, k, :], in_=xg[xs][:, k * 128:(k + 1) * 128], identity=ident[:]),
                             reads=[("xg", xs), "ident"], writes=["ptx"])
                    S.op("act", lambda: A_.copy(out=xT2[e % 2][:, :, rt * 128:(rt + 1) * 128], in_=ptx[:]), reads=["ptx"], writes=[("xT", e % 2)])

                def prep_rt(e, rt):
                    if rt + 1 < NRT:
                        prep_g(e, rt + 1)
                    prep_t(e, rt)

                load_w(0)
                S.waitfor("sp", sc_keys)
                S.waitfor("pool", h1_keys)
                prep_idx(0)
                prep_g(0, 0)
                for rt in range(NRT):
                    prep_rt(0, rt)
                cc = 0
                for e in range(NEXP):
                    s_ = e % 2
                    xTe = xT2[s_]
                    if e + 1 < NEXP:
                        prep_idx(e + 1)
                        prep_g(e + 1, 0)
                    nxt = list(range(NRT)) if e + 1 < NEXP else []
                    for fj in range(8):
                        for (c0, c1) in chunks:
                            b_ = cc % 2; cc += 1
                            n_ = c1 - c0
                            for (W, kw, P, kp) in ((wgs[s_], ("wg", s_), pgc[b_], ("pgc", b_)), (wus[s_], ("wu", s_), puc[b_], ("puc", b_))):
                                for k in range(8):
                                    S.op("pe", lambda: T_.matmul(P[:, 0:n_], lhsT=W[:, k, fj * 128:(fj + 1) * 128], rhs=xTe[:, k, c0:c1],
                                                                 start=(k == 0), stop=(k == 7)), reads=[kw + (k,), ("xT", s_)], writes=[kp])
                            gs, sg_, us = gsb[b_], sgs[b_], usb[b_]
                            S.op("dve", lambda: V.tensor_scalar(out=gs[:, 0:n_], in0=pgc[b_][:, 0:n_], scalar1=bgall[:, e, fj:fj + 1], scalar2=7.0,
                                                                op0=ALU.add, op1=ALU.min), reads=[("pgc", b_), ("bgall", e)], writes=[("gsb", b_)])
                            S.op("act", lambda: A_.activation(out=sg_[:, 0:n_], in_=gs[:, 0:n_], func=AF.Sigmoid, scale=1.702), reads=[("gsb", b_)], writes=[("sgs", b_)])
                            S.op("dve", lambda: V.tensor_scalar(out=us[:, 0:n_], in0=puc[b_][:, 0:n_], scalar1=buall[:, e, fj:fj + 1], scalar2=7.0,
                                                                op0=ALU.add, op1=ALU.min), reads=[("puc", b_), ("buall", e)], writes=[("usb", b_)])
                            S.op("dve", lambda: V.tensor_scalar(out=us[:, 0:n_], in0=us[:, 0:n_], scalar1=-7.0, scalar2=1.0, op0=ALU.max, op1=ALU.add),
                                 reads=[("usb", b_)], writes=[("usb", b_)])
                            S.op("dve", lambda: V.tensor_tensor(out=gs[:, 0:n_], in0=gs[:, 0:n_], in1=sg_[:, 0:n_], op=ALU.mult),
                                 reads=[("gsb", b_), ("sgs", b_)], writes=[("gsb", b_)])
                            S.op("dve", lambda: V.tensor_tensor(out=actT[:, fj, c0:c1], in0=gs[:, 0:n_], in1=us[:, 0:n_], op=ALU.mult),
                                 reads=[("gsb", b_), ("usb", b_)], writes=["actT"])
                        if e + 1 < NEXP:
                            load_wk(e + 1, fj)
                        if nxt:
                            prep_rt(e + 1, nxt.pop(0))
                    while nxt:
                        prep_rt(e + 1, nxt.pop(0))
                    for rt in range(NRT):
                        ys = rt % 2
                        for half in range(2):
                            for k in range(8):
                                S.op("pe", lambda: T_.matmul(pdh[half][:, :], lhsT=actT[:, k, rt * 128:(rt + 1) * 128],
                                                             rhs=wds[s_][:, k, half * 512:(half + 1) * 512], start=(k == 0), stop=(k == 7)),
                                     reads=["actT", ("wd", s_, k)], writes=[("pd", half)])
                            S.op("dve", lambda: V.tensor_tensor(out=yo[ys][:, half * 512:(half + 1) * 512], in0=pdh[half][:, :],
                                                                in1=bdbc[s_][:, half * 512:(half + 1) * 512], op=ALU.add),
                                 reads=[("pd", half), ("bdbc", s_)], writes=[("yo", ys, half)])
                        r0 = e * CAP + rt * 128
                        S.dma("sp", lambda: nc.sync.dma_start(out=ybuf_d[r0:r0 + 128, :], in_=yo[ys][:]), reads=[("yo", ys, 0), ("yo", ys, 1)],
                              writes=[("ybuf", e, rt)])
                        y_keys.append(("ybuf", e, rt))
                barrier()
            print("P2b done: inst", S.n_inst, "waits", S.n_wait, "sems", S.nsem, flush=True)

            with ExitStack() as st7:
                sb7, ps7 = mk(st7)
                h1c = [sb7(f"h1c{i}", [128, D], F32) for i in range(2)]
                yk = [[sb7(f"yk{i}_{k}", [128, D], F32) for k in range(4)] for i in range(2)]
                accs = [sb7(f"acc{i}", [128, D], F32) for i in range(2)]
                tmpc = sb7("tmpc", [128, D], F32)
                ot = [sb7(f"ot{i}", [128, D], F32) for i in range(2)]
                S.waitfor("pool", y_keys)

                def c_fetch(t2):
                    s2 = t2 % 2
                    S.dma("sp", lambda: nc.sync.dma_start(out=h1c[s2][:], in_=h1_d[t2 * 128:(t2 + 1) * 128, :]), reads=[("h1_d", t2)], writes=[("h1c", s2)])
                    for k in range(4):
                        S.dma("pool", lambda: P_.indirect_dma_start(out=yk[s2][k][:, :], out_offset=None, in_=ybuf_d[:, :],
                                                                    in_offset=IOA(ap=slot_ga[:, t2, k:k + 1], axis=0)),
                              reads=["slot_ga"], writes=[("yk", s2, k)])

                def c_comp(t2):
                    s2 = t2 % 2
                    ac = accs[s2]; ka = ("acc", s2)
                    S.op("act", lambda: A_.activation(out=ac[:], in_=h1c[s2][:], func=AF.Copy, scale=ALPHA), reads=[("h1c", s2)], writes=[ka])
                    for k in range(4):
                        S.op("dve", lambda: V.scalar_tensor_tensor(out=ac[:], in0=yk[s2][k][:], scalar=gate_all[:, t2, k:k + 1], in1=ac[:],
                                                                   op0=ALU.mult, op1=ALU.add), reads=[("yk", s2, k), "gate_all", ka], writes=[ka])
                    layernorm(ac, ka, g2, "g2", b2, "b2", tmpc, "tmpc", ot[s2][:], ("ot", s2), lnsb2[s2])
                    S.dma("sp", lambda: nc.sync.dma_start(out=out_d[t2 * 128:(t2 + 1) * 128, :], in_=ot[s2][:]), reads=[("ot", s2)], writes=[("out", t2)])

                c_fetch(0)
                for t2 in range(NT2):
                    if t2 + 1 < NT2:
                        c_fetch(t2 + 1)
                    c_comp(t2)
                barrier()
        for e in ("pe", "act", "dve", "pool", "sp"):
            S.wait_all(e)
    return nc


def make_maps(inp, NX, full=True):
    f = lambda a: np.ascontiguousarray(np.asarray(a, dtype=np.float32))
    NG = NX // 512
    NTOK2 = NX // 4
    NG2 = NTOK2 // 512
    XCH = min(1024, 128 * NG)
    w_in = np.asarray(inp["w_in"])[0]
    maps = []
    for c in range(8):
        b, r = c // 4, c % 4
        m = {}
        m["x_b"] = f(inp["x"][b, :NX])
        m["meta"] = f(inp["meta_tokens"])
        m["ln_in_g"] = f(inp["ln_in_g"]).reshape(1, D)
        m["ln_in_b"] = f(inp["ln_in_b"]).reshape(1, D)
        m["w4"] = f(np.concatenate([w_in[:, r * 128:(r + 1) * 128], w_in[:, 512 + r * 128:512 + (r + 1) * 128],
                                    w_in[:, 1024 + r * 128:1024 + (r + 1) * 128], w_in[:, 1536 + r * 128:1536 + (r + 1) * 128]], axis=1))
        m["relb"] = f(np.asarray(inp["rel_bias"])[:, r]).reshape(1, 32)
        m["lam4"] = f(np.stack([inp["lambda_q1"][0], inp["lambda_k1"][0], inp["lambda_q2"][0], inp["lambda_k2"][0]]))
        m["subln_g"] = f(inp["subln_g"][0]).reshape(1, 128)
        gs = slice(8 * r, 8 * r + 8)
        m["a_re"] = f(inp["a_re"][0][gs]).reshape(1, 512)
        m["a_im"] = f(inp["a_im"][0][gs]).reshape(1, 512)
        m["log_step"] = f(inp["log_step"][0][gs]).reshape(1, 8)
        m["b_re"] = f(inp["b_re"][0][gs]); m["b_im"] = f(inp["b_im"][0][gs])
        m["c_re"] = f(inp["c_re"][0][gs]); m["c_im"] = f(inp["c_im"][0][gs])
        m["d_skip"] = f(inp["d_skip"][0][128 * r:128 * r + 128]).reshape(1, 128)
        if full:
            m["x2"] = f(inp["x"][b, r * NTOK2:(r + 1) * NTOK2])
            gi = np.zeros((128, 8 * NG2), np.int32)
            p = np.arange(128)
            for tg in range(NG2):
                for j in range(4):
                    for half in range(2):
                        rho = (r * NG2 + tg) * 256 + half * 128 + p
                        gi[:, (tg * 4 + j) * 2 + half] = (rho // XCH) * 4 * XCH + j * XCH + rho % XCH
            m["gidx"] = gi
            m["w_glu"] = f(inp["w_glu"][0]); m["b_glu"] = f(inp["b_glu"][0]).reshape(1, 512)
            m["w_out"] = f(inp["w_out"][0])
            m["ln1_g"] = f(inp["ln1_g"][0]).reshape(1, D); m["ln1_b"] = f(inp["ln1_b"][0]).reshape(1, D)
            m["w_router"] = f(inp["w_router"][0]); m["b_router"] = f(inp["b_router"][0]).reshape(1, NEXP)
            m["w_gate"] = f(inp["w_gate"][0]); m["b_gate"] = f(inp["b_gate"][0])
            m["w_up"] = f(inp["w_up"][0]); m["b_up"] = f(inp["b_up"][0])
            m["w_down"] = f(inp["w_down"][0]); m["b_down"] = f(inp["b_down"][0])
            m["ln2_g"] = f(inp["ln2_g"][0]).reshape(1, D); m["ln2_b"] = f(inp["ln2_b"][0]).reshape(1, D)
        maps.append(m)
    return maps


def kernel(**inputs):
    NX = int(np.asarray(inputs["x"]).shape[1])
    CAP = 768 if NX >= 16384 else 128 * max(1, int(math.ceil(1.5 * NX / 8 / 128)))
    nc = build(NX, CAP, debug=False, full=True)
    maps = make_maps(inputs, NX, full=True)
    res = run_bass_kernel_spmd(nc, maps, core_ids=list(range(8)))
    NTOK2 = NX // 4
    out = np.zeros((2, NX, D), np.float32)
    for c in range(8):
        b, r = c // 4, c % 4
        out[b, r * NTOK2:(r + 1) * NTOK2] = np.asarray(res.results[c]["out"], dtype=np.float32)
    return out
```

```python
import math
from contextlib import ExitStack

import numpy as np
import concourse.bass as bass
import concourse.mybir as mybir
from concourse.bass_utils import run_bass_kernel_spmd

F32 = mybir.dt.float32
BF16 = mybir.dt.bfloat16
I32 = mybir.dt.int32
AF = mybir.ActivationFunctionType
ALU = mybir.AluOpType
AX = mybir.AxisListType

D = 1024
NEXP = 32
LN_EPS = 1e-5
ALPHA = 2.0 ** 0.25
NEG = -30000.0
EPOCH = 30000


class Sync:
    def __init__(self, nc, stack, n_dma_sems=10):
        self.nc = nc
        self.stack = stack
        self.eng = {"pe": nc.tensor, "act": nc.scalar, "dve": nc.vector,
                    "pool": nc.gpsimd, "sp": nc.sync}
        self.sems = {}
        self.nsem = 0
        self.cur = {e: [self._new_sem(), 0] for e in self.eng}
        self.seen = {e: {} for e in self.eng}
        self.lastw = {}
        self.readers = {}
        self.dma_pool = {}
        self.dma_rr = {}
        for q in ("sp", "pool", "act"):
            self.dma_pool[q] = [[self._new_sem(), 0] for _ in range(n_dma_sems)]
            self.dma_rr[q] = 0
        self.n_inst = 0
        self.n_wait = 0

    def _new_sem(self):
        sid = self.nsem
        self.nsem += 1
        self.sems[sid] = self.stack.enter_context(self.nc.semaphore(f"s{sid}"))
        return sid

    def _wait(self, e, ticket):
        sid, val, owner = ticket
        if owner == e and e == "pe":
            return
        if self.seen[e].get(sid, 0) >= val:
            return
        self.eng[e].wait_ge(self.sems[sid], val)
        self.seen[e][sid] = val
        self.n_wait += 1

    def _deps(self, e, reads, writes):
        for b in list(reads) + list(writes):
            t = self.lastw.get(b)
            if t is not None:
                self._wait(e, t)
        for b in writes:
            for t in self.readers.get(b, ()):
                self._wait(e, t)

    def _commit(self, ticket, reads, writes):
        for b in writes:
            self.lastw[b] = ticket
            self.readers[b] = []
        for b in reads:
            lst = self.readers.setdefault(b, [])
            lst.append(ticket)
            if len(lst) > 24:
                best = {}
                for t in lst:
                    if t[0] not in best or best[t[0]][1] < t[1]:
                        best[t[0]] = t
                self.readers[b] = list(best.values())

    def op(self, e, fn, reads=(), writes=()):
        self._deps(e, reads, writes)
        c = self.cur[e]
        if c[1] >= EPOCH:
            c[0] = self._new_sem()
            c[1] = 0
        ins = fn()
        c[1] += 1
        ins.then_inc(self.sems[c[0]], 1)
        ticket = (c[0], c[1], e)
        self._commit(ticket, reads, writes)
        self.n_inst += 1
        return ticket

    def dma(self, q, fn, reads=(), writes=(), grp=None, ngrp=6):
        self._deps(q, reads, writes)
        if grp is None:
            pk = q
        else:
            pk = (q, grp)
            if pk not in self.dma_pool:
                self.dma_pool[pk] = [[self._new_sem(), 0] for _ in range(ngrp)]
                self.dma_rr[pk] = 0
        pool = self.dma_pool[pk]
        i = self.dma_rr[pk]
        self.dma_rr[pk] = (i + 1) % len(pool)
        slot = pool[i]
        if slot[1] > 0:
            self._wait(q, (slot[0], slot[1], "dma"))
        if slot[1] >= EPOCH:
            slot[0] = self._new_sem()
            slot[1] = 0
        ins = fn()
        slot[1] += 16
        ins.then_inc(self.sems[slot[0]], 16)
        ticket = (slot[0], slot[1], "dma")
        self._commit(ticket, reads, writes)
        self.n_inst += 1
        return ticket

    def waitfor(self, e, keys):
        for b in keys:
            t = self.lastw.get(b)
            if t is not None:
                self._wait(e, t)

    def wait_all(self, e):
        for b, t in list(self.lastw.items()):
            self._wait(e, t)
        for q, pool in self.dma_pool.items():
            for slot in pool:
                if slot[1] > 0:
                    self._wait(e, (slot[0], slot[1], "dma"))


def _bucket_thresholds():
    n = np.arange(1, 400, dtype=np.int32)
    n_f = np.maximum(n, 1).astype(np.float32)
    large = 8 + (np.log(n_f / np.float32(8)) / np.float32(math.log(128 / 8)) * np.float32(8)).astype(np.int32)
    large = np.minimum(large, 15)
    bucket = np.where(n < 8, n, large)
    thr = {}
    for j in range(9, 16):
        thr[j] = int(n[np.argmax(bucket >= j)])
    return thr


def build(NX, CAP, debug=False, full=True, stop_after=None):
    nc = bass.Bass("TRN2", target_bir_lowering=False)
    NT = NX // 128 + 1
    NCOL = NT * 128
    NG = NX // 512
    NTOK2 = NX // 4
    NT2 = NTOK2 // 128
    NG2 = NTOK2 // 512
    NRT = CAP // 128
    NSLOT = NEXP * CAP

    def din(name, shape, dt=F32):
        return nc.dram_tensor(name, list(shape), dt, kind="ExternalInput").ap()

    x_b = din("x_b", [NX, D])
    meta = din("meta", [16, D])
    ln_in_g = din("ln_in_g", [1, D]); ln_in_b = din("ln_in_b", [1, D])
    w4 = din("w4", [D, 512])
    relb = din("relb", [1, 32])
    lam4 = din("lam4", [4, 64])
    subln_g = din("subln_g", [1, 128])
    a_re = din("a_re", [1, 512]); a_im = din("a_im", [1, 512]); log_step = din("log_step", [1, 8])
    b_re = din("b_re", [8, 64, 16]); b_im = din("b_im", [8, 64, 16])
    c_re = din("c_re", [8, 16, 64]); c_im = din("c_im", [8, 16, 64])
    d_skip = din("d_skip", [1, 128])
    if full:
        x2 = din("x2", [NTOK2, D])
        gidx = din("gidx", [128, 8 * NG2], I32)
        w_glu = din("w_glu", [512, 512]); b_glu = din("b_glu", [1, 512])
        w_out = din("w_out", [D, D])
        ln1_g = din("ln1_g", [1, D]); ln1_b = din("ln1_b", [1, D])
        w_router = din("w_router", [D, NEXP]); b_router = din("b_router", [1, NEXP])
        w_gate = din("w_gate", [NEXP, D, D]); b_gate = din("b_gate", [NEXP, D])
        w_up = din("w_up", [NEXP, D, D]); b_up = din("b_up", [NEXP, D])
        w_down = din("w_down", [NEXP, D, D]); b_down = din("b_down", [NEXP, D])
        ln2_g = din("ln2_g", [1, D]); ln2_b = din("ln2_b", [1, D])
        out_d = nc.dram_tensor("out", [NTOK2, D], F32, kind="ExternalOutput").ap()

    slab = nc.dram_tensor("slab", [256 * NG, 512], BF16).ap()
    gath = nc.dram_tensor("gath", [4 * 256 * NG, 512], BF16).ap()
    slab3 = slab.rearrange("(g f) c -> f g c", f=256)
    vaug_d = nc.dram_tensor("vaug_d", [128, NT, 129], BF16).ap()
    h1_d = nc.dram_tensor("h1_d", [NTOK2, D], F32).ap()
    h1b_d = nc.dram_tensor("h1b_d", [NTOK2, D], BF16).ap()
    tokslot_d = nc.dram_tensor("tokslot_d", [NSLOT + 128, 1], I32).ap()
    ybuf_d = nc.dram_tensor("ybuf_d", [NSLOT, D], F32).ap()
    dbg = {}
    if debug:
        dbg["qT"] = nc.dram_tensor("dbg_qT", [128, NCOL], BF16, kind="ExternalOutput").ap()
        dbg["kT"] = nc.dram_tensor("dbg_kT", [128, NCOL], BF16, kind="ExternalOutput").ap()
        dbg["uT"] = nc.dram_tensor("dbg_uT", [128, NCOL], BF16, kind="ExternalOutput").ap()
        dbg["v"] = nc.dram_tensor("dbg_v", [128, NT, 129], BF16, kind="ExternalOutput").ap()
        dbg["slab"] = nc.dram_tensor("dbg_slab", [256 * NG, 512], BF16, kind="ExternalOutput").ap()
        dbg["Wb"] = nc.dram_tensor("dbg_Wb", [128, 1024], F32, kind="ExternalOutput").ap()
        dbg["ident"] = nc.dram_tensor("dbg_ident", [128, 128], BF16, kind="ExternalOutput").ap()
        dbg["ob"] = nc.dram_tensor("dbg_ob", [128, 4, 128], BF16, kind="ExternalOutput").ap()
        dbg["g08"] = nc.dram_tensor("dbg_g08", [128, 128], F32, kind="ExternalOutput").ap()
        dbg["po"] = nc.dram_tensor("dbg_po", [128, 3, 512], F32, kind="ExternalOutput").ap()
        dbg["rr"] = nc.dram_tensor("dbg_rr", [128, 8], F32, kind="ExternalOutput").ap()
        dbg["pT"] = nc.dram_tensor("dbg_pT", [128, 2, 512], BF16, kind="ExternalOutput").ap()
        if full:
            dbg["h1"] = nc.dram_tensor("dbg_h1", [NTOK2, D], F32, kind="ExternalOutput").ap()
            dbg["lg"] = nc.dram_tensor("dbg_lg", [NTOK2, 32], F32, kind="ExternalOutput").ap()

    thr = _bucket_thresholds()

    with ExitStack() as st0:
        S = Sync(nc, st0)
        V, A_, P_, T_ = nc.vector, nc.scalar, nc.gpsimd, nc.tensor

        def mk(stack):
            def sb(name, shape, dt=F32):
                return stack.enter_context(nc.sbuf_tensor(name, list(shape), dt))

            def ps(name, shape, dt=F32):
                return stack.enter_context(nc.psum_tensor(name, list(shape), dt))
            return sb, ps

        sb0, ps0 = mk(st0)

        ident = sb0("ident", [128, 128], BF16)
        identf = sb0("identf", [128, 128], F32)
        tri = sb0("tri", [128, 128], BF16)
        stri = sb0("stri", [128, 128], BF16)
        ones = sb0("ones", [128, 128], BF16)
        iop = sb0("iop", [128, 1], F32)
        iop_i = sb0("iop_i", [128, 1], I32)
        S.op("pool", lambda: P_.memset(identf[:], 0.0), writes=["identf"])
        S.op("pool", lambda: P_.affine_select(out=identf[:], in_=identf[:], pattern=[[-1, 128]],
                                              compare_op=ALU.not_equal, fill=1.0, base=0, channel_multiplier=1),
             reads=["identf"], writes=["identf"])
        S.op("dve", lambda: V.tensor_copy(out=ident[:], in_=identf[:]), reads=["identf"], writes=["ident"])
        S.op("pool", lambda: P_.memset(ones[:], 1.0), writes=["ones"])
        S.op("pool", lambda: P_.affine_select(out=tri[:], in_=ones[:], pattern=[[1, 128]],
                                              compare_op=ALU.is_ge, fill=0.0, base=0, channel_multiplier=-1),
             reads=["ones"], writes=["tri"])
        S.op("pool", lambda: P_.affine_select(out=stri[:], in_=ones[:], pattern=[[1, 128]],
                                              compare_op=ALU.is_gt, fill=0.0, base=0, channel_multiplier=-1),
             reads=["ones"], writes=["stri"])
        S.op("pool", lambda: P_.iota(iop_i[:], pattern=[[0, 1]], base=0, channel_multiplier=1), writes=["iop_i"])
        S.op("dve", lambda: V.tensor_copy(out=iop[:], in_=iop_i[:]), reads=["iop_i"], writes=["iop"])

        def bc_load(name, src, n, stack_sb, q="sp"):
            t = stack_sb(name, [128, n], F32)
            S.dma(q, lambda: nc.sync.dma_start(out=t[:], in_=src.partition_broadcast(128)), writes=[name])
            return t

        lng = bc_load("lng", ln_in_g[0], D, sb0)
        lnb = bc_load("lnb", ln_in_b[0], D, sb0)

        cnt = {"ln": 0}

        def layernorm(xt, kx, g_bc, kg, b_bc, kb, tmp, kt, out_ap, kout, sbs):
            i = cnt["ln"]; cnt["ln"] += 1
            st6, mv, rstd, nmr = sbs
            ks = ("lnstat", id(st6))
            S.op("dve", lambda: V.bn_stats(out=st6[:, 0:6], in_=xt[:, 0:512]), reads=[kx], writes=[ks])
            S.op("dve", lambda: V.bn_stats(out=st6[:, 6:12], in_=xt[:, 512:1024]), reads=[kx], writes=[ks])
            S.op("dve", lambda: V.bn_aggr(out=mv[:, 0:2], in_=st6[:, 0:12]), reads=[ks], writes=[ks])
            S.op("dve", lambda: V.tensor_scalar(out=rstd[:], in0=mv[:, 1:2], scalar1=LN_EPS, scalar2=None, op0=ALU.add), reads=[ks], writes=[ks])
            S.op("dve", lambda: V.tensor_scalar(out=nmr[:], in0=mv[:, 0:1], scalar1=-1.0, scalar2=None, op0=ALU.mult), reads=[ks], writes=[ks])
            S.op("act", lambda: A_.activation(out=rstd[:], in_=rstd[:], func=AF.Ln), reads=[ks], writes=[ks])
            S.op("act", lambda: A_.activation(out=rstd[:], in_=rstd[:], func=AF.Exp, scale=-0.5), reads=[ks], writes=[ks])
            S.op("act", lambda: A_.activation(out=nmr[:], in_=nmr[:], func=AF.Copy, scale=rstd[:, 0:1]), reads=[ks], writes=[ks])
            S.op("act", lambda: A_.activation(out=tmp[:], in_=xt[:], func=AF.Identity, bias=nmr[:, 0:1], scale=rstd[:, 0:1]),
                 reads=[kx, ks], writes=[kt])
            S.op("dve", lambda: V.tensor_tensor(out=tmp[:], in0=tmp[:], in1=g_bc[:], op=ALU.mult), reads=[kt, kg], writes=[kt])
            S.op("dve", lambda: V.tensor_tensor(out=out_ap, in0=tmp[:], in1=b_bc[:], op=ALU.add), reads=[kt, kb], writes=[kout])

        st_att = ExitStack()
        sbA, psA = mk(st_att)
        qT = sbA("qT", [128, NCOL], BF16)
        kT = sbA("kT", [128, NCOL], BF16)
        st_u = ExitStack()
        sbU, _ = mk(st_u)
        uT = sbU("uT", [128, NCOL], BF16)

        st3 = ExitStack()
        sb, ps = mk(st3)
        PI = math.pi
        Tnr = sb("Tnr", [128, 512], F32); Tni = sb("Tni", [128, 512], F32)
        Ppr = sb("Ppr", [128, 4, 128], F32); Ppi = sb("Ppi", [128, 4, 128], F32)
        Bblk = sb("Bblk", [128, 1024], BF16)
        Cmat = sb("Cmat", [128, 8, 128], BF16)
        dsk = sb("dsk", [128, 1], F32)
        S.dma("sp", lambda: nc.sync.dma_start(out=dsk[:], in_=d_skip.rearrange("o (p q) -> (o p) q", q=1)), writes=["dsk"])
        negpi = sb("negpi", [128, 1], F32)
        S.op("dve", lambda: V.memset(negpi[:], -PI), writes=["negpi"])
        with ExitStack() as stt:
            sbt, _ = mk(stt)
            are = bc_load("are", a_re[0], 512, sbt)
            aim = bc_load("aim", a_im[0], 512, sbt)
            ls8 = bc_load("ls8", log_step[0], 8, sbt)
            S.op("act", lambda: A_.activation(out=ls8[:], in_=ls8[:], func=AF.Exp), reads=["ls8"], writes=["ls8"])
            S.op("dve", lambda: V.tensor_scalar(out=are[:], in0=are[:], scalar1=-1e-4, scalar2=None, op0=ALU.min), reads=["are"], writes=["are"])
            sa = sbt("sa", [128, 512], F32); sp_ = sbt("sp_", [128, 512], F32)
            st8 = ls8[:, 0:8].unsqueeze(2).to_broadcast([128, 8, 64])
            S.op("dve", lambda: V.tensor_tensor(out=sa[:].rearrange("p (g n) -> p g n", n=64), in0=are[:].rearrange("p (g n) -> p g n", n=64),
                                                in1=st8, op=ALU.mult), reads=["are", "ls8"], writes=["sa"])
            S.op("dve", lambda: V.tensor_tensor(out=sp_[:].rearrange("p (g n) -> p g n", n=64), in0=aim[:].rearrange("p (g n) -> p g n", n=64),
                                                in1=st8, op=ALU.mult), reads=["aim", "ls8"], writes=["sp_"])
            t_a = sbt("t_a", [128, 512], F32); t_b = sbt("t_b", [128, 512], F32); t_c = sbt("t_c", [128, 512], F32)
            t_d = sbt("t_d", [128, 512], F32); t_e = sbt("t_e", [128, 512], F32)
            sp1 = sbt("sp1", [128, 1], F32); nsp1 = sbt("nsp1", [128, 1], F32)
            S.op("dve", lambda: V.tensor_scalar(out=sp1[:], in0=iop[:], scalar1=1.0, scalar2=None, op0=ALU.add), reads=["iop"], writes=["sp1"])
            S.op("dve", lambda: V.tensor_scalar(out=nsp1[:], in0=sp1[:], scalar1=-1.0, scalar2=None, op0=ALU.mult), reads=["sp1"], writes=["nsp1"])

            rki = sbt("rki", [128, 512], I32)
            rkf = sbt("rkf", [128, 512], F32)
            C1 = 6.28125
            C2 = 2 * PI - C1

            def reduce_sin(ph, kph, shift, out_ap, kout, scratch, ksc):
                n = ph.shape[-1]
                S.op("dve", lambda: V.tensor_scalar(out=scratch, in0=ph, scalar1=shift, scalar2=1.0 / (2 * PI), op0=ALU.add, op1=ALU.mult),
                     reads=[kph], writes=[ksc])
                S.op("dve", lambda: V.tensor_copy(out=rki[:, 0:n], in_=scratch), reads=[ksc], writes=["rki"])
                S.op("dve", lambda: V.tensor_copy(out=rkf[:, 0:n], in_=rki[:, 0:n]), reads=["rki"], writes=["rkf"])
                S.op("dve", lambda: V.tensor_scalar(out=scratch, in0=ph, scalar1=shift, scalar2=None, op0=ALU.add), reads=[kph], writes=[ksc])
                S.op("dve", lambda: V.scalar_tensor_tensor(out=scratch, in0=rkf[:, 0:n], scalar=-C1, in1=scratch, op0=ALU.mult, op1=ALU.add),
                     reads=["rkf", ksc], writes=[ksc])
                S.op("dve", lambda: V.scalar_tensor_tensor(out=scratch, in0=rkf[:, 0:n], scalar=-C2, in1=scratch, op0=ALU.mult, op1=ALU.add),
                     reads=["rkf", ksc], writes=[ksc])
                S.op("dve", lambda: V.tensor_scalar(out=rkf[:, 0:n], in0=scratch, scalar1=PI, scalar2=-2 * PI, op0=ALU.is_gt, op1=ALU.mult),
                     reads=[ksc], writes=["rkf"])
                S.op("dve", lambda: V.tensor_tensor(out=scratch, in0=scratch, in1=rkf[:, 0:n], op=ALU.add), reads=["rkf", ksc], writes=[ksc])
                S.op("dve", lambda: V.tensor_scalar(out=rkf[:, 0:n], in0=scratch, scalar1=-PI, scalar2=2 * PI, op0=ALU.is_lt, op1=ALU.mult),
                     reads=[ksc], writes=["rkf"])
                S.op("dve", lambda: V.tensor_tensor(out=scratch, in0=scratch, in1=rkf[:, 0:n], op=ALU.add), reads=["rkf", ksc], writes=[ksc])
                S.op("act", lambda: A_.activation(out=out_ap, in_=scratch, func=AF.Sin), reads=[ksc], writes=[kout])

            def sincos(ph, kph, out_s, ks, out_c, kc, scratch, ksc):
                reduce_sin(ph, kph, 0.0, out_s, ks, scratch, ksc)
                reduce_sin(ph, kph, 0.5 * PI, out_c, kc, scratch, ksc)

            S.op("dve", lambda: V.tensor_scalar(out=t_a[:], in0=sa[:], scalar1=nsp1[:, 0:1], scalar2=None, op0=ALU.mult), reads=["sa", "nsp1"], writes=["t_a"])
            S.op("act", lambda: A_.activation(out=t_a[:], in_=t_a[:], func=AF.Exp), reads=["t_a"], writes=["t_a"])
            S.op("dve", lambda: V.tensor_scalar(out=t_b[:], in0=sp_[:], scalar1=sp1[:, 0:1], scalar2=None, op0=ALU.mult), reads=["sp_", "sp1"], writes=["t_b"])
            sincos(t_b[:], "t_b", t_c[:], "t_c", t_d[:], "t_d", t_e[:], "t_e")
            S.op("dve", lambda: V.tensor_tensor(out=Tnr[:], in0=t_a[:], in1=t_d[:], op=ALU.mult), reads=["t_a", "t_d"], writes=["Tnr"])
            S.op("dve", lambda: V.scalar_tensor_tensor(out=Tni[:], in0=t_a[:], scalar=-1.0, in1=t_c[:], op0=ALU.mult, op1=ALU.mult),
                 reads=["t_a", "t_c"], writes=["Tni"])
            S.op("act", lambda: A_.activation(out=t_a[:], in_=sa[:], func=AF.Exp), reads=["sa", "Tnr", "Tni"], writes=["t_a"])
            sincos(sp_[:], "sp_", t_c[:], "t_c", t_d[:], "t_d", t_e[:], "t_e")
            S.op("dve", lambda: V.tensor_tensor(out=t_d[:], in0=t_a[:], in1=t_d[:], op=ALU.mult), reads=["t_a", "t_d"], writes=["t_d"])
            S.op("dve", lambda: V.tensor_scalar(out=t_d[:], in0=t_d[:], scalar1=-1.0, scalar2=None, op0=ALU.add), reads=["t_d"], writes=["t_d"])
            S.op("dve", lambda: V.tensor_tensor(out=t_c[:], in0=t_a[:], in1=t_c[:], op=ALU.mult), reads=["t_a", "t_c"], writes=["t_c"])
            S.op("dve", lambda: V.tensor_tensor(out=t_a[:], in0=are[:], in1=are[:], op=ALU.mult), reads=["are", "t_c", "t_d"], writes=["t_a"])
            S.op("dve", lambda: V.tensor_tensor(out=t_b[:], in0=aim[:], in1=aim[:], op=ALU.mult), reads=["aim"], writes=["t_b"])
            S.op("dve", lambda: V.tensor_tensor(out=t_a[:], in0=t_a[:], in1=t_b[:], op=ALU.add), reads=["t_a", "t_b"], writes=["t_a"])
            S.op("dve", lambda: V.reciprocal(out=t_a[:], in_=t_a[:]), reads=["t_a"], writes=["t_a"])
            S.op("dve", lambda: V.tensor_tensor(out=t_b[:], in0=t_d[:], in1=are[:], op=ALU.mult), reads=["t_d", "are"], writes=["t_b"])
            S.op("dve", lambda: V.tensor_tensor(out=t_e[:], in0=t_c[:], in1=aim[:], op=ALU.mult), reads=["t_c", "aim"], writes=["t_e"])
            S.op("dve", lambda: V.tensor_tensor(out=t_b[:], in0=t_b[:], in1=t_e[:], op=ALU.add), reads=["t_b", "t_e"], writes=["t_b"])
            S.op("dve", lambda: V.tensor_tensor(out=t_b[:], in0=t_b[:], in1=t_a[:], op=ALU.mult), reads=["t_b", "t_a"], writes=["t_b"])
            S.op("dve", lambda: V.tensor_tensor(out=t_e[:], in0=t_c[:], in1=are[:], op=ALU.mult), reads=["t_c", "are", "t_b"], writes=["t_e"])
            S.op("dve", lambda: V.tensor_tensor(out=t_c[:], in0=t_d[:], in1=aim[:], op=ALU.mult), reads=["t_d", "aim", "t_e"], writes=["t_c"])
            S.op("dve", lambda: V.tensor_tensor(out=t_e[:], in0=t_e[:], in1=t_c[:], op=ALU.subtract), reads=["t_e", "t_c"], writes=["t_e"])
            S.op("dve", lambda: V.tensor_tensor(out=t_e[:], in0=t_e[:], in1=t_a[:], op=ALU.mult), reads=["t_e", "t_a"], writes=["t_e"])
            Brr = sbt("Brr", [128, 512], F32); Bri = sbt("Bri", [128, 512], F32)
            S.op("pool", lambda: P_.memset(Brr[:], 0.0), writes=["Brr"])
            S.op("pool", lambda: P_.memset(Bri[:], 0.0), writes=["Bri"])
            for gi in range(8):
                S.dma("sp", lambda: nc.sync.dma_start(out=Brr[16 * gi:16 * gi + 16, 64 * gi:64 * gi + 64], in_=b_re[gi].rearrange("n c -> c n"),
                                                      allow_slow_non_contiguous=True), reads=["Brr"], writes=[("Brr", gi)])
                S.dma("sp", lambda: nc.sync.dma_start(out=Bri[16 * gi:16 * gi + 16, 64 * gi:64 * gi + 64], in_=b_im[gi].rearrange("n c -> c n"),
                                                      allow_slow_non_contiguous=True), reads=["Bri"], writes=[("Bri", gi)])
            bk = [("Brr", gi) for gi in range(8)] + [("Bri", gi) for gi in range(8)]
            S.op("dve", lambda: V.tensor_tensor(out=t_a[:], in0=t_b[:], in1=Brr[:], op=ALU.mult), reads=["t_b", "t_a"] + bk, writes=["t_a"])
            S.op("dve", lambda: V.tensor_tensor(out=t_c[:], in0=t_e[:], in1=Bri[:], op=ALU.mult), reads=["t_e", "t_c"] + bk, writes=["t_c"])
            S.op("dve", lambda: V.tensor_tensor(out=Bblk[:, 0:512], in0=t_a[:], in1=t_c[:], op=ALU.subtract), reads=["t_a", "t_c"], writes=["Bblk"])
            S.op("dve", lambda: V.tensor_tensor(out=t_a[:], in0=t_b[:], in1=Bri[:], op=ALU.mult), reads=["t_b", "t_a", "Bblk"] + bk, writes=["t_a"])
            S.op("dve", lambda: V.tensor_tensor(out=t_c[:], in0=t_e[:], in1=Brr[:], op=ALU.mult), reads=["t_e", "t_c", "Bblk"] + bk, writes=["t_c"])
            S.op("dve", lambda: V.tensor_tensor(out=Bblk[:, 512:1024], in0=t_a[:], in1=t_c[:], op=ALU.add), reads=["t_a", "t_c"], writes=["Bblk"])
            Cst = sbt("Cst", [128, 8, 128], F32)
            S.op("pool", lambda: P_.memset(Cst[:], 0.0), writes=["Cst"])
            ck = []
            for gi in range(8):
                k, gl = gi // 2, gi % 2
                S.dma("sp", lambda: nc.sync.dma_start(out=Cst[64 * gl:64 * gl + 64, k, 16 * gi:16 * gi + 16], in_=c_re[gi].rearrange("c n -> n c"),
                                                      allow_slow_non_contiguous=True), reads=["Cst"], writes=[("Cst", gi, 0)])
                S.dma("sp", lambda: nc.sync.dma_start(out=Cst[64 * gl:64 * gl + 64, 4 + k, 16 * gi:16 * gi + 16], in_=c_im[gi].rearrange("c n -> n c"),
                                                      allow_slow_non_contiguous=True), reads=["Cst"], writes=[("Cst", gi, 1)])
                ck += [("Cst", gi, 0), ("Cst", gi, 1)]
            S.op("dve", lambda: V.tensor_copy(out=Cmat[:, 0:4, :], in_=Cst[:, 0:4, :]), reads=ck, writes=["Cmat"])
            S.op("dve", lambda: V.tensor_scalar(out=Cmat[:, 4:8, :], in0=Cst[:, 4:8, :], scalar1=-1.0, scalar2=None, op0=ALU.mult), reads=ck, writes=["Cmat"])
            arc = sbt("arc", [128, 4], F32); aic = sbt("aic", [128, 4], F32); stc = sbt("stc", [128, 4], F32)
            S.dma("sp", lambda: nc.sync.dma_start(out=arc[:], in_=a_re.rearrange("o (k p) -> (o p) k", p=128), allow_slow_non_contiguous=True), writes=["arc"])
            S.dma("sp", lambda: nc.sync.dma_start(out=aic[:], in_=a_im.rearrange("o (k p) -> (o p) k", p=128), allow_slow_non_contiguous=True), writes=["aic"])
            for gi in range(8):
                k, gl = gi // 2, gi % 2
                S.dma("sp", lambda: nc.sync.dma_start(out=stc[64 * gl:64 * gl + 64, k:k + 1], in_=log_step[0, gi:gi + 1].partition_broadcast(64)),
                      writes=[("stc", gi)])
            S.op("act", lambda: A_.activation(out=stc[:], in_=stc[:], func=AF.Exp), reads=[("stc", gi) for gi in range(8)], writes=["stc"])
            S.op("dve", lambda: V.tensor_scalar(out=arc[:], in0=arc[:], scalar1=-1e-4, scalar2=None, op0=ALU.min), reads=["arc"], writes=["arc"])
            S.op("dve", lambda: V.tensor_tensor(out=arc[:], in0=arc[:], in1=stc[:], op=ALU.mult), reads=["arc", "stc"], writes=["arc"])
            S.op("dve", lambda: V.tensor_tensor(out=aic[:], in0=aic[:], in1=stc[:], op=ALU.mult), reads=["aic", "stc"], writes=["aic"])
            tp1i = sbt("tp1i", [128, 128], I32); tp1 = sbt("tp1", [128, 128], F32)
            S.op("pool", lambda: P_.iota(tp1i[:], pattern=[[1, 128]], base=1, channel_multiplier=0), writes=["tp1i"])
            S.op("dve", lambda: V.tensor_copy(out=tp1[:], in_=tp1i[:]), reads=["tp1i"], writes=["tp1"])
            for k in range(4):
                S.op("dve", lambda: V.tensor_scalar(out=t_a[:, 0:128], in0=tp1[:], scalar1=arc[:, k:k + 1], scalar2=None, op0=ALU.mult),
                     reads=["tp1", "arc", "Bblk"], writes=["t_a"])
                S.op("act", lambda: A_.activation(out=t_a[:, 0:128], in_=t_a[:, 0:128], func=AF.Exp), reads=["t_a"], writes=["t_a"])
                S.op("dve", lambda: V.tensor_scalar(out=t_b[:, 0:128], in0=tp1[:], scalar1=aic[:, k:k + 1], scalar2=None, op0=ALU.mult),
                     reads=["tp1", "aic", "Bblk"], writes=["t_b"])
                sincos(t_b[:, 0:128], "t_b", t_c[:, 0:128], "t_c", t_d[:, 0:128], "t_d", t_e[:, 0:128], "t_e")
                S.op("dve", lambda: V.tensor_tensor(out=Ppr[:, k, :], in0=t_a[:, 0:128], in1=t_d[:, 0:128], op=ALU.mult), reads=["t_a", "t_d"], writes=["Ppr"])
                S.op("dve", lambda: V.tensor_tensor(out=Ppi[:, k, :], in0=t_a[:, 0:128], in1=t_c[:, 0:128], op=ALU.mult), reads=["t_a", "t_c"], writes=["Ppi"])
            for e in ("pe", "act", "dve", "pool", "sp"):
                S.wait_all(e)

        pbu = ps("pbu", [128, 2, 512], F32)
        pz = [ps("pz0", [128, 8, 128], F32)] * 2
        py = ps("py", [128, 512], F32)
        wq = [[sb(f"w{k}_{i}", [128, 512], F32) for k in range(4)] for i in range(2)]
        Wbs = [sb(f"Wbs{i}", [128, 1024], BF16) for i in range(2)]
        zp = sb("zp", [128, 8, 128], F32)
        xq = [[sb(f"x{k}_0", [128, 4, 128], F32) for k in range(4)]] * 2
        XTs = [sb(f"XT{i}", [128, 8, 128], BF16) for i in range(2)]
        car = [sb(f"car{i}", [128, 8], F32) for i in range(2)]
        yf = sb("yf", [128, 512], F32); y2 = sb("y2", [128, 512], F32); ysg = sb("ysg", [128, 512], F32)
        ybs = [sb(f"yb{i}", [128, 512], BF16) for i in range(2)]
        S.op("dve", lambda: V.memset(car[0][:], 0.0), writes=[("car", 0)])

        def bu(ct):
            for half in range(2):
                S.op("pe", lambda: T_.matmul(pbu[:, half, :], lhsT=uT[:, ct * 128:(ct + 1) * 128], rhs=Bblk[:, half * 512:(half + 1) * 512],
                                             start=True, stop=True), reads=[("uT", (ct + 3) // 4), "Bblk"], writes=["pbu"])

        def wmod(ct):
            b2 = ct % 2
            w1, w2, w3, w4_ = wq[b2]
            kw = ("wq", b2)
            S.op("dve", lambda: V.tensor_tensor(out=w1[:], in0=pbu[:, 0, :], in1=Tnr[:], op=ALU.mult), reads=["pbu", "Tnr"], writes=[kw])
            S.op("dve", lambda: V.tensor_tensor(out=w2[:], in0=pbu[:, 1, :], in1=Tni[:], op=ALU.mult), reads=["pbu", "Tni"], writes=[kw])
            S.op("dve", lambda: V.tensor_tensor(out=w3[:], in0=pbu[:, 1, :], in1=Tnr[:], op=ALU.mult), reads=["pbu", "Tnr"], writes=[kw])
            S.op("dve", lambda: V.tensor_tensor(out=w4_[:], in0=pbu[:, 0, :], in1=Tni[:], op=ALU.mult), reads=["pbu", "Tni"], writes=[kw])
            S.op("pool", lambda: P_.tensor_tensor(out=Wbs[b2][:, 0:512], in0=w1[:], in1=w2[:], op=ALU.subtract), reads=[kw], writes=[("Wbs", b2)])
            S.op("pool", lambda: P_.tensor_tensor(out=Wbs[b2][:, 512:1024], in0=w3[:], in1=w4_[:], op=ALU.add), reads=[kw], writes=[("Wbs", b2)])

        def zmm(ct):
            b2 = ct % 2
            for k in range(8):
                S.op("pe", lambda: T_.matmul(pz[b2][:, k, :], lhsT=Wbs[b2][:, k * 128:(k + 1) * 128], rhs=tri[:], start=True, stop=True),
                     reads=[("Wbs", b2), "tri"], writes=[("pz", 0)])

        def xmod(ct):
            b2 = ct % 2
            xa, xb_, xc, xd = xq[b2]
            kx = ("xq", 0)
            cin, cout = car[b2], car[1 - b2]
            S.op("dve", lambda: V.tensor_tensor(out=zp[:], in0=pz[b2][:], in1=cin[:, 0:8].unsqueeze(2).to_broadcast([128, 8, 128]), op=ALU.add),
                 reads=[("pz", 0), ("car", b2)], writes=["zp"])
            S.op("dve", lambda: V.tensor_tensor(out=xa[:], in0=zp[:, 0:4, :], in1=Ppr[:], op=ALU.mult), reads=["zp", "Ppr"], writes=[kx])
            S.op("dve", lambda: V.tensor_tensor(out=xb_[:], in0=zp[:, 4:8, :], in1=Ppi[:], op=ALU.mult), reads=["zp", "Ppi"], writes=[kx])
            S.op("dve", lambda: V.tensor_tensor(out=xc[:], in0=zp[:, 0:4, :], in1=Ppi[:], op=ALU.mult), reads=["zp", "Ppi"], writes=[kx])
            S.op("dve", lambda: V.tensor_tensor(out=xd[:], in0=zp[:, 4:8, :], in1=Ppr[:], op=ALU.mult), reads=["zp", "Ppr"], writes=[kx])
            S.op("dve", lambda: V.tensor_tensor(out=cout[:, 0:4], in0=xa[:, :, 127], in1=xb_[:, :, 127], op=ALU.subtract),
                 reads=[kx], writes=[("car", 1 - b2)])
            S.op("dve", lambda: V.tensor_tensor(out=cout[:, 4:8], in0=xc[:, :, 127], in1=xd[:, :, 127], op=ALU.add),
                 reads=[kx], writes=[("car", 1 - b2)])
            if ct == 0:
                return
            S.op("pool", lambda: P_.tensor_tensor(out=XTs[b2][:, 0:4, :], in0=xa[:], in1=xb_[:], op=ALU.subtract), reads=[kx], writes=[("XT", b2)])
            S.op("pool", lambda: P_.tensor_tensor(out=XTs[b2][:, 4:8, :], in0=xc[:], in1=xd[:], op=ALU.add), reads=[kx], writes=[("XT", b2)])

        def ymm(ct):
            if ct == 0:
                return
            b2 = ct % 2
            ci = (ct - 1) % 4
            for k in range(8):
                S.op("pe", lambda: T_.matmul(py[:, ci * 128:(ci + 1) * 128], lhsT=Cmat[:, k, :], rhs=XTs[b2][:, k, :], start=(k == 0), stop=(k == 7)),
                     reads=["Cmat", ("XT", b2)], writes=["py"])
            if ci == 3:
                gq = (ct - 1) // 4
                c0 = 128 + 512 * gq
                yb = ybs[gq % 2]; kyb = ("yb", gq % 2)
                S.op("dve", lambda: V.scalar_tensor_tensor(out=yf[:], in0=uT[:, c0:c0 + 512], scalar=dsk[:, 0:1], in1=py[:], op0=ALU.mult, op1=ALU.add),
                     reads=[("uT", gq + 1), "dsk", "py"], writes=["yf"])
                S.op("pool", lambda: P_.tensor_tensor(out=y2[:], in0=yf[:], in1=yf[:], op=ALU.mult), reads=["yf"], writes=["y2"])
                S.op("pool", lambda: P_.tensor_scalar(out=y2[:], in0=y2[:], scalar1=0.044715, scalar2=1.0, op0=ALU.mult, op1=ALU.add), reads=["y2"], writes=["y2"])
                S.op("pool", lambda: P_.tensor_tensor(out=y2[:], in0=y2[:], in1=yf[:], op=ALU.mult), reads=["y2", "yf"], writes=["y2"])
                S.op("act", lambda: A_.activation(out=ysg[:], in_=y2[:], func=AF.Sigmoid, scale=1.5957691216057308), reads=["y2"], writes=["ysg"])
                S.op("pool", lambda: P_.tensor_tensor(out=yb[:], in0=yf[:], in1=ysg[:], op=ALU.mult), reads=["yf", "ysg"], writes=[kyb])
                S.dma("sp", lambda: nc.sync.dma_start(out=slab3[128:256, gq, :], in_=yb[:]), reads=[kyb], writes=[("slabY", gq)])


        with ExitStack() as st1:
            sb, ps = mk(st1)
            w4b = sb("w4b", [128, 8, 512], BF16)
            biasq = sb("biasq", [128, 1], F32); biask = sb("biask", [128, 1], F32); biasu = sb("biasu", [128, 1], F32)
            bv_bc = sb("bv_bc", [128, 128], F32)
            with ExitStack() as stw:
                sbw, psw = mk(stw)
                wf = sbw("wf", [128, 8, 512], F32)
                gcol = sbw("gcol", [128, 8], F32); bcol = sbw("bcol", [128, 8], F32)
                bcr = sbw("bcr", [128, 8, 128], F32)
                pb = psw("pb", [128, 4], F32)
                pbv = psw("pbv", [128, 128], F32)
                S.dma("sp", lambda: nc.sync.dma_start(out=wf[:], in_=w4.rearrange("(k p) n -> p k n", p=128)), writes=["wf"])
                S.dma("sp", lambda: nc.sync.dma_start(out=gcol[:], in_=ln_in_g.rearrange("o (k p) -> (o p) k", p=128), allow_slow_non_contiguous=True), writes=["gcol"])
                S.dma("sp", lambda: nc.sync.dma_start(out=bcol[:], in_=ln_in_b.rearrange("o (k p) -> (o p) k", p=128), allow_slow_non_contiguous=True), writes=["bcol"])
                for k in range(8):
                    S.op("dve", lambda: V.tensor_scalar(out=w4b[:, k, :], in0=wf[:, k, :], scalar1=gcol[:, k:k + 1], scalar2=None, op0=ALU.mult),
                         reads=["wf", "gcol"], writes=["w4b"])
                    S.op("pool", lambda: P_.tensor_copy(out=bcr[:, k, :], in_=bcol[:, k:k + 1].to_broadcast([128, 128])), reads=["bcol"], writes=["bcr"])
                for bi, (dstb, kb, sc) in enumerate(((biasq, "biasq", 0.125), (biask, "biask", 1.0), (None, None, None), (biasu, "biasu", 1.0))):
                    if dstb is None:
                        continue
                    for k in range(8):
                        S.op("pe", lambda: T_.matmul(pb[:, bi:bi + 1], lhsT=wf[:, k, bi * 128:(bi + 1) * 128], rhs=bcol[:, k:k + 1],
                                                     start=(k == 0), stop=(k == 7)), reads=["wf", "bcol"], writes=["pb"])
                    S.op("dve", lambda: V.tensor_scalar(out=dstb[:], in0=pb[:, bi:bi + 1], scalar1=sc, scalar2=None, op0=ALU.mult), reads=["pb"], writes=[kb])
                for k in range(8):
                    S.op("pe", lambda: T_.matmul(pbv[:], lhsT=bcr[:, k, :], rhs=wf[:, k, 256:384], start=(k == 0), stop=(k == 7)),
                         reads=["wf", "bcr"], writes=["pbv"])
                S.op("dve", lambda: V.tensor_copy(out=bv_bc[:], in_=pbv[:]), reads=["pbv"], writes=["bv_bc"])
                for e in ("pe", "act", "dve", "pool", "sp"):
                    S.wait_all(e)
            NB = 3
            xts = [sb(f"xt{i}", [128, D], F32) for i in range(NB)]
            tmps = [None] * NB
            hbs = [sb(f"hb{i}", [128, D], BF16) for i in range(NB)]
            hTs = [sb(f"hT{i}", [128, 8, 512], BF16) for i in range(2)]
            Vst = [sb(f"Vst{i}", [128, 4, 129], BF16) for i in range(2)]
            for i_ in range(2):
                S.op("pool", lambda: P_.memset(Vst[i_][:, :, 128:129], 1.0), writes=[("Vst1", i_)])
            lnsb = [(sb(f"st6_{i}", [128, 12], F32), sb(f"mv_{i}", [128, 2], F32), sb(f"rstd_{i}", [128, 1], F32),
                     sb(f"nmr_{i}", [128, 1], F32)) for i in range(NB)]
            ptr = [ps("ptr0", [128, 8, 128], BF16)] * 2
            pproj = [ps("pproj0", [128, 512], F32)] * 2
            pv = ps("pv", [128, 4, 128], F32)
            groups = [[0]] + [list(range(1 + 4 * g, 5 + 4 * g)) for g in range(NG)]
            flat = [(gi, ti, ct) for gi, tiles in enumerate(groups) for ti, ct in enumerate(tiles)]
            pcnt = [0]

            def stA(t):
                gi, ti, ct = flat[t]
                s = t % NB
                xt, tmp, hb = xts[s], tmps[s], hbs[s]
                if ct == 0:
                    S.op("dve", lambda: V.memset(xt[0:112, :], 0.0), writes=[("xt", s)])
                    S.dma("sp", lambda: nc.sync.dma_start(out=xt[112:128, :], in_=meta), writes=[("xt", s)])
                else:
                    S.dma("sp", lambda: nc.sync.dma_start(out=xt[:], in_=x_b[(ct - 1) * 128:ct * 128, :]), writes=[("xt", s)])
                st6, mv, rstd, nmr = lnsb[s]
                ks = ("lnstat", id(st6))
                kx = ("xt", s)
                S.op("dve", lambda: V.bn_stats(out=st6[:, 0:6], in_=xt[:, 0:512]), reads=[kx], writes=[ks])
                S.op("dve", lambda: V.bn_stats(out=st6[:, 6:12], in_=xt[:, 512:1024]), reads=[kx], writes=[ks])
                S.op("dve", lambda: V.bn_aggr(out=mv[:, 0:2], in_=st6[:, 0:12]), reads=[ks], writes=[ks])
                S.op("dve", lambda: V.tensor_scalar(out=rstd[:], in0=mv[:, 1:2], scalar1=LN_EPS, scalar2=None, op0=ALU.add), reads=[ks], writes=[ks])
                S.op("dve", lambda: V.tensor_scalar(out=nmr[:], in0=mv[:, 0:1], scalar1=-1.0, scalar2=None, op0=ALU.mult), reads=[ks], writes=[ks])
                S.op("act", lambda: A_.activation(out=rstd[:], in_=rstd[:], func=AF.Ln), reads=[ks], writes=[ks])
                S.op("act", lambda: A_.activation(out=rstd[:], in_=rstd[:], func=AF.Exp, scale=-0.5), reads=[ks], writes=[ks])
                S.op("act", lambda: A_.activation(out=nmr[:], in_=nmr[:], func=AF.Copy, scale=rstd[:, 0:1]), reads=[ks], writes=[ks])
                S.op("act", lambda: A_.activation(out=hb[:], in_=xt[:], func=AF.Identity, bias=nmr[:, 0:1], scale=rstd[:, 0:1]),
                     reads=[kx, ks], writes=[("hb", s)])

            def stB(t):
                gi, ti, ct = flat[t]
                s = t % NB
                hb = hbs[s]
                hT = hTs[gi % 2]; khT = ("hT", gi % 2)
                pt = ptr[0]
                for k in range(8):
                    S.op("pe", lambda: T_.transpose(out=pt[:, k, :], in_=hb[:, k * 128:(k + 1) * 128], identity=ident[:]),
                         reads=[("hb", s), "ident"], writes=[("ptr", 0)])
                S.op("act", lambda: A_.copy(out=hT[:, :, ti * 128:(ti + 1) * 128], in_=pt[:]),
                     reads=[("ptr", 0)], writes=[khT])

            def stC(gi):
                tiles = groups[gi]
                hT = hTs[gi % 2]; khT = ("hT", gi % 2)
                ncols = 128 * len(tiles)
                c0 = tiles[0] * 128
                for bi, (dst, kd) in enumerate(((qT, "qT"), (kT, "kT"), (None, None), (uT, ("uT", gi)))):
                    if dst is None:
                        continue
                    pp = pproj[0]; kp = ("pproj", 0)
                    for k in range(8):
                        S.op("pe", lambda: T_.matmul(pp[:, 0:ncols], lhsT=w4b[:, k, bi * 128:(bi + 1) * 128], rhs=hT[:, k, 0:ncols],
                                                     start=(k == 0), stop=(k == 7)), reads=["w4b", khT], writes=[kp])
                    if bi == 0:
                        S.op("act", lambda: A_.activation(out=dst[:, c0:c0 + ncols], in_=pp[:, 0:ncols], func=AF.Identity, bias=biasq[:, 0:1], scale=0.125),
                             reads=[kp, "biasq"], writes=[kd])
                    else:
                        bb_ = biask if bi == 1 else biasu
                        S.op("dve", lambda: V.tensor_scalar(out=dst[:, c0:c0 + ncols], in0=pp[:, 0:ncols], scalar1=bb_[:, 0:1], scalar2=None, op0=ALU.add),
                             reads=[kp, "biask", "biasu"], writes=[kd])
                        if bi == 3 and gi == 0:
                            S.op("dve", lambda: V.memset(uT[:, 0:112], 0.0), reads=[kd], writes=[kd])
                for ti, ct in enumerate(tiles):
                    for k in range(8):
                        S.op("pe", lambda: T_.matmul(pv[:, ti, :], lhsT=hT[:, k, ti * 128:(ti + 1) * 128], rhs=w4b[:, k, 256:384],
                                                     start=(k == 0), stop=(k == 7)), reads=["w4b", khT], writes=["pv"])
                nt_ = len(tiles)
                vs_ = gi % 2
                S.op("dve", lambda: V.tensor_tensor(out=Vst[vs_][:, 0:nt_, 0:128], in0=pv[:, 0:nt_, :],
                                                    in1=bv_bc[:, 0:128].unsqueeze(1).to_broadcast([128, nt_, 128]), op=ALU.add),
                     reads=["pv", "bv_bc"], writes=[("Vst", vs_)])
                S.dma("sp", lambda: nc.sync.dma_start(out=vaug_d[:, tiles[0]:tiles[0] + nt_, :], in_=Vst[vs_][:, 0:nt_, :]),
                      reads=[("Vst", vs_), ("Vst1", vs_)], writes=[("vaug_d", gi)])

            nfl = len(flat)
            stA(0)
            if nfl > 1:
                stA(1)
            ssm_next = [0]

            def ssm_run(upto):
                while ssm_next[0] < upto:
                    ct_ = ssm_next[0]
                    if ct_ == 0:
                        bu(0)
                        wmod(0)
                    if ct_ + 1 < NT:
                        bu(ct_ + 1)
                    zmm(ct_)
                    if ct_ >= 1:
                        ymm(ct_ - 1)
                    if ct_ + 1 < NT:
                        wmod(ct_ + 1)
                    xmod(ct_)
                    ssm_next[0] += 1

            print("SBUF bytes remaining in fused phase:", nc.sbuf_bytes_remaining, flush=True)
            allowed = 0
            for t in range(nfl):
                if t + 2 < nfl:
                    stA(t + 2)
                stB(t)
                gi, ti, ct = flat[t]
                if ti == len(groups[gi]) - 1:
                    stC(gi)
                    if gi >= 1:
                        allowed = groups[gi - 1][-1]
                if ssm_next[0] < allowed:
                    ssm_run(ssm_next[0] + 1)
                    if allowed - ssm_next[0] > 4:
                        ssm_run(ssm_next[0] + 1)
            ssm_run(NT)
            ymm(NT - 1)
            if debug:
                S.dma("sp", lambda: nc.sync.dma_start(out=dbg["qT"], in_=qT[:]), reads=["qT"], writes=["dq"])
                S.dma("sp", lambda: nc.sync.dma_start(out=dbg["kT"], in_=kT[:]), reads=["kT"], writes=["dk"])
            for e in ("pe", "act", "dve", "pool", "sp"):
                S.wait_all(e)
        st3.close()
        st_u.close()
        print("P1a+S5 done: inst", S.n_inst, "waits", S.n_wait, "sems", S.nsem, flush=True)

        XCH = min(1024, 128 * NG)
        NXC = 256 * NG // XCH

        GPC = XCH // 256

        def ag_chunk(c_):
            keys = []
            for g_ in range(c_ * GPC, (c_ + 1) * GPC):
                keys += [("slabA", g_), ("slabY", g_)]
            S.waitfor("pool", keys)
            S.op("pool", lambda: P_.collective_compute("AllGather", ALU.bypass, replica_groups=[[0, 1, 2, 3], [4, 5, 6, 7]],
                                                       ins=[slab[c_ * XCH:(c_ + 1) * XCH, :].opt()],
                                                       outs=[gath[c_ * 4 * XCH:(c_ + 1) * 4 * XCH, :].opt()]), writes=[("gath", c_)])

        with ExitStack() as st2:
            sb, ps = mk(st2)
            Vaug = sb("Vaug", [128, NT, 129], BF16)
            S.dma("sp", lambda: nc.sync.dma_start(out=Vaug[:], in_=vaug_d), reads=[("vaug_d", gi_) for gi_ in range(NG + 1)], writes=["Vaug"])
            tb = bc_load("tb", relb[0], 32, sb)
            Wb = sb("Wb", [128, 1024], F32)
            Bm0 = sb("Bm0", [128, 512], F32)
            with ExitStack() as stt:
                sbt, _ = mk(stt)
                reli = sbt("reli", [128, 1024], I32)
                relv = sbt("relv", [128, 1024], F32)
                stp = sbt("stp", [128, 1024], F32)
                iom = sbt("iom", [128, 1024], F32)
                dl = sbt("dl", [128, 32], F32)
                thrp = sbt("thrp", [128, 1], F32)
                S.op("pool", lambda: P_.iota(reli[:], pattern=[[-1, 1024]], base=384, channel_multiplier=1), writes=["reli"])
                S.op("dve", lambda: V.tensor_copy(out=relv[:], in_=reli[:]), reads=["reli"], writes=["relv"])
                S.op("pool", lambda: P_.iota(reli[:], pattern=[[1, 1024]], base=0, channel_multiplier=0), reads=["relv"], writes=["reli"])
                S.op("dve", lambda: V.tensor_copy(out=iom[:], in_=reli[:]), reads=["reli"], writes=["iom"])
                steps = []
                for j in range(15, 8, -1):
                    steps.append((-thr[j] + 1, j - 1))
                for n in range(7, -1, -1):
                    steps.append((-n, n))
                for n in range(1, 8):
                    steps.append((n, 16 + n))
                steps.append((8, 24))
                for j in range(9, 16):
                    steps.append((thr[j], 16 + j))
                prev = 15
                S.op("dve", lambda: V.tensor_scalar(out=Wb[:], in0=relv[:], scalar1=0.0, scalar2=tb[:, 15:16],
                                                    op0=ALU.mult, op1=ALU.add), reads=["relv", "tb"], writes=["Wb"])
                for si, (tv, bk) in enumerate(steps):
                    S.op("dve", lambda: V.tensor_tensor(out=dl[:, si:si + 1], in0=tb[:, bk:bk + 1], in1=tb[:, prev:prev + 1],
                                                        op=ALU.subtract), reads=["tb"], writes=["dl"])
                    S.op("dve", lambda: V.tensor_scalar(out=stp[:], in0=relv[:], scalar1=float(tv), scalar2=dl[:, si:si + 1],
                                                        op0=ALU.is_ge, op1=ALU.mult), reads=["relv", "dl"], writes=["stp"])
                    S.op("dve", lambda: V.tensor_tensor(out=Wb[:], in0=Wb[:], in1=stp[:], op=ALU.add), reads=["stp", "Wb"], writes=["Wb"])
                    prev = bk
                S.op("pool", lambda: P_.affine_select(out=Wb[0:64, :], in_=Wb[0:64, :], pattern=[[1, 1024]], compare_op=ALU.is_ge,
                                                      fill=NEG, base=-384, channel_multiplier=0), reads=["Wb"], writes=["Wb"])
                S.op("pool", lambda: P_.affine_select(out=Wb[64:128, :], in_=Wb[64:128, :], pattern=[[1, 1024]], compare_op=ALU.is_ge,
                                                      fill=NEG, base=-448, channel_multiplier=0), reads=["Wb"], writes=["Wb"])
                S.op("dve", lambda: V.tensor_copy(out=Bm0[:], in_=Wb[:, 512:1024]), reads=["Wb"], writes=["Bm0"])
                S.op("dve", lambda: V.memset(Bm0[0:112, :], NEG), reads=["Bm0"], writes=["Bm0"])
                for e in ("pe", "act", "dve", "pool", "sp"):
                    S.wait_all(e)
            Wbb = sb("Wbb", [128, 1024], BF16)
            Bm0b = sb("Bm0b", [128, 512], BF16)
            S.op("dve", lambda: V.tensor_copy(out=Wbb[:], in_=Wb[:]), reads=["Wb"], writes=["Wbb"])
            S.op("dve", lambda: V.tensor_copy(out=Bm0b[:], in_=Bm0[:]), reads=["Bm0"], writes=["Bm0b"])
            bfar = sb("bfar", [128, 1], F32)
            bfarm = sb("bfarm", [128, 1], F32)
            S.op("dve", lambda: V.tensor_copy(out=bfar[:], in_=tb[:, 15:16]), reads=["tb"], writes=["bfar"])
            S.op("dve", lambda: V.tensor_copy(out=bfarm[:], in_=tb[:, 15:16]), reads=["tb"], writes=["bfarm"])
            S.op("dve", lambda: V.memset(bfarm[0:112, :], NEG), reads=["bfarm"], writes=["bfarm"])
            lamt = sb("lamt", [128, 4, 64], F32)
            S.dma("sp", lambda: nc.sync.dma_start(out=lamt[:], in_=lam4.rearrange("a n -> (a n)").partition_broadcast(128)), writes=["lamt"])
            lsum = sb("lsum", [128, 2], F32)
            lprod = sb("lprod", [128, 2, 64], F32)
            neglam = sb("neglam", [128, 1], F32)
            S.op("dve", lambda: V.tensor_tensor(out=lprod[:, 0, :], in0=lamt[:, 0, :], in1=lamt[:, 1, :], op=ALU.mult), reads=["lamt"], writes=["lprod"])
            S.op("dve", lambda: V.tensor_tensor(out=lprod[:, 1, :], in0=lamt[:, 2, :], in1=lamt[:, 3, :], op=ALU.mult), reads=["lamt"], writes=["lprod"])
            S.op("dve", lambda: V.reduce_sum(out=lsum[:], in_=lprod[:], axis=AX.X), reads=["lprod"], writes=["lsum"])
            S.op("act", lambda: A_.activation(out=lsum[:], in_=lsum[:], func=AF.Exp), reads=["lsum"], writes=["lsum"])
            S.op("dve", lambda: V.tensor_tensor(out=neglam[:], in0=lsum[:, 1:2], in1=lsum[:, 0:1], op=ALU.subtract), reads=["lsum"], writes=["neglam"])
            S.op("dve", lambda: V.tensor_scalar(out=neglam[:], in0=neglam[:], scalar1=-0.2, scalar2=None, op0=ALU.add), reads=["neglam"], writes=["neglam"])
            g08 = bc_load("g08", subln_g[0], 128, sb)
            S.op("dve", lambda: V.tensor_scalar(out=g08[:], in0=g08[:], scalar1=0.8, scalar2=None, op0=ALU.mult), reads=["g08"], writes=["g08"])

            pss = [ps(f"pss{i}", [128, 2, 512], F32) for i in range(2)]
            po = ps("po", [128, 3, 512], F32)
            ptt = ps("ptt", [128, 4, 128], BF16)
            pTs = [sb(f"pT{i}", [128, 2, 512], BF16) for i in range(3)]
            sn = [sb(f"sn{i}", [128, 2, 512], F32) for i in range(2)]
            osb = sb("osb", [128, 128], F32)
            o2 = sb("o2", [128, 128], F32)
            ob = sb("ob", [128, 4, 128], BF16)
            rr = sb("rr", [128, 8], F32)
            attT = [sb(f"attT{i}", [128, 512], BF16) for i in range(2)]

            def acc(a):
                return po[:, a // 3, (a % 3) * 129:(a % 3) * 129 + 129]

            o4 = sb("o4", [128, 4, 128], F32)
            ms = sb("ms", [128, 4], F32)
            units = [(g, j) for g in range(NG) for j in range(4 * g + 5)]
            ncnt = [0]

            def near_of(g, j):
                if j == 0:
                    return Bm0b[:, :] if g == 0 else None
                if j < 4 * g:
                    return None
                tp = j - (4 * g + 1)
                return Wbb[:, 384 - 128 * tp:384 - 128 * tp + 512]

            def qk(i):
                g, j = units[i]
                u = i % 2
                q0 = 128 + 512 * g
                nb = near_of(g, j)
                for m in range(2):
                    S.op("pe", lambda: T_.matmul(pss[u][:, m, :], lhsT=kT[64 * m:64 * m + 64, j * 128:(j + 1) * 128],
                                                 rhs=qT[64 * m:64 * m + 64, q0:q0 + 512], start=True, stop=(nb is None)),
                         reads=["kT", "qT"], writes=[("pss", u)])
                    if nb is not None:
                        S.op("pe", lambda: T_.matmul(pss[u][:, m, :], lhsT=ident[:], rhs=nb, start=False, stop=True),
                             reads=["ident", "Wbb", "Bm0b"], writes=[("pss", u)])

            def ex(i):
                g, j = units[i]
                u = i % 2
                v3 = i % 3
                pS, pT = pss[u], pTs[v3]
                if j == 0 and g > 0:
                    S.op("act", lambda: A_.activation(out=pT[:], in_=pS[:], func=AF.Exp, bias=bfarm[:, 0:1], scale=1.0),
                         reads=[("pss", u), "bfarm"], writes=[("pT", v3)])
                elif 0 < j < 4 * g:
                    S.op("act", lambda: A_.activation(out=pT[:], in_=pS[:], func=AF.Exp, bias=bfar[:, 0:1], scale=1.0),
                         reads=[("pss", u), "bfar"], writes=[("pT", v3)])
                else:
                    S.op("act", lambda: A_.activation(out=pT[:], in_=pS[:], func=AF.Exp), reads=[("pss", u)], writes=[("pT", v3)])

            def pv(i):
                g, j = units[i]
                u = i % 3
                last = 4 * g + 4
                for m in range(2):
                    for qs in range(4):
                        S.op("pe", lambda: T_.matmul(acc(m * 4 + qs), lhsT=pTs[u][:, m, qs * 128:(qs + 1) * 128], rhs=Vaug[:, j, :],
                                                     start=(j == 0 and (m * 4 + qs) % 3 == 0), stop=(j == last), skip_group_check=True),
                             reads=[("pT", u), "Vaug"], writes=["po"])

            posn = sb("posn", [128, 3, 512], F32)

            def accs_(a):
                return posn[:, a // 3, (a % 3) * 129:(a % 3) * 129 + 129]

            def fin_dve1(g):
                S.op("dve", lambda: V.tensor_copy(out=posn[:, 0:2, 0:387], in_=po[:, 0:2, 0:387]), reads=["po"], writes=["posn"])
                S.op("dve", lambda: V.tensor_copy(out=posn[:, 2, 0:258], in_=po[:, 2, 0:258]), reads=["po"], writes=["posn"])
                for a in range(8):
                    S.op("dve", lambda: V.reciprocal(out=rr[:, a:a + 1], in_=accs_(a)[:, 128:129]), reads=["posn"], writes=["rr"])
                S.op("dve", lambda: V.tensor_scalar(out=rr[:, 4:8], in0=rr[:, 4:8], scalar1=neglam[:, 0:1], scalar2=None, op0=ALU.mult),
                     reads=["rr", "neglam"], writes=["rr"])
                for qs in range(4):
                    S.op("dve", lambda: V.tensor_scalar(out=o4[:, qs, :], in0=accs_(qs)[:, 0:128], scalar1=rr[:, qs:qs + 1], scalar2=None, op0=ALU.mult),
                         reads=["posn", "rr"], writes=["o4"])
                    S.op("dve", lambda: V.scalar_tensor_tensor(out=o4[:, qs, :], in0=accs_(4 + qs)[:, 0:128], scalar=rr[:, 4 + qs:5 + qs], in1=o4[:, qs, :],
                                                               op0=ALU.mult, op1=ALU.add), reads=["posn", "rr", "o4"], writes=["o4"])
                    S.op("dve", lambda: V.tensor_tensor(out=o2[:], in0=o4[:, qs, :], in1=o4[:, qs, :], op=ALU.mult), reads=["o4"], writes=["o2"])
                    S.op("dve", lambda: V.reduce_sum(out=ms[:, qs:qs + 1], in_=o2[:], axis=AX.X), reads=["o2", "ms"], writes=["ms"])
                S.op("dve", lambda: V.tensor_scalar(out=ms[:], in0=ms[:], scalar1=1.0 / 128.0, scalar2=LN_EPS, op0=ALU.mult, op1=ALU.add),
                     reads=["ms"], writes=["ms"])

            def fin_rest(g):
                aT = attT[g % 2]; kaT = ("attT", g % 2)
                S.op("act", lambda: A_.activation(out=ms[:], in_=ms[:], func=AF.Ln), reads=["ms"], writes=["ms"])
                S.op("act", lambda: A_.activation(out=ms[:], in_=ms[:], func=AF.Exp, scale=-0.5), reads=["ms"], writes=["ms"])
                for qs in range(4):
                    S.op("dve", lambda: V.scalar_tensor_tensor(out=ob[:, qs, :], in0=o4[:, qs, :], scalar=ms[:, qs:qs + 1], in1=g08[:],
                                                               op0=ALU.mult, op1=ALU.mult), reads=["o4", "ms", "g08"], writes=["ob"])

            def fin_out(g):
                aT = attT[g % 2]; kaT = ("attT", g % 2)
                for qs in range(4):
                    S.op("pe", lambda: T_.transpose(out=ptt[:, qs, :], in_=ob[:, qs, :], identity=ident[:]), reads=["ob", "ident"], writes=["ptt"])
                S.op("dve", lambda: V.tensor_copy(out=aT[:], in_=ptt[:].rearrange("p a b -> p (a b)")), reads=["ptt"], writes=[kaT])
                S.dma("sp", lambda: nc.sync.dma_start(out=slab3[0:128, g, :], in_=aT[:]), reads=[kaT], writes=[("slabA", g)])
                if (g + 1) % GPC == 0:
                    ag_chunk(g // GPC)

            qk(0)
            pending = None
            pend_out = None
            nun = len(units)
            for i in range(nun + 1):
                if i + 1 < nun:
                    qk(i + 1)
                if i < nun:
                    ex(i)
                if i >= 1:
                    pv(i - 1)
                    gp, jp = units[i - 1]
                    if pending is not None and jp == min(8, 4 * gp + 4):
                        fin_rest(pending)
                        pend_out = pending
                        pending = None
                    if pend_out is not None and jp == min(10, 4 * gp + 4):
                        fin_out(pend_out)
                        pend_out = None
                    if jp == 4 * gp + 4:
                        fin_dve1(gp)
                        pending = gp
            if pend_out is not None:
                fin_out(pend_out)
            fin_rest(pending)
            fin_out(pending)
            for e in ("pe", "act", "dve", "pool", "sp"):
                S.wait_all(e)
        st_att.close()
        print("P1b done: inst", S.n_inst, "waits", S.n_wait, "sems", S.nsem, flush=True)
        if debug:
            S.waitfor("sp", [("slabA", g) for g in range(NG)] + [("slabY", g) for g in range(NG)])
            S.dma("sp", lambda: nc.sync.dma_start(out=dbg["slab"], in_=slab), writes=["dslab"])
        if not full:
            for e in ("pe", "act", "dve", "pool", "sp"):
                S.wait_all(e)
            return nc
        S.waitfor("pool", [("gath", c_) for c_ in range(NXC)])

        def barrier():
            for e_ in ("pe", "act", "dve", "pool", "sp"):
                S.wait_all(e_)

        IOA = bass.IndirectOffsetOnAxis
        with ExitStack() as st4:
            sb, ps = mk(st4)
            g1 = bc_load("g1", ln1_g[0], D, sb); b1 = bc_load("b1", ln1_b[0], D, sb)
            g2 = bc_load("g2", ln2_g[0], D, sb); b2 = bc_load("b2", ln2_b[0], D, sb)
            slot_ga = sb("slot_ga", [128, NT2, 4], I32)
            gate_all = sb("gate_all", [128, NT2, 4], F32)
            lnsb2 = [(sb(f"st6b_{i}", [128, 12], F32), sb(f"mvb_{i}", [128, 2], F32), sb(f"rstdb_{i}", [128, 1], F32),
                      sb(f"nmrb_{i}", [128, 1], F32)) for i in range(2)]
            ztile = sb("ztile", [128, NSLOT // 128 + 1], I32)
            S.op("pool", lambda: P_.memset(ztile[:], 0), writes=["ztile"])
            S.dma("sp", lambda: nc.sync.dma_start(out=tokslot_d.rearrange("(p f) o -> p (f o)", p=128), in_=ztile[:]), reads=["ztile"], writes=["tokslot0"])
            sc_keys = []
            h1_keys = []
            with ExitStack() as st5:
                sb5, ps5 = mk(st5)
                wglu = sb5("wglu", [128, 4, 512], BF16)
                wout = sb5("wout", [128, 8, 1024], BF16)
                wr = sb5("wr", [128, 8, 32], F32)
                S.dma("pool", lambda: P_.dma_start(out=wglu[:], in_=w_glu.rearrange("(k p) n -> p k n", p=128)), writes=["wglu"])
                S.dma("pool", lambda: P_.dma_start(out=wout[:], in_=w_out.rearrange("(k p) n -> p k n", p=128)), writes=["wout"])
                S.dma("sp", lambda: nc.sync.dma_start(out=wr[:], in_=w_router.rearrange("(k p) n -> p k n", p=128)), writes=["wr"])
                bglu = sb5("bglu", [128, 4], F32)
                S.dma("sp", lambda: nc.sync.dma_start(out=bglu[:], in_=b_glu.rearrange("o (j p) -> (o p) j", p=128), allow_slow_non_contiguous=True), writes=["bglu"])
                brt = bc_load("brt", b_router[0], NEXP, sb5)
                gidx_t = sb5("gidx_t", [128, 8 * NG2], I32)
                S.dma("sp", lambda: nc.sync.dma_start(out=gidx_t[:], in_=gidx), writes=["gidx"])
                ebi = sb5("ebi", [128, NEXP], I32); ebase = sb5("ebase", [128, NEXP], F32)
                S.op("pool", lambda: P_.iota(ebi[:], pattern=[[CAP, NEXP]], base=0, channel_multiplier=0), writes=["ebi"])
                S.op("dve", lambda: V.tensor_copy(out=ebase[:], in_=ebi[:]), reads=["ebi"], writes=["ebase"])
                cntbc = sb5("cntbc", [128, NEXP], F32)
                S.op("dve", lambda: V.memset(cntbc[:], 0.0), writes=["cntbc"])
                catT = [sb5(f"catT{i}", [128, 4, 512], BF16) for i in range(2)]
                yT = [sb5(f"yT{i}", [128, 4, 512], BF16) for i in range(2)]
                ssmT = sb5("ssmT", [128, 4, 512], BF16)
                sg = sb5("sg", [128, 512], F32)
                xt2 = [sb5(f"xt2_{i}", [128, D], F32) for i in range(2)]
                tmpa = sb5("tmpa", [128, D], F32)
                h0 = sb5("h0", [128, D], F32)
                pre = sb5("pre", [128, D], F32)
                h1 = [sb5(f"h1_{i}", [128, D], F32) for i in range(2)]
                h1b = [sb5(f"h1b_{i}", [128, D], BF16) for i in range(2)]
                h1T = sb5("h1T", [128, 8, 128], F32)
                lg = sb5("lg", [128, NEXP], F32); mx8 = sb5("mx8", [128, 8], F32); negm = sb5("negm", [128, 1], F32)
                ex = sb5("ex", [128, NEXP], F32); msk = sb5("msk", [128, NEXP], F32); mskb = sb5("mskb", [128, NEXP], BF16)
                den = sb5("den", [128, 1], F32); gfull = sb5("gfull", [128, NEXP], F32)
                rank = sb5("rank", [128, NEXP], F32); ovf = sb5("ovf", [128, NEXP], F32); keep = sb5("keep", [128, NEXP], F32)
                sga = sb5("sga", [128, NEXP], F32); ssc = sb5("ssc", [128, NEXP], F32)
                oh = sb5("oh", [128, NEXP], F32); tmq = sb5("tmq", [128, NEXP], F32)
                sgak = sb5("sgak", [128, 4], F32); ssck = sb5("ssck", [128, 4], F32)
                ssci = [sb5(f"ssci{i}", [128, 4], I32) for i in range(2)]
                tokid = [sb5(f"tokid{i}", [128, 1], I32) for i in range(2)]
                pzg = [ps5(f"pzg{i}", [128, 512], F32) for i in range(2)]
                pms = [ps5(f"pm{i}", [128, 1024], F32) for i in range(2)]
                ptf = [ps5("ptf0", [128, 4, 128], F32)] * 2
                pk = ps5("pk", [128, 512], F32)
                Q3 = sb5("Q3", [128, 3, NEXP], F32)
                tm3 = sb5("tm3", [128, 3, NEXP], F32)
                R4 = sb5("R4", [128, 4, 3], F32)
                lnsb3 = [(sb5(f"st6c_{i}", [128, 12], F32), sb5(f"mvc_{i}", [128, 2], F32), sb5(f"rstdc_{i}", [128, 1], F32),
                          sb5(f"nmrc_{i}", [128, 1], F32)) for i in range(2)]

                h0s = [h0, sb5("h0b", [128, D], F32)]
                tmpb = sb5("tmpb", [128, D], F32)

                def stage0(t2):
                    s2 = t2 % 2
                    xt = xt2[s2]
                    S.dma("sp", lambda: nc.sync.dma_start(out=xt[:], in_=x2[t2 * 128:(t2 + 1) * 128, :]), writes=[("xt2", s2)])
                    layernorm(xt, ("xt2", s2), lng, "lng", lnb, "lnb", tmpb, "tmpb", h0s[s2][:], ("h0", s2), lnsb3[0])

                def stage1_mm(t2, tt, cT, kc):
                    pm = pms[t2 % 2]
                    for half in range(2):
                        for k in range(8):
                            lhs = cT[:, k, tt * 128:(tt + 1) * 128] if k < 4 else ssmT[:, k - 4, tt * 128:(tt + 1) * 128]
                            S.op("pe", lambda: T_.matmul(pm[:, half * 512:(half + 1) * 512], lhsT=lhs, rhs=wout[:, k, half * 512:(half + 1) * 512],
                                                         start=(k == 0), stop=(k == 7)), reads=["wout", kc, "ssmT"], writes=[("pm", t2 % 2)])

                def stage1(t2, tg, tt, cT, kc):
                    s2 = t2 % 2
                    pm = pms[s2]
                    S.op("dve", lambda: V.scalar_tensor_tensor(out=pre[:], in0=h0s[s2][:], scalar=ALPHA, in1=pm[:], op0=ALU.mult, op1=ALU.add),
                         reads=[("h0", s2), ("pm", s2)], writes=["pre"])
                    h1t = h1[s2]; kh1 = ("h1", s2)
                    layernorm(pre, "pre", g1, "g1", b1, "b1", tmpa, "tmpa", h1t[:], kh1, lnsb3[1])
                    S.op("act", lambda: A_.copy(out=h1b[s2][:], in_=h1t[:]), reads=[kh1], writes=[("h1b", s2)])
                    S.dma("sp", lambda: nc.sync.dma_start(out=h1_d[t2 * 128:(t2 + 1) * 128, :], in_=h1t[:]), reads=[kh1], writes=[("h1_d", t2)])
                    S.dma("sp", lambda: nc.sync.dma_start(out=h1b_d[t2 * 128:(t2 + 1) * 128, :], in_=h1b[s2][:]), reads=[("h1b", s2)], writes=[("h1b_d", t2)])
                    h1_keys.append(("h1b_d", t2))
                    if debug:
                        S.dma("sp", lambda: nc.sync.dma_start(out=dbg["h1"][t2 * 128:(t2 + 1) * 128, :], in_=h1t[:]), reads=[kh1], writes=[("dh1", t2)])

                def stage2(t2):
                    s2 = t2 % 2
                    h1t = h1[s2]; kh1 = ("h1", s2)
                    for hh in range(2):
                        pf = ptf[hh]
                        for k4 in range(4):
                            k = hh * 4 + k4
                            S.op("pe", lambda: T_.transpose(out=pf[:, k4, :], in_=h1t[:, k * 128:(k + 1) * 128], identity=identf[:]),
                                 reads=[kh1, "identf"], writes=[("ptf", 0)])
                        S.op("act", lambda: A_.copy(out=h1T[:, hh * 4:hh * 4 + 4, :], in_=pf[:]), reads=[("ptf", 0)], writes=["h1T"])
                    for k in range(8):
                        S.op("pe", lambda: T_.matmul(pk[:, 0:32], lhsT=h1T[:, k, :], rhs=wr[:, k, :], start=(k == 0), stop=(k == 7)),
                             reads=["h1T", "wr"], writes=["pk"])
                    S.op("dve", lambda: V.tensor_tensor(out=lg[:], in0=pk[:, 0:32], in1=brt[:], op=ALU.add), reads=["pk", "brt"], writes=["lg"])
                    if debug:
                        S.dma("sp", lambda: nc.sync.dma_start(out=dbg["lg"][t2 * 128:(t2 + 1) * 128, :], in_=lg[:]), reads=["lg"], writes=[("dlg", t2)])
                    S.op("dve", lambda: V.max(out=mx8[:], in_=lg[:]), reads=["lg"], writes=["mx8"])
                    S.op("dve", lambda: V.tensor_scalar(out=negm[:], in0=mx8[:, 0:1], scalar1=-1.0, scalar2=None, op0=ALU.mult), reads=["mx8"], writes=["negm"])
                    S.op("act", lambda: A_.activation(out=ex[:], in_=lg[:], func=AF.Exp, bias=negm[:, 0:1], scale=1.0), reads=["lg", "negm"], writes=["ex"])
                    S.op("dve", lambda: V.tensor_scalar(out=msk[:], in0=lg[:], scalar1=mx8[:, 3:4], scalar2=None, op0=ALU.is_ge), reads=["lg", "mx8"], writes=["msk"])
                    S.op("dve", lambda: V.tensor_copy(out=mskb[:], in_=msk[:]), reads=["msk"], writes=["mskb"])
                    S.op("pe", lambda: T_.matmul(pk[:, 32:64], lhsT=stri[:], rhs=mskb[:], start=True, stop=True), reads=["stri", "mskb"], writes=["pk"])
                    S.op("pe", lambda: T_.matmul(pk[:, 64:96], lhsT=ones[:], rhs=mskb[:], start=True, stop=True), reads=["ones", "mskb"], writes=["pk"])
                    S.op("dve", lambda: V.tensor_tensor(out=ex[:], in0=ex[:], in1=msk[:], op=ALU.mult), reads=["ex", "msk"], writes=["ex"])
                    S.op("dve", lambda: V.reduce_sum(out=den[:], in_=ex[:], axis=AX.X), reads=["ex"], writes=["den"])
                    S.op("dve", lambda: V.reciprocal(out=den[:], in_=den[:]), reads=["den"], writes=["den"])
                    S.op("dve", lambda: V.tensor_tensor(out=rank[:], in0=pk[:, 32:64], in1=cntbc[:], op=ALU.add), reads=["pk", "cntbc"], writes=["rank"])
                    S.op("dve", lambda: V.tensor_tensor(out=cntbc[:], in0=pk[:, 64:96], in1=cntbc[:], op=ALU.add), reads=["pk", "cntbc", "rank"], writes=["cntbc"])
                    S.op("dve", lambda: V.tensor_scalar(out=ovf[:], in0=rank[:], scalar1=float(CAP), scalar2=None, op0=ALU.is_ge), reads=["rank"], writes=["ovf"])
                    S.op("dve", lambda: V.tensor_scalar(out=keep[:], in0=ovf[:], scalar1=-1.0, scalar2=1.0, op0=ALU.mult, op1=ALU.add), reads=["ovf"], writes=["keep"])
                    S.op("dve", lambda: V.scalar_tensor_tensor(out=Q3[:, 2, :], in0=ex[:], scalar=den[:, 0:1], in1=keep[:], op0=ALU.mult, op1=ALU.mult),
                         reads=["ex", "den", "keep"], writes=["Q3"])
                    S.op("dve", lambda: V.scalar_tensor_tensor(out=Q3[:, 0, :], in0=rank[:], scalar=float(CAP - 1), in1=ebase[:], op0=ALU.min, op1=ALU.add),
                         reads=["rank", "ebase"], writes=["Q3"])
                    S.op("dve", lambda: V.tensor_tensor(out=ssc[:], in0=rank[:], in1=ebase[:], op=ALU.add), reads=["rank", "ebase"], writes=["ssc"])
                    S.op("dve", lambda: V.tensor_tensor(out=ssc[:], in0=ssc[:], in1=keep[:], op=ALU.mult), reads=["ssc", "keep"], writes=["ssc"])
                    S.op("dve", lambda: V.scalar_tensor_tensor(out=Q3[:, 1, :], in0=ovf[:], scalar=float(NSLOT), in1=ssc[:], op0=ALU.mult, op1=ALU.add),
                         reads=["ovf", "ssc"], writes=["Q3"])
                    for k in range(4):
                        S.op("dve", lambda: V.tensor_scalar(out=oh[:], in0=lg[:], scalar1=mx8[:, k:k + 1], scalar2=None, op0=ALU.is_equal), reads=["lg", "mx8"], writes=["oh"])
                        S.op("dve", lambda: V.tensor_tensor(out=tm3[:], in0=Q3[:], in1=oh[:, 0:NEXP].unsqueeze(1).to_broadcast([128, 3, NEXP]), op=ALU.mult),
                             reads=["Q3", "oh"], writes=["tm3"])
                        S.op("dve", lambda: V.reduce_sum(out=R4[:, k, :], in_=tm3[:], axis=AX.X), reads=["tm3"], writes=["R4"])
                    S.op("dve", lambda: V.tensor_copy(out=slot_ga[:, t2, :], in_=R4[:, :, 0]), reads=["R4"], writes=["slot_ga"])
                    S.op("dve", lambda: V.tensor_copy(out=ssci[s2][:], in_=R4[:, :, 1]), reads=["R4"], writes=[("ssci", s2)])
                    S.op("dve", lambda: V.tensor_copy(out=gate_all[:, t2, :], in_=R4[:, :, 2]), reads=["R4"], writes=["gate_all"])
                    S.op("pool", lambda: P_.iota(tokid[s2][:], pattern=[[0, 1]], base=t2 * 128, channel_multiplier=1), writes=[("tokid", s2)])
                    for k in range(4):
                        S.dma("pool", lambda: P_.indirect_dma_start(out=tokslot_d[:, :], out_offset=IOA(ap=ssci[s2][:, k:k + 1], axis=0),
                                                                    in_=tokid[s2][:, 0:1], in_offset=None),
                              reads=[("ssci", s2), ("tokid", s2), "tokslot0"], writes=[("sc", t2, k)])
                        sc_keys.append(("sc", t2, k))

                def fetch_group(tg):
                    for j in range(4):
                        for half in range(2):
                            col = (tg * 4 + j) * 2 + half
                            dst = catT[tg % 2][:, j, :] if half == 0 else yT[tg % 2][:, j, :]
                            S.dma("pool", lambda: P_.indirect_dma_start(out=dst, out_offset=None, in_=gath[:, :],
                                                                        in_offset=IOA(ap=gidx_t[:, col:col + 1], axis=0)),
                                  reads=["gidx"], writes=[("catT", tg % 2) if half == 0 else ("yT", tg % 2)])

                fetch_group(0)
                for tg in range(NG2):
                    cT, yT_ = catT[tg % 2], yT[tg % 2]
                    kc, ky = ("catT", tg % 2), ("yT", tg % 2)
                    if tg + 1 < NG2:
                        fetch_group(tg + 1)
                    for j2 in range(4):
                        pz_ = pzg[j2 % 2]; kpz = ("pzg", j2 % 2)
                        for j1 in range(4):
                            S.op("pe", lambda: T_.matmul(pz_[:], lhsT=wglu[:, j1, j2 * 128:(j2 + 1) * 128], rhs=yT_[:, j1, :],
                                                         start=(j1 == 0), stop=(j1 == 3)), reads=["wglu", ky], writes=[kpz])
                        S.op("act", lambda: A_.activation(out=sg[:], in_=pz_[:], func=AF.Sigmoid, bias=bglu[:, j2:j2 + 1], scale=1.0),
                             reads=[kpz, "bglu"], writes=["sg"])
                        S.op("dve", lambda: V.tensor_tensor(out=ssmT[:, j2, :], in0=yT_[:, j2, :], in1=sg[:], op=ALU.mult),
                             reads=[ky, "sg"], writes=["ssmT"])
                    stage1_mm(tg * 4, 0, cT, kc)
                    for tt in range(4):
                        t2 = tg * 4 + tt
                        if t2 == 0:
                            stage0(0)
                        if t2 + 1 < NT2:
                            stage0(t2 + 1)
                        if tt < 3:
                            stage1_mm(t2 + 1, tt + 1, cT, kc)
                        stage1(t2, tg, tt, cT, kc)
                        if t2 >= 1:
                            stage2(t2 - 1)
                stage2(NT2 - 1)
                barrier()
            print("P2a done: inst", S.n_inst, "waits", S.n_wait, "sems", S.nsem, flush=True)

            chunks = [(c0, min(c0 + 512, CAP)) for c0 in range(0, CAP, 512)]
            y_keys = []
            with ExitStack() as st6:
                sb6, ps6 = mk(st6)
                wgs = [sb6(f"wg{i}", [128, 8, 1024], BF16) for i in range(2)]
                wus = [sb6(f"wu{i}", [128, 8, 1024], BF16) for i in range(2)]
                wds = [sb6(f"wd{i}", [128, 8, 1024], BF16) for i in range(2)]
                bgall = sb6("bgall", [128, NEXP, 8], F32); buall = sb6("buall", [128, NEXP, 8], F32)
                for e in range(NEXP):
                    S.dma("sp", lambda: nc.sync.dma_start(out=bgall[:, e, :], in_=b_gate[e].rearrange("(j p) -> p j", p=128), allow_slow_non_contiguous=True),
                          writes=[("bgall", e)])
                    S.dma("sp", lambda: nc.sync.dma_start(out=buall[:, e, :], in_=b_up[e].rearrange("(j p) -> p j", p=128), allow_slow_non_contiguous=True),
                          writes=[("buall", e)])
                bdbc = [sb6(f"bdbc{i}", [128, D], F32) for i in range(2)]
                idxs = [sb6(f"idx{i}", [128, NRT], I32) for i in range(2)]
                xg = [sb6(f"xg{i}", [128, D], BF16) for i in range(2)]
                xT = sb6("xT", [128, 8, CAP], BF16)
                actT = sb6("actT", [128, 8, CAP], BF16)
                gsb = [sb6(f"gsb{i}", [128, 512], F32) for i in range(2)]
                sgs = [sb6(f"sgs{i}", [128, 512], F32) for i in range(2)]
                usb = [sb6(f"usb{i}", [128, 512], F32) for i in range(2)]
                yo = [sb6(f"yo{i}", [128, D], F32) for i in range(2)]
                ptx = ps6("ptx", [128, 8, 128], BF16)
                pgc = [ps6(f"pgc{i}", [128, 512], F32) for i in range(2)]
                puc = [ps6(f"puc{i}", [128, 512], F32) for i in range(2)]
                pdh = [ps6(f"pd{i}", [128, 512], F32) for i in range(2)]

                WSRC = {"wg": (wgs, w_gate), "wu": (wus, w_up), "wd": (wds, w_down)}

                def load_piece(e, nm, k2):
                    s_ = e % 2
                    wt, src = WSRC[nm]
                    S.dma("pool", lambda: P_.dma_start(out=wt[s_][:, 2 * k2:2 * k2 + 2, :],
                                                       in_=src[e, k2 * 256:(k2 + 1) * 256, :].rearrange("(k p) n -> p k n", p=128)),
                          writes=[(nm, s_, 2 * k2), (nm, s_, 2 * k2 + 1)], grp="w", ngrp=9)

                WPLAN = [[("wg", 0), ("wu", 0)], [("wd", 0)], [("wg", 1), ("wu", 1)], [("wd", 1)],
                         [("wg", 2), ("wu", 2)], [("wd", 2)], [("wg", 3), ("wu", 3)], [("wd", 3)]]

                def load_wk(e, fj):
                    for nm, k2 in WPLAN[fj]:
                        load_piece(e, nm, k2)

                def load_w(e):
                    for fj in range(8):
                        load_wk(e, fj)

                xT2 = [xT, sb6("xTb", [128, 8, CAP], BF16)]

                def prep_idx(e):
                    S.dma("sp", lambda: nc.sync.dma_start(out=idxs[e % 2][:], in_=tokslot_d[e * CAP:(e + 1) * CAP, :].rearrange("(r p) o -> p (r o)", p=128),
                                                          allow_slow_non_contiguous=True), writes=[("idx", e % 2)])
                    S.dma("sp", lambda: nc.sync.dma_start(out=bdbc[e % 2][:], in_=b_down[e].partition_broadcast(128)), writes=[("bdbc", e % 2)])

                gcnt = [0]

                def prep_g(e, rt):
                    xs = rt % 2
                    S.dma("pool", lambda: P_.indirect_dma_start(out=xg[xs][:, :], out_offset=None, in_=h1b_d[:, :],
                                                                in_offset=IOA(ap=idxs[e % 2][:, rt:rt + 1], axis=0)),
                          reads=[("idx", e % 2)], writes=[("xg", xs)])

                def prep_t(e, rt):
                    xs = rt % 2
                    for k in range(8):
                        S.op("pe", lambda: T_.transpose(out=ptx[:, k, :], in_=xg[xs][:, k * 128:(k + 1) * 128], identity=ident[:]),
                             reads=[("xg", xs), "ident"], writes=["ptx"])
                    S.op("act", lambda: A_.copy(out=xT2[e % 2][:, :, rt * 128:(rt + 1) * 128], in_=ptx[:]), reads=["ptx"], writes=[("xT", e % 2)])

                def prep_rt(e, rt):
                    if rt + 1 < NRT:
                        prep_g(e, rt + 1)
                    prep_t(e, rt)

                load_w(0)
                S.waitfor("sp", sc_keys)
                S.waitfor("pool", h1_keys)
                prep_idx(0)
                prep_g(0, 0)
                for rt in range(NRT):
                    prep_rt(0, rt)
                cc = 0
                for e in range(NEXP):
                    s_ = e % 2
                    xTe = xT2[s_]
                    if e + 1 < NEXP:
                        prep_idx(e + 1)
                        prep_g(e + 1, 0)
                    nxt = list(range(NRT)) if e + 1 < NEXP else []
                    for fj in range(8):
                        for (c0, c1) in chunks:
                            b_ = cc % 2; cc += 1
                            n_ = c1 - c0
                            for (W, kw, P, kp) in ((wgs[s_], ("wg", s_), pgc[b_], ("pgc", b_)), (wus[s_], ("wu", s_), puc[b_], ("puc", b_))):
                                for k in range(8):
                                    S.op("pe", lambda: T_.matmul(P[:, 0:n_], lhsT=W[:, k, fj * 128:(fj + 1) * 128], rhs=xTe[:, k, c0:c1],
                                                                 start=(k == 0), stop=(k == 7)), reads=[kw + (k,), ("xT", s_)], writes=[kp])
                            gs, sg_, us = gsb[b_], sgs[b_], usb[b_]
                            S.op("dve", lambda: V.tensor_scalar(out=gs[:, 0:n_], in0=pgc[b_][:, 0:n_], scalar1=bgall[:, e, fj:fj + 1], scalar2=7.0,
                                                                op0=ALU.add, op1=ALU.min), reads=[("pgc", b_), ("bgall", e)], writes=[("gsb", b_)])
                            S.op("act", lambda: A_.activation(out=sg_[:, 0:n_], in_=gs[:, 0:n_], func=AF.Sigmoid, scale=1.702), reads=[("gsb", b_)], writes=[("sgs", b_)])
                            S.op("dve", lambda: V.tensor_scalar(out=us[:, 0:n_], in0=puc[b_][:, 0:n_], scalar1=buall[:, e, fj:fj + 1], scalar2=7.0,
                                                                op0=ALU.add, op1=ALU.min), reads=[("puc", b_), ("buall", e)], writes=[("usb", b_)])
                            S.op("dve", lambda: V.tensor_scalar(out=us[:, 0:n_], in0=us[:, 0:n_], scalar1=-7.0, scalar2=1.0, op0=ALU.max, op1=ALU.add),
                                 reads=[("usb", b_)], writes=[("usb", b_)])
                            S.op("dve", lambda: V.tensor_tensor(out=gs[:, 0:n_], in0=gs[:, 0:n_], in1=sg_[:, 0:n_], op=ALU.mult),
                                 reads=[("gsb", b_), ("sgs", b_)], writes=[("gsb", b_)])
                            S.op("dve", lambda: V.tensor_tensor(out=actT[:, fj, c0:c1], in0=gs[:, 0:n_], in1=us[:, 0:n_], op=ALU.mult),
                                 reads=[("gsb", b_), ("usb", b_)], writes=["actT"])
                        if e + 1 < NEXP:
                            load_wk(e + 1, fj)
                        if nxt:
                            prep_rt(e + 1, nxt.pop(0))
                    while nxt:
                        prep_rt(e + 1, nxt.pop(0))
                    for rt in range(NRT):
                        ys = rt % 2
                        for half in range(2):
                            for k in range(8):
                                S.op("pe", lambda: T_.matmul(pdh[half][:, :], lhsT=actT[:, k, rt * 128:(rt + 1) * 128],
                                                             rhs=wds[s_][:, k, half * 512:(half + 1) * 512], start=(k == 0), stop=(k == 7)),
                                     reads=["actT", ("wd", s_, k)], writes=[("pd", half)])
                            S.op("dve", lambda: V.tensor_tensor(out=yo[ys][:, half * 512:(half + 1) * 512], in0=pdh[half][:, :],
                                                                in1=bdbc[s_][:, half * 512:(half + 1) * 512], op=ALU.add),
                                 reads=[("pd", half), ("bdbc", s_)], writes=[("yo", ys, half)])
                        r0 = e * CAP + rt * 128
                        S.dma("sp", lambda: nc.sync.dma_start(out=ybuf_d[r0:r0 + 128, :], in_=yo[ys][:]), reads=[("yo", ys, 0), ("yo", ys, 1)],
                              writes=[("ybuf", e, rt)])
                        y_keys.append(("ybuf", e, rt))
                barrier()
            print("P2b done: inst", S.n_inst, "waits", S.n_wait, "sems", S.nsem, flush=True)

            with ExitStack() as st7:
                sb7, ps7 = mk(st7)
                h1c = [sb7(f"h1c{i}", [128, D], F32) for i in range(2)]
                yk = [[sb7(f"yk{i}_{k}", [128, D], F32) for k in range(4)] for i in range(2)]
                accs = [sb7(f"acc{i}", [128, D], F32) for i in range(2)]
                tmpc = sb7("tmpc", [128, D], F32)
                ot = [sb7(f"ot{i}", [128, D], F32) for i in range(2)]
                S.waitfor("pool", y_keys)

                def c_fetch(t2):
                    s2 = t2 % 2
                    S.dma("sp", lambda: nc.sync.dma_start(out=h1c[s2][:], in_=h1_d[t2 * 128:(t2 + 1) * 128, :]), reads=[("h1_d", t2)], writes=[("h1c", s2)])
                    for k in range(4):
                        S.dma("pool", lambda: P_.indirect_dma_start(out=yk[s2][k][:, :], out_offset=None, in_=ybuf_d[:, :],
                                                                    in_offset=IOA(ap=slot_ga[:, t2, k:k + 1], axis=0)),
                              reads=["slot_ga"], writes=[("yk", s2, k)])

                def c_comp(t2):
                    s2 = t2 % 2
                    ac = accs[s2]; ka = ("acc", s2)
                    S.op("act", lambda: A_.activation(out=ac[:], in_=h1c[s2][:], func=AF.Copy, scale=ALPHA), reads=[("h1c", s2)], writes=[ka])
                    for k in range(4):
                        S.op("dve", lambda: V.scalar_tensor_tensor(out=ac[:], in0=yk[s2][k][:], scalar=gate_all[:, t2, k:k + 1], in1=ac[:],
                                                                   op0=ALU.mult, op1=ALU.add), reads=[("yk", s2, k), "gate_all", ka], writes=[ka])
                    layernorm(ac, ka, g2, "g2", b2, "b2", tmpc, "tmpc", ot[s2][:], ("ot", s2), lnsb2[s2])
                    S.dma("sp", lambda: nc.sync.dma_start(out=out_d[t2 * 128:(t2 + 1) * 128, :], in_=ot[s2][:]), reads=[("ot", s2)], writes=[("out", t2)])

                c_fetch(0)
                for t2 in range(NT2):
                    if t2 + 1 < NT2:
                        c_fetch(t2 + 1)
                    c_comp(t2)
                barrier()
        for e in ("pe", "act", "dve", "pool", "sp"):
            S.wait_all(e)
    return nc


def make_maps(inp, NX, full=True):
    f = lambda a: np.ascontiguousarray(np.asarray(a, dtype=np.float32))
    NG = NX // 512
    NTOK2 = NX // 4
    NG2 = NTOK2 // 512
    XCH = min(1024, 128 * NG)
    w_in = np.asarray(inp["w_in"])[0]
    maps = []
    for c in range(8):
        b, r = c // 4, c % 4
        m = {}
        m["x_b"] = f(inp["x"][b, :NX])
        m["meta"] = f(inp["meta_tokens"])
        m["ln_in_g"] = f(inp["ln_in_g"]).reshape(1, D)
        m["ln_in_b"] = f(inp["ln_in_b"]).reshape(1, D)
        m["w4"] = f(np.concatenate([w_in[:, r * 128:(r + 1) * 128], w_in[:, 512 + r * 128:512 + (r + 1) * 128],
                                    w_in[:, 1024 + r * 128:1024 + (r + 1) * 128], w_in[:, 1536 + r * 128:1536 + (r + 1) * 128]], axis=1))
        m["relb"] = f(np.asarray(inp["rel_bias"])[:, r]).reshape(1, 32)
        m["lam4"] = f(np.stack([inp["lambda_q1"][0], inp["lambda_k1"][0], inp["lambda_q2"][0], inp["lambda_k2"][0]]))
        m["subln_g"] = f(inp["subln_g"][0]).reshape(1, 128)
        gs = slice(8 * r, 8 * r + 8)
        m["a_re"] = f(inp["a_re"][0][gs]).reshape(1, 512)
        m["a_im"] = f(inp["a_im"][0][gs]).reshape(1, 512)
        m["log_step"] = f(inp["log_step"][0][gs]).reshape(1, 8)
        m["b_re"] = f(inp["b_re"][0][gs]); m["b_im"] = f(inp["b_im"][0][gs])
        m["c_re"] = f(inp["c_re"][0][gs]); m["c_im"] = f(inp["c_im"][0][gs])
        m["d_skip"] = f(inp["d_skip"][0][128 * r:128 * r + 128]).reshape(1, 128)
        if full:
            m["x2"] = f(inp["x"][b, r * NTOK2:(r + 1) * NTOK2])
            gi = np.zeros((128, 8 * NG2), np.int32)
            p = np.arange(128)
            for tg in range(NG2):
                for j in range(4):
                    for half in range(2):
                        rho = (r * NG2 + tg) * 256 + half * 128 + p
                        gi[:, (tg * 4 + j) * 2 + half] = (rho // XCH) * 4 * XCH + j * XCH + rho % XCH
            m["gidx"] = gi
            m["w_glu"] = f(inp["w_glu"][0]); m["b_glu"] = f(inp["b_glu"][0]).reshape(1, 512)
            m["w_out"] = f(inp["w_out"][0])
            m["ln1_g"] = f(inp["ln1_g"][0]).reshape(1, D); m["ln1_b"] = f(inp["ln1_b"][0]).reshape(1, D)
            m["w_router"] = f(inp["w_router"][0]); m["b_router"] = f(inp["b_router"][0]).reshape(1, NEXP)
            m["w_gate"] = f(inp["w_gate"][0]); m["b_gate"] = f(inp["b_gate"][0])
            m["w_up"] = f(inp["w_up"][0]); m["b_up"] = f(inp["b_up"][0])
            m["w_down"] = f(inp["w_down"][0]); m["b_down"] = f(inp["b_down"][0])
            m["ln2_g"] = f(inp["ln2_g"][0]).reshape(1, D); m["ln2_b"] = f(inp["ln2_b"][0]).reshape(1, D)
        maps.append(m)
    return maps


def kernel(**inputs):
    NX = int(np.asarray(inputs["x"]).shape[1])
    CAP = 768 if NX >= 16384 else 128 * max(1, int(math.ceil(1.5 * NX / 8 / 128)))
    nc = build(NX, CAP, debug=False, full=True)
    maps = make_maps(inputs, NX, full=True)
    res = run_bass_kernel_spmd(nc, maps, core_ids=list(range(8)))
    NTOK2 = NX // 4
    out = np.zeros((2, NX, D), np.float32)
    for c in range(8):
        b, r = c // 4, c % 4
        out[b, r * NTOK2:(r + 1) * NTOK2] = np.asarray(res.results[c]["out"], dtype=np.float32)
    return out
```

```python
import math
from contextlib import ExitStack

import numpy as np
import concourse.bass as bass
import concourse.mybir as mybir
from concourse.bass_utils import run_bass_kernel_spmd

F32 = mybir.dt.float32
BF16 = mybir.dt.bfloat16
I32 = mybir.dt.int32
AF = mybir.ActivationFunctionType
ALU = mybir.AluOpType
AX = mybir.AxisListType

D = 1024
NEXP = 32
LN_EPS = 1e-5
ALPHA = 2.0 ** 0.25
NEG = -30000.0
EPOCH = 30000


class Sync:
    def __init__(self, nc, stack, n_dma_sems=10):
        self.nc = nc
        self.stack = stack
        self.eng = {"pe": nc.tensor, "act": nc.scalar, "dve": nc.vector,
                    "pool": nc.gpsimd, "sp": nc.sync}
        self.sems = {}
        self.nsem = 0
        self.cur = {e: [self._new_sem(), 0] for e in self.eng}
        self.seen = {e: {} for e in self.eng}
        self.lastw = {}
        self.readers = {}
        self.dma_pool = {}
        self.dma_rr = {}
        for q in ("sp", "pool", "act"):
            self.dma_pool[q] = [[self._new_sem(), 0] for _ in range(n_dma_sems)]
            self.dma_rr[q] = 0
        self.n_inst = 0
        self.n_wait = 0

    def _new_sem(self):
        sid = self.nsem
        self.nsem += 1
        self.sems[sid] = self.stack.enter_context(self.nc.semaphore(f"s{sid}"))
        return sid

    def _wait(self, e, ticket):
        sid, val, owner = ticket
        if owner == e and e == "pe":
            return
        if self.seen[e].get(sid, 0) >= val:
            return
        self.eng[e].wait_ge(self.sems[sid], val)
        self.seen[e][sid] = val
        self.n_wait += 1

    def _deps(self, e, reads, writes):
        for b in list(reads) + list(writes):
            t = self.lastw.get(b)
            if t is not None:
                self._wait(e, t)
        for b in writes:
            for t in self.readers.get(b, ()):
                self._wait(e, t)

    def _commit(self, ticket, reads, writes):
        for b in writes:
            self.lastw[b] = ticket
            self.readers[b] = []
        for b in reads:
            lst = self.readers.setdefault(b, [])
            lst.append(ticket)
            if len(lst) > 24:
                best = {}
                for t in lst:
                    if t[0] not in best or best[t[0]][1] < t[1]:
                        best[t[0]] = t
                self.readers[b] = list(best.values())

    def op(self, e, fn, reads=(), writes=()):
        self._deps(e, reads, writes)
        c = self.cur[e]
        if c[1] >= EPOCH:
            c[0] = self._new_sem()
            c[1] = 0
        ins = fn()
        c[1] += 1
        ins.then_inc(self.sems[c[0]], 1)
        ticket = (c[0], c[1], e)
        self._commit(ticket, reads, writes)
        self.n_inst += 1
        return ticket

    def dma(self, q, fn, reads=(), writes=(), grp=None, ngrp=6):
        self._deps(q, reads, writes)
        if grp is None:
            pk = q
        else:
            pk = (q, grp)
            if pk not in self.dma_pool:
                self.dma_pool[pk] = [[self._new_sem(), 0] for _ in range(ngrp)]
                self.dma_rr[pk] = 0
        pool = self.dma_pool[pk]
        i = self.dma_rr[pk]
        self.dma_rr[pk] = (i + 1) % len(pool)
        slot = pool[i]
        if slot[1] > 0:
            self._wait(q, (slot[0], slot[1], "dma"))
        if slot[1] >= EPOCH:
            slot[0] = self._new_sem()
            slot[1] = 0
        ins = fn()
        slot[1] += 16
        ins.then_inc(self.sems[slot[0]], 16)
        ticket = (slot[0], slot[1], "dma")
        self._commit(ticket, reads, writes)
        self.n_inst += 1
        return ticket

    def waitfor(self, e, keys):
        for b in keys:
            t = self.lastw.get(b)
            if t is not None:
                self._wait(e, t)

    def wait_all(self, e):
        for b, t in list(self.lastw.items()):
            self._wait(e, t)
        for q, pool in self.dma_pool.items():
            for slot in pool:
                if slot[1] > 0:
                    self._wait(e, (slot[0], slot[1], "dma"))


def _bucket_thresholds():
    n = np.arange(1, 400, dtype=np.int32)
    n_f = np.maximum(n, 1).astype(np.float32)
    large = 8 + (np.log(n_f / np.float32(8)) / np.float32(math.log(128 / 8)) * np.float32(8)).astype(np.int32)
    large = np.minimum(large, 15)
    bucket = np.where(n < 8, n, large)
    thr = {}
    for j in range(9, 16):
        thr[j] = int(n[np.argmax(bucket >= j)])
    return thr


def build(NX, CAP, debug=False, full=True, stop_after=None):
    nc = bass.Bass("TRN2", target_bir_lowering=False)
    NT = NX // 128 + 1
    NCOL = NT * 128
    NG = NX // 512
    NTOK2 = NX // 4
    NT2 = NTOK2 // 128
    NG2 = NTOK2 // 512
    NRT = CAP // 128
    NSLOT = NEXP * CAP

    def din(name, shape, dt=F32):
        return nc.dram_tensor(name, list(shape), dt, kind="ExternalInput").ap()

    x_b = din("x_b", [NX, D])
    meta = din("meta", [16, D])
    ln_in_g = din("ln_in_g", [1, D]); ln_in_b = din("ln_in_b", [1, D])
    w4 = din("w4", [D, 512])
    relb = din("relb", [1, 32])
    lam4 = din("lam4", [4, 64])
    subln_g = din("subln_g", [1, 128])
    a_re = din("a_re", [1, 512]); a_im = din("a_im", [1, 512]); log_step = din("log_step", [1, 8])
    b_re = din("b_re", [8, 64, 16]); b_im = din("b_im", [8, 64, 16])
    c_re = din("c_re", [8, 16, 64]); c_im = din("c_im", [8, 16, 64])
    d_skip = din("d_skip", [1, 128])
    if full:
        x2 = din("x2", [NTOK2, D])
        gidx = din("gidx", [128, 8 * NG2], I32)
        w_glu = din("w_glu", [512, 512]); b_glu = din("b_glu", [1, 512])
        w_out = din("w_out", [D, D])
        ln1_g = din("ln1_g", [1, D]); ln1_b = din("ln1_b", [1, D])
        w_router = din("w_router", [D, NEXP]); b_router = din("b_router", [1, NEXP])
        w_gate = din("w_gate", [NEXP, D, D]); b_gate = din("b_gate", [NEXP, D])
        w_up = din("w_up", [NEXP, D, D]); b_up = din("b_up", [NEXP, D])
        w_down = din("w_down", [NEXP, D, D]); b_down = din("b_down", [NEXP, D])
        ln2_g = din("ln2_g", [1, D]); ln2_b = din("ln2_b", [1, D])
        out_d = nc.dram_tensor("out", [NTOK2, D], F32, kind="ExternalOutput").ap()

    slab = nc.dram_tensor("slab", [256 * NG, 512], BF16).ap()
    gath = nc.dram_tensor("gath", [4 * 256 * NG, 512], BF16).ap()
    slab3 = slab.rearrange("(g f) c -> f g c", f=256)
    vaug_d = nc.dram_tensor("vaug_d", [128, NT, 129], BF16).ap()
    h1_d = nc.dram_tensor("h1_d", [NTOK2, D], F32).ap()
    h1b_d = nc.dram_tensor("h1b_d", [NTOK2, D], BF16).ap()
    tokslot_d = nc.dram_tensor("tokslot_d", [NSLOT + 128, 1], I32).ap()
    ybuf_d = nc.dram_tensor("ybuf_d", [NSLOT, D], F32).ap()
    dbg = {}
    if debug:
        dbg["qT"] = nc.dram_tensor("dbg_qT", [128, NCOL], BF16, kind="ExternalOutput").ap()
        dbg["kT"] = nc.dram_tensor("dbg_kT", [128, NCOL], BF16, kind="ExternalOutput").ap()
        dbg["uT"] = nc.dram_tensor("dbg_uT", [128, NCOL], BF16, kind="ExternalOutput").ap()
        dbg["v"] = nc.dram_tensor("dbg_v", [128, NT, 129], BF16, kind="ExternalOutput").ap()
        dbg["slab"] = nc.dram_tensor("dbg_slab", [256 * NG, 512], BF16, kind="ExternalOutput").ap()
        dbg["Wb"] = nc.dram_tensor("dbg_Wb", [128, 1024], F32, kind="ExternalOutput").ap()
        dbg["ident"] = nc.dram_tensor("dbg_ident", [128, 128], BF16, kind="ExternalOutput").ap()
        dbg["ob"] = nc.dram_tensor("dbg_ob", [128, 4, 128], BF16, kind="ExternalOutput").ap()
        dbg["g08"] = nc.dram_tensor("dbg_g08", [128, 128], F32, kind="ExternalOutput").ap()
        dbg["po"] = nc.dram_tensor("dbg_po", [128, 3, 512], F32, kind="ExternalOutput").ap()
        dbg["rr"] = nc.dram_tensor("dbg_rr", [128, 8], F32, kind="ExternalOutput").ap()
        dbg["pT"] = nc.dram_tensor("dbg_pT", [128, 2, 512], BF16, kind="ExternalOutput").ap()
        if full:
            dbg["h1"] = nc.dram_tensor("dbg_h1", [NTOK2, D], F32, kind="ExternalOutput").ap()
            dbg["lg"] = nc.dram_tensor("dbg_lg", [NTOK2, 32], F32, kind="ExternalOutput").ap()

    thr = _bucket_thresholds()

    with ExitStack() as st0:
        S = Sync(nc, st0)
        V, A_, P_, T_ = nc.vector, nc.scalar, nc.gpsimd, nc.tensor

        def mk(stack):
            def sb(name, shape, dt=F32):
                return stack.enter_context(nc.sbuf_tensor(name, list(shape), dt))

            def ps(name, shape, dt=F32):
                return stack.enter_context(nc.psum_tensor(name, list(shape), dt))
            return sb, ps

        sb0, ps0 = mk(st0)

        ident = sb0("ident", [128, 128], BF16)
        identf = sb0("identf", [128, 128], F32)
        tri = sb0("tri", [128, 128], BF16)
        stri = sb0("stri", [128, 128], BF16)
        ones = sb0("ones", [128, 128], BF16)
        iop = sb0("iop", [128, 1], F32)
        iop_i = sb0("iop_i", [128, 1], I32)
        S.op("pool", lambda: P_.memset(identf[:], 0.0), writes=["identf"])
        S.op("pool", lambda: P_.affine_select(out=identf[:], in_=identf[:], pattern=[[-1, 128]],
                                              compare_op=ALU.not_equal, fill=1.0, base=0, channel_multiplier=1),
             reads=["identf"], writes=["identf"])
        S.op("dve", lambda: V.tensor_copy(out=ident[:], in_=identf[:]), reads=["identf"], writes=["ident"])
        S.op("pool", lambda: P_.memset(ones[:], 1.0), writes=["ones"])
        S.op("pool", lambda: P_.affine_select(out=tri[:], in_=ones[:], pattern=[[1, 128]],
                                              compare_op=ALU.is_ge, fill=0.0, base=0, channel_multiplier=-1),
             reads=["ones"], writes=["tri"])
        S.op("pool", lambda: P_.affine_select(out=stri[:], in_=ones[:], pattern=[[1, 128]],
                                              compare_op=ALU.is_gt, fill=0.0, base=0, channel_multiplier=-1),
             reads=["ones"], writes=["stri"])
        S.op("pool", lambda: P_.iota(iop_i[:], pattern=[[0, 1]], base=0, channel_multiplier=1), writes=["iop_i"])
        S.op("dve", lambda: V.tensor_copy(out=iop[:], in_=iop_i[:]), reads=["iop_i"], writes=["iop"])

        def bc_load(name, src, n, stack_sb, q="sp"):
            t = stack_sb(name, [128, n], F32)
            S.dma(q, lambda: nc.sync.dma_start(out=t[:], in_=src.partition_broadcast(128)), writes=[name])
            return t

        lng = bc_load("lng", ln_in_g[0], D, sb0)
        lnb = bc_load("lnb", ln_in_b[0], D, sb0)

        cnt = {"ln": 0}

        def layernorm(xt, kx, g_bc, kg, b_bc, kb, tmp, kt, out_ap, kout, sbs):
            i = cnt["ln"]; cnt["ln"] += 1
            st6, mv, rstd, nmr = sbs
            ks = ("lnstat", id(st6))
            S.op("dve", lambda: V.bn_stats(out=st6[:, 0:6], in_=xt[:, 0:512]), reads=[kx], writes=[ks])
            S.op("dve", lambda: V.bn_stats(out=st6[:, 6:12], in_=xt[:, 512:1024]), reads=[kx], writes=[ks])
            S.op("dve", lambda: V.bn_aggr(out=mv[:, 0:2], in_=st6[:, 0:12]), reads=[ks], writes=[ks])
            S.op("dve", lambda: V.tensor_scalar(out=rstd[:], in0=mv[:, 1:2], scalar1=LN_EPS, scalar2=None, op0=ALU.add), reads=[ks], writes=[ks])
            S.op("dve", lambda: V.tensor_scalar(out=nmr[:], in0=mv[:, 0:1], scalar1=-1.0, scalar2=None, op0=ALU.mult), reads=[ks], writes=[ks])
            S.op("act", lambda: A_.activation(out=rstd[:], in_=rstd[:], func=AF.Ln), reads=[ks], writes=[ks])
            S.op("act", lambda: A_.activation(out=rstd[:], in_=rstd[:], func=AF.Exp, scale=-0.5), reads=[ks], writes=[ks])
            S.op("act", lambda: A_.activation(out=nmr[:], in_=nmr[:], func=AF.Copy, scale=rstd[:, 0:1]), reads=[ks], writes=[ks])
            S.op("act", lambda: A_.activation(out=tmp[:], in_=xt[:], func=AF.Identity, bias=nmr[:, 0:1], scale=rstd[:, 0:1]),
                 reads=[kx, ks], writes=[kt])
            S.op("dve", lambda: V.tensor_tensor(out=tmp[:], in0=tmp[:], in1=g_bc[:], op=ALU.mult), reads=[kt, kg], writes=[kt])
            S.op("dve", lambda: V.tensor_tensor(out=out_ap, in0=tmp[:], in1=b_bc[:], op=ALU.add), reads=[kt, kb], writes=[kout])

        st_att = ExitStack()
        sbA, psA = mk(st_att)
        qT = sbA("qT", [128, NCOL], BF16)
        kT = sbA("kT", [128, NCOL], BF16)
        st_u = ExitStack()
        sbU, _ = mk(st_u)
        uT = sbU("uT", [128, NCOL], BF16)

        st3 = ExitStack()
        sb, ps = mk(st3)
        PI = math.pi
        Tnr = sb("Tnr", [128, 512], F32); Tni = sb("Tni", [128, 512], F32)
        Ppr = sb("Ppr", [128, 4, 128], F32); Ppi = sb("Ppi", [128, 4, 128], F32)
        Bblk = sb("Bblk", [128, 1024], BF16)
        Cmat = sb("Cmat", [128, 8, 128], BF16)
        dsk = sb("dsk", [128, 1], F32)
        S.dma("sp", lambda: nc.sync.dma_start(out=dsk[:], in_=d_skip.rearrange("o (p q) -> (o p) q", q=1)), writes=["dsk"])
        negpi = sb("negpi", [128, 1], F32)
        S.op("dve", lambda: V.memset(negpi[:], -PI), writes=["negpi"])
        with ExitStack() as stt:
            sbt, _ = mk(stt)
            are = bc_load("are", a_re[0], 512, sbt)
            aim = bc_load("aim", a_im[0], 512, sbt)
            ls8 = bc_load("ls8", log_step[0], 8, sbt)
            S.op("act", lambda: A_.activation(out=ls8[:], in_=ls8[:], func=AF.Exp), reads=["ls8"], writes=["ls8"])
            S.op("dve", lambda: V.tensor_scalar(out=are[:], in0=are[:], scalar1=-1e-4, scalar2=None, op0=ALU.min), reads=["are"], writes=["are"])
            sa = sbt("sa", [128, 512], F32); sp_ = sbt("sp_", [128, 512], F32)
            st8 = ls8[:, 0:8].unsqueeze(2).to_broadcast([128, 8, 64])
            S.op("dve", lambda: V.tensor_tensor(out=sa[:].rearrange("p (g n) -> p g n", n=64), in0=are[:].rearrange("p (g n) -> p g n", n=64),
                                                in1=st8, op=ALU.mult), reads=["are", "ls8"], writes=["sa"])
            S.op("dve", lambda: V.tensor_tensor(out=sp_[:].rearrange("p (g n) -> p g n", n=64), in0=aim[:].rearrange("p (g n) -> p g n", n=64),
                                                in1=st8, op=ALU.mult), reads=["aim", "ls8"], writes=["sp_"])
            t_a = sbt("t_a", [128, 512], F32); t_b = sbt("t_b", [128, 512], F32); t_c = sbt("t_c", [128, 512], F32)
            t_d = sbt("t_d", [128, 512], F32); t_e = sbt("t_e", [128, 512], F32)
            sp1 = sbt("sp1", [128, 1], F32); nsp1 = sbt("nsp1", [128, 1], F32)
            S.op("dve", lambda: V.tensor_scalar(out=sp1[:], in0=iop[:], scalar1=1.0, scalar2=None, op0=ALU.add), reads=["iop"], writes=["sp1"])
            S.op("dve", lambda: V.tensor_scalar(out=nsp1[:], in0=sp1[:], scalar1=-1.0, scalar2=None, op0=ALU.mult), reads=["sp1"], writes=["nsp1"])

            rki = sbt("rki", [128, 512], I32)
            rkf = sbt("rkf", [128, 512], F32)
            C1 = 6.28125
            C2 = 2 * PI - C1

            def reduce_sin(ph, kph, shift, out_ap, kout, scratch, ksc):
                n = ph.shape[-1]
                S.op("dve", lambda: V.tensor_scalar(out=scratch, in0=ph, scalar1=shift, scalar2=1.0 / (2 * PI), op0=ALU.add, op1=ALU.mult),
                     reads=[kph], writes=[ksc])
                S.op("dve", lambda: V.tensor_copy(out=rki[:, 0:n], in_=scratch), reads=[ksc], writes=["rki"])
                S.op("dve", lambda: V.tensor_copy(out=rkf[:, 0:n], in_=rki[:, 0:n]), reads=["rki"], writes=["rkf"])
                S.op("dve", lambda: V.tensor_scalar(out=scratch, in0=ph, scalar1=shift, scalar2=None, op0=ALU.add), reads=[kph], writes=[ksc])
                S.op("dve", lambda: V.scalar_tensor_tensor(out=scratch, in0=rkf[:, 0:n], scalar=-C1, in1=scratch, op0=ALU.mult, op1=ALU.add),
                     reads=["rkf", ksc], writes=[ksc])
                S.op("dve", lambda: V.scalar_tensor_tensor(out=scratch, in0=rkf[:, 0:n], scalar=-C2, in1=scratch, op0=ALU.mult, op1=ALU.add),
                     reads=["rkf", ksc], writes=[ksc])
                S.op("dve", lambda: V.tensor_scalar(out=rkf[:, 0:n], in0=scratch, scalar1=PI, scalar2=-2 * PI, op0=ALU.is_gt, op1=ALU.mult),
                     reads=[ksc], writes=["rkf"])
                S.op("dve", lambda: V.tensor_tensor(out=scratch, in0=scratch, in1=rkf[:, 0:n], op=ALU.add), reads=["rkf", ksc], writes=[ksc])
                S.op("dve", lambda: V.tensor_scalar(out=rkf[:, 0:n], in0=scratch, scalar1=-PI, scalar2=2 * PI, op0=ALU.is_lt, op1=ALU.mult),
                     reads=[ksc], writes=["rkf"])
                S.op("dve", lambda: V.tensor_tensor(out=scratch, in0=scratch, in1=rkf[:, 0:n], op=ALU.add), reads=["rkf", ksc], writes=[ksc])
                S.op("act", lambda: A_.activation(out=out_ap, in_=scratch, func=AF.Sin), reads=[ksc], writes=[kout])

            def sincos(ph, kph, out_s, ks, out_c, kc, scratch, ksc):
                reduce_sin(ph, kph, 0.0, out_s, ks, scratch, ksc)
                reduce_sin(ph, kph, 0.5 * PI, out_c, kc, scratch, ksc)

            S.op("dve", lambda: V.tensor_scalar(out=t_a[:], in0=sa[:], scalar1=nsp1[:, 0:1], scalar2=None, op0=ALU.mult), reads=["sa", "nsp1"], writes=["t_a"])
            S.op("act", lambda: A_.activation(out=t_a[:], in_=t_a[:], func=AF.Exp), reads=["t_a"], writes=["t_a"])
            S.op("dve", lambda: V.tensor_scalar(out=t_b[:], in0=sp_[:], scalar1=sp1[:, 0:1], scalar2=None, op0=ALU.mult), reads=["sp_", "sp1"], writes=["t_b"])
            sincos(t_b[:], "t_b", t_c[:], "t_c", t_d[:], "t_d", t_e[:], "t_e")
            S.op("dve", lambda: V.tensor_tensor(out=Tnr[:], in0=t_a[:], in1=t_d[:], op=ALU.mult), reads=["t_a", "t_d"], writes=["Tnr"])
            S.op("dve", lambda: V.scalar_tensor_tensor(out=Tni[:], in0=t_a[:], scalar=-1.0, in1=t_c[:], op0=ALU.mult, op1=ALU.mult),
                 reads=["t_a", "t_c"], writes=["Tni"])
            S.op("act", lambda: A_.activation(out=t_a[:], in_=sa[:], func=AF.Exp), reads=["sa", "Tnr", "Tni"], writes=["t_a"])
            sincos(sp_[:], "sp_", t_c[:], "t_c", t_d[:], "t_d", t_e[:], "t_e")
            S.op("dve", lambda: V.tensor_tensor(out=t_d[:], in0=t_a[:], in1=t_d[:], op=ALU.mult), reads=["t_a", "t_d"], writes=["t_d"])
            S.op("dve", lambda: V.tensor_scalar(out=t_d[:], in0=t_d[:], scalar1=-1.0, scalar2=None, op0=ALU.add), reads=["t_d"], writes=["t_d"])
            S.op("dve", lambda: V.tensor_tensor(out=t_c[:], in0=t_a[:], in1=t_c[:], op=ALU.mult), reads=["t_a", "t_c"], writes=["t_c"])
            S.op("dve", lambda: V.tensor_tensor(out=t_a[:], in0=are[:], in1=are[:], op=ALU.mult), reads=["are", "t_c", "t_d"], writes=["t_a"])
            S.op("dve", lambda: V.tensor_tensor(out=t_b[:], in0=aim[:], in1=aim[:], op=ALU.mult), reads=["aim"], writes=["t_b"])
            S.op("dve", lambda: V.tensor_tensor(out=t_a[:], in0=t_a[:], in1=t_b[:], op=ALU.add), reads=["t_a", "t_b"], writes=["t_a"])
            S.op("dve", lambda: V.reciprocal(out=t_a[:], in_=t_a[:]), reads=["t_a"], writes=["t_a"])
            S.op("dve", lambda: V.tensor_tensor(out=t_b[:], in0=t_d[:], in1=are[:], op=ALU.mult), reads=["t_d", "are"], writes=["t_b"])
            S.op("dve", lambda: V.tensor_tensor(out=t_e[:], in0=t_c[:], in1=aim[:], op=ALU.mult), reads=["t_c", "aim"], writes=["t_e"])
            S.op("dve", lambda: V.tensor_tensor(out=t_b[:], in0=t_b[:], in1=t_e[:], op=ALU.add), reads=["t_b", "t_e"], writes=["t_b"])
            S.op("dve", lambda: V.tensor_tensor(out=t_b[:], in0=t_b[:], in1=t_a[:], op=ALU.mult), reads=["t_b", "t_a"], writes=["t_b"])
            S.op("dve", lambda: V.tensor_tensor(out=t_e[:], in0=t_c[:], in1=are[:], op=ALU.mult), reads=["t_c", "are", "t_b"], writes=["t_e"])
            S.op("dve", lambda: V.tensor_tensor(out=t_c[:], in0=t_d[:], in1=aim[:], op=ALU.mult), reads=["t_d", "aim", "t_e"], writes=["t_c"])
            S.op("dve", lambda: V.tensor_tensor(out=t_e[:], in0=t_e[:], in1=t_c[:], op=ALU.subtract), reads=["t_e", "t_c"], writes=["t_e"])
            S.op("dve", lambda: V.tensor_tensor(out=t_e[:], in0=t_e[:], in1=t_a[:], op=ALU.mult), reads=["t_e", "t_a"], writes=["t_e"])
            Brr = sbt("Brr", [128, 512], F32); Bri = sbt("Bri", [128, 512], F32)
            S.op("pool", lambda: P_.memset(Brr[:], 0.0), writes=["Brr"])
            S.op("pool", lambda: P_.memset(Bri[:], 0.0), writes=["Bri"])
            for gi in range(8):
                S.dma("sp", lambda: nc.sync.dma_start(out=Brr[16 * gi:16 * gi + 16, 64 * gi:64 * gi + 64], in_=b_re[gi].rearrange("n c -> c n"),
                                                      allow_slow_non_contiguous=True), reads=["Brr"], writes=[("Brr", gi)])
                S.dma("sp", lambda: nc.sync.dma_start(out=Bri[16 * gi:16 * gi + 16, 64 * gi:64 * gi + 64], in_=b_im[gi].rearrange("n c -> c n"),
                                                      allow_slow_non_contiguous=True), reads=["Bri"], writes=[("Bri", gi)])
            bk = [("Brr", gi) for gi in range(8)] + [("Bri", gi) for gi in range(8)]
            S.op("dve", lambda: V.tensor_tensor(out=t_a[:], in0=t_b[:], in1=Brr[:], op=ALU.mult), reads=["t_b", "t_a"] + bk, writes=["t_a"])
            S.op("dve", lambda: V.tensor_tensor(out=t_c[:], in0=t_e[:], in1=Bri[:], op=ALU.mult), reads=["t_e", "t_c"] + bk, writes=["t_c"])
            S.op("dve", lambda: V.tensor_tensor(out=Bblk[:, 0:512], in0=t_a[:], in1=t_c[:], op=ALU.subtract), reads=["t_a", "t_c"], writes=["Bblk"])
            S.op("dve", lambda: V.tensor_tensor(out=t_a[:], in0=t_b[:], in1=Bri[:], op=ALU.mult), reads=["t_b", "t_a", "Bblk"] + bk, writes=["t_a"])
            S.op("dve", lambda: V.tensor_tensor(out=t_c[:], in0=t_e[:], in1=Brr[:], op=ALU.mult), reads=["t_e", "t_c", "Bblk"] + bk, writes=["t_c"])
            S.op("dve", lambda: V.tensor_tensor(out=Bblk[:, 512:1024], in0=t_a[:], in1=t_c[:], op=ALU.add), reads=["t_a", "t_c"], writes=["Bblk"])
            Cst = sbt("Cst", [128, 8, 128], F32)
            S.op("pool", lambda: P_.memset(Cst[:], 0.0), writes=["Cst"])
            ck = []
            for gi in range(8):
                k, gl = gi // 2, gi % 2
                S.dma("sp", lambda: nc.sync.dma_start(out=Cst[64 * gl:64 * gl + 64, k, 16 * gi:16 * gi + 16], in_=c_re[gi].rearrange("c n -> n c"),
                                                      allow_slow_non_contiguous=True), reads=["Cst"], writes=[("Cst", gi, 0)])
                S.dma("sp", lambda: nc.sync.dma_start(out=Cst[64 * gl:64 * gl + 64, 4 + k, 16 * gi:16 * gi + 16], in_=c_im[gi].rearrange("c n -> n c"),
                                                      allow_slow_non_contiguous=True), reads=["Cst"], writes=[("Cst", gi, 1)])
                ck += [("Cst", gi, 0), ("Cst", gi, 1)]
            S.op("dve", lambda: V.tensor_copy(out=Cmat[:, 0:4, :], in_=Cst[:, 0:4, :]), reads=ck, writes=["Cmat"])
            S.op("dve", lambda: V.tensor_scalar(out=Cmat[:, 4:8, :], in0=Cst[:, 4:8, :], scalar1=-1.0, scalar2=None, op0=ALU.mult), reads=ck, writes=["Cmat"])
            arc = sbt("arc", [128, 4], F32); aic = sbt("aic", [128, 4], F32); stc = sbt("stc", [128, 4], F32)
            S.dma("sp", lambda: nc.sync.dma_start(out=arc[:], in_=a_re.rearrange("o (k p) -> (o p) k", p=128), allow_slow_non_contiguous=True), writes=["arc"])
            S.dma("sp", lambda: nc.sync.dma_start(out=aic[:], in_=a_im.rearrange("o (k p) -> (o p) k", p=128), allow_slow_non_contiguous=True), writes=["aic"])
            for gi in range(8):
                k, gl = gi // 2, gi % 2
                S.dma("sp", lambda: nc.sync.dma_start(out=stc[64 * gl:64 * gl + 64, k:k + 1], in_=log_step[0, gi:gi + 1].partition_broadcast(64)),
                      writes=[("stc", gi)])
            S.op("act", lambda: A_.activation(out=stc[:], in_=stc[:], func=AF.Exp), reads=[("stc", gi) for gi in range(8)], writes=["stc"])
            S.op("dve", lambda: V.tensor_scalar(out=arc[:], in0=arc[:], scalar1=-1e-4, scalar2=None, op0=ALU.min), reads=["arc"], writes=["arc"])
            S.op("dve", lambda: V.tensor_tensor(out=arc[:], in0=arc[:], in1=stc[:], op=ALU.mult), reads=["arc", "stc"], writes=["arc"])
            S.op("dve", lambda: V.tensor_tensor(out=aic[:], in0=aic[:], in1=stc[:], op=ALU.mult), reads=["aic", "stc"], writes=["aic"])
            tp1i = sbt("tp1i", [128, 128], I32); tp1 = sbt("tp1", [128, 128], F32)
            S.op("pool", lambda: P_.iota(tp1i[:], pattern=[[1, 128]], base=1, channel_multiplier=0), writes=["tp1i"])
            S.op("dve", lambda: V.tensor_copy(out=tp1[:], in_=tp1i[:]), reads=["tp1i"], writes=["tp1"])
            for k in range(4):
                S.op("dve", lambda: V.tensor_scalar(out=t_a[:, 0:128], in0=tp1[:], scalar1=arc[:, k:k + 1], scalar2=None, op0=ALU.mult),
                     reads=["tp1", "arc", "Bblk"], writes=["t_a"])
                S.op("act", lambda: A_.activation(out=t_a[:, 0:128], in_=t_a[:, 0:128], func=AF.Exp), reads=["t_a"], writes=["t_a"])
                S.op("dve", lambda: V.tensor_scalar(out=t_b[:, 0:128], in0=tp1[:], scalar1=aic[:, k:k + 1], scalar2=None, op0=ALU.mult),
                     reads=["tp1", "aic", "Bblk"], writes=["t_b"])
                sincos(t_b[:, 0:128], "t_b", t_c[:, 0:128], "t_c", t_d[:, 0:128], "t_d", t_e[:, 0:128], "t_e")
                S.op("dve", lambda: V.tensor_tensor(out=Ppr[:, k, :], in0=t_a[:, 0:128], in1=t_d[:, 0:128], op=ALU.mult), reads=["t_a", "t_d"], writes=["Ppr"])
                S.op("dve", lambda: V.tensor_tensor(out=Ppi[:, k, :], in0=t_a[:, 0:128], in1=t_c[:, 0:128], op=ALU.mult), reads=["t_a", "t_c"], writes=["Ppi"])
            for e in ("pe", "act", "dve", "pool", "sp"):
                S.wait_all(e)

        pbu = ps("pbu", [128, 2, 512], F32)
        pz = [ps("pz0", [128, 8, 128], F32)] * 2
        py = ps("py", [128, 512], F32)
        wq = [[sb(f"w{k}_{i}", [128, 512], F32) for k in range(4)] for i in range(2)]
        Wbs = [sb(f"Wbs{i}", [128, 1024], BF16) for i in range(2)]
        zp = sb("zp", [128, 8, 128], F32)
        xq = [[sb(f"x{k}_0", [128, 4, 128], F32) for k in range(4)]] * 2
        XTs = [sb(f"XT{i}", [128, 8, 128], BF16) for i in range(2)]
        car = [sb(f"car{i}", [128, 8], F32) for i in range(2)]
        yf = sb("yf", [128, 512], F32); y2 = sb("y2", [128, 512], F32); ysg = sb("ysg", [128, 512], F32)
        ybs = [sb(f"yb{i}", [128, 512], BF16) for i in range(2)]
        S.op("dve", lambda: V.memset(car[0][:], 0.0), writes=[("car", 0)])

        def bu(ct):
            for half in range(2):
                S.op("pe", lambda: T_.matmul(pbu[:, half, :], lhsT=uT[:, ct * 128:(ct + 1) * 128], rhs=Bblk[:, half * 512:(half + 1) * 512],
                                             start=True, stop=True), reads=[("uT", (ct + 3) // 4), "Bblk"], writes=["pbu"])

        def wmod(ct):
            b2 = ct % 2
            w1, w2, w3, w4_ = wq[b2]
            kw = ("wq", b2)
            S.op("dve", lambda: V.tensor_tensor(out=w1[:], in0=pbu[:, 0, :], in1=Tnr[:], op=ALU.mult), reads=["pbu", "Tnr"], writes=[kw])
            S.op("dve", lambda: V.tensor_tensor(out=w2[:], in0=pbu[:, 1, :], in1=Tni[:], op=ALU.mult), reads=["pbu", "Tni"], writes=[kw])
            S.op("dve", lambda: V.tensor_tensor(out=w3[:], in0=pbu[:, 1, :], in1=Tnr[:], op=ALU.mult), reads=["pbu", "Tnr"], writes=[kw])
            S.op("dve", lambda: V.tensor_tensor(out=w4_[:], in0=pbu[:, 0, :], in1=Tni[:], op=ALU.mult), reads=["pbu", "Tni"], writes=[kw])
            S.op("pool", lambda: P_.tensor_tensor(out=Wbs[b2][:, 0:512], in0=w1[:], in1=w2[:], op=ALU.subtract), reads=[kw], writes=[("Wbs", b2)])
            S.op("pool", lambda: P_.tensor_tensor(out=Wbs[b2][:, 512:1024], in0=w3[:], in1=w4_[:], op=ALU.add), reads=[kw], writes=[("Wbs", b2)])

        def zmm(ct):
            b2 = ct % 2
            for k in range(8):
                S.op("pe", lambda: T_.matmul(pz[b2][:, k, :], lhsT=Wbs[b2][:, k * 128:(k + 1) * 128], rhs=tri[:], start=True, stop=True),
                     reads=[("Wbs", b2), "tri"], writes=[("pz", 0)])

        def xmod(ct):
            b2 = ct % 2
            xa, xb_, xc, xd = xq[b2]
            kx = ("xq", 0)
            cin, cout = car[b2], car[1 - b2]
            S.op("dve", lambda: V.tensor_tensor(out=zp[:], in0=pz[b2][:], in1=cin[:, 0:8].unsqueeze(2).to_broadcast([128, 8, 128]), op=ALU.add),
                 reads=[("pz", 0), ("car", b2)], writes=["zp"])
            S.op("dve", lambda: V.tensor_tensor(out=xa[:], in0=zp[:, 0:4, :], in1=Ppr[:], op=ALU.mult), reads=["zp", "Ppr"], writes=[kx])
            S.op("dve", lambda: V.tensor_tensor(out=xb_[:], in0=zp[:, 4:8, :], in1=Ppi[:], op=ALU.mult), reads=["zp", "Ppi"], writes=[kx])
            S.op("dve", lambda: V.tensor_tensor(out=xc[:], in0=zp[:, 0:4, :], in1=Ppi[:], op=ALU.mult), reads=["zp", "Ppi"], writes=[kx])
            S.op("dve", lambda: V.tensor_tensor(out=xd[:], in0=zp[:, 4:8, :], in1=Ppr[:], op=ALU.mult), reads=["zp", "Ppr"], writes=[kx])
            S.op("dve", lambda: V.tensor_tensor(out=cout[:, 0:4], in0=xa[:, :, 127], in1=xb_[:, :, 127], op=ALU.subtract),
                 reads=[kx], writes=[("car", 1 - b2)])
            S.op("dve", lambda: V.tensor_tensor(out=cout[:, 4:8], in0=xc[:, :, 127], in1=xd[:, :, 127], op=ALU.add),
                 reads=[kx], writes=[("car", 1 - b2)])
            if ct == 0:
                return
            S.op("pool", lambda: P_.tensor_tensor(out=XTs[b2][:, 0:4, :], in0=xa[:], in1=xb_[:], op=ALU.subtract), reads=[kx], writes=[("XT", b2)])
            S.op("pool", lambda: P_.tensor_tensor(out=XTs[b2][:, 4:8, :], in0=xc[:], in1=xd[:], op=ALU.add), reads=[kx], writes=[("XT", b2)])

        def ymm(ct):
            if ct == 0:
                return
            b2 = ct % 2
            ci = (ct - 1) % 4
            for k in range(8):
                S.op("pe", lambda: T_.matmul(py[:, ci * 128:(ci + 1) * 128], lhsT=Cmat[:, k, :], rhs=XTs[b2][:, k, :], start=(k == 0), stop=(k == 7)),
                     reads=["Cmat", ("XT", b2)], writes=["py"])
            if ci == 3:
                gq = (ct - 1) // 4
                c0 = 128 + 512 * gq
                yb = ybs[gq % 2]; kyb = ("yb", gq % 2)
                S.op("dve", lambda: V.scalar_tensor_tensor(out=yf[:], in0=uT[:, c0:c0 + 512], scalar=dsk[:, 0:1], in1=py[:], op0=ALU.mult, op1=ALU.add),
                     reads=[("uT", gq + 1), "dsk", "py"], writes=["yf"])
                S.op("pool", lambda: P_.tensor_tensor(out=y2[:], in0=yf[:], in1=yf[:], op=ALU.mult), reads=["yf"], writes=["y2"])
                S.op("pool", lambda: P_.tensor_scalar(out=y2[:], in0=y2[:], scalar1=0.044715, scalar2=1.0, op0=ALU.mult, op1=ALU.add), reads=["y2"], writes=["y2"])
                S.op("pool", lambda: P_.tensor_tensor(out=y2[:], in0=y2[:], in1=yf[:], op=ALU.mult), reads=["y2", "yf"], writes=["y2"])
                S.op("act", lambda: A_.activation(out=ysg[:], in_=y2[:], func=AF.Sigmoid, scale=1.5957691216057308), reads=["y2"], writes=["ysg"])
                S.op("pool", lambda: P_.tensor_tensor(out=yb[:], in0=yf[:], in1=ysg[:], op=ALU.mult), reads=["yf", "ysg"], writes=[kyb])
                S.dma("sp", lambda: nc.sync.dma_start(out=slab3[128:256, gq, :], in_=yb[:]), reads=[kyb], writes=[("slabY", gq)])


        with ExitStack() as st1:
            sb, ps = mk(st1)
            w4b = sb("w4b", [128, 8, 512], BF16)
            biasq = sb("biasq", [128, 1], F32); biask = sb("biask", [128, 1], F32); biasu = sb("biasu", [128, 1], F32)
            bv_bc = sb("bv_bc", [128, 128], F32)
            with ExitStack() as stw:
                sbw, psw = mk(stw)
                wf = sbw("wf", [128, 8, 512], F32)
                gcol = sbw("gcol", [128, 8], F32); bcol = sbw("bcol", [128, 8], F32)
                bcr = sbw("bcr", [128, 8, 128], F32)
                pb = psw("pb", [128, 4], F32)
                pbv = psw("pbv", [128, 128], F32)
                S.dma("sp", lambda: nc.sync.dma_start(out=wf[:], in_=w4.rearrange("(k p) n -> p k n", p=128)), writes=["wf"])
                S.dma("sp", lambda: nc.sync.dma_start(out=gcol[:], in_=ln_in_g.rearrange("o (k p) -> (o p) k", p=128), allow_slow_non_contiguous=True), writes=["gcol"])
                S.dma("sp", lambda: nc.sync.dma_start(out=bcol[:], in_=ln_in_b.rearrange("o (k p) -> (o p) k", p=128), allow_slow_non_contiguous=True), writes=["bcol"])
                for k in range(8):
                    S.op("dve", lambda: V.tensor_scalar(out=w4b[:, k, :], in0=wf[:, k, :], scalar1=gcol[:, k:k + 1], scalar2=None, op0=ALU.mult),
                         reads=["wf", "gcol"], writes=["w4b"])
                    S.op("pool", lambda: P_.tensor_copy(out=bcr[:, k, :], in_=bcol[:, k:k + 1].to_broadcast([128, 128])), reads=["bcol"], writes=["bcr"])
                for bi, (dstb, kb, sc) in enumerate(((biasq, "biasq", 0.125), (biask, "biask", 1.0), (None, None, None), (biasu, "biasu", 1.0))):
                    if dstb is None:
                        continue
                    for k in range(8):
                        S.op("pe", lambda: T_.matmul(pb[:, bi:bi + 1], lhsT=wf[:, k, bi * 128:(bi + 1) * 128], rhs=bcol[:, k:k + 1],
                                                     start=(k == 0), stop=(k == 7)), reads=["wf", "bcol"], writes=["pb"])
                    S.op("dve", lambda: V.tensor_scalar(out=dstb[:], in0=pb[:, bi:bi + 1], scalar1=sc, scalar2=None, op0=ALU.mult), reads=["pb"], writes=[kb])
                for k in range(8):
                    S.op("pe", lambda: T_.matmul(pbv[:], lhsT=bcr[:, k, :], rhs=wf[:, k, 256:384], start=(k == 0), stop=(k == 7)),
                         reads=["wf", "bcr"], writes=["pbv"])
                S.op("dve", lambda: V.tensor_copy(out=bv_bc[:], in_=pbv[:]), reads=["pbv"], writes=["bv_bc"])
                for e in ("pe", "act", "dve", "pool", "sp"):
                    S.wait_all(e)
            NB = 3
            xts = [sb(f"xt{i}", [128, D], F32) for i in range(NB)]
            tmps = [None] * NB
            hbs = [sb(f"hb{i}", [128, D], BF16) for i in range(NB)]
            hTs = [sb(f"hT{i}", [128, 8, 512], BF16) for i in range(2)]
            Vst = [sb(f"Vst{i}", [128, 4, 129], BF16) for i in range(2)]
            for i_ in range(2):
                S.op("pool", lambda: P_.memset(Vst[i_][:, :, 128:129], 1.0), writes=[("Vst1", i_)])
            lnsb = [(sb(f"st6_{i}", [128, 12], F32), sb(f"mv_{i}", [128, 2], F32), sb(f"rstd_{i}", [128, 1], F32),
                     sb(f"nmr_{i}", [128, 1], F32)) for i in range(NB)]
            ptr = [ps("ptr0", [128, 8, 128], BF16)] * 2
            pproj = [ps("pproj0", [128, 512], F32)] * 2
            pv = ps("pv", [128, 4, 128], F32)
            groups = [[0]] + [list(range(1 + 4 * g, 5 + 4 * g)) for g in range(NG)]
            flat = [(gi, ti, ct) for gi, tiles in enumerate(groups) for ti, ct in enumerate(tiles)]
            pcnt = [0]

            def stA(t):
                gi, ti, ct = flat[t]
                s = t % NB
                xt, tmp, hb = xts[s], tmps[s], hbs[s]
                if ct == 0:
                    S.op("dve", lambda: V.memset(xt[0:112, :], 0.0), writes=[("xt", s)])
                    S.dma("sp", lambda: nc.sync.dma_start(out=xt[112:128, :], in_=meta), writes=[("xt", s)])
                else:
                    S.dma("sp", lambda: nc.sync.dma_start(out=xt[:], in_=x_b[(ct - 1) * 128:ct * 128, :]), writes=[("xt", s)])
                st6, mv, rstd, nmr = lnsb[s]
                ks = ("lnstat", id(st6))
                kx = ("xt", s)
                S.op("dve", lambda: V.bn_stats(out=st6[:, 0:6], in_=xt[:, 0:512]), reads=[kx], writes=[ks])
                S.op("dve", lambda: V.bn_stats(out=st6[:, 6:12], in_=xt[:, 512:1024]), reads=[kx], writes=[ks])
                S.op("dve", lambda: V.bn_aggr(out=mv[:, 0:2], in_=st6[:, 0:12]), reads=[ks], writes=[ks])
                S.op("dve", lambda: V.tensor_scalar(out=rstd[:], in0=mv[:, 1:2], scalar1=LN_EPS, scalar2=None, op0=ALU.add), reads=[ks], writes=[ks])
                S.op("dve", lambda: V.tensor_scalar(out=nmr[:], in0=mv[:, 0:1], scalar1=-1.0, scalar2=None, op0=ALU.mult), reads=[ks], writes=[ks])
                S.op("act", lambda: A_.activation(out=rstd[:], in_=rstd[:], func=AF.Ln), reads=[ks], writes=[ks])
                S.op("act", lambda: A_.activation(out=rstd[:], in_=rstd[:], func=AF.Exp, scale=-0.5), reads=[ks], writes=[ks])
                S.op("act", lambda: A_.activation(out=nmr[:], in_=nmr[:], func=AF.Copy, scale=rstd[:, 0:1]), reads=[ks], writes=[ks])
                S.op("act", lambda: A_.activation(out=hb[:], in_=xt[:], func=AF.Identity, bias=nmr[:, 0:1], scale=rstd[:, 0:1]),
                     reads=[kx, ks], writes=[("hb", s)])

            def stB(t):
                gi, ti, ct = flat[t]
                s = t % NB
                hb = hbs[s]
                hT = hTs[gi % 2]; khT = ("hT", gi % 2)
                pt = ptr[0]
                for k in range(8):
                    S.op("pe", lambda: T_.transpose(out=pt[:, k, :], in_=hb[:, k * 128:(k + 1) * 128], identity=ident[:]),
                         reads=[("hb", s), "ident"], writes=[("ptr", 0)])
                S.op("act", lambda: A_.copy(out=hT[:, :, ti * 128:(ti + 1) * 128], in_=pt[:]),
                     reads=[("ptr", 0)], writes=[khT])

            def stC(gi):
                tiles = groups[gi]
                hT = hTs[gi % 2]; khT = ("hT", gi % 2)
                ncols = 128 * len(tiles)
                c0 = tiles[0] * 128
                for bi, (dst, kd) in enumerate(((qT, "qT"), (kT, "kT"), (None, None), (uT, ("uT", gi)))):
                    if dst is None:
                        continue
                    pp = pproj[0]; kp = ("pproj", 0)
                    for k in range(8):
                        S.op("pe", lambda: T_.matmul(pp[:, 0:ncols], lhsT=w4b[:, k, bi * 128:(bi + 1) * 128], rhs=hT[:, k, 0:ncols],
                                                     start=(k == 0), stop=(k == 7)), reads=["w4b", khT], writes=[kp])
                    if bi == 0:
                        S.op("act", lambda: A_.activation(out=dst[:, c0:c0 + ncols], in_=pp[:, 0:ncols], func=AF.Identity, bias=biasq[:, 0:1], scale=0.125),
                             reads=[kp, "biasq"], writes=[kd])
                    else:
                        bb_ = biask if bi == 1 else biasu
                        S.op("act", lambda: A_.activation(out=dst[:, c0:c0 + ncols], in_=pp[:, 0:ncols], func=AF.Identity, bias=bb_[:, 0:1], scale=1.0),
                             reads=[kp, "biask", "biasu"], writes=[kd])
                        if bi == 3 and gi == 0:
                            S.op("dve", lambda: V.memset(uT[:, 0:112], 0.0), reads=[kd], writes=[kd])
                for ti, ct in enumerate(tiles):
                    for k in range(8):
                        S.op("pe", lambda: T_.matmul(pv[:, ti, :], lhsT=hT[:, k, ti * 128:(ti + 1) * 128], rhs=w4b[:, k, 256:384],
                                                     start=(k == 0), stop=(k == 7)), reads=["w4b", khT], writes=["pv"])
                nt_ = len(tiles)
                vs_ = gi % 2
                S.op("dve", lambda: V.tensor_tensor(out=Vst[vs_][:, 0:nt_, 0:128], in0=pv[:, 0:nt_, :],
                                                    in1=bv_bc[:, 0:128].unsqueeze(1).to_broadcast([128, nt_, 128]), op=ALU.add),
                     reads=["pv", "bv_bc"], writes=[("Vst", vs_)])
                S.dma("sp", lambda: nc.sync.dma_start(out=vaug_d[:, tiles[0]:tiles[0] + nt_, :], in_=Vst[vs_][:, 0:nt_, :]),
                      reads=[("Vst", vs_), ("Vst1", vs_)], writes=[("vaug_d", gi)])

            nfl = len(flat)
            stA(0)
            if nfl > 1:
                stA(1)
            ssm_next = [0]

            def ssm_run(upto):
                while ssm_next[0] < upto:
                    ct_ = ssm_next[0]
                    if ct_ == 0:
                        bu(0)
                        wmod(0)
                    if ct_ + 1 < NT:
                        bu(ct_ + 1)
                    zmm(ct_)
                    if ct_ >= 1:
                        ymm(ct_ - 1)
                    if ct_ + 1 < NT:
                        wmod(ct_ + 1)
                    xmod(ct_)
                    ssm_next[0] += 1

            print("SBUF bytes remaining in fused phase:", nc.sbuf_bytes_remaining, flush=True)
            allowed = 0
            for t in range(nfl):
                if t + 2 < nfl:
                    stA(t + 2)
                stB(t)
                gi, ti, ct = flat[t]
                if ti == len(groups[gi]) - 1:
                    stC(gi)
                    if gi >= 1:
                        allowed = groups[gi - 1][-1]
                if ssm_next[0] < allowed:
                    ssm_run(ssm_next[0] + 1)
                    if allowed - ssm_next[0] > 4:
                        ssm_run(ssm_next[0] + 1)
            ssm_run(NT)
            ymm(NT - 1)
            if debug:
                S.dma("sp", lambda: nc.sync.dma_start(out=dbg["qT"], in_=qT[:]), reads=["qT"], writes=["dq"])
                S.dma("sp", lambda: nc.sync.dma_start(out=dbg["kT"], in_=kT[:]), reads=["kT"], writes=["dk"])
            for e in ("pe", "act", "dve", "pool", "sp"):
                S.wait_all(e)
        st3.close()
        st_u.close()
        print("P1a+S5 done: inst", S.n_inst, "waits", S.n_wait, "sems", S.nsem, flush=True)

        XCH = min(1024, 128 * NG)
        NXC = 256 * NG // XCH

        GPC = XCH // 256

        def ag_chunk(c_):
            keys = []
            for g_ in range(c_ * GPC, (c_ + 1) * GPC):
                keys += [("slabA", g_), ("slabY", g_)]
            S.waitfor("pool", keys)
            S.op("pool", lambda: P_.collective_compute("AllGather", ALU.bypass, replica_groups=[[0, 1, 2, 3], [4, 5, 6, 7]],
                                                       ins=[slab[c_ * XCH:(c_ + 1) * XCH, :].opt()],
                                                       outs=[gath[c_ * 4 * XCH:(c_ + 1) * 4 * XCH, :].opt()]), writes=[("gath", c_)])

        with ExitStack() as st2:
            sb, ps = mk(st2)
            Vaug = sb("Vaug", [128, NT, 129], BF16)
            S.dma("sp", lambda: nc.sync.dma_start(out=Vaug[:], in_=vaug_d), reads=[("vaug_d", gi_) for gi_ in range(NG + 1)], writes=["Vaug"])
            tb = bc_load("tb", relb[0], 32, sb)
            Wb = sb("Wb", [128, 1024], F32)
            Bm0 = sb("Bm0", [128, 512], F32)
            with ExitStack() as stt:
                sbt, _ = mk(stt)
                reli = sbt("reli", [128, 1024], I32)
                relv = sbt("relv", [128, 1024], F32)
                stp = sbt("stp", [128, 1024], F32)
                iom = sbt("iom", [128, 1024], F32)
                dl = sbt("dl", [128, 32], F32)
                thrp = sbt("thrp", [128, 1], F32)
                S.op("pool", lambda: P_.iota(reli[:], pattern=[[-1, 1024]], base=384, channel_multiplier=1), writes=["reli"])
                S.op("dve", lambda: V.tensor_copy(out=relv[:], in_=reli[:]), reads=["reli"], writes=["relv"])
                S.op("pool", lambda: P_.iota(reli[:], pattern=[[1, 1024]], base=0, channel_multiplier=0), reads=["relv"], writes=["reli"])
                S.op("dve", lambda: V.tensor_copy(out=iom[:], in_=reli[:]), reads=["reli"], writes=["iom"])
                steps = []
                for j in range(15, 8, -1):
                    steps.append((-thr[j] + 1, j - 1))
                for n in range(7, -1, -1):
                    steps.append((-n, n))
                for n in range(1, 8):
                    steps.append((n, 16 + n))
                steps.append((8, 24))
                for j in range(9, 16):
                    steps.append((thr[j], 16 + j))
                prev = 15
                S.op("dve", lambda: V.tensor_scalar(out=Wb[:], in0=relv[:], scalar1=0.0, scalar2=tb[:, 15:16],
                                                    op0=ALU.mult, op1=ALU.add), reads=["relv", "tb"], writes=["Wb"])
                for si, (tv, bk) in enumerate(steps):
                    S.op("dve", lambda: V.tensor_tensor(out=dl[:, si:si + 1], in0=tb[:, bk:bk + 1], in1=tb[:, prev:prev + 1],
                                                        op=ALU.subtract), reads=["tb"], writes=["dl"])
                    S.op("dve", lambda: V.tensor_scalar(out=stp[:], in0=relv[:], scalar1=float(tv), scalar2=dl[:, si:si + 1],
                                                        op0=ALU.is_ge, op1=ALU.mult), reads=["relv", "dl"], writes=["stp"])
                    S.op("dve", lambda: V.tensor_tensor(out=Wb[:], in0=Wb[:], in1=stp[:], op=ALU.add), reads=["stp", "Wb"], writes=["Wb"])
                    prev = bk
                S.op("pool", lambda: P_.affine_select(out=Wb[0:64, :], in_=Wb[0:64, :], pattern=[[1, 1024]], compare_op=ALU.is_ge,
                                                      fill=NEG, base=-384, channel_multiplier=0), reads=["Wb"], writes=["Wb"])
                S.op("pool", lambda: P_.affine_select(out=Wb[64:128, :], in_=Wb[64:128, :], pattern=[[1, 1024]], compare_op=ALU.is_ge,
                                                      fill=NEG, base=-448, channel_multiplier=0), reads=["Wb"], writes=["Wb"])
                S.op("dve", lambda: V.tensor_copy(out=Bm0[:], in_=Wb[:, 512:1024]), reads=["Wb"], writes=["Bm0"])
                S.op("dve", lambda: V.memset(Bm0[0:112, :], NEG), reads=["Bm0"], writes=["Bm0"])
                for e in ("pe", "act", "dve", "pool", "sp"):
                    S.wait_all(e)
            Wbb = sb("Wbb", [128, 1024], BF16)
            Bm0b = sb("Bm0b", [128, 512], BF16)
            S.op("dve", lambda: V.tensor_copy(out=Wbb[:], in_=Wb[:]), reads=["Wb"], writes=["Wbb"])
            S.op("dve", lambda: V.tensor_copy(out=Bm0b[:], in_=Bm0[:]), reads=["Bm0"], writes=["Bm0b"])
            bfar = sb("bfar", [128, 1], F32)
            bfarm = sb("bfarm", [128, 1], F32)
            S.op("dve", lambda: V.tensor_copy(out=bfar[:], in_=tb[:, 15:16]), reads=["tb"], writes=["bfar"])
            S.op("dve", lambda: V.tensor_copy(out=bfarm[:], in_=tb[:, 15:16]), reads=["tb"], writes=["bfarm"])
            S.op("dve", lambda: V.memset(bfarm[0:112, :], NEG), reads=["bfarm"], writes=["bfarm"])
            lamt = sb("lamt", [128, 4, 64], F32)
            S.dma("sp", lambda: nc.sync.dma_start(out=lamt[:], in_=lam4.rearrange("a n -> (a n)").partition_broadcast(128)), writes=["lamt"])
            lsum = sb("lsum", [128, 2], F32)
            lprod = sb("lprod", [128, 2, 64], F32)
            neglam = sb("neglam", [128, 1], F32)
            S.op("dve", lambda: V.tensor_tensor(out=lprod[:, 0, :], in0=lamt[:, 0, :], in1=lamt[:, 1, :], op=ALU.mult), reads=["lamt"], writes=["lprod"])
            S.op("dve", lambda: V.tensor_tensor(out=lprod[:, 1, :], in0=lamt[:, 2, :], in1=lamt[:, 3, :], op=ALU.mult), reads=["lamt"], writes=["lprod"])
            S.op("dve", lambda: V.reduce_sum(out=lsum[:], in_=lprod[:], axis=AX.X), reads=["lprod"], writes=["lsum"])
            S.op("act", lambda: A_.activation(out=lsum[:], in_=lsum[:], func=AF.Exp), reads=["lsum"], writes=["lsum"])
            S.op("dve", lambda: V.tensor_tensor(out=neglam[:], in0=lsum[:, 1:2], in1=lsum[:, 0:1], op=ALU.subtract), reads=["lsum"], writes=["neglam"])
            S.op("dve", lambda: V.tensor_scalar(out=neglam[:], in0=neglam[:], scalar1=-0.2, scalar2=None, op0=ALU.add), reads=["neglam"], writes=["neglam"])
            g08 = bc_load("g08", subln_g[0], 128, sb)
            S.op("dve", lambda: V.tensor_scalar(out=g08[:], in0=g08[:], scalar1=0.8, scalar2=None, op0=ALU.mult), reads=["g08"], writes=["g08"])

            pss = [ps(f"pss{i}", [128, 2, 512], F32) for i in range(2)]
            po = ps("po", [128, 3, 512], F32)
            ptt = ps("ptt", [128, 4, 128], BF16)
            pTs = [sb(f"pT{i}", [128, 2, 512], BF16) for i in range(3)]
            sn = [sb(f"sn{i}", [128, 2, 512], F32) for i in range(2)]
            osb = sb("osb", [128, 128], F32)
            o2 = sb("o2", [128, 128], F32)
            ob = sb("ob", [128, 4, 128], BF16)
            rr = sb("rr", [128, 8], F32)
            attT = [sb(f"attT{i}", [128, 512], BF16) for i in range(2)]

            def acc(a):
                return po[:, a // 3, (a % 3) * 129:(a % 3) * 129 + 129]

            o4 = sb("o4", [128, 4, 128], F32)
            ms = sb("ms", [128, 4], F32)
            units = [(g, j) for g in range(NG) for j in range(4 * g + 5)]
            ncnt = [0]

            def near_of(g, j):
                if j == 0:
                    return Bm0b[:, :] if g == 0 else None
                if j < 4 * g:
                    return None
                tp = j - (4 * g + 1)
                return Wbb[:, 384 - 128 * tp:384 - 128 * tp + 512]

            def qk(i):
                g, j = units[i]
                u = i % 2
                q0 = 128 + 512 * g
                nb = near_of(g, j)
                for m in range(2):
                    S.op("pe", lambda: T_.matmul(pss[u][:, m, :], lhsT=kT[64 * m:64 * m + 64, j * 128:(j + 1) * 128],
                                                 rhs=qT[64 * m:64 * m + 64, q0:q0 + 512], start=True, stop=(nb is None)),
                         reads=["kT", "qT"], writes=[("pss", u)])
                    if nb is not None:
                        S.op("pe", lambda: T_.matmul(pss[u][:, m, :], lhsT=ident[:], rhs=nb, start=False, stop=True),
                             reads=["ident", "Wbb", "Bm0b"], writes=[("pss", u)])

            def ex(i):
                g, j = units[i]
                u = i % 2
                v3 = i % 3
                pS, pT = pss[u], pTs[v3]
                if j == 0 and g > 0:
                    S.op("act", lambda: A_.activation(out=pT[:], in_=pS[:], func=AF.Exp, bias=bfarm[:, 0:1], scale=1.0),
                         reads=[("pss", u), "bfarm"], writes=[("pT", v3)])
                elif 0 < j < 4 * g:
                    S.op("act", lambda: A_.activation(out=pT[:], in_=pS[:], func=AF.Exp, bias=bfar[:, 0:1], scale=1.0),
                         reads=[("pss", u), "bfar"], writes=[("pT", v3)])
                else:
                    S.op("act", lambda: A_.activation(out=pT[:], in_=pS[:], func=AF.Exp), reads=[("pss", u)], writes=[("pT", v3)])

            def pv(i):
                g, j = units[i]
                u = i % 3
                last = 4 * g + 4
                for m in range(2):
                    for qs in range(4):
                        S.op("pe", lambda: T_.matmul(acc(m * 4 + qs), lhsT=pTs[u][:, m, qs * 128:(qs + 1) * 128], rhs=Vaug[:, j, :],
                                                     start=(j == 0 and (m * 4 + qs) % 3 == 0), stop=(j == last), skip_group_check=True),
                             reads=[("pT", u), "Vaug"], writes=["po"])

            posn = sb("posn", [128, 3, 512], F32)

            def accs_(a):
                return posn[:, a // 3, (a % 3) * 129:(a % 3) * 129 + 129]

            def fin_dve1(g):
                S.op("dve", lambda: V.tensor_copy(out=posn[:, 0:2, 0:387], in_=po[:, 0:2, 0:387]), reads=["po"], writes=["posn"])
                S.op("dve", lambda: V.tensor_copy(out=posn[:, 2, 0:258], in_=po[:, 2, 0:258]), reads=["po"], writes=["posn"])
                for a in range(8):
                    S.op("dve", lambda: V.reciprocal(out=rr[:, a:a + 1], in_=accs_(a)[:, 128:129]), reads=["posn"], writes=["rr"])
                S.op("dve", lambda: V.tensor_scalar(out=rr[:, 4:8], in0=rr[:, 4:8], scalar1=neglam[:, 0:1], scalar2=None, op0=ALU.mult),
                     reads=["rr", "neglam"], writes=["rr"])
                for qs in range(4):
                    S.op("dve", lambda: V.tensor_scalar(out=o4[:, qs, :], in0=accs_(qs)[:, 0:128], scalar1=rr[:, qs:qs + 1], scalar2=None, op0=ALU.mult),
                         reads=["posn", "rr"], writes=["o4"])
                    S.op("dve", lambda: V.scalar_tensor_tensor(out=o4[:, qs, :], in0=accs_(4 + qs)[:, 0:128], scalar=rr[:, 4 + qs:5 + qs], in1=o4[:, qs, :],
                                                               op0=ALU.mult, op1=ALU.add), reads=["posn", "rr", "o4"], writes=["o4"])
                    S.op("dve", lambda: V.tensor_tensor(out=o2[:], in0=o4[:, qs, :], in1=o4[:, qs, :], op=ALU.mult), reads=["o4"], writes=["o2"])
                    S.op("dve", lambda: V.reduce_sum(out=ms[:, qs:qs + 1], in_=o2[:], axis=AX.X), reads=["o2", "ms"], writes=["ms"])
                S.op("dve", lambda: V.tensor_scalar(out=ms[:], in0=ms[:], scalar1=1.0 / 128.0, scalar2=LN_EPS, op0=ALU.mult, op1=ALU.add),
                     reads=["ms"], writes=["ms"])

            def fin_rest(g):
                aT = attT[g % 2]; kaT = ("attT", g % 2)
                S.op("act", lambda: A_.activation(out=ms[:], in_=ms[:], func=AF.Ln), reads=["ms"], writes=["ms"])
                S.op("act", lambda: A_.activation(out=ms[:], in_=ms[:], func=AF.Exp, scale=-0.5), reads=["ms"], writes=["ms"])
                for qs in range(4):
                    S.op("dve", lambda: V.scalar_tensor_tensor(out=ob[:, qs, :], in0=o4[:, qs, :], scalar=ms[:, qs:qs + 1], in1=g08[:],
                                                               op0=ALU.mult, op1=ALU.mult), reads=["o4", "ms", "g08"], writes=["ob"])

            def fin_out(g):
                aT = attT[g % 2]; kaT = ("attT", g % 2)
                for qs in range(4):
                    S.op("pe", lambda: T_.transpose(out=ptt[:, qs, :], in_=ob[:, qs, :], identity=ident[:]), reads=["ob", "ident"], writes=["ptt"])
                S.op("dve", lambda: V.tensor_copy(out=aT[:], in_=ptt[:].rearrange("p a b -> p (a b)")), reads=["ptt"], writes=[kaT])
                S.dma("sp", lambda: nc.sync.dma_start(out=slab3[0:128, g, :], in_=aT[:]), reads=[kaT], writes=[("slabA", g)])
                if (g + 1) % GPC == 0:
                    ag_chunk(g // GPC)

            qk(0)
            pending = None
            pend_out = None
            nun = len(units)
            for i in range(nun + 1):
                if i + 1 < nun:
                    qk(i + 1)
                if i < nun:
                    ex(i)
                if i >= 1:
                    pv(i - 1)
                    gp, jp = units[i - 1]
                    if pending is not None and jp == min(8, 4 * gp + 4):
                        fin_rest(pending)
                        pend_out = pending
                        pending = None
                    if pend_out is not None and jp == min(10, 4 * gp + 4):
                        fin_out(pend_out)
                        pend_out = None
                    if jp == 4 * gp + 4:
                        fin_dve1(gp)
                        pending = gp
            if pend_out is not None:
                fin_out(pend_out)
            fin_rest(pending)
            fin_out(pending)
            for e in ("pe", "act", "dve", "pool", "sp"):
                S.wait_all(e)
        st_att.close()
        print("P1b done: inst", S.n_inst, "waits", S.n_wait, "sems", S.nsem, flush=True)
        if debug:
            S.waitfor("sp", [("slabA", g) for g in range(NG)] + [("slabY", g) for g in range(NG)])
            S.dma("sp", lambda: nc.sync.dma_start(out=dbg["slab"], in_=slab), writes=["dslab"])
        if not full:
            for e in ("pe", "act", "dve", "pool", "sp"):
                S.wait_all(e)
            return nc
        S.waitfor("pool", [("gath", c_) for c_ in range(NXC)])

        def barrier():
            for e_ in ("pe", "act", "dve", "pool", "sp"):
                S.wait_all(e_)

        IOA = bass.IndirectOffsetOnAxis
        with ExitStack() as st4:
            sb, ps = mk(st4)
            g1 = bc_load("g1", ln1_g[0], D, sb); b1 = bc_load("b1", ln1_b[0], D, sb)
            g2 = bc_load("g2", ln2_g[0], D, sb); b2 = bc_load("b2", ln2_b[0], D, sb)
            slot_ga = sb("slot_ga", [128, NT2, 4], I32)
            gate_all = sb("gate_all", [128, NT2, 4], F32)
            lnsb2 = [(sb(f"st6b_{i}", [128, 12], F32), sb(f"mvb_{i}", [128, 2], F32), sb(f"rstdb_{i}", [128, 1], F32),
                      sb(f"nmrb_{i}", [128, 1], F32)) for i in range(2)]
            ztile = sb("ztile", [128, NSLOT // 128 + 1], I32)
            S.op("pool", lambda: P_.memset(ztile[:], 0), writes=["ztile"])
            S.dma("sp", lambda: nc.sync.dma_start(out=tokslot_d.rearrange("(p f) o -> p (f o)", p=128), in_=ztile[:]), reads=["ztile"], writes=["tokslot0"])
            sc_keys = []
            h1_keys = []
            with ExitStack() as st5:
                sb5, ps5 = mk(st5)
                wglu = sb5("wglu", [128, 4, 512], BF16)
                wout = sb5("wout", [128, 8, 1024], BF16)
                wr = sb5("wr", [128, 8, 32], F32)
                S.dma("pool", lambda: P_.dma_start(out=wglu[:], in_=w_glu.rearrange("(k p) n -> p k n", p=128)), writes=["wglu"])
                S.dma("pool", lambda: P_.dma_start(out=wout[:], in_=w_out.rearrange("(k p) n -> p k n", p=128)), writes=["wout"])
                S.dma("sp", lambda: nc.sync.dma_start(out=wr[:], in_=w_router.rearrange("(k p) n -> p k n", p=128)), writes=["wr"])
                bglu = sb5("bglu", [128, 4], F32)
                S.dma("sp", lambda: nc.sync.dma_start(out=bglu[:], in_=b_glu.rearrange("o (j p) -> (o p) j", p=128), allow_slow_non_contiguous=True), writes=["bglu"])
                brt = bc_load("brt", b_router[0], NEXP, sb5)
                gidx_t = sb5("gidx_t", [128, 8 * NG2], I32)
                S.dma("sp", lambda: nc.sync.dma_start(out=gidx_t[:], in_=gidx), writes=["gidx"])
                ebi = sb5("ebi", [128, NEXP], I32); ebase = sb5("ebase", [128, NEXP], F32)
                S.op("pool", lambda: P_.iota(ebi[:], pattern=[[CAP, NEXP]], base=0, channel_multiplier=0), writes=["ebi"])
                S.op("dve", lambda: V.tensor_copy(out=ebase[:], in_=ebi[:]), reads=["ebi"], writes=["ebase"])
                cntbc = sb5("cntbc", [128, NEXP], F32)
                S.op("dve", lambda: V.memset(cntbc[:], 0.0), writes=["cntbc"])
                catT = [sb5(f"catT{i}", [128, 4, 512], BF16) for i in range(2)]
                yT = [sb5(f"yT{i}", [128, 4, 512], BF16) for i in range(2)]
                ssmT = sb5("ssmT", [128, 4, 512], BF16)
                sg = sb5("sg", [128, 512], F32)
                xt2 = [sb5(f"xt2_{i}", [128, D], F32) for i in range(2)]
                tmpa = sb5("tmpa", [128, D], F32)
                h0 = sb5("h0", [128, D], F32)
                pre = sb5("pre", [128, D], F32)
                h1 = [sb5(f"h1_{i}", [128, D], F32) for i in range(2)]
                h1b = [sb5(f"h1b_{i}", [128, D], BF16) for i in range(2)]
                h1T = sb5("h1T", [128, 8, 128], F32)
                lg = sb5("lg", [128, NEXP], F32); mx8 = sb5("mx8", [128, 8], F32); negm = sb5("negm", [128, 1], F32)
                ex = sb5("ex", [128, NEXP], F32); msk = sb5("msk", [128, NEXP], F32); mskb = sb5("mskb", [128, NEXP], BF16)
                den = sb5("den", [128, 1], F32); gfull = sb5("gfull", [128, NEXP], F32)
                rank = sb5("rank", [128, NEXP], F32); ovf = sb5("ovf", [128, NEXP], F32); keep = sb5("keep", [128, NEXP], F32)
                sga = sb5("sga", [128, NEXP], F32); ssc = sb5("ssc", [128, NEXP], F32)
                oh = sb5("oh", [128, NEXP], F32); tmq = sb5("tmq", [128, NEXP], F32)
                sgak = sb5("sgak", [128, 4], F32); ssck = sb5("ssck", [128, 4], F32)
                ssci = [sb5(f"ssci{i}", [128, 4], I32) for i in range(2)]
                tokid = [sb5(f"tokid{i}", [128, 1], I32) for i in range(2)]
                pzg = [ps5(f"pzg{i}", [128, 512], F32) for i in range(2)]
                pms = [ps5(f"pm{i}", [128, 1024], F32) for i in range(2)]
                ptf = [ps5("ptf0", [128, 4, 128], F32)] * 2
                pk = ps5("pk", [128, 512], F32)
                Q3 = sb5("Q3", [128, 3, NEXP], F32)
                tm3 = sb5("tm3", [128, 3, NEXP], F32)
                R4 = sb5("R4", [128, 4, 3], F32)
                lnsb3 = [(sb5(f"st6c_{i}", [128, 12], F32), sb5(f"mvc_{i}", [128, 2], F32), sb5(f"rstdc_{i}", [128, 1], F32),
                          sb5(f"nmrc_{i}", [128, 1], F32)) for i in range(2)]

                h0s = [h0, sb5("h0b", [128, D], F32)]
                tmpb = sb5("tmpb", [128, D], F32)

                def stage0(t2):
                    s2 = t2 % 2
                    xt = xt2[s2]
                    S.dma("sp", lambda: nc.sync.dma_start(out=xt[:], in_=x2[t2 * 128:(t2 + 1) * 128, :]), writes=[("xt2", s2)])
                    layernorm(xt, ("xt2", s2), lng, "lng", lnb, "lnb", tmpb, "tmpb", h0s[s2][:], ("h0", s2), lnsb3[0])

                def stage1_mm(t2, tt, cT, kc):
                    pm = pms[t2 % 2]
                    for half in range(2):
                        for k in range(8):
                            lhs = cT[:, k, tt * 128:(tt + 1) * 128] if k < 4 else ssmT[:, k - 4, tt * 128:(tt + 1) * 128]
                            S.op("pe", lambda: T_.matmul(pm[:, half * 512:(half + 1) * 512], lhsT=lhs, rhs=wout[:, k, half * 512:(half + 1) * 512],
                                                         start=(k == 0), stop=(k == 7)), reads=["wout", kc, "ssmT"], writes=[("pm", t2 % 2)])

                def stage1(t2, tg, tt, cT, kc):
                    s2 = t2 % 2
                    pm = pms[s2]
                    S.op("dve", lambda: V.scalar_tensor_tensor(out=pre[:], in0=h0s[s2][:], scalar=ALPHA, in1=pm[:], op0=ALU.mult, op1=ALU.add),
                         reads=[("h0", s2), ("pm", s2)], writes=["pre"])
                    h1t = h1[s2]; kh1 = ("h1", s2)
                    layernorm(pre, "pre", g1, "g1", b1, "b1", tmpa, "tmpa", h1t[:], kh1, lnsb3[1])
                    S.op("act", lambda: A_.copy(out=h1b[s2][:], in_=h1t[:]), reads=[kh1], writes=[("h1b", s2)])
                    S.dma("sp", lambda: nc.sync.dma_start(out=h1_d[t2 * 128:(t2 + 1) * 128, :], in_=h1t[:]), reads=[kh1], writes=[("h1_d", t2)])
                    S.dma("sp", lambda: nc.sync.dma_start(out=h1b_d[t2 * 128:(t2 + 1) * 128, :], in_=h1b[s2][:]), reads=[("h1b", s2)], writes=[("h1b_d", t2)])
                    h1_keys.append(("h1b_d", t2))
                    if debug:
                        S.dma("sp", lambda: nc.sync.dma_start(out=dbg["h1"][t2 * 128:(t2 + 1) * 128, :], in_=h1t[:]), reads=[kh1], writes=[("dh1", t2)])

                def stage2(t2):
                    s2 = t2 % 2
                    h1t = h1[s2]; kh1 = ("h1", s2)
                    for hh in range(2):
                        pf = ptf[hh]
                        for k4 in range(4):
                            k = hh * 4 + k4
                            S.op("pe", lambda: T_.transpose(out=pf[:, k4, :], in_=h1t[:, k * 128:(k + 1) * 128], identity=identf[:]),
                                 reads=[kh1, "identf"], writes=[("ptf", 0)])
                        S.op("act", lambda: A_.copy(out=h1T[:, hh * 4:hh * 4 + 4, :], in_=pf[:]), reads=[("ptf", 0)], writes=["h1T"])
                    for k in range(8):
                        S.op("pe", lambda: T_.matmul(pk[:, 0:32], lhsT=h1T[:, k, :], rhs=wr[:, k, :], start=(k == 0), stop=(k == 7)),
                             reads=["h1T", "wr"], writes=["pk"])
                    S.op("dve", lambda: V.tensor_tensor(out=lg[:], in0=pk[:, 0:32], in1=brt[:], op=ALU.add), reads=["pk", "brt"], writes=["lg"])
                    if debug:
                        S.dma("sp", lambda: nc.sync.dma_start(out=dbg["lg"][t2 * 128:(t2 + 1) * 128, :], in_=lg[:]), reads=["lg"], writes=[("dlg", t2)])
                    S.op("dve", lambda: V.max(out=mx8[:], in_=lg[:]), reads=["lg"], writes=["mx8"])
                    S.op("dve", lambda: V.tensor_scalar(out=negm[:], in0=mx8[:, 0:1], scalar1=-1.0, scalar2=None, op0=ALU.mult), reads=["mx8"], writes=["negm"])
                    S.op("act", lambda: A_.activation(out=ex[:], in_=lg[:], func=AF.Exp, bias=negm[:, 0:1], scale=1.0), reads=["lg", "negm"], writes=["ex"])
                    S.op("dve", lambda: V.tensor_scalar(out=msk[:], in0=lg[:], scalar1=mx8[:, 3:4], scalar2=None, op0=ALU.is_ge), reads=["lg", "mx8"], writes=["msk"])
                    S.op("dve", lambda: V.tensor_copy(out=mskb[:], in_=msk[:]), reads=["msk"], writes=["mskb"])
                    S.op("pe", lambda: T_.matmul(pk[:, 32:64], lhsT=stri[:], rhs=mskb[:], start=True, stop=True), reads=["stri", "mskb"], writes=["pk"])
                    S.op("pe", lambda: T_.matmul(pk[:, 64:96], lhsT=ones[:], rhs=mskb[:], start=True, stop=True), reads=["ones", "mskb"], writes=["pk"])
                    S.op("dve", lambda: V.tensor_tensor(out=ex[:], in0=ex[:], in1=msk[:], op=ALU.mult), reads=["ex", "msk"], writes=["ex"])
                    S.op("dve", lambda: V.reduce_sum(out=den[:], in_=ex[:], axis=AX.X), reads=["ex"], writes=["den"])
                    S.op("dve", lambda: V.reciprocal(out=den[:], in_=den[:]), reads=["den"], writes=["den"])
                    S.op("dve", lambda: V.tensor_tensor(out=rank[:], in0=pk[:, 32:64], in1=cntbc[:], op=ALU.add), reads=["pk", "cntbc"], writes=["rank"])
                    S.op("dve", lambda: V.tensor_tensor(out=cntbc[:], in0=pk[:, 64:96], in1=cntbc[:], op=ALU.add), reads=["pk", "cntbc", "rank"], writes=["cntbc"])
                    S.op("dve", lambda: V.tensor_scalar(out=ovf[:], in0=rank[:], scalar1=float(CAP), scalar2=None, op0=ALU.is_ge), reads=["rank"], writes=["ovf"])
                    S.op("dve", lambda: V.tensor_scalar(out=keep[:], in0=ovf[:], scalar1=-1.0, scalar2=1.0, op0=ALU.mult, op1=ALU.add), reads=["ovf"], writes=["keep"])
                    S.op("dve", lambda: V.scalar_tensor_tensor(out=Q3[:, 2, :], in0=ex[:], scalar=den[:, 0:1], in1=keep[:], op0=ALU.mult, op1=ALU.mult),
                         reads=["ex", "den", "keep"], writes=["Q3"])
                    S.op("dve", lambda: V.scalar_tensor_tensor(out=Q3[:, 0, :], in0=rank[:], scalar=float(CAP - 1), in1=ebase[:], op0=ALU.min, op1=ALU.add),
                         reads=["rank", "ebase"], writes=["Q3"])
                    S.op("dve", lambda: V.tensor_tensor(out=ssc[:], in0=rank[:], in1=ebase[:], op=ALU.add), reads=["rank", "ebase"], writes=["ssc"])
                    S.op("dve", lambda: V.tensor_tensor(out=ssc[:], in0=ssc[:], in1=keep[:], op=ALU.mult), reads=["ssc", "keep"], writes=["ssc"])
                    S.op("dve", lambda: V.scalar_tensor_tensor(out=Q3[:, 1, :], in0=ovf[:], scalar=float(NSLOT), in1=ssc[:], op0=ALU.mult, op1=ALU.add),
                         reads=["ovf", "ssc"], writes=["Q3"])
                    for k in range(4):
                        S.op("dve", lambda: V.tensor_scalar(out=oh[:], in0=lg[:], scalar1=mx8[:, k:k + 1], scalar2=None, op0=ALU.is_equal), reads=["lg", "mx8"], writes=["oh"])
                        S.op("dve", lambda: V.tensor_tensor(out=tm3[:], in0=Q3[:], in1=oh[:, 0:NEXP].unsqueeze(1).to_broadcast([128, 3, NEXP]), op=ALU.mult),
                             reads=["Q3", "oh"], writes=["tm3"])
                        S.op("dve", lambda: V.reduce_sum(out=R4[:, k, :], in_=tm3[:], axis=AX.X), reads=["tm3"], writes=["R4"])
                    S.op("dve", lambda: V.tensor_copy(out=slot_ga[:, t2, :], in_=R4[:, :, 0]), reads=["R4"], writes=["slot_ga"])
                    S.op("dve", lambda: V.tensor_copy(out=ssci[s2][:], in_=R4[:, :, 1]), reads=["R4"], writes=[("ssci", s2)])
                    S.op("dve", lambda: V.tensor_copy(out=gate_all[:, t2, :], in_=R4[:, :, 2]), reads=["R4"], writes=["gate_all"])
                    S.op("pool", lambda: P_.iota(tokid[s2][:], pattern=[[0, 1]], base=t2 * 128, channel_multiplier=1), writes=[("tokid", s2)])
                    for k in range(4):
                        S.dma("pool", lambda: P_.indirect_dma_start(out=tokslot_d[:, :], out_offset=IOA(ap=ssci[s2][:, k:k + 1], axis=0),
                                                                    in_=tokid[s2][:, 0:1], in_offset=None),
                              reads=[("ssci", s2), ("tokid", s2), "tokslot0"], writes=[("sc", t2, k)])
                        sc_keys.append(("sc", t2, k))

                def fetch_group(tg):
                    for j in range(4):
                        for half in range(2):
                            col = (tg * 4 + j) * 2 + half
                            dst = catT[tg % 2][:, j, :] if half == 0 else yT[tg % 2][:, j, :]
                            S.dma("pool", lambda: P_.indirect_dma_start(out=dst, out_offset=None, in_=gath[:, :],
                                                                        in_offset=IOA(ap=gidx_t[:, col:col + 1], axis=0)),
                                  reads=["gidx"], writes=[("catT", tg % 2) if half == 0 else ("yT", tg % 2)])

                fetch_group(0)
                for tg in range(NG2):
                    cT, yT_ = catT[tg % 2], yT[tg % 2]
                    kc, ky = ("catT", tg % 2), ("yT", tg % 2)
                    if tg + 1 < NG2:
                        fetch_group(tg + 1)
                    for j2 in range(4):
                        pz_ = pzg[j2 % 2]; kpz = ("pzg", j2 % 2)
                        for j1 in range(4):
                            S.op("pe", lambda: T_.matmul(pz_[:], lhsT=wglu[:, j1, j2 * 128:(j2 + 1) * 128], rhs=yT_[:, j1, :],
                                                         start=(j1 == 0), stop=(j1 == 3)), reads=["wglu", ky], writes=[kpz])
                        S.op("act", lambda: A_.activation(out=sg[:], in_=pz_[:], func=AF.Sigmoid, bias=bglu[:, j2:j2 + 1], scale=1.0),
                             reads=[kpz, "bglu"], writes=["sg"])
                        S.op("dve", lambda: V.tensor_tensor(out=ssmT[:, j2, :], in0=yT_[:, j2, :], in1=sg[:], op=ALU.mult),
                             reads=[ky, "sg"], writes=["ssmT"])
                    stage1_mm(tg * 4, 0, cT, kc)
                    for tt in range(4):
                        t2 = tg * 4 + tt
                        if t2 == 0:
                            stage0(0)
                        if t2 + 1 < NT2:
                            stage0(t2 + 1)
                        if tt < 3:
                            stage1_mm(t2 + 1, tt + 1, cT, kc)
                        stage1(t2, tg, tt, cT, kc)
                        if t2 >= 1:
                            stage2(t2 - 1)
                stage2(NT2 - 1)
                barrier()
            print("P2a done: inst", S.n_inst, "waits", S.n_wait, "sems", S.nsem, flush=True)

            chunks = [(c0, min(c0 + 512, CAP)) for c0 in range(0, CAP, 512)]
            y_keys = []
            with ExitStack() as st6:
                sb6, ps6 = mk(st6)
                wgs = [sb6(f"wg{i}", [128, 8, 1024], BF16) for i in range(2)]
                wus = [sb6(f"wu{i}", [128, 8, 1024], BF16) for i in range(2)]
                wds = [sb6(f"wd{i}", [128, 8, 1024], BF16) for i in range(2)]
                bgall = sb6("bgall", [128, NEXP, 8], F32); buall = sb6("buall", [128, NEXP, 8], F32)
                for e in range(NEXP):
                    S.dma("sp", lambda: nc.sync.dma_start(out=bgall[:, e, :], in_=b_gate[e].rearrange("(j p) -> p j", p=128), allow_slow_non_contiguous=True),
                          writes=[("bgall", e)])
                    S.dma("sp", lambda: nc.sync.dma_start(out=buall[:, e, :], in_=b_up[e].rearrange("(j p) -> p j", p=128), allow_slow_non_contiguous=True),
                          writes=[("buall", e)])
                bdbc = [sb6(f"bdbc{i}", [128, D], F32) for i in range(2)]
                idxs = [sb6(f"idx{i}", [128, NRT], I32) for i in range(2)]
                xg = [sb6(f"xg{i}", [128, D], BF16) for i in range(2)]
                xT = sb6("xT", [128, 8, CAP], BF16)
                actT = sb6("actT", [128, 8, CAP], BF16)
                gsb = [sb6(f"gsb{i}", [128, 512], F32) for i in range(2)]
                sgs = [sb6(f"sgs{i}", [128, 512], F32) for i in range(2)]
                usb = [sb6(f"usb{i}", [128, 512], F32) for i in range(2)]
                yo = [sb6(f"yo{i}", [128, D], F32) for i in range(2)]
                ptx = ps6("ptx", [128, 8, 128], BF16)
                pgc = [ps6(f"pgc{i}", [128, 512], F32) for i in range(2)]
                puc = [ps6(f"puc{i}", [128, 512], F32) for i in range(2)]
                pdh = [ps6(f"pd{i}", [128, 512], F32) for i in range(2)]

                WSRC = {"wg": (wgs, w_gate), "wu": (wus, w_up), "wd": (wds, w_down)}

                def load_piece(e, nm, k2):
                    s_ = e % 2
                    wt, src = WSRC[nm]
                    S.dma("pool", lambda: P_.dma_start(out=wt[s_][:, 2 * k2:2 * k2 + 2, :],
                                                       in_=src[e, k2 * 256:(k2 + 1) * 256, :].rearrange("(k p) n -> p k n", p=128)),
                          writes=[(nm, s_, 2 * k2), (nm, s_, 2 * k2 + 1)], grp="w", ngrp=9)

                WPLAN = [[("wg", 0), ("wu", 0)], [("wd", 0)], [("wg", 1), ("wu", 1)], [("wd", 1)],
                         [("wg", 2), ("wu", 2)], [("wd", 2)], [("wg", 3), ("wu", 3)], [("wd", 3)]]

                def load_wk(e, fj):
                    for nm, k2 in WPLAN[fj]:
                        load_piece(e, nm, k2)

                def load_w(e):
                    for fj in range(8):
                        load_wk(e, fj)

                xT2 = [xT, sb6("xTb", [128, 8, CAP], BF16)]

                def prep_idx(e):
                    S.dma("sp", lambda: nc.sync.dma_start(out=idxs[e % 2][:], in_=tokslot_d[e * CAP:(e + 1) * CAP, :].rearrange("(r p) o -> p (r o)", p=128),
                                                          allow_slow_non_contiguous=True), writes=[("idx", e % 2)])
                    S.dma("sp", lambda: nc.sync.dma_start(out=bdbc[e % 2][:], in_=b_down[e].partition_broadcast(128)), writes=[("bdbc", e % 2)])

                gcnt = [0]

                def prep_g(e, rt):
                    xs = rt % 2
                    S.dma("pool", lambda: P_.indirect_dma_start(out=xg[xs][:, :], out_offset=None, in_=h1b_d[:, :],
                                                                in_offset=IOA(ap=idxs[e % 2][:, rt:rt + 1], axis=0)),
                          reads=[("idx", e % 2)], writes=[("xg", xs)])

                def prep_t(e, rt):
                    xs = rt % 2
                    for k in range(8):
                        S.op("pe", lambda: T_.transpose(out=ptx[:, k, :], in_=xg[xs][:, k * 128:(k + 1) * 128], identity=ident[:]),
                             reads=[("xg", xs), "ident"], writes=["ptx"])
                    S.op("act", lambda: A_.copy(out=xT2[e % 2][:, :, rt * 128:(rt + 1) * 128], in_=ptx[:]), reads=["ptx"], writes=[("xT", e % 2)])

                def prep_rt(e, rt):
                    if rt + 1 < NRT:
                        prep_g(e, rt + 1)
                    prep_t(e, rt)

                load_w(0)
                S.waitfor("sp", sc_keys)
                S.waitfor("pool", h1_keys)
                prep_idx(0)
                prep_g(0, 0)
                for rt in range(NRT):
                    prep_rt(0, rt)
                cc = 0
                for e in range(NEXP):
                    s_ = e % 2
                    xTe = xT2[s_]
                    if e + 1 < NEXP:
                        prep_idx(e + 1)
                        prep_g(e + 1, 0)
                    nxt = list(range(NRT)) if e + 1 < NEXP else []
                    for fj in range(8):
                        for (c0, c1) in chunks:
                            b_ = cc % 2; cc += 1
                            n_ = c1 - c0
                            for (W, kw, P, kp) in ((wgs[s_], ("wg", s_), pgc[b_], ("pgc", b_)), (wus[s_], ("wu", s_), puc[b_], ("puc", b_))):
                                for k in range(8):
                                    S.op("pe", lambda: T_.matmul(P[:, 0:n_], lhsT=W[:, k, fj * 128:(fj + 1) * 128], rhs=xTe[:, k, c0:c1],
                                                                 start=(k == 0), stop=(k == 7)), reads=[kw + (k,), ("xT", s_)], writes=[kp])
                            gs, sg_, us = gsb[b_], sgs[b_], usb[b_]
                            S.op("dve", lambda: V.tensor_scalar(out=gs[:, 0:n_], in0=pgc[b_][:, 0:n_], scalar1=bgall[:, e, fj:fj + 1], scalar2=7.0,
                                                                op0=ALU.add, op1=ALU.min), reads=[("pgc", b_), ("bgall", e)], writes=[("gsb", b_)])
                            S.op("act", lambda: A_.activation(out=sg_[:, 0:n_], in_=gs[:, 0:n_], func=AF.Sigmoid, scale=1.702), reads=[("gsb", b_)], writes=[("sgs", b_)])
                            S.op("dve", lambda: V.tensor_scalar(out=us[:, 0:n_], in0=puc[b_][:, 0:n_], scalar1=buall[:, e, fj:fj + 1], scalar2=7.0,
                                                                op0=ALU.add, op1=ALU.min), reads=[("puc", b_), ("buall", e)], writes=[("usb", b_)])
                            S.op("dve", lambda: V.tensor_scalar(out=us[:, 0:n_], in0=us[:, 0:n_], scalar1=-7.0, scalar2=1.0, op0=ALU.max, op1=ALU.add),
                                 reads=[("usb", b_)], writes=[("usb", b_)])
                            S.op("dve", lambda: V.tensor_tensor(out=gs[:, 0:n_], in0=gs[:, 0:n_], in1=sg_[:, 0:n_], op=ALU.mult),
                                 reads=[("gsb", b_), ("sgs", b_)], writes=[("gsb", b_)])
                            S.op("dve", lambda: V.tensor_tensor(out=actT[:, fj, c0:c1], in0=gs[:, 0:n_], in1=us[:, 0:n_], op=ALU.mult),
                                 reads=[("gsb", b_), ("usb", b_)], writes=["actT"])
                        if e + 1 < NEXP:
                            load_wk(e + 1, fj)
                        if nxt:
                            prep_rt(e + 1, nxt.pop(0))
                    while nxt:
                        prep_rt(e + 1, nxt.pop(0))
                    for rt in range(NRT):
                        ys = rt % 2
                        for half in range(2):
                            for k in range(8):
                                S.op("pe", lambda: T_.matmul(pdh[half][:, :], lhsT=actT[:, k, rt * 128:(rt + 1) * 128],
                                                             rhs=wds[s_][:, k, half * 512:(half + 1) * 512], start=(k == 0), stop=(k == 7)),
                                     reads=["actT", ("wd", s_, k)], writes=[("pd", half)])
                            S.op("dve", lambda: V.tensor_tensor(out=yo[ys][:, half * 512:(half + 1) * 512], in0=pdh[half][:, :],
                                                                in1=bdbc[s_][:, half * 512:(half + 1) * 512], op=ALU.add),
                                 reads=[("pd", half), ("bdbc", s_)], writes=[("yo", ys, half)])
                        r0 = e * CAP + rt * 128
                        S.dma("sp", lambda: nc.sync.dma_start(out=ybuf_d[r0:r0 + 128, :], in_=yo[ys][:]), reads=[("yo", ys, 0), ("yo", ys, 1)],
                              writes=[("ybuf", e, rt)])
                        y_keys.append(("ybuf", e, rt))
                barrier()
            print("P2b done: inst", S.n_inst, "waits", S.n_wait, "sems", S.nsem, flush=True)

            with ExitStack() as st7:
                sb7, ps7 = mk(st7)
                h1c = [sb7(f"h1c{i}", [128, D], F32) for i in range(2)]
                yk = [[sb7(f"yk{i}_{k}", [128, D], F32) for k in range(4)] for i in range(2)]
                accs = [sb7(f"acc{i}", [128, D], F32) for i in range(2)]
                tmpc = sb7("tmpc", [128, D], F32)
                ot = [sb7(f"ot{i}", [128, D], F32) for i in range(2)]
                S.waitfor("pool", y_keys)

                def c_fetch(t2):
                    s2 = t2 % 2
                    S.dma("sp", lambda: nc.sync.dma_start(out=h1c[s2][:], in_=h1_d[t2 * 128:(t2 + 1) * 128, :]), reads=[("h1_d", t2)], writes=[("h1c", s2)])
                    for k in range(4):
                        S.dma("pool", lambda: P_.indirect_dma_start(out=yk[s2][k][:, :], out_offset=None, in_=ybuf_d[:, :],
                                                                    in_offset=IOA(ap=slot_ga[:, t2, k:k + 1], axis=0)),
                              reads=["slot_ga"], writes=[("yk", s2, k)])

                def c_comp(t2):
                    s2 = t2 % 2
                    ac = accs[s2]; ka = ("acc", s2)
                    S.op("act", lambda: A_.activation(out=ac[:], in_=h1c[s2][:], func=AF.Copy, scale=ALPHA), reads=[("h1c", s2)], writes=[ka])
                    for k in range(4):
                        S.op("dve", lambda: V.scalar_tensor_tensor(out=ac[:], in0=yk[s2][k][:], scalar=gate_all[:, t2, k:k + 1], in1=ac[:],
                                                                   op0=ALU.mult, op1=ALU.add), reads=[("yk", s2, k), "gate_all", ka], writes=[ka])
                    layernorm(ac, ka, g2, "g2", b2, "b2", tmpc, "tmpc", ot[s2][:], ("ot", s2), lnsb2[s2])
                    S.dma("sp", lambda: nc.sync.dma_start(out=out_d[t2 * 128:(t2 + 1) * 128, :], in_=ot[s2][:]), reads=[("ot", s2)], writes=[("out", t2)])

                c_fetch(0)
                for t2 in range(NT2):
                    if t2 + 1 < NT2:
                        c_fetch(t2 + 1)
                    c_comp(t2)
                barrier()
        for e in ("pe", "act", "dve", "pool", "sp"):
            S.wait_all(e)
    return nc


def make_maps(inp, NX, full=True):
    f = lambda a: np.ascontiguousarray(np.asarray(a, dtype=np.float32))
    NG = NX // 512
    NTOK2 = NX // 4
    NG2 = NTOK2 // 512
    XCH = min(1024, 128 * NG)
    w_in = np.asarray(inp["w_in"])[0]
    maps = []
    for c in range(8):
        b, r = c // 4, c % 4
        m = {}
        m["x_b"] = f(inp["x"][b, :NX])
        m["meta"] = f(inp["meta_tokens"])
        m["ln_in_g"] = f(inp["ln_in_g"]).reshape(1, D)
        m["ln_in_b"] = f(inp["ln_in_b"]).reshape(1, D)
        m["w4"] = f(np.concatenate([w_in[:, r * 128:(r + 1) * 128], w_in[:, 512 + r * 128:512 + (r + 1) * 128],
                                    w_in[:, 1024 + r * 128:1024 + (r + 1) * 128], w_in[:, 1536 + r * 128:1536 + (r + 1) * 128]], axis=1))
        m["relb"] = f(np.asarray(inp["rel_bias"])[:, r]).reshape(1, 32)
        m["lam4"] = f(np.stack([inp["lambda_q1"][0], inp["lambda_k1"][0], inp["lambda_q2"][0], inp["lambda_k2"][0]]))
        m["subln_g"] = f(inp["subln_g"][0]).reshape(1, 128)
        gs = slice(8 * r, 8 * r + 8)
        m["a_re"] = f(inp["a_re"][0][gs]).reshape(1, 512)
        m["a_im"] = f(inp["a_im"][0][gs]).reshape(1, 512)
        m["log_step"] = f(inp["log_step"][0][gs]).reshape(1, 8)
        m["b_re"] = f(inp["b_re"][0][gs]); m["b_im"] = f(inp["b_im"][0][gs])
        m["c_re"] = f(inp["c_re"][0][gs]); m["c_im"] = f(inp["c_im"][0][gs])
        m["d_skip"] = f(inp["d_skip"][0][128 * r:128 * r + 128]).reshape(1, 128)
        if full:
            m["x2"] = f(inp["x"][b, r * NTOK2:(r + 1) * NTOK2])
            gi = np.zeros((128, 8 * NG2), np.int32)
            p = np.arange(128)
            for tg in range(NG2):
                for j in range(4):
                    for half in range(2):
                        rho = (r * NG2 + tg) * 256 + half * 128 + p
                        gi[:, (tg * 4 + j) * 2 + half] = (rho // XCH) * 4 * XCH + j * XCH + rho % XCH
            m["gidx"] = gi
            m["w_glu"] = f(inp["w_glu"][0]); m["b_glu"] = f(inp["b_glu"][0]).reshape(1, 512)
            m["w_out"] = f(inp["w_out"][0])
            m["ln1_g"] = f(inp["ln1_g"][0]).reshape(1, D); m["ln1_b"] = f(inp["ln1_b"][0]).reshape(1, D)
            m["w_router"] = f(inp["w_router"][0]); m["b_router"] = f(inp["b_router"][0]).reshape(1, NEXP)
            m["w_gate"] = f(inp["w_gate"][0]); m["b_gate"] = f(inp["b_gate"][0])
            m["w_up"] = f(inp["w_up"][0]); m["b_up"] = f(inp["b_up"][0])
            m["w_down"] = f(inp["w_down"][0]); m["b_down"] = f(inp["b_down"][0])
            m["ln2_g"] = f(inp["ln2_g"][0]).reshape(1, D); m["ln2_b"] = f(inp["ln2_b"][0]).reshape(1, D)
        maps.append(m)
    return maps


def kernel(**inputs):
    NX = int(np.asarray(inputs["x"]).shape[1])
    CAP = 768 if NX >= 16384 else 128 * max(1, int(math.ceil(1.5 * NX / 8 / 128)))
    nc = build(NX, CAP, debug=False, full=True)
    maps = make_maps(inputs, NX, full=True)
    res = run_bass_kernel_spmd(nc, maps, core_ids=list(range(8)))
    NTOK2 = NX // 4
    out = np.zeros((2, NX, D), np.float32)
    for c in range(8):
        b, r = c // 4, c % 4
        out[b, r * NTOK2:(r + 1) * NTOK2] = np.asarray(res.results[c]["out"], dtype=np.float32)
    return out
```
